# Optimizing a Trainium2 kernel written in Bass

```python
import math
import jax, jax.numpy as jnp
from jax import lax
import numpy as np

D_MODEL = 1024
BATCH = 8
SEQ = 4096
DEPTH = 2

HEAD_DIM = 64
RWKV_HEADS = 8
ATTN_HEADS = 8
RWKV_WIDTH = RWKV_HEADS * HEAD_DIM
ATTN_WIDTH = ATTN_HEADS * HEAD_DIM
MIX_WIDTH = RWKV_WIDTH + ATTN_WIDTH
IDX_HEADS = 8
IDX_DIM = 64
TOPK_MAX = 256
QUERY_BLOCK = 128
DECAY_LORA = 64
ICLR_LORA = 64
VRES_LORA = 32
GATE_LORA = 128
D_FF = 4 * D_MODEL
N_BUCKETS = 32
MAX_DISTANCE = 128
RMS_EPS = 1e-6
LNX_EPS = 64e-5
IN_COLS = 3 * RWKV_WIDTH + 3 * ATTN_WIDTH + IDX_HEADS * IDX_DIM + IDX_DIM + IDX_HEADS

kernel_name = "hymba_rwkv7_dsa_adaln_trunk"


def rmsnorm(x, g):
    xf = x.astype(jnp.float32)
    y = xf * lax.rsqrt(jnp.mean(xf * xf, axis=-1, keepdims=True) + RMS_EPS)
    return (y * g.astype(jnp.float32)).astype(x.dtype)


def token_shift(z):
    return jnp.pad(z[:, :-1], ((0, 0), (1, 0), (0, 0)))


def split_heads(z, n):
    return z.reshape(*z.shape[:-1], n, z.shape[-1] // n)


def t5_bucket(dist):
    max_exact = N_BUCKETS // 2
    n = jnp.maximum(dist, 0)
    nf = jnp.maximum(n, 1).astype(jnp.float32)
    large = max_exact + (jnp.log(nf / max_exact) / math.log(MAX_DISTANCE / max_exact)
                         * (N_BUCKETS - max_exact)).astype(jnp.int32)
    large = jnp.minimum(large, N_BUCKETS - 1)
    return jnp.where(n < max_exact, n, large)


def rwkv7_recurrence(r, decay, k, v, kk, a):
    B, T, H, N = r.shape

    def step(S, inp):
        r_t, w_t, k_t, v_t, kk_t, a_t = inp
        s_kk = jnp.einsum('bhvk,bhk->bhv', S, kk_t)
        S = (S * w_t[:, :, None, :]
             - s_kk[..., None] * (kk_t * a_t)[:, :, None, :]
             + v_t[..., None] * k_t[:, :, None, :])
        return S, jnp.einsum('bhvk,bhk->bhv', S, r_t)

    xs = tuple(jnp.moveaxis(z.astype(jnp.float32), 1, 0) for z in (r, decay, k, v, kk, a))
    s0 = jnp.zeros((B, H, N, N), jnp.float32)
    _, ys = lax.scan(step, s0, xs)
    return jnp.moveaxis(ys, 0, 1)


def rwkv7_group(h, rkv, v_first, mu_rkv, mu_lora, decay_w0, decay_a, decay_b, iclr_a0, iclr_a,
                iclr_b, gate_a, gate_b, k_k, k_a, r_k, lnx_g, lnx_b, vres):
    B, T, _ = h.shape
    f32 = jnp.float32
    rkv = rkv + (token_shift(rkv) - rkv) * mu_rkv.reshape(-1)
    r, k, v = jnp.split(rkv, 3, axis=-1)
    dx = token_shift(h) - h
    xw = h + dx * mu_lora[0]
    xa = h + dx * mu_lora[1]
    xg = h + dx * mu_lora[2]
    wlog = (decay_w0 + jnp.tanh(xw @ decay_a) @ decay_b).astype(f32)
    wlog = -jax.nn.softplus(-wlog) - 0.5
    decay = jnp.exp(-jnp.exp(wlog))
    a = jax.nn.sigmoid(iclr_a0 + (xa @ iclr_a) @ iclr_b)
    g = jax.nn.sigmoid(xg @ gate_a) @ gate_b
    kk = split_heads(k * k_k, RWKV_HEADS).astype(f32)
    kk = kk / jnp.maximum(jnp.sqrt(jnp.sum(kk * kk, axis=-1, keepdims=True)), 1e-12)
    k = k * (1.0 + (a - 1.0) * k_a)
    if vres is None:
        v_first = v
    else:
        mu_v, v0, v_a, v_b = vres
        xv = h + dx * mu_v
        v = v + (v_first - v) * jax.nn.sigmoid(v0 + (xv @ v_a) @ v_b)
    rh = split_heads(r, RWKV_HEADS).astype(f32)
    kh = split_heads(k, RWKV_HEADS).astype(f32)
    vh = split_heads(v, RWKV_HEADS).astype(f32)
    ah = split_heads(a, RWKV_HEADS).astype(f32)
    y = rwkv7_recurrence(rh, split_heads(decay, RWKV_HEADS), kh, vh, kk, ah)
    mean = jnp.mean(y, axis=-1, keepdims=True)
    var = jnp.mean((y - mean) ** 2, axis=-1, keepdims=True)
    yn = ((y - mean) * lax.rsqrt(var + LNX_EPS) * lnx_g.reshape(RWKV_HEADS, HEAD_DIM).astype(f32)
          + lnx_b.reshape(RWKV_HEADS, HEAD_DIM).astype(f32))
    bonus = jnp.sum(rh * kh * r_k.astype(f32), axis=-1, keepdims=True) * vh
    out = (yn + bonus).reshape(B, T, RWKV_WIDTH) * g.astype(f32)
    return out.astype(h.dtype), v_first


def dsa_attention(q, k, v, qi, ki, wi, rel_bias):
    B, T, H, Dh = q.shape
    L = k.shape[1]
    n_sel = min(TOPK_MAX, L // 4)
    nb = T // QUERY_BLOCK
    key_pos = jnp.arange(L, dtype=jnp.int32)
    kif = ki.astype(jnp.float32)

    def blocks(z):
        return jnp.moveaxis(z.reshape(B, nb, QUERY_BLOCK, *z.shape[2:]), 1, 0)

    def one_block(args):
        start, q_b, qi_b, wi_b = args
        q_pos = start + jnp.arange(QUERY_BLOCK, dtype=jnp.int32)
        dots = jnp.einsum('bqhd,bsd->bqhs', qi_b.astype(jnp.float32), kif) * (IDX_DIM ** -0.5)
        w_b = wi_b.astype(jnp.float32) * (IDX_HEADS ** -0.5)
        score = jnp.einsum('bqh,bqhs->bqs', w_b, jax.nn.relu(dots))
        admissible = key_pos[None, :] <= q_pos[:, None]
        score = jnp.where(admissible[None], score, -jnp.inf)
        _, idx = lax.top_k(score, n_sel)
        valid = idx <= q_pos[None, :, None]
        k_sel = jax.vmap(lambda kb, ib: kb[ib])(k, idx)
        v_sel = jax.vmap(lambda vb, ib: vb[ib])(v, idx)
        logits = jnp.einsum('bqhd,bqkhd->bqhk', q_b, k_sel).astype(jnp.float32) * (Dh ** -0.5)
        bias = rel_bias[t5_bucket(q_pos[None, :, None] - idx)]
        logits = logits + jnp.moveaxis(bias, -1, 2).astype(jnp.float32)
        logits = jnp.where(valid[:, :, None, :], logits, -jnp.inf)
        p = jax.nn.softmax(logits, axis=-1)
        return jnp.einsum('bqhk,bqkhd->bqhd', p.astype(v.dtype), v_sel)

    starts = jnp.arange(nb, dtype=jnp.int32) * QUERY_BLOCK
    out = lax.map(one_block, (starts, blocks(q), blocks(qi), blocks(wi)))
    return jnp.moveaxis(out, 0, 1).reshape(B, T, H, Dh)


def setup_inputs(seed: int = 0) -> dict:
    key = jax.random.key(seed)
    ks = iter(jax.random.split(key, 40))
    nrm = lambda shape, scale: jax.random.normal(next(ks), shape, jnp.float32) * scale
    uni = lambda shape, lo, hi: jax.random.uniform(next(ks), shape, jnp.float32, lo, hi)
    L, D, R, A = DEPTH, D_MODEL, RWKV_WIDTH, ATTN_WIDTH
    return {
        "x": nrm((BATCH, SEQ, D), 1.0),
        "c": nrm((BATCH, D), 1.0),
        "w_ada": nrm((L, D, 6 * D), 0.5 * D ** -0.5),
        "b_ada": nrm((L, 6 * D), 0.01),
        "norm1_g": 1.0 + nrm((L, D), 0.02),
        "norm2_g": 1.0 + nrm((L, D), 0.02),
        "w_in": nrm((L, D, IN_COLS), D ** -0.5),
        "mu_rkv": uni((L, 3, R), 0.0, 1.0),
        "mu_lora": uni((L, 3, D), 0.0, 1.0),
        "decay_w0": uni((L, R), -4.0, -0.5),
        "decay_a": nrm((L, D, DECAY_LORA), D ** -0.5),
        "decay_b": nrm((L, DECAY_LORA, R), 0.1 * DECAY_LORA ** -0.5),
        "iclr_a0": nrm((L, R), 0.1),
        "iclr_a": nrm((L, D, ICLR_LORA), D ** -0.5),
        "iclr_b": nrm((L, ICLR_LORA, R), 0.3 * ICLR_LORA ** -0.5),
        "gate_a": nrm((L, D, GATE_LORA), D ** -0.5),
        "gate_b": nrm((L, GATE_LORA, R), GATE_LORA ** -0.5),
        "k_k": 0.85 + nrm((L, R), 0.05),
        "k_a": 1.0 + nrm((L, R), 0.05),
        "r_k": nrm((L, RWKV_HEADS, HEAD_DIM), 0.1),
        "lnx_g": 1.0 + nrm((L, R), 0.02),
        "lnx_b": nrm((L, R), 0.01),
        "vres_mu": uni((L - 1, D), 0.0, 1.0),
        "vres_v0": nrm((L - 1, R), 0.1),
        "vres_a": nrm((L - 1, D, VRES_LORA), D ** -0.5),
        "vres_b": nrm((L - 1, VRES_LORA, R), 0.3 * VRES_LORA ** -0.5),
        "attn_out_g": 1.0 + nrm((L, A), 0.02),
        "rel_bias": nrm((N_BUCKETS, ATTN_HEADS), 0.5),
        "w_out": nrm((L, MIX_WIDTH, D), MIX_WIDTH ** -0.5),
        "w_mlp1": nrm((L, D, D_FF), D ** -0.5),
        "w_mlp2": nrm((L, D_FF, D), D_FF ** -0.5),
        "final_g": 1.0 + nrm((D,), 0.02),
    }


def reference(x, c, w_ada, b_ada, norm1_g, norm2_g, w_in, mu_rkv, mu_lora, decay_w0, decay_a,
              decay_b, iclr_a0, iclr_a, iclr_b, gate_a, gate_b, k_k, k_a, r_k, lnx_g, lnx_b,
              vres_mu, vres_v0, vres_a, vres_b, attn_out_g, rel_bias, w_out, w_mlp1, w_mlp2,
              final_g):
    B, T, D = x.shape
    R, A = RWKV_WIDTH, ATTN_WIDTH
    cuts = np.cumsum([R, R, R, A, A, A, IDX_HEADS * IDX_DIM, IDX_DIM]).tolist()
    c_act = jax.nn.silu(c)
    v_first = None
    for l in range(DEPTH):
        mod = c_act @ w_ada[l] + b_ada[l]
        sh1, sc1, gt1, sh2, sc2, gt2 = jnp.split(mod, 6, axis=-1)
        h = rmsnorm(x, norm1_g[l]) * (1.0 + sc1[:, None]) + sh1[:, None]
        proj = h @ w_in[l]
        r_p, k_p, v_p, q_a, k_att, v_att, qi, ki, wi = jnp.split(proj, cuts, axis=-1)
        vres = None if l == 0 else (vres_mu[l - 1], vres_v0[l - 1], vres_a[l - 1], vres_b[l - 1])
        rwkv_out, v_first = rwkv7_group(
            h, jnp.concatenate([r_p, k_p, v_p], axis=-1), v_first, mu_rkv[l], mu_lora[l],
            decay_w0[l], decay_a[l], decay_b[l], iclr_a0[l], iclr_a[l], iclr_b[l], gate_a[l],
            gate_b[l], k_k[l], k_a[l], r_k[l], lnx_g[l], lnx_b[l], vres)
        att = dsa_attention(split_heads(q_a, ATTN_HEADS), split_heads(k_att, ATTN_HEADS),
                            split_heads(v_att, ATTN_HEADS), split_heads(qi, IDX_HEADS), ki, wi,
                            rel_bias)
        att = rmsnorm(att, attn_out_g[l].reshape(ATTN_HEADS, HEAD_DIM)).reshape(B, T, A)
        mixed = jnp.concatenate([rwkv_out, att], axis=-1) @ w_out[l]
        x = x + gt1[:, None] * mixed
        h2 = rmsnorm(x, norm2_g[l]) * (1.0 + sc2[:, None]) + sh2[:, None]
        u = jnp.square(jax.nn.relu(h2 @ w_mlp1[l]))
        x = x + gt2[:, None] * (u @ w_mlp2[l])
    return rmsnorm(x, final_g)
```

```python
import contextlib
import os
import math
import numpy as np
import ml_dtypes
import concourse.bass as bass
import concourse.mybir as mybir
from concourse.bass_utils import run_bass_kernel_spmd

F32 = mybir.dt.float32
BF16 = mybir.dt.bfloat16
AF = mybir.ActivationFunctionType
ALU = mybir.AluOpType
AX = mybir.AxisListType

ENG = ("pe", "dve", "act", "pool", "sp")
NDMA = 12
NBIS = 16


class _Stop(Exception):
    pass


_DEAD = [False]


def chk(n):
    if int(os.environ.get("A2STOP", "0")) == n:
        _DEAD[0] = True


class Op:
    __slots__ = ("eng", "fn", "deps", "signal", "sig_val", "dma", "dma_slot", "dma_val", "sem")

    def __init__(self, eng, fn, dma):
        self.eng = eng
        self.fn = fn
        self.deps = []
        self.signal = False
        self.sig_val = None
        self.dma = dma
        self.dma_slot = None
        self.dma_val = None
        self.sem = None


class Sched:
    def __init__(self, nc, stack):
        self.nc = nc
        self.stack = stack
        self.nsw = 0
        self.sw_dmas = []
        self.esem = {e: stack.enter_context(nc.semaphore("s_" + e)) for e in ENG if e != "sp"}
        self.dsem = [stack.enter_context(nc.semaphore("d_%d" % i)) for i in range(NDMA)]
        self.ops = {e: [] for e in ENG}
        self.last_w = {}
        self.readers = {}
        self.dma_count = 0
        self.dma_last = [None] * NDMA
        self.sigc = {e: 0 for e in ENG}
        self.waited = {e: {} for e in ENG}
        self.bar = {e: [] for e in ENG}
        self.nops = 0

    def _dep(self, op, prod):
        if prod is None or prod is op:
            return
        if (not prod.dma) and (not op.dma) and prod.eng == op.eng == "pe":
            return
        if not prod.dma:
            prod.signal = True
        op.deps.append(prod)

    def add(self, eng, fn, r=(), w=(), dma=False):
        op = Op(eng, fn, dma)
        if _DEAD[0]:
            return op
        if self.bar[eng]:
            for p in self.bar[eng]:
                op.deps.append(p)
            self.bar[eng] = []
        for k in r:
            self._dep(op, self.last_w.get(k))
        for k in w:
            self._dep(op, self.last_w.get(k))
            for rd in self.readers.get(k, ()):
                self._dep(op, rd)
        for k in r:
            self.readers.setdefault(k, []).append(op)
        for k in w:
            self.last_w[k] = op
            self.readers[k] = []
        if dma and eng == "pool":
            op.sem = self.stack.enter_context(self.nc.semaphore("w_%d" % self.nsw))
            op.dma_slot = "w%d" % self.nsw
            self.nsw += 1
            op.dma_val = 16
            self.sw_dmas.append(op)
        elif dma:
            slot = self.dma_count % NDMA
            self.dma_count += 1
            prev = self.dma_last[slot]
            op.dma_slot = slot
            op.sem = self.dsem[slot]
            op.dma_val = (prev.dma_val if prev else 0) + 16
            if prev is not None:
                op.deps.append(prev)
            self.dma_last[slot] = op
        self.ops[eng].append(op)
        self.nops += 1
        return op

    def pe(self, fn, r=(), w=()):
        return self.add("pe", fn, r, w)

    def dve(self, fn, r=(), w=()):
        return self.add("dve", fn, r, w)

    def act(self, fn, r=(), w=()):
        return self.add("act", fn, r, w)

    def pool(self, fn, r=(), w=()):
        return self.add("pool", fn, r, w)

    def dma(self, out, in_, r=(), w=(), eng="sp"):
        return self.add(eng, lambda e: e.dma_start(out=out, in_=in_), r, w, dma=True)

    def flush(self, final=False):
        nc = self.nc
        lasts = []
        for e in ENG:
            nd = [op for op in self.ops[e] if not op.dma]
            if nd:
                nd[-1].signal = True
                lasts.append(nd[-1])
        for e in ENG:
            for op in self.ops[e]:
                if op.signal and not op.dma:
                    self.sigc[e] += 1
                    op.sig_val = self.sigc[e]
        dlast = [p for p in self.dma_last if p is not None] + self.sw_dmas
        self.sw_dmas = []
        with nc.Block() as block:
            engobj = {"pe": block.tensor, "dve": block.vector, "act": block.scalar,
                      "pool": block.gpsimd, "sp": block.sync}
            for ename in ENG:
                ops = self.ops[ename]
                if not ops and not (final and ename == "sp"):
                    continue

                def body(e, ops=ops, ename=ename):
                    waited = self.waited[ename]
                    semof = {}
                    for op in ops:
                        need = {}
                        for p in op.deps:
                            if p.dma:
                                key = ("d", p.dma_slot)
                                semof[key] = p.sem
                                val = p.dma_val
                            else:
                                key = ("e", p.eng)
                                val = p.sig_val
                            if need.get(key, 0) < val:
                                need[key] = val
                        for key, val in need.items():
                            if waited.get(key, 0) >= val:
                                continue
                            waited[key] = val
                            sem = semof[key] if key[0] == "d" else self.esem[key[1]]
                            e.wait_ge(sem, val)
                        ins = op.fn(e)
                        if op.dma:
                            ins.then_inc(op.sem, 16)
                        elif op.signal:
                            ins.then_inc(self.esem[ename], 1)
                    if final and ename == "sp":
                        for p in dlast:
                            if waited.get(("d", p.dma_slot), 0) < p.dma_val:
                                e.wait_ge(p.sem, p.dma_val)
                        for p in lasts:
                            e.wait_ge(self.esem[p.eng], p.sig_val)
                engobj[ename](body)
        barrier = lasts + dlast
        self.ops = {e: [] for e in ENG}
        self.last_w = {}
        self.readers = {}
        self.bar = {e: list(barrier) for e in ENG}


def MM(out, lhsT, rhs, start=True, stop=True):
    return lambda e: e.matmul(out, lhsT=lhsT, rhs=rhs, start=start, stop=stop)


def TR(out, in_, ident):
    return lambda e: e.transpose(out, in_, ident)


def ACTF(out, in_, func, bias=0.0, scale=1.0, accum=None):
    if accum is None:
        return lambda e: e.activation(out=out, in_=in_, func=func, bias=bias, scale=scale)
    return lambda e: e.activation(out=out, in_=in_, func=func, bias=bias, scale=scale, accum_out=accum)


def TT(out, a, b, op):
    return lambda e: e.tensor_tensor(out=out, in0=a, in1=b, op=op)


def TS(out, a, s1, s2=None, op0=ALU.mult, op1=None, accum=None):
    if accum is not None:
        return lambda e: e.tensor_scalar(out=out, in0=a, scalar1=s1, scalar2=s2, op0=op0, op1=op1, accum_out=accum)
    if op1 is None:
        return lambda e: e.tensor_scalar(out=out, in0=a, scalar1=s1, scalar2=None, op0=op0)
    return lambda e: e.tensor_scalar(out=out, in0=a, scalar1=s1, scalar2=s2, op0=op0, op1=op1)


def STT(out, a, s, b, op0, op1):
    return lambda e: e.scalar_tensor_tensor(out=out, in0=a, scalar=s, in1=b, op0=op0, op1=op1)


def CP(out, in_):
    return lambda e: e.tensor_copy(out, in_)


def RED(out, in_, op, axis=AX.X):
    return lambda e: e.tensor_reduce(out=out, in_=in_, axis=axis, op=op)


def MEMSET(ap, v):
    return lambda e: e.memset(ap, v)


WSHAPES = {
    "w_ada": (1024, 6144), "b_ada": (6144,), "norm1_g": (1024,), "norm2_g": (1024,),
    "w_in": (1024, 3656), "mu_rkv": (3, 512), "mu_lora": (3, 1024), "decay_w0": (512,),
    "decay_a": (1024, 64), "decay_b": (64, 512), "iclr_a0": (512,), "iclr_a": (1024, 64),
    "iclr_b": (64, 512), "gate_a": (1024, 128), "gate_b": (128, 512), "k_k": (512,), "k_a": (512,),
    "r_k": (8, 64), "lnx_g": (512,), "lnx_b": (512,), "attn_out_g": (512,),
    "w_out": (1024, 1024), "w_mlp1": (1024, 4096), "w_mlp2": (4096, 1024),
}
VSHAPES = {"vres_mu": (1024,), "vres_v0": (512,), "vres_a": (1024, 32), "vres_b": (32, 512)}


def build(T, L, dbg=False, stop_after=None):
    _DEAD[0] = False
    NT = T // 128
    NB = T // 512
    NSEL = min(256, T // 4)
    nc = bass.Bass("TRN2", target_bir_lowering=False)
    W = {}

    def din(name, shape):
        W[name] = nc.dram_tensor(name, list(shape), F32, kind="ExternalInput").ap()
        return W[name]

    x_d = din("x", [T, 1024])
    c8_d = din("c8", [128, 8])
    consts_d = din("consts", [128, 640])
    bnear_d = din("bnear", [128, 2048])
    c31_d = din("c31", [1, 8])
    for k, s in WSHAPES.items():
        din(k, (L,) + s)
    for k, s in VSHAPES.items():
        din(k, (max(L - 1, 1),) + s)
    din("final_g", [1, 1024])
    out_d = nc.dram_tensor("out", [T, 1024], F32, kind="ExternalOutput").ap()

    def dscr(name, shape, dt):
        return nc.dram_tensor(name, list(shape), dt, kind=("ExternalOutput" if dbg else "Internal")).ap()

    xs_d = dscr("xs", [T, 1024], F32)
    mod_d = dscr("modd", [128, 6144], F32)
    featT_d = dscr("featT", [1600, T], BF16)
    vaug_d = dscr("vaug", [T, 520], BF16)
    wi_d = dscr("wid", [T, 8], F32)
    hT_d = dscr("hTd", [1024, T], BF16)
    vfirst_d = dscr("vfirst", [T, 512], F32)
    rwkvT_d = dscr("rwkvT", [512, T], BF16)
    attT_d = dscr("attT", [512, T], BF16)
    h2T_d = dscr("h2T", [1024, T], BF16)
    dbg_d = {}
    if dbg:
        for nm, shp in (("dbg_y", [T, 512]), ("dbg_score", [T, T]), ("dbg_thr", [T, 1]), ("dbg_v", [T, 512]),
                        ("dbg_r", [T, 512]), ("dbg_k", [T, 512]), ("dbg_a", [T, 512]), ("dbg_lw", [T, 512])):
            dbg_d[nm] = nc.dram_tensor(nm, shp, F32, kind="ExternalOutput").ap()

    uniq = [0]

    def SBT(name, shape, dt):
        uniq[0] += 1
        return nc.sbuf_tensor("%s_%d" % (name, uniq[0]), shape, dt)

    with contextlib.ExitStack() as gst:
        S = Sched(nc, gst)

        def gsb(name, shape, dt):
            return gst.enter_context(SBT(name, shape, dt))

        banks = [gst.enter_context(nc.psum_tensor("bank%d" % i, [128, 512], F32)) for i in range(8)]
        bank_i = [0]

        NROT = 6

        def nb():
            i = bank_i[0] % NROT
            bank_i[0] += 1
            return banks[i], "bank%d" % i

        def bfv(bank):
            return bank[:].bitcast(BF16)

        cst = gsb("cst", [128, 640], F32)
        identb = gsb("identb", [128, 128], BF16)
        mask4 = gsb("mask4", [128, 512], F32)
        lowm4 = gsb("lowm4", [128, 512], F32)
        onesf = gsb("onesf", [128, 128], F32)
        m05 = gsb("m05", [128, 512], F32)
        cbc = gsb("cbc", [128, 8, 128], F32)
        identf = cst[:, 0:128]
        tri_incl = cst[:, 128:256]
        tri_strict = cst[:, 256:384]
        low_strict = cst[:, 384:512]
        caus = cst[:, 512:640]

        with contextlib.ExitStack() as st:
            c8 = st.enter_context(SBT("c8s", [128, 8], F32))
            c8t = st.enter_context(SBT("c8t", [128, 8], F32))
            S.dma(cst[:], consts_d, w=["cst"])
            S.dma(c8[:], c8_d, w=["c8"])
            S.dve(CP(identb[:], identf), r=["cst"], w=["identb"])
            for i in range(4):
                S.dve(CP(mask4[:, i * 128:(i + 1) * 128], tri_strict if i % 2 == 0 else tri_incl), r=["cst"], w=["mask4"])
                S.pool(CP(lowm4[:, i * 128:(i + 1) * 128], low_strict), r=["cst"], w=["lowm4"])
            S.pool(MEMSET(onesf[:], 1.0), w=["onesf"])
            S.pool(MEMSET(m05[:], -0.5), w=["m05"])
            S.act(ACTF(c8t[:], c8[:], AF.Tanh, scale=0.5), r=["c8"], w=["c8t"])
            S.dve(TS(c8t[:], c8t[:], 0.5, 0.5, ALU.mult, ALU.add), r=["c8t"], w=["c8t"])
            S.dve(TT(c8t[:], c8t[:], c8[:], ALU.mult), r=["c8t", "c8"], w=["c8t"])
            S.dve(CP(cbc[:], c8t[:].unsqueeze(2).to_broadcast([128, 8, 128])), r=["c8t"], w=["cbc"])
            S.flush()

        def phase_M(l):
            with contextlib.ExitStack() as st:
                def sb(name, shape, dt):
                    return st.enter_context(SBT(name, shape, dt))
                wst = [sb("wst%d" % i, [128, 8, 512], F32) for i in range(2)]
                modt = sb("modt", [128, 6144], F32)
                gbc = sb("gbc", [128, 2048], F32)
                bada = sb("bada", [1, 6144], F32)
                S.dma(gbc[:, 0:1024], W["norm1_g"][l:l + 1, :].partition_broadcast(128), w=["gbc"])
                S.dma(gbc[:, 1024:2048], W["norm2_g"][l:l + 1, :].partition_broadcast(128), w=["gbc"])
                S.dma(bada[:], W["b_ada"][l:l + 1, :], w=["bada"])
                wa = W["w_ada"][l].rearrange("(kc p) n -> p kc n", p=128)
                for n in range(12):
                    buf = wst[n % 2]
                    key = "wst%d" % (n % 2)
                    S.dma(buf[:], wa[:, :, n * 512:(n + 1) * 512], w=[key])
                    bk, kb = nb()
                    for kc in range(8):
                        S.pe(MM(bk[:], cbc[:, kc, :], buf[:, kc, :], kc == 0, False), r=[key, "cbc"], w=[kb])
                    S.pe(MM(bk[:], onesf[0:1, :], bada[0:1, n * 512:(n + 1) * 512], False, True), r=["bada", "onesf"], w=[kb])
                    S.act(ACTF(modt[:, n * 512:(n + 1) * 512], bk[:], AF.Copy), r=[kb], w=["modt"])
                S.dve(STT(modt[:, 1024:2048], modt[:, 1024:2048], 1.0, gbc[:, 0:1024], ALU.add, ALU.mult), r=["modt", "gbc"], w=["modt"])
                S.dve(STT(modt[:, 4096:5120], modt[:, 4096:5120], 1.0, gbc[:, 1024:2048], ALU.add, ALU.mult), r=["modt", "gbc"], w=["modt"])
                S.dma(mod_d, modt[:], r=["modt"], w=["mod_d"])
                S.flush()

        def norm_mod_T(X, kx, At, sht, hb, khb, junk, ssq, ms, rstd, tmp, idx, dstT, kdst):
            S.act(ACTF(junk[:], X, AF.Square, accum=ssq[:]), r=[kx], w=["ssq%d" % idx])
            S.dve(TS(ms[:], ssq[:], 1.0 / 1024, 1e-6, ALU.mult, ALU.add), r=["ssq%d" % idx], w=["ms%d" % idx])
            S.pool(TT(rstd[:], ms[:], m05[:, 0:1], ALU.pow), r=["ms%d" % idx, "m05"], w=["rstd%d" % idx])
            S.dve(STT(tmp[:], X, rstd[:, 0:1], At, ALU.mult, ALU.mult), r=[kx, "rstd%d" % idx, "modp"], w=["tmpn"])
            S.pool(TT(hb[:], tmp[:], sht, ALU.add), r=["tmpn", "modp"], w=[khb])
            bk, kb = nb()
            bkb = bfv(bk)
            for kc in range(8):
                S.pe(TR(bkb[:, kc * 128:(kc + 1) * 128], hb[:, kc * 128:(kc + 1) * 128], identb[:]), r=[khb, "identb"], w=[kb])
            S.act(ACTF(dstT, bkb.rearrange("p (k t) -> p k t", k=8), AF.Copy), r=[kb], w=[kdst])

        def phase_A1(l, xsrc):
            with contextlib.ExitStack() as st:
                def sb(name, shape, dt):
                    return st.enter_context(SBT(name, shape, dt))
                WF = sb("WF", [128, 8, 1600], BF16)
                WV = sb("WV", [128, 8, 520], BF16)
                A1t = sb("A1t", [128, 1024], F32)
                sh1t = sb("sh1t", [128, 1024], F32)
                xb = [sb("xb%d" % i, [128, 1024], F32) for i in range(2)]
                junk = sb("junk", [128, 1024], F32)
                tmp = sb("tmpn", [128, 1024], F32)
                hb = [sb("hb%d" % i, [128, 1024], BF16) for i in range(2)]
                hT = [sb("hT%d" % i, [128, 8, 512], BF16) for i in range(2)]
                fst = [sb("fst%d" % i, [128, 512], BF16) for i in range(2)]
                vst = [sb("vst%d" % i, [128, 8, 65], BF16) for i in range(2)]
                wist = [sb("wist%d" % i, [128, 8], F32) for i in range(2)]
                ssq = [sb("ssq%d" % i, [128, 1], F32) for i in range(2)]
                ms = [sb("ms%d" % i, [128, 1], F32) for i in range(2)]
                rstd = [sb("rstd%d" % i, [128, 1], F32) for i in range(2)]
                wv = W["w_in"][l].rearrange("(kc p) n -> p kc n", p=128)
                S.dma(WF[:, :, 0:1024], wv[:, :, 1536:2560], w=["WF"], eng="pool")
                S.dma(WF[:, :, 1024:1600], wv[:, :, 3072:3648], w=["WF"], eng="pool")
                S.dma(WV[:, :, 0:512], wv[:, :, 2560:3072], w=["WV"], eng="pool")
                S.dma(WV[:, :, 512:520], wv[:, :, 3648:3656], w=["WV"], eng="pool")
                S.dma(A1t[:], mod_d[:, 1024:2048], r=["mod_d"], w=["modp"])
                S.dma(sh1t[:], mod_d[:, 0:1024], r=["mod_d"], w=["modp"])
                for i in range(2):
                    S.pool(MEMSET(vst[i][:, :, 64:65], 1.0), w=["vst%d" % i])

                def load(t):
                    S.dma(xb[t % 2][:], xsrc[t * 128:(t + 1) * 128, :], r=["xs_d"], w=["xb%d" % (t % 2)])
                load(0)
                for b in range(NB):
                    hTb = hT[b % 2]
                    kh = "hT%d" % (b % 2)
                    for j in range(4):
                        t = b * 4 + j
                        if t + 1 < NT:
                            load(t + 1)
                        i2 = t % 2
                        norm_mod_T(xb[i2][:], "xb%d" % i2, A1t[:], sh1t[:], hb[i2], "hb%d" % i2, junk, ssq[i2], ms[i2],
                                   rstd[i2], tmp, i2, hTb[:, :, j * 128:(j + 1) * 128], kh)
                        bk, kb = nb()
                        bk2, kb2 = nb()
                        for kc in range(8):
                            S.pe(MM(bk[:], hTb[:, kc, j * 128:(j + 1) * 128], WV[:, kc, 0:512], kc == 0, kc == 7), r=[kh, "WV"], w=[kb])
                        for kc in range(8):
                            S.pe(MM(bk2[:, 0:8], hTb[:, kc, j * 128:(j + 1) * 128], WV[:, kc, 512:520], kc == 0, kc == 7), r=[kh, "WV"], w=[kb2])
                        S.dve(CP(vst[i2][:, :, 0:64], bk[:].rearrange("p (h d) -> p h d", h=8)), r=[kb], w=["vst%d" % i2])
                        S.act(ACTF(wist[i2][:], bk2[:, 0:8], AF.Copy), r=[kb2], w=["wist%d" % i2])
                        S.dma(vaug_d[t * 128:(t + 1) * 128, :], vst[i2][:].rearrange("p h d -> p (h d)"), r=["vst%d" % i2], w=["vaug_d"])
                        S.dma(wi_d[t * 128:(t + 1) * 128, :], wist[i2][:], r=["wist%d" % i2], w=["wi_d"])
                    for c in range(13):
                        rows = 128 if c < 12 else 64
                        bk, kb = nb()
                        for kc in range(8):
                            S.pe(MM(bk[0:rows, :], WF[:, kc, c * 128:c * 128 + rows], hTb[:, kc, :], kc == 0, kc == 7), r=[kh, "WF"], w=[kb])
                        f = fst[c % 2]
                        kf = "fst%d" % (c % 2)
                        if c % 2 == 0:
                            S.act(ACTF(f[0:rows, :], bk[0:rows, :], AF.Copy), r=[kb], w=[kf])
                        else:
                            S.dve(CP(f[0:rows, :], bk[0:rows, :]), r=[kb], w=[kf])
                        S.dma(featT_d[c * 128:c * 128 + rows, b * 512:(b + 1) * 512], f[0:rows, :], r=[kf], w=["featT_d"])
                    S.dma(hT_d[:, b * 512:(b + 1) * 512].rearrange("(kc p) t -> p kc t", p=128), hTb[:], r=[kh], w=["hT_d"])
                S.flush()

        def phase_A2(l):
            with contextlib.ExitStack() as st:
                def sb(name, shape, dt):
                    return st.enter_context(SBT(name, shape, dt))
                WT1 = sb("WT1", [128, 8, 1536], BF16)
                WT2 = sb("WT2", [128, 8, 1536], BF16)
                LA1 = sb("LA1", [128, 8, 288], BF16)
                LA2 = sb("LA2", [128, 8, 288], BF16)
                Bwa = sb("Bwa", [128, 512], BF16)
                Bg = sb("Bg", [128, 512], BF16)
                Bv = sb("Bv", [32, 512], BF16)
                brow = sb("brow", [1, 3, 512], F32)
                pbc = sb("pbc", [128, 5, 512], F32)
                muT = sb("muT", [128, 32], F32)
                omT = sb("omT", [128, 32], F32)
                with contextlib.ExitStack() as st2:
                    Wr = st2.enter_context(SBT("Wr", [128, 8, 1536], BF16))
                    mubc = st2.enter_context(SBT("mubc", [128, 1536], F32))
                    ombc = st2.enter_context(SBT("ombc", [128, 1536], F32))
                    LA = st2.enter_context(SBT("LA", [128, 8, 288], F32))
                    mu32 = st2.enter_context(SBT("mu32", [32, 128], F32))
                    wv = W["w_in"][l].rearrange("(kc p) n -> p kc n", p=128)
                    S.dma(Wr[:, :, 0:768], wv[:, :, 0:768], w=["Wr"], eng="pool")
                    S.dma(Wr[:, :, 768:1536], wv[:, :, 768:1536], w=["Wr"], eng="pool")
                    S.dma(mubc[:], W["mu_rkv"][l:l + 1].rearrange("o a b -> o (a b)").partition_broadcast(128), w=["mubc"])
                    S.dve(TS(ombc[:], mubc[:], -1.0, 1.0, ALU.mult, ALU.add), r=["mubc"], w=["ombc"])
                    S.dve(TT(WT2[:], Wr[:], mubc[:].unsqueeze(1).to_broadcast([128, 8, 1536]), ALU.mult), r=["Wr", "mubc"], w=["WT2"])
                    S.pool(TT(WT1[:], Wr[:], ombc[:].unsqueeze(1).to_broadcast([128, 8, 1536]), ALU.mult), r=["Wr", "ombc"], w=["WT1"])
                    S.dma(LA[:, :, 0:64], W["decay_a"][l].rearrange("(kc p) n -> p kc n", p=128), w=["LA"])
                    S.dma(LA[:, :, 64:128], W["iclr_a"][l].rearrange("(kc p) n -> p kc n", p=128), w=["LA"])
                    S.dma(LA[:, :, 128:256], W["gate_a"][l].rearrange("(kc p) n -> p kc n", p=128), w=["LA"])
                    if l > 0:
                        S.dma(LA[:, :, 256:288], W["vres_a"][l - 1].rearrange("(kc p) n -> p kc n", p=128), w=["LA"])
                    else:
                        S.dve(MEMSET(LA[:, :, 256:288], 0.0), w=["LA"])
                    S.dve(MEMSET(mu32[:], 0.0), w=["mu32"])
                    S.dma(mu32[0:24, :], W["mu_lora"][l].rearrange("a (kc p) -> (a kc) p", p=128), r=["mu32"], w=["mu32"])
                    if l > 0:
                        S.dma(mu32[24:32, :], W["vres_mu"][l - 1:l, :].rearrange("o (kc p) -> (o kc) p", p=128), r=["mu32"], w=["mu32"])
                    bk, kb = nb()
                    S.pe(MM(bk[:, 0:32], mu32[:], identf[0:32, 0:32], True, True), r=["mu32", "cst"], w=[kb])
                    S.act(ACTF(muT[:], bk[:, 0:32], AF.Copy), r=[kb], w=["muT"])
                    S.dve(TS(omT[:], muT[:], -1.0, 1.0, ALU.mult, ALU.add), r=["muT"], w=["omT"])
                    for gi, (c0, c1) in enumerate(((0, 64), (64, 128), (128, 256), (256, 288))):
                        wdt = c1 - c0
                        S.dve(TT(LA2[:, :, c0:c1], LA[:, :, c0:c1], muT[:, gi * 8:(gi + 1) * 8].unsqueeze(2).to_broadcast([128, 8, wdt]), ALU.mult), r=["LA", "muT"], w=["LA2"])
                        S.dve(TT(LA1[:, :, c0:c1], LA[:, :, c0:c1], omT[:, gi * 8:(gi + 1) * 8].unsqueeze(2).to_broadcast([128, 8, wdt]), ALU.mult), r=["LA", "omT"], w=["LA1"])
                    S.dma(Bwa[0:64, :], W["decay_b"][l], w=["Bwa"], eng="pool")
                    S.dma(Bwa[64:128, :], W["iclr_b"][l], w=["Bwa"], eng="pool")
                    S.dma(Bg[:], W["gate_b"][l], w=["Bg"], eng="pool")
                    if l > 0:
                        S.dma(Bv[:], W["vres_b"][l - 1], w=["Bv"], eng="pool")
                    S.dma(brow[:, 0, :], W["decay_w0"][l:l + 1, :], w=["brow"])
                    S.dma(brow[:, 1, :], W["iclr_a0"][l:l + 1, :], w=["brow"])
                    if l > 0:
                        S.dma(brow[:, 2, :], W["vres_v0"][l - 1:l, :], w=["brow"])
                    S.dma(pbc[:, 0, :], W["k_k"][l:l + 1, :].partition_broadcast(128), w=["pbc"])
                    S.dma(pbc[:, 1, :], W["k_a"][l:l + 1, :].partition_broadcast(128), w=["pbc"])
                    S.dma(pbc[:, 2, :], W["r_k"][l:l + 1].rearrange("o a b -> o (a b)").partition_broadcast(128), w=["pbc"])
                    S.dma(pbc[:, 3, :], W["lnx_g"][l:l + 1, :].partition_broadcast(128), w=["pbc"])
                    S.dma(pbc[:, 4, :], W["lnx_b"][l:l + 1, :].partition_broadcast(128), w=["pbc"])
                    S.flush()

                chk(1)
                hTb2 = [sb("hTb%d" % i, [128, 8, 512], BF16) for i in range(1)]
                hTp2 = [sb("hTp%d" % i, [128, 8, 512], BF16) for i in range(1)]
                L1wa = sb("L1wa", [128, 512], BF16)
                sg = sb("sg", [128, 512], BF16)
                sgt = sb("sgt", [128, 512], F32)
                L1v = sb("L1v", [32, 512], BF16)

                def f32t(name):
                    return sb(name, [128, 512], F32)

                def b16t(name):
                    return sb(name, [128, 512], BF16)
                r32, k32, v32 = f32t("r32"), f32t("k32"), f32t("v32")
                kkr, kk, a32, b32, km = f32t("kkr"), f32t("kk"), f32t("a32"), f32t("b32"), f32t("km")
                lw, Ginc, Ginv, Gexc, g32 = f32t("lw"), f32t("Ginc"), f32t("Ginv"), f32t("Gexc"), f32t("g32")
                t1, t2, U0, cen, vf = f32t("t1"), f32t("t2"), f32t("U0"), f32t("cen"), f32t("vf")
                y32 = f32t("y32")
                Kd, Rd, Bi, Ki, Vb, Zb, Ub, ob = (b16t(n) for n in ("Kd", "Rd", "Bi", "Ki", "Vb", "Zb", "Ub", "ob"))
                s8 = [sb("s8_%d" % i, [128, 8], F32) for i in range(6)]
                FT = sb("FT", [64, 8, 4, 128], BF16)
                GE = sb("GE", [128, 8, 512], BF16)
                MMb = [sb("MMb%d" % i, [128, 8, 128], F32) for i in range(2)]
                NNb = [sb("NNb%d" % i, [128, 8, 128], F32) for i in range(2)]
                TTb = [sb("TTb%d" % i, [128, 8, 128], F32) for i in range(2)]
                TTh = sb("TTh", [128, 8, 128], BF16)
                WTs = sb("WTs", [64, 8, 128], BF16)
                P32 = sb("P32", [64, 8, 64], F32)
                Pb = sb("Pb", [64, 8, 64], BF16)
                GC = sb("GC", [64, 8], F32)
                rwT = sb("rwT", [128, 4, 128], BF16)
                S.dve(MEMSET(P32[:], 0.0), w=["P32"])
                S.dve(MEMSET(Pb[:], 0.0), w=["Pb"])
                P32f = P32[:].rearrange("p h v -> p (h v)")
                ev = [0]

                def evac(out, in_, r, w):
                    ev[0] += 1
                    if ev[0] % 2:
                        S.act(ACTF(out, in_, AF.Copy), r=r, w=w)
                    else:
                        S.dve(CP(out, in_), r=r, w=w)

                for b in range(NB):
                    hTb = hTb2[0]
                    kh = "hTb0"
                    src = hT_d.rearrange("(kc p) t -> p kc t", p=128)
                    if b == 1:
                        chk(8)
                    hTp = hTp2[0]
                    S.dma(hTb[:], src[:, :, b * 512:(b + 1) * 512], r=["hT_d"], w=[kh])
                    if b == 0:
                        S.dve(MEMSET(hTp[:, :, 0:1], 0.0), w=[kh])
                        S.dma(hTp[:, :, 1:512], src[:, :, 0:511], r=["hT_d"], w=[kh])
                    else:
                        S.dma(hTp[:], src[:, :, b * 512 - 1:b * 512 + 511], r=["hT_d"], w=[kh])
                    if b == 1:
                        chk(9)
                    for gi, (c0, c1) in enumerate(((0, 128), (128, 256), (256, 288))):
                        if gi == 2 and l == 0:
                            continue
                        rows = c1 - c0
                        bk, kb = nb()
                        for kc in range(8):
                            S.pe(MM(bk[0:rows, :], LA1[:, kc, c0:c1], hTb[:, kc, :], kc == 0, False), r=[kh, "LA1"], w=[kb])
                        for kc in range(8):
                            S.pe(MM(bk[0:rows, :], LA2[:, kc, c0:c1], hTp[:, kc, :], False, kc == 7), r=[kh, "LA2"], w=[kb])
                        if gi == 0:
                            S.act(ACTF(L1wa[0:64, :], bk[0:64, :], AF.Tanh), r=[kb], w=["L1wa"])
                            S.act(ACTF(L1wa[64:128, :], bk[64:128, :], AF.Copy), r=[kb], w=["L1wa"])
                        elif gi == 1:
                            S.act(ACTF(sgt[:], bk[:], AF.Tanh, scale=0.5), r=[kb], w=["sgt"])
                            S.dve(TS(sg[:], sgt[:], 0.5, 0.5, ALU.mult, ALU.add), r=["sgt"], w=["sg"])
                        else:
                            S.act(ACTF(L1v[:], bk[0:32, :], AF.Copy), r=[kb], w=["L1v"])
                    chk(2)
                    for j in range(4):
                        t = b * 4 + j
                        lo = j * 128
                        tsl = slice(j * 128, (j + 1) * 128)
                        if l > 0:
                            S.dma(vf[:], vfirst_d[t * 128:(t + 1) * 128, :], r=["vfirst_d"], w=["vf"])
                        for g, (dst, kd) in enumerate(((r32, "r32"), (k32, "k32"), (v32, "v32"))):
                            bk, kb = nb()
                            for kc in range(8):
                                S.pe(MM(bk[:], hTb[:, kc, lo:lo + 128], WT1[:, kc, g * 512:(g + 1) * 512], kc == 0, False), r=[kh, "WT1"], w=[kb])
                            for kc in range(8):
                                S.pe(MM(bk[:], hTp[:, kc, lo:lo + 128], WT2[:, kc, g * 512:(g + 1) * 512], False, kc == 7), r=[kh, "WT2"], w=[kb])
                            evac(dst[:], bk[:], [kb], [kd])
                        bkw, kbw = nb()
                        S.pe(MM(bkw[:], L1wa[0:64, tsl], Bwa[0:64, :], True, False), r=["L1wa", "Bwa"], w=[kbw])
                        S.pe(MM(bkw[:], onesf[0:1, :], brow[0:1, 0, :], False, True), r=["onesf", "brow"], w=[kbw])
                        bka, kba = nb()
                        S.pe(MM(bka[:], L1wa[64:128, tsl], Bwa[64:128, :], True, False), r=["L1wa", "Bwa"], w=[kba])
                        S.pe(MM(bka[:], onesf[0:1, :], brow[0:1, 1, :], False, True), r=["onesf", "brow"], w=[kba])
                        bkg, kbg = nb()
                        S.pe(MM(bkg[:], sg[:, tsl], Bg[:], True, True), r=["sg", "Bg"], w=[kbg])
                        S.act(ACTF(lw[:], bkw[:], AF.Tanh, scale=0.5), r=[kbw], w=["lw"])
                        S.dve(TS(lw[:], lw[:], -0.5 * math.exp(-0.5), -0.5 * math.exp(-0.5), ALU.mult, ALU.add), r=["lw"], w=["lw"])
                        S.act(ACTF(a32[:], bka[:], AF.Tanh, scale=0.5), r=[kba], w=["a32"])
                        S.pool(TS(a32[:], a32[:], 0.5, 0.5, ALU.mult, ALU.add), r=["a32"], w=["a32"])
                        S.act(ACTF(g32[:], bkg[:], AF.Copy), r=[kbg], w=["g32"])
                        if l > 0:
                            bkv, kbv = nb()
                            S.pe(MM(bkv[:], L1v[0:32, tsl], Bv[0:32, :], True, False), r=["L1v", "Bv"], w=[kbv])
                            S.pe(MM(bkv[:], onesf[0:1, :], brow[0:1, 2, :], False, True), r=["onesf", "brow"], w=[kbv])
                            S.act(ACTF(t1[:], bkv[:], AF.Tanh, scale=0.5), r=[kbv], w=["t1"])
                            S.pool(TS(t1[:], t1[:], 0.5, 0.5, ALU.mult, ALU.add), r=["t1"], w=["t1"])
                            S.pool(TT(t2[:], vf[:], v32[:], ALU.subtract), r=["vf", "v32"], w=["t2"])
                            S.pool(TT(t2[:], t2[:], t1[:], ALU.mult), r=["t2", "t1"], w=["t2"])
                            S.pool(TT(v32[:], v32[:], t2[:], ALU.add), r=["v32", "t2"], w=["v32"])
                        else:
                            S.dma(vfirst_d[t * 128:(t + 1) * 128, :], v32[:], r=["v32"], w=["vfirst_d"])
                        S.act(ACTF(Vb[:], v32[:], AF.Copy), r=["v32"], w=["Vb"])
                        if dbg:
                            rows = slice(t * 128, (t + 1) * 128)
                            S.dma(dbg_d["dbg_v"][rows, :], v32[:], r=["v32"], w=["dbgv"])
                            S.dma(dbg_d["dbg_r"][rows, :], r32[:], r=["r32"], w=["dbgr"])
                            S.dma(dbg_d["dbg_k"][rows, :], k32[:], r=["k32"], w=["dbgk"])
                            S.dma(dbg_d["dbg_a"][rows, :], a32[:], r=["a32"], w=["dbga"])
                            S.dma(dbg_d["dbg_lw"][rows, :], lw[:], r=["lw"], w=["dbglw"])
                        bkc, kbc = nb()
                        S.pe(MM(bkc[:], tri_incl, lw[:], True, True), r=["cst", "lw"], w=[kbc])
                        bke, kbe = nb()
                        S.pe(MM(bke[:], tri_strict, lw[:], True, True), r=["cst", "lw"], w=[kbe])
                        bkG, kbG = nb()
                        for h in range(8):
                            S.pe(MM(bkG[0:64, h:h + 1], lw[:, h * 64:(h + 1) * 64], onesf[:, 0:1], True, True), r=["lw", "onesf"], w=[kbG])
                        S.act(ACTF(Ginc[:], bkc[:], AF.Exp), r=[kbc], w=["Ginc"])
                        S.act(ACTF(Ginv[:], bkc[:], AF.Exp, scale=-1.0), r=[kbc], w=["Ginv"])
                        S.act(ACTF(Gexc[:], bke[:], AF.Exp), r=[kbe], w=["Gexc"])
                        S.act(ACTF(GC[:], bkG[0:64, 0:8], AF.Exp), r=[kbG], w=["GC"])
                        S.dve(TT(kkr[:], k32[:], pbc[:, 0, :], ALU.mult), r=["k32", "pbc"], w=["kkr"])
                        S.act(ACTF(t2[:], kkr[:], AF.Square), r=["kkr"], w=["t2"])
                        S.dve(RED(s8[0][:], t2[:].rearrange("p (h d) -> p h d", h=8), ALU.add), r=["t2"], w=["s8_0"])
                        S.dve(TS(s8[0][:], s8[0][:], 1e-24, None, ALU.max), r=["s8_0"], w=["s8_0"])
                        S.pool(TT(s8[1][:], s8[0][:], m05[:, 0:8], ALU.pow), r=["s8_0", "m05"], w=["s8_1"])
                        S.dve(TT(kk[:].rearrange("p (h d) -> p h d", h=8), kkr[:].rearrange("p (h d) -> p h d", h=8),
                                 s8[1][:].unsqueeze(2).to_broadcast([128, 8, 64]), ALU.mult), r=["kkr", "s8_1"], w=["kk"])
                        S.pool(TT(b32[:], kk[:], a32[:], ALU.mult), r=["kk", "a32"], w=["b32"])
                        S.dve(STT(t1[:], a32[:], -1.0, pbc[:, 1, :], ALU.add, ALU.mult), r=["a32", "pbc"], w=["t1"])
                        S.dve(STT(km[:], t1[:], 1.0, k32[:], ALU.add, ALU.mult), r=["t1", "k32"], w=["km"])
                        S.dve(TT(Kd[:], kk[:], Gexc[:], ALU.mult), r=["kk", "Gexc"], w=["Kd"])
                        S.pool(TT(Bi[:], b32[:], Ginv[:], ALU.mult), r=["b32", "Ginv"], w=["Bi"])
                        S.dve(TT(Ki[:], km[:], Ginv[:], ALU.mult), r=["km", "Ginv"], w=["Ki"])
                        S.pool(TT(Rd[:], r32[:], Ginc[:], ALU.mult), r=["r32", "Ginc"], w=["Rd"])
                        S.pool(TT(t2[:], r32[:], km[:], ALU.mult), r=["r32", "km"], w=["t2"])
                        S.pool(TT(t2[:], t2[:], pbc[:, 2, :], ALU.mult), r=["t2", "pbc"], w=["t2"])
                        S.dve(RED(s8[2][:], t2[:].rearrange("p (h d) -> p h d", h=8), ALU.add), r=["t2"], w=["s8_2"])
                        chk(3)
                        for q, (srcT, ks) in enumerate(((Kd, "Kd"), (Rd, "Rd"), (Bi, "Bi"), (Ki, "Ki"))):
                            bk, kb = nb()
                            bkb = bfv(bk)
                            for h in range(8):
                                S.pe(TR(bkb[0:64, h * 128:(h + 1) * 128], srcT[:, h * 64:(h + 1) * 64], identb[:]), r=[ks, "identb"], w=[kb])
                            evac(FT[:, :, q, :], bkb[0:64, :].rearrange("p (h t) -> p h t", h=8), [kb], ["FT"])
                        for h in range(8):
                            bk, kb = nb()
                            rhs = FT[:, h, 0:2, :].rearrange("p a t -> p (a t)")
                            S.pe(MM(bk[:, 0:256], FT[:, h, 2, :], rhs, True, True), r=["FT"], w=[kb])
                            S.pe(MM(bk[:, 256:512], FT[:, h, 3, :], rhs, True, True), r=["FT"], w=[kb])
                            S.dve(TT(GE[:, h, :], bk[:], mask4[:], ALU.mult), r=[kb, "mask4"], w=["GE"])
                            S.dve(TT(NNb[0][:, h, :], bk[:, 0:128], tri_strict, ALU.mult), r=[kb, "cst"], w=["NNb0"])
                        for g in range(2):
                            bk, kb = nb()
                            for hh in range(4):
                                h = g * 4 + hh
                                S.pe(MM(bk[:, hh * 128:(hh + 1) * 128], FT[:, h, 0, :], FT[:, h, 2, :], True, True), r=["FT"], w=[kb])
                            S.dve(TT(MMb[0][:, g * 4:(g + 1) * 4, :], bk[:].rearrange("p (h t) -> p h t", h=4),
                                     lowm4[:].rearrange("p (h t) -> p h t", h=4), ALU.mult), r=[kb, "lowm4"], w=["MMb0"])
                        chk(4)
                        S.pool(TT(TTb[0][:], identf.unsqueeze(1).to_broadcast([128, 8, 128]), NNb[0][:], ALU.subtract),
                               r=["cst", "NNb0"], w=["TTb0"])
                        cur = 0
                        for step in range(1, 7):
                            nxt = 1 - cur
                            for g in range(2):
                                hs = range(g * 4, g * 4 + 4)
                                bkM, kbM = nb()
                                for hh, h in enumerate(hs):
                                    Nprev = NNb[cur][:, h, :]
                                    S.pe(MM(bkM[:, hh * 128:(hh + 1) * 128], Nprev, MMb[cur][:, h, :], True, True),
                                         r=["NNb%d" % cur, "MMb%d" % cur], w=[kbM])
                                evac(MMb[nxt][:, g * 4:(g + 1) * 4, :], bkM[:].rearrange("p (h t) -> p h t", h=4), [kbM], ["MMb%d" % nxt])
                                if step < 6:
                                    bkN, kbN = nb()
                                    for hh, h in enumerate(hs):
                                        Nprev = NNb[cur][:, h, :]
                                        S.pe(MM(bkN[:, hh * 128:(hh + 1) * 128], MMb[cur][:, h, :], Nprev, True, True),
                                             r=["NNb%d" % cur, "MMb%d" % cur], w=[kbN])
                                    evac(NNb[nxt][:, g * 4:(g + 1) * 4, :], bkN[:].rearrange("p (h t) -> p h t", h=4), [kbN], ["NNb%d" % nxt])
                                bkT, kbT = nb()
                                for hh, h in enumerate(hs):
                                    S.pe(MM(bkT[:, hh * 128:(hh + 1) * 128], MMb[nxt][:, h, :], TTb[cur][:, h, :], True, False),
                                         r=["MMb%d" % nxt, "TTb%d" % cur], w=[kbT])
                                    S.pe(MM(bkT[:, hh * 128:(hh + 1) * 128], identf, TTb[cur][:, h, :], False, True),
                                         r=["cst", "TTb%d" % cur], w=[kbT])
                                evac(TTb[nxt][:, g * 4:(g + 1) * 4, :], bkT[:].rearrange("p (h t) -> p h t", h=4), [kbT], ["TTb%d" % nxt])
                            cur = nxt
                        chk(5)
                        S.act(ACTF(TTh[:], TTb[cur][:], AF.Copy), r=["TTb%d" % cur], w=["TTh"])
                        TTf = TTh
                        kT = "TTh"
                        for g in range(2):
                            bk, kb = nb()
                            for hh in range(4):
                                h = g * 4 + hh
                                S.pe(MM(bk[0:64, hh * 128:(hh + 1) * 128], Kd[:, h * 64:(h + 1) * 64], TTf[:, h, :], True, True), r=["Kd", kT], w=[kb])
                            evac(WTs[:, g * 4:(g + 1) * 4, :], bk[0:64, :].rearrange("p (h t) -> p h t", h=4), [kb], ["WTs"])
                        bk, kb = nb()
                        for h in range(8):
                            S.pe(MM(bk[:, h * 64:(h + 1) * 64], GE[:, h, 256:384], Vb[:, h * 64:(h + 1) * 64], True, True), r=["GE", "Vb"], w=[kb])
                        evac(Zb[:], bk[:], [kb], ["Zb"])
                        bk, kb = nb()
                        for h in range(8):
                            S.pe(MM(bk[:, h * 64:(h + 1) * 64], TTf[:, h, :], Zb[:, h * 64:(h + 1) * 64], True, True), r=[kT, "Zb"], w=[kb])
                        evac(U0[:], bk[:], [kb], ["U0"])
                        chk(6)
                        bk, kb = nb()
                        for h in range(8):
                            S.pe(MM(bk[:, h * 64:(h + 1) * 64], WTs[:, h, :], Pb[:, h, :], True, True), r=["WTs", "Pb"], w=[kb])
                        S.dve(STT(Ub[:], bk[:], -1.0, U0[:], ALU.mult, ALU.subtract), r=[kb, "U0"], w=["Ub"])
                        bkY, kbY = nb()
                        for h in range(8):
                            hs_ = slice(h * 64, (h + 1) * 64)
                            S.pe(MM(bkY[:, hs_], FT[:, h, 1, :], Pb[:, h, :], True, False), r=["FT", "Pb"], w=[kbY])
                            S.pe(MM(bkY[:, hs_], GE[:, h, 128:256], Ub[:, hs_], False, False), r=["GE", "Ub"], w=[kbY])
                            S.pe(MM(bkY[:, hs_], GE[:, h, 384:512], Vb[:, hs_], False, True), r=["GE", "Vb"], w=[kbY])
                        bkX, kbX = nb()
                        S.pe(MM(bkX[0:64, :], identf[0:64, 0:64], P32f, True, False), r=["cst", "P32"], w=[kbX])
                        for h in range(8):
                            hs_ = slice(h * 64, (h + 1) * 64)
                            S.pe(MM(bkX[0:64, hs_], Bi[:, hs_], Ub[:, hs_], False, False), r=["Bi", "Ub"], w=[kbX])
                            S.pe(MM(bkX[0:64, hs_], Ki[:, hs_], Vb[:, hs_], False, h == 7), r=["Ki", "Vb"], w=[kbX])
                        S.dve(TT(P32[:], bkX[0:64, :].rearrange("p (h v) -> p h v", h=8), GC[:].unsqueeze(2).to_broadcast([64, 8, 64]), ALU.mult),
                              r=[kbX, "GC"], w=["P32"])
                        S.act(ACTF(Pb[:], P32[:], AF.Copy), r=["P32"], w=["Pb"])
                        chk(7)
                        S.act(ACTF(y32[:], bkY[:], AF.Copy), r=[kbY], w=["y32"])
                        if dbg:
                            S.dma(dbg_d["dbg_y"][t * 128:(t + 1) * 128, :], y32[:], r=["y32"], w=["dbgy"])
                        chk(10)
                        Y3 = y32[:].rearrange("p (h d) -> p h d", h=8)
                        S.dve(RED(s8[3][:], Y3, ALU.add), r=["y32"], w=["s8_3"])
                        S.dve(TS(s8[3][:], s8[3][:], 1.0 / 64, None, ALU.mult), r=["s8_3"], w=["s8_3"])
                        S.dve(TT(cen[:].rearrange("p (h d) -> p h d", h=8), Y3, s8[3][:].unsqueeze(2).to_broadcast([128, 8, 64]), ALU.subtract),
                              r=["y32", "s8_3"], w=["cen"])
                        S.act(ACTF(t2[:], cen[:], AF.Square), r=["cen"], w=["t2"])
                        S.dve(RED(s8[4][:], t2[:].rearrange("p (h d) -> p h d", h=8), ALU.add), r=["t2"], w=["s8_4"])
                        S.dve(TS(s8[4][:], s8[4][:], 1.0 / 64, 64e-5, ALU.mult, ALU.add), r=["s8_4"], w=["s8_4"])
                        S.pool(TT(s8[5][:], s8[4][:], m05[:, 0:8], ALU.pow), r=["s8_4", "m05"], w=["s8_5"])
                        S.dve(TT(cen[:].rearrange("p (h d) -> p h d", h=8), cen[:].rearrange("p (h d) -> p h d", h=8),
                                 s8[5][:].unsqueeze(2).to_broadcast([128, 8, 64]), ALU.mult), r=["cen", "s8_5"], w=["cen"])
                        S.pool(TT(cen[:], cen[:], pbc[:, 3, :], ALU.mult), r=["cen", "pbc"], w=["cen"])
                        S.pool(TT(cen[:], cen[:], pbc[:, 4, :], ALU.add), r=["cen", "pbc"], w=["cen"])
                        S.dve(TT(t2[:].rearrange("p (h d) -> p h d", h=8), v32[:].rearrange("p (h d) -> p h d", h=8),
                                 s8[2][:].unsqueeze(2).to_broadcast([128, 8, 64]), ALU.mult), r=["v32", "s8_2"], w=["t2"])
                        S.pool(TT(cen[:], cen[:], t2[:], ALU.add), r=["cen", "t2"], w=["cen"])
                        S.dve(TT(ob[:], cen[:], g32[:], ALU.mult), r=["cen", "g32"], w=["ob"])
                        chk(11)
                        bk, kb = nb()
                        bkb = bfv(bk)
                        for c in range(4):
                            S.pe(TR(bkb[:, c * 128:(c + 1) * 128], ob[:, c * 128:(c + 1) * 128], identb[:]), r=["ob", "identb"], w=[kb])
                        evac(rwT[:], bkb[:, 0:512].rearrange("p (c t) -> p c t", c=4), [kb], ["rwT"])
                        S.dma(rwkvT_d[:, t * 128:(t + 1) * 128].rearrange("(c p) t -> p c t", p=128), rwT[:], r=["rwT"], w=["rwkvT_d"])
                        chk(12)
                S.flush()

        def phase_B(l):
            with contextlib.ExitStack() as st:
                def sb(name, shape, dt):
                    return st.enter_context(SBT(name, shape, dt))
                c31b = sb("c31b", [128, 8], F32)
                nc31 = sb("nc31", [128, 8], F32)
                Rn = sb("Rn", [128, 8, 256], BF16)
                aog = sb("aog", [64, 8], F32)
                statL = sb("statL", [65, 64], F32)
                S.dma(c31b[:], c31_d.partition_broadcast(128), w=["c31b"])
                aog8 = sb("aog8", [8, 64], F32)
                S.dma(aog8[:], W["attn_out_g"][l].rearrange("(h d) -> h d", d=64), w=["aog8"])
                bk, kb = nb()
                S.pe(MM(bk[0:64, 0:8], aog8[:], identf[0:8, 0:8], True, True), r=["aog8", "cst"], w=[kb])
                S.dve(CP(aog[:], bk[0:64, 0:8]), r=[kb], w=["aog"])
                S.dve(TS(nc31[:], c31b[:], -1.0, None, ALU.mult), r=["c31b"], w=["nc31"])
                with contextlib.ExitStack() as st2:
                    bnr = st2.enter_context(SBT("bnr", [128, 2048], F32))
                    S.dma(bnr[:], bnear_d, w=["bnr"])
                    for h in range(8):
                        S.act(ACTF(Rn[:, h, :], bnr[:, h * 256:(h + 1) * 256], AF.Exp, bias=nc31[:, h:h + 1]), r=["bnr", "nc31"], w=["Rn"])
                    S.flush()
                kaT = sb("kaT", [128, 4, T], BF16)
                kiT2 = sb("kiT2", [128, T], BF16)
                Va = sb("Va", [128, NT, 520], BF16)
                qaTb = [sb("qaTb%d" % i, [128, 4, 512], BF16) for i in range(2)]
                qiTb = [sb("qiTb%d" % i, [128, 4, 512], BF16) for i in range(2)]
                wib = [sb("wib%d" % i, [128, 4, 8], F32) for i in range(2)]
                wabs = sb("wabs", [128, 4, 8], F32)
                wsgn = sb("wsgn", [128, 4, 8], F32)
                sc = sb("sc", [128, T], F32)
                scj = sb("scj", [128, T], BF16)
                rl = [sb("rl%d" % i, [128, 512], F32) for i in range(3)]
                msk = sb("msk", [128, T], BF16)
                maskT = sb("maskT", [128, NT, 512], BF16)
                Eb = [sb("Eb%d" % i, [128, 512], BF16) for i in range(3)]
                Pt = [sb("Pt%d" % i, [128, 512], BF16) for i in range(3)]
                attTb = sb("attTb", [128, 4, 512], BF16)
                Osb = sb("Osb", [65, 512], F32)
                SQ = sb("SQ", [65, 512], F32)
                rs = sb("rs", [64, 512], F32)
                bis = [sb("bis%d" % i, [128, 1], F32) for i in range(6)]
                wtab = sb("wtab", [128, NBIS + 2], F32)
                ctab = sb("ctab", [128, NBIS + 2], F32)
                S.dma(kaT[:], featT_d[512:1024, :].rearrange("(c p) t -> p c t", p=128), r=["featT_d"], w=["kaT"])
                S.dma(kiT2[0:64, :], featT_d[1536:1600, :], r=["featT_d"], w=["kiT2"])
                S.dma(kiT2[64:128, :], featT_d[1536:1600, :], r=["featT_d"], w=["kiT2"])
                vsrc = vaug_d.rearrange("(n p) c -> p n c", p=128)
                for n0 in range(0, NT, 8):
                    S.dma(Va[:, n0:n0 + 8, :], vsrc[:, n0:n0 + 8, :], r=["vaug_d"], w=["Va"])
                S.dve(MEMSET(statL[0:64, :], 1.0 / 64), w=["statL"])
                S.dve(MEMSET(statL[64:65, :], 1e-6), w=["statL"])
                for k in range(NBIS + 2):
                    S.pool(MEMSET(ctab[:, k:k + 1], 2.0 ** (-k)), w=["ctab"])
                cw = 0.125 * (8 ** -0.5)

                def loadq(b):
                    i = b % 2
                    S.dma(qaTb[i][:], featT_d[0:512, b * 512:(b + 1) * 512].rearrange("(c p) t -> p c t", p=128), r=["featT_d"], w=["qaTb%d" % i])
                    S.dma(qiTb[i][:], featT_d[1024:1536, b * 512:(b + 1) * 512].rearrange("(c p) t -> p c t", p=128), r=["featT_d"], w=["qiTb%d" % i])
                    S.dma(wib[i][:], wi_d[b * 512:(b + 1) * 512, :].rearrange("(j p) h -> p j h", p=128), r=["wi_d"], w=["wib%d" % i])
                loadq(0)
                eidx = [0]
                for b in range(NB):
                    if b + 1 < NB:
                        loadq(b + 1)
                    i2 = b % 2
                    qa, qi, wi_ = qaTb[i2], qiTb[i2], wib[i2]
                    kqa, kqi, kwi = "qaTb%d" % i2, "qiTb%d" % i2, "wib%d" % i2
                    S.dve(STT(wabs[:], wi_[:], -1.0, wi_[:], ALU.mult, ALU.max), r=[kwi], w=["wabs"])
                    S.dve(TS(wabs[:], wabs[:], cw, None, ALU.mult), r=["wabs"], w=["wabs"])
                    S.act(ACTF(wsgn[:], wi_[:], AF.Sign), r=[kwi], w=["wsgn"])
                    nk = 4 * b + 4
                    for jq in range(4):
                        j = 4 * b + jq
                        Lk = (j + 1) * 128
                        qsl = slice(jq * 128, (jq + 1) * 128)
                        nch = (Lk + 511) // 512
                        for kc in range(nch):
                            ncol = min(512, Lk - kc * 512)
                            csl = slice(kc * 512, kc * 512 + ncol)
                            for ih in range(8):
                                pr, hf = ih // 2, ih % 2
                                ps_ = slice(hf * 64, (hf + 1) * 64)
                                bk, kb = nb()
                                S.pe(MM(bk[:, 0:ncol], qi[ps_, pr, qsl], kiT2[ps_, csl], True, True), r=[kqi, "kiT2"], w=[kb])
                                rb = rl[eidx[0] % 3]
                                krb = "rl%d" % (eidx[0] % 3)
                                eidx[0] += 1
                                S.act(ACTF(rb[:, 0:ncol], bk[:, 0:ncol], AF.Relu, scale=wabs[:, jq, ih:ih + 1]), r=[kb, "wabs"], w=[krb])
                                if ih == 0:
                                    S.dve(TS(sc[:, csl], rb[:, 0:ncol], wsgn[:, jq, 0:1], None, ALU.mult), r=[krb, "wsgn"], w=["sc"])
                                else:
                                    S.dve(STT(sc[:, csl], rb[:, 0:ncol], wsgn[:, jq, ih:ih + 1], sc[:, csl], ALU.mult, ALU.add), r=[krb, "wsgn", "sc"], w=["sc"])
                        A_, mid, cnt, inc = bis[0], bis[1], bis[2], bis[3]
                        S.dve(lambda e, A_=A_, Lk=Lk: e.tensor_reduce(out=A_[:], in_=sc[:, 0:Lk], axis=AX.X, op=ALU.max, apply_absolute_value=True),
                              r=["sc"], w=["bis0"])
                        S.dve(TS(A_[:], A_[:], 1.001, 1e-6, ALU.mult, ALU.add), r=["bis0"], w=["bis0"])
                        S.dve(TT(sc[:, j * 128:(j + 1) * 128], sc[:, j * 128:(j + 1) * 128], caus, ALU.add), r=["sc", "cst"], w=["sc"])
                        if dbg:
                            S.dma(dbg_d["dbg_score"][j * 128:(j + 1) * 128, 0:Lk], sc[:, 0:Lk], r=["sc"], w=["dbgs"])
                        S.dve(TS(wtab[:], ctab[:], A_[:, 0:1], None, ALU.mult), r=["ctab", "bis0"], w=["wtab"])
                        S.dve(TS(mid[:], A_[:], 0.0, None, ALU.mult), r=["bis0"], w=["bis1"])
                        for k in range(1, NBIS + 1):
                            if k % 2 == 1:
                                S.dve(TS(scj[:, 0:Lk], sc[:, 0:Lk], mid[:, 0:1], 0.0, ALU.is_ge, ALU.add, accum=cnt[:]), r=["sc", "bis1"], w=["bis2"])
                                S.dve(STT(inc[:], cnt[:], NSEL - 0.5, wtab[:, k - 1:k], ALU.is_ge, ALU.mult), r=["bis2", "wtab"], w=["bis3"])
                            else:
                                S.dve(TS(bis[4][:], mid[:], -1.0, None, ALU.mult), r=["bis1"], w=["bis4"])
                                S.act(ACTF(scj[:, 0:Lk], sc[:, 0:Lk], AF.Sign, bias=bis[4][:, 0:1], accum=cnt[:]), r=["sc", "bis4"], w=["bis2"])
                                S.dve(STT(inc[:], cnt[:], 2.0 * NSEL - 1.0 - Lk, wtab[:, k - 1:k], ALU.is_ge, ALU.mult), r=["bis2", "wtab"], w=["bis3"])
                            S.dve(STT(mid[:], inc[:], wtab[:, k:k + 1], mid[:], ALU.subtract, ALU.add), r=["bis3", "wtab", "bis1"], w=["bis1"])
                        S.dve(TT(bis[5][:], mid[:], wtab[:, NBIS:NBIS + 1], ALU.subtract), r=["bis1", "wtab"], w=["bis5"])
                        if dbg:
                            S.dma(dbg_d["dbg_thr"][j * 128:(j + 1) * 128, :], bis[5][:], r=["bis5"], w=["dbgt"])
                        S.dve(TS(msk[:, 0:Lk], sc[:, 0:Lk], bis[5][:, 0:1], None, ALU.is_ge), r=["sc", "bis5"], w=["msk"])
                        for i0 in range(0, j + 1, 8):
                            n8 = min(8, j + 1 - i0)
                            bk, kb = nb()
                            bkb = bfv(bk)
                            for ii in range(n8):
                                S.pe(TR(bkb[:, ii * 128:(ii + 1) * 128], msk[:, (i0 + ii) * 128:(i0 + ii + 1) * 128], identb[:]), r=["msk", "identb"], w=[kb])
                            S.act(ACTF(maskT[:, i0:i0 + n8, qsl], bkb[:, 0:n8 * 128].rearrange("p (i t) -> p i t", i=n8), AF.Copy), r=[kb], w=["maskT"])
                    for h in range(8):
                        pr, hf = h // 2, h % 2
                        ps_ = slice(hf * 64, (hf + 1) * 64)
                        bkO, kbO = banks[6 + h % 2], "bank%d" % (6 + h % 2)
                        for i in range(nk):
                            m = i - 4 * b
                            c0 = max(0, m) * 128
                            ncol = 512 - c0
                            bk, kb = nb()
                            S.pe(MM(bk[:, 0:ncol], kaT[ps_, pr, i * 128:(i + 1) * 128], qa[ps_, pr, c0:512], True, True), r=["kaT", kqa], w=[kb])
                            ei = eidx[0] % 3
                            eidx[0] += 1
                            E, kE = Eb[ei], "Eb%d" % ei
                            P_, kP = Pt[ei], "Pt%d" % ei
                            S.act(ACTF(E[:, 0:ncol], bk[:, 0:ncol], AF.Exp, bias=c31b[:, h:h + 1], scale=0.125), r=[kb, "c31b"], w=[kE])
                            eng = S.dve if (eidx[0] % 2) else S.pool
                            eng(TT(P_[:, 0:ncol], E[:, 0:ncol], maskT[:, i, c0:512], ALU.mult), r=[kE, "maskT"], w=[kP])
                            if m >= 0:
                                nn = min(256, ncol)
                                eng(TT(P_[:, 0:nn], P_[:, 0:nn], Rn[:, h, 0:nn], ALU.mult), r=[kP, "Rn"], w=[kP])
                            elif m == -1:
                                eng(TT(P_[:, 0:128], P_[:, 0:128], Rn[:, h, 128:256], ALU.mult), r=[kP, "Rn"], w=[kP])
                            S.pe(MM(bkO[0:65, c0:512], Va[:, i, h * 65:(h + 1) * 65], P_[:, 0:ncol], i == 0, i == nk - 1), r=["Va", kP], w=[kbO])
                        S.act(ACTF(Osb[:], bkO[0:65, :], AF.Copy), r=[kbO], w=["Osb"])
                        S.act(ACTF(SQ[:], bkO[0:65, :], AF.Square), r=[kbO], w=["SQ"])
                        bk, kb = nb()
                        S.pe(MM(bk[0:64, :], statL[:], SQ[:], True, True), r=["statL", "SQ"], w=[kb])
                        S.act(ACTF(rs[:], bk[0:64, :], AF.Ln), r=[kb], w=["rs"])
                        S.act(ACTF(rs[:], rs[:], AF.Exp, scale=-0.5), r=["rs"], w=["rs"])
                        S.dve(STT(attTb[ps_, pr, :], Osb[0:64, :], aog[:, h:h + 1], rs[:], ALU.mult, ALU.mult), r=["Osb", "aog", "rs"], w=["attTb"])
                    S.dma(attT_d[:, b * 512:(b + 1) * 512].rearrange("(c p) t -> p c t", p=128), attTb[:], r=["attTb"], w=["attT_d"])
                S.flush()

        def phase_B2(l, xsrc):
            with contextlib.ExitStack() as st:
                def sb(name, shape, dt):
                    return st.enter_context(SBT(name, shape, dt))
                wo = sb("wo", [128, 8, 1024], BF16)
                gt1 = sb("gt1", [128, 1024], F32)
                A2t = sb("A2t", [128, 1024], F32)
                sh2t = sb("sh2t", [128, 1024], F32)
                xb = [sb("xb%d" % i, [128, 1024], F32) for i in range(2)]
                mixT = [sb("mixT%d" % i, [128, 8, 128], BF16) for i in range(2)]
                x1 = [sb("x1_%d" % i, [128, 1024], F32) for i in range(2)]
                junk = sb("junk", [128, 1024], F32)
                tmp = sb("tmpn", [128, 1024], F32)
                tmp2 = sb("tmpm", [128, 1024], F32)
                hb = [sb("hb%d" % i, [128, 1024], BF16) for i in range(2)]
                h2s = [sb("h2s%d" % i, [128, 8, 128], BF16) for i in range(2)]
                ssq = [sb("ssq%d" % i, [128, 1], F32) for i in range(2)]
                ms = [sb("ms%d" % i, [128, 1], F32) for i in range(2)]
                rstd = [sb("rstd%d" % i, [128, 1], F32) for i in range(2)]
                wov = W["w_out"][l].rearrange("(kc p) n -> p kc n", p=128)
                S.dma(wo[:], wov, w=["wo"], eng="pool")
                S.dma(gt1[:], mod_d[:, 2048:3072], r=["mod_d"], w=["modp"])
                S.dma(A2t[:], mod_d[:, 4096:5120], r=["mod_d"], w=["modp"])
                S.dma(sh2t[:], mod_d[:, 3072:4096], r=["mod_d"], w=["modp"])

                def load(t):
                    i = t % 2
                    S.dma(xb[i][:], xsrc[t * 128:(t + 1) * 128, :], r=["xs_d"], w=["xb%d" % i])
                    S.dma(mixT[i][:, 0:4, :], rwkvT_d[:, t * 128:(t + 1) * 128].rearrange("(c p) t -> p c t", p=128), r=["rwkvT_d"], w=["mixT%d" % i])
                    S.dma(mixT[i][:, 4:8, :], attT_d[:, t * 128:(t + 1) * 128].rearrange("(c p) t -> p c t", p=128), r=["attT_d"], w=["mixT%d" % i])
                load(0)
                for t in range(NT):
                    if t + 1 < NT:
                        load(t + 1)
                    i = t % 2
                    for hf in range(2):
                        bk, kb = nb()
                        for kc in range(8):
                            S.pe(MM(bk[:], mixT[i][:, kc, :], wo[:, kc, hf * 512:(hf + 1) * 512], kc == 0, kc == 7), r=["mixT%d" % i, "wo"], w=[kb])
                        csl = slice(hf * 512, (hf + 1) * 512)
                        S.dve(TT(tmp2[:, csl], bk[:], gt1[:, csl], ALU.mult), r=[kb, "modp"], w=["tmpm"])
                    S.pool(TT(x1[i][:], tmp2[:], xb[i][:], ALU.add), r=["tmpm", "xb%d" % i], w=["x1_%d" % i])
                    S.dma(xs_d[t * 128:(t + 1) * 128, :], x1[i][:], r=["x1_%d" % i], w=["xs_d2"])
                    norm_mod_T(x1[i][:], "x1_%d" % i, A2t[:], sh2t[:], hb[i], "hb%d" % i, junk, ssq[i], ms[i], rstd[i], tmp, i,
                               h2s[i][:], "h2s%d" % i)
                    S.dma(h2T_d[:, t * 128:(t + 1) * 128].rearrange("(kc p) t -> p kc t", p=128), h2s[i][:], r=["h2s%d" % i], w=["h2T_d"])
                S.flush()

        def phase_C(l, last):
            TB = 256
            NBC = T // TB
            with contextlib.ExitStack() as st:
                def sb(name, shape, dt):
                    return st.enter_context(SBT(name, shape, dt))
                w1 = sb("w1", [128, 8, 4096], BF16)
                w2 = sb("w2", [128, 32, 1024], BF16)
                gt2 = sb("gt2", [128, 1024], F32)
                fg = sb("fg", [128, 1024], F32)
                h2b = [sb("h2b%d" % i, [128, 8, TB], BF16) for i in range(2)]
                uT = sb("uT", [128, 32, TB], BF16)
                sq = [sb("sq%d" % i, [128, TB], F32) for i in range(2)]
                xb = [sb("xb%d" % i, [128, 1024], F32) for i in range(2)]
                x2 = [sb("x2_%d" % i, [128, 1024], F32) for i in range(2)]
                tmp2 = sb("tmpm", [128, 1024], F32)
                junk = sb("junk", [128, 1024], F32)
                ssq = [sb("ssq%d" % i, [128, 1], F32) for i in range(2)]
                ms = [sb("ms%d" % i, [128, 1], F32) for i in range(2)]
                rstd = [sb("rstd%d" % i, [128, 1], F32) for i in range(2)]
                w1v = W["w_mlp1"][l].rearrange("(kc p) n -> p kc n", p=128)
                w2v = W["w_mlp2"][l].rearrange("(fc p) n -> p fc n", p=128)
                for q in range(4):
                    S.dma(w1[:, :, q * 1024:(q + 1) * 1024], w1v[:, :, q * 1024:(q + 1) * 1024], w=["w1"], eng="pool")
                for q in range(4):
                    S.dma(w2[:, q * 8:(q + 1) * 8, :], w2v[:, q * 8:(q + 1) * 8, :], w=["w2"], eng="pool")
                S.dma(gt2[:], mod_d[:, 5120:6144], r=["mod_d"], w=["modp"])
                if last:
                    S.dma(fg[:], W["final_g"].partition_broadcast(128), w=["fg"])

                def load(bb):
                    i = bb % 2
                    S.dma(h2b[i][:], h2T_d[:, bb * TB:(bb + 1) * TB].rearrange("(kc p) t -> p kc t", p=128), r=["h2T_d"], w=["h2b%d" % i])
                load(0)
                xi = [0]
                for bb in range(NBC):
                    if bb + 1 < NBC:
                        load(bb + 1)
                    i = bb % 2
                    for fc in range(32):
                        bk, kb = nb()
                        for kc in range(8):
                            S.pe(MM(bk[:, 0:TB], w1[:, kc, fc * 128:(fc + 1) * 128], h2b[i][:, kc, :], kc == 0, kc == 7), r=["w1", "h2b%d" % i], w=[kb])
                        s_ = sq[fc % 2]
                        ks_ = "sq%d" % (fc % 2)
                        S.act(ACTF(s_[:], bk[:, 0:TB], AF.Square), r=[kb], w=[ks_])
                        S.dve(STT(uT[:, fc, :], bk[:, 0:TB], 0.0, s_[:], ALU.is_gt, ALU.mult), r=[kb, ks_], w=["uT"])
                    for jj in range(TB // 128):
                        t = bb * (TB // 128) + jj
                        xi_ = xi[0] % 2
                        xi[0] += 1
                        S.dma(xb[xi_][:], xs_d[t * 128:(t + 1) * 128, :], r=["xs_d"], w=["xb%d" % xi_])
                        for hf in range(2):
                            bk, kb = nb()
                            for fc in range(32):
                                S.pe(MM(bk[:], uT[:, fc, jj * 128:(jj + 1) * 128], w2[:, fc, hf * 512:(hf + 1) * 512], fc == 0, fc == 31), r=["uT", "w2"], w=[kb])
                            csl = slice(hf * 512, (hf + 1) * 512)
                            S.dve(TT(tmp2[:, csl], bk[:], gt2[:, csl], ALU.mult), r=[kb, "modp"], w=["tmpm"])
                        S.pool(TT(x2[xi_][:], tmp2[:], xb[xi_][:], ALU.add), r=["tmpm", "xb%d" % xi_], w=["x2_%d" % xi_])
                        if not last:
                            S.dma(xs_d[t * 128:(t + 1) * 128, :], x2[xi_][:], r=["x2_%d" % xi_], w=["xs_d2"])
                        else:
                            S.act(ACTF(junk[:], x2[xi_][:], AF.Square, accum=ssq[xi_][:]), r=["x2_%d" % xi_], w=["ssq%d" % xi_])
                            S.dve(TS(ms[xi_][:], ssq[xi_][:], 1.0 / 1024, 1e-6, ALU.mult, ALU.add), r=["ssq%d" % xi_], w=["ms%d" % xi_])
                            S.pool(TT(rstd[xi_][:], ms[xi_][:], m05[:, 0:1], ALU.pow), r=["ms%d" % xi_, "m05"], w=["rstd%d" % xi_])
                            S.dve(STT(x2[xi_][:], x2[xi_][:], rstd[xi_][:, 0:1], fg[:], ALU.mult, ALU.mult), r=["x2_%d" % xi_, "rstd%d" % xi_, "fg"], w=["x2_%d" % xi_])
                            S.dma(out_d[t * 128:(t + 1) * 128, :], x2[xi_][:], r=["x2_%d" % xi_], w=["out_d"])
                S.flush()

        order = []
        for l in range(L):
            order += [("M", l), ("A1", l), ("A2", l), ("B", l), ("B2", l), ("C", l)]
        for ph, l in order:
            xsrc = x_d if l == 0 else xs_d
            if ph == "M":
                phase_M(l)
            elif ph == "A1":
                phase_A1(l, xsrc)
            elif ph == "A2":
                try:
                    phase_A2(l)
                except _Stop:
                    S.flush()
                    break
            elif ph == "B":
                phase_B(l)
            elif ph == "B2":
                phase_B2(l, xsrc)
            elif ph == "C":
                phase_C(l, l == L - 1)
            if stop_after == (ph, l):
                break
        S.flush(final=True)
        nops = S.nops
    return nc, nops


def _t5_bucket(n):
    n = np.maximum(n, 0)
    nf = np.maximum(n, 1).astype(np.float32)
    large = 16 + (np.log(nf / np.float32(16)) / np.float32(math.log(128 / 16)) * np.float32(16)).astype(np.int32)
    large = np.minimum(large, 31)
    return np.where(n < 16, n, large)


def _consts():
    s = np.arange(128)[:, None]
    t = np.arange(128)[None, :]
    c = np.zeros((128, 640), np.float32)
    c[:, 0:128] = (s == t)
    c[:, 128:256] = (s <= t)
    c[:, 256:384] = (s < t)
    c[:, 384:512] = (s > t)
    c[:, 512:640] = np.where(t <= s, 0.0, -1e30)
    return c


def host_inputs(inputs, T, L):
    B = inputs["x"].shape[0]
    f = lambda a: np.ascontiguousarray(np.asarray(a, dtype=np.float32))
    rel_bias = f(inputs["rel_bias"])
    tk = np.arange(128)[:, None, None, None]
    d = np.arange(2)[None, None, :, None]
    tq = np.arange(128)[None, None, None, :]
    hh = np.arange(8)[None, :, None, None]
    bidx = _t5_bucket(128 * d + tq - tk) + 0 * hh
    bnear = rel_bias[bidx, hh + 0 * bidx].reshape(128, 2048)
    shared = {"consts": _consts(), "bnear": f(bnear), "c31": f(rel_bias[31:32, :])}
    for k in WSHAPES:
        shared[k] = f(inputs[k])[:L]
    for k in VSHAPES:
        shared[k] = f(inputs[k])[:max(L - 1, 1)]
    shared["final_g"] = f(inputs["final_g"]).reshape(1, 1024)
    maps = []
    for bi in range(B):
        m = dict(shared)
        m["x"] = f(inputs["x"][bi, :T])
        m["c8"] = f(np.asarray(inputs["c"][bi]).reshape(8, 128).T)
        maps.append(m)
    return maps


_CACHE = {}


def kernel(**inputs):
    T = inputs["x"].shape[1]
    L = inputs["w_ada"].shape[0]
    B = inputs["x"].shape[0]
    key = (T, L)
    if key not in _CACHE:
        _CACHE[key] = build(T, L)[0]
    nc = _CACHE[key]
    maps = host_inputs(inputs, T, L)
    res = run_bass_kernel_spmd(nc, maps, core_ids=list(range(B)))
    return np.stack([np.asarray(r["out"], dtype=np.float32) for r in res.results], axis=0)
```

```python
import contextlib
import os
import math
import numpy as np
import ml_dtypes
import concourse.bass as bass
import concourse.mybir as mybir
from concourse.bass_utils import run_bass_kernel_spmd

F32 = mybir.dt.float32
BF16 = mybir.dt.bfloat16
AF = mybir.ActivationFunctionType
ALU = mybir.AluOpType
AX = mybir.AxisListType

ENG = ("pe", "dve", "act", "pool", "sp")
NDMA = 12
NBIS = 16


class _Stop(Exception):
    pass


_DEAD = [False]


def chk(n):
    if int(os.environ.get("A2STOP", "0")) == n:
        _DEAD[0] = True


class Op:
    __slots__ = ("eng", "fn", "deps", "signal", "sig_val", "dma", "dma_slot", "dma_val", "sem")

    def __init__(self, eng, fn, dma):
        self.eng = eng
        self.fn = fn
        self.deps = []
        self.signal = False
        self.sig_val = None
        self.dma = dma
        self.dma_slot = None
        self.dma_val = None
        self.sem = None


class Sched:
    def __init__(self, nc, stack):
        self.nc = nc
        self.stack = stack
        self.nsw = 0
        self.sw_dmas = []
        self.esem = {e: stack.enter_context(nc.semaphore("s_" + e)) for e in ENG if e != "sp"}
        self.dsem = [stack.enter_context(nc.semaphore("d_%d" % i)) for i in range(NDMA)]
        self.ops = {e: [] for e in ENG}
        self.last_w = {}
        self.readers = {}
        self.dma_count = 0
        self.dma_last = [None] * NDMA
        self.sigc = {e: 0 for e in ENG}
        self.waited = {e: {} for e in ENG}
        self.bar = {e: [] for e in ENG}
        self.nops = 0

    def _dep(self, op, prod):
        if prod is None or prod is op:
            return
        if (not prod.dma) and (not op.dma) and prod.eng == op.eng == "pe":
            return
        if not prod.dma:
            prod.signal = True
        op.deps.append(prod)

    def add(self, eng, fn, r=(), w=(), dma=False):
        op = Op(eng, fn, dma)
        if _DEAD[0]:
            return op
        if self.bar[eng]:
            for p in self.bar[eng]:
                op.deps.append(p)
            self.bar[eng] = []
        for k in r:
            self._dep(op, self.last_w.get(k))
        for k in w:
            self._dep(op, self.last_w.get(k))
            for rd in self.readers.get(k, ()):
                self._dep(op, rd)
        for k in r:
            self.readers.setdefault(k, []).append(op)
        for k in w:
            self.last_w[k] = op
            self.readers[k] = []
        if dma and eng == "pool":
            op.sem = self.stack.enter_context(self.nc.semaphore("w_%d" % self.nsw))
            op.dma_slot = "w%d" % self.nsw
            self.nsw += 1
            op.dma_val = 16
            self.sw_dmas.append(op)
        elif dma:
            slot = self.dma_count % NDMA
            self.dma_count += 1
            prev = self.dma_last[slot]
            op.dma_slot = slot
            op.sem = self.dsem[slot]
            op.dma_val = (prev.dma_val if prev else 0) + 16
            if prev is not None:
                op.deps.append(prev)
            self.dma_last[slot] = op
        self.ops[eng].append(op)
        self.nops += 1
        return op

    def pe(self, fn, r=(), w=()):
        return self.add("pe", fn, r, w)

    def dve(self, fn, r=(), w=()):
        return self.add("dve", fn, r, w)

    def act(self, fn, r=(), w=()):
        return self.add("act", fn, r, w)

    def pool(self, fn, r=(), w=()):
        return self.add("pool", fn, r, w)

    def dma(self, out, in_, r=(), w=(), eng="sp"):
        return self.add(eng, lambda e: e.dma_start(out=out, in_=in_), r, w, dma=True)

    def flush(self, final=False):
        nc = self.nc
        lasts = []
        for e in ENG:
            nd = [op for op in self.ops[e] if not op.dma]
            if nd:
                nd[-1].signal = True
                lasts.append(nd[-1])
        for e in ENG:
            for op in self.ops[e]:
                if op.signal and not op.dma:
                    self.sigc[e] += 1
                    op.sig_val = self.sigc[e]
        dlast = [p for p in self.dma_last if p is not None] + self.sw_dmas
        self.sw_dmas = []
        with nc.Block() as block:
            engobj = {"pe": block.tensor, "dve": block.vector, "act": block.scalar,
                      "pool": block.gpsimd, "sp": block.sync}
            for ename in ENG:
                ops = self.ops[ename]
                if not ops and not (final and ename == "sp"):
                    continue

                def body(e, ops=ops, ename=ename):
                    waited = self.waited[ename]
                    semof = {}
                    for op in ops:
                        need = {}
                        for p in op.deps:
                            if p.dma:
                                key = ("d", p.dma_slot)
                                semof[key] = p.sem
                                val = p.dma_val
                            else:
                                key = ("e", p.eng)
                                val = p.sig_val
                            if need.get(key, 0) < val:
                                need[key] = val
                        for key, val in need.items():
                            if waited.get(key, 0) >= val:
                                continue
                            waited[key] = val
                            sem = semof[key] if key[0] == "d" else self.esem[key[1]]
                            e.wait_ge(sem, val)
                        ins = op.fn(e)
                        if op.dma:
                            ins.then_inc(op.sem, 16)
                        elif op.signal:
                            ins.then_inc(self.esem[ename], 1)
                    if final and ename == "sp":
                        for p in dlast:
                            if waited.get(("d", p.dma_slot), 0) < p.dma_val:
                                e.wait_ge(p.sem, p.dma_val)
                        for p in lasts:
                            e.wait_ge(self.esem[p.eng], p.sig_val)
                engobj[ename](body)
        barrier = lasts + dlast
        self.ops = {e: [] for e in ENG}
        self.last_w = {}
        self.readers = {}
        self.bar = {e: list(barrier) for e in ENG}


def MM(out, lhsT, rhs, start=True, stop=True):
    return lambda e: e.matmul(out, lhsT=lhsT, rhs=rhs, start=start, stop=stop)


def TR(out, in_, ident):
    return lambda e: e.transpose(out, in_, ident)


def ACTF(out, in_, func, bias=0.0, scale=1.0, accum=None):
    if accum is None:
        return lambda e: e.activation(out=out, in_=in_, func=func, bias=bias, scale=scale)
    return lambda e: e.activation(out=out, in_=in_, func=func, bias=bias, scale=scale, accum_out=accum)


def TT(out, a, b, op):
    return lambda e: e.tensor_tensor(out=out, in0=a, in1=b, op=op)


def TS(out, a, s1, s2=None, op0=ALU.mult, op1=None, accum=None):
    if accum is not None:
        return lambda e: e.tensor_scalar(out=out, in0=a, scalar1=s1, scalar2=s2, op0=op0, op1=op1, accum_out=accum)
    if op1 is None:
        return lambda e: e.tensor_scalar(out=out, in0=a, scalar1=s1, scalar2=None, op0=op0)
    return lambda e: e.tensor_scalar(out=out, in0=a, scalar1=s1, scalar2=s2, op0=op0, op1=op1)


def STT(out, a, s, b, op0, op1):
    return lambda e: e.scalar_tensor_tensor(out=out, in0=a, scalar=s, in1=b, op0=op0, op1=op1)


def CP(out, in_):
    return lambda e: e.tensor_copy(out, in_)


def RED(out, in_, op, axis=AX.X, absval=False):
    if absval:
        return lambda e: e.tensor_reduce(out=out, in_=in_, axis=axis, op=op, apply_absolute_value=True)
    return lambda e: e.tensor_reduce(out=out, in_=in_, axis=axis, op=op)


def MEMSET(ap, v):
    return lambda e: e.memset(ap, v)


WSHAPES = {
    "w_ada": (1024, 6144), "b_ada": (6144,), "norm1_g": (1024,), "norm2_g": (1024,),
    "w_in": (1024, 3656), "mu_rkv": (3, 512), "mu_lora": (3, 1024), "decay_w0": (512,),
    "decay_a": (1024, 64), "decay_b": (64, 512), "iclr_a0": (512,), "iclr_a": (1024, 64),
    "iclr_b": (64, 512), "gate_a": (1024, 128), "gate_b": (128, 512), "k_k": (512,), "k_a": (512,),
    "r_k": (8, 64), "lnx_g": (512,), "lnx_b": (512,), "attn_out_g": (512,),
    "w_out": (1024, 1024), "w_mlp1": (1024, 4096), "w_mlp2": (4096, 1024),
}
VSHAPES = {"vres_mu": (1024,), "vres_v0": (512,), "vres_a": (1024, 32), "vres_b": (32, 512)}


def build(T, L, dbg=False, stop_after=None):
    _DEAD[0] = False
    NT = T // 128
    NB = T // 512
    NSEL = min(256, T // 4)
    nc = bass.Bass("TRN2", target_bir_lowering=False)
    W = {}

    def din(name, shape):
        W[name] = nc.dram_tensor(name, list(shape), F32, kind="ExternalInput").ap()
        return W[name]

    x_d = din("x", [T, 1024])
    c8_d = din("c8", [128, 8])
    consts_d = din("consts", [128, 640])
    bnear_d = din("bnear", [128, 2048])
    c31_d = din("c31", [1, 8])
    for k, s in WSHAPES.items():
        din(k, (L,) + s)
    for k, s in VSHAPES.items():
        din(k, (max(L - 1, 1),) + s)
    din("final_g", [1, 1024])
    out_d = nc.dram_tensor("out", [T, 1024], F32, kind="ExternalOutput").ap()

    def dscr(name, shape, dt):
        return nc.dram_tensor(name, list(shape), dt, kind=("ExternalOutput" if dbg else "Internal")).ap()

    xs_d = dscr("xs", [T, 1024], F32)
    mod_d = dscr("modd", [128, 6144], F32)
    featT_d = dscr("featT", [1600, T], BF16)
    vaug_d = dscr("vaug", [T, 520], BF16)
    wi_d = dscr("wid", [T, 8], F32)
    hT_d = dscr("hTd", [1024, T], BF16)
    vfirst_d = dscr("vfirst", [T, 512], F32)
    rwkvT_d = dscr("rwkvT", [512, T], BF16)
    attT_d = dscr("attT", [512, T], BF16)
    h2T_d = dscr("h2T", [1024, T], BF16)
    dbg_d = {}
    if dbg:
        for nm, shp in (("dbg_y", [T, 512]), ("dbg_score", [T, T]), ("dbg_thr", [T, 1]), ("dbg_v", [T, 512]),
                        ("dbg_r", [T, 512]), ("dbg_k", [T, 512]), ("dbg_a", [T, 512]), ("dbg_lw", [T, 512])):
            dbg_d[nm] = nc.dram_tensor(nm, shp, F32, kind="ExternalOutput").ap()

    uniq = [0]

    def SBT(name, shape, dt):
        uniq[0] += 1
        return nc.sbuf_tensor("%s_%d" % (name, uniq[0]), shape, dt)

    with contextlib.ExitStack() as gst:
        S = Sched(nc, gst)

        def gsb(name, shape, dt):
            return gst.enter_context(SBT(name, shape, dt))

        banks = [gst.enter_context(nc.psum_tensor("bank%d" % i, [128, 512], F32)) for i in range(8)]
        bank_i = [0]

        NROT = 6

        def nb():
            i = bank_i[0] % NROT
            bank_i[0] += 1
            return banks[i], "bank%d" % i

        def bfv(bank):
            return bank[:].bitcast(BF16)

        cst = gsb("cst", [128, 640], F32)
        identb = gsb("identb", [128, 128], BF16)
        mask4 = gsb("mask4", [128, 512], F32)
        lowm4 = gsb("lowm4", [128, 512], F32)
        onesf = gsb("onesf", [128, 128], F32)
        m05 = gsb("m05", [128, 512], F32)
        cbc = gsb("cbc", [128, 8, 128], F32)
        identf = cst[:, 0:128]
        tri_incl = cst[:, 128:256]
        tri_strict = cst[:, 256:384]
        low_strict = cst[:, 384:512]
        caus = cst[:, 512:640]

        with contextlib.ExitStack() as st:
            c8 = st.enter_context(SBT("c8s", [128, 8], F32))
            c8t = st.enter_context(SBT("c8t", [128, 8], F32))
            S.dma(cst[:], consts_d, w=["cst"])
            S.dma(c8[:], c8_d, w=["c8"])
            S.dve(CP(identb[:], identf), r=["cst"], w=["identb"])
            for i in range(4):
                S.dve(CP(mask4[:, i * 128:(i + 1) * 128], tri_strict if i % 2 == 0 else tri_incl), r=["cst"], w=["mask4"])
                S.pool(CP(lowm4[:, i * 128:(i + 1) * 128], low_strict), r=["cst"], w=["lowm4"])
            S.pool(MEMSET(onesf[:], 1.0), w=["onesf"])
            S.pool(MEMSET(m05[:], -0.5), w=["m05"])
            S.act(ACTF(c8t[:], c8[:], AF.Tanh, scale=0.5), r=["c8"], w=["c8t"])
            S.dve(TS(c8t[:], c8t[:], 0.5, 0.5, ALU.mult, ALU.add), r=["c8t"], w=["c8t"])
            S.dve(TT(c8t[:], c8t[:], c8[:], ALU.mult), r=["c8t", "c8"], w=["c8t"])
            S.dve(CP(cbc[:], c8t[:].unsqueeze(2).to_broadcast([128, 8, 128])), r=["c8t"], w=["cbc"])
            S.flush()

        def phase_M(l):
            with contextlib.ExitStack() as st:
                def sb(name, shape, dt):
                    return st.enter_context(SBT(name, shape, dt))
                wst = [sb("wst%d" % i, [128, 8, 512], F32) for i in range(2)]
                modt = sb("modt", [128, 6144], F32)
                gbc = sb("gbc", [128, 2048], F32)
                bada = sb("bada", [1, 6144], F32)
                S.dma(gbc[:, 0:1024], W["norm1_g"][l:l + 1, :].partition_broadcast(128), w=["gbc"])
                S.dma(gbc[:, 1024:2048], W["norm2_g"][l:l + 1, :].partition_broadcast(128), w=["gbc"])
                S.dma(bada[:], W["b_ada"][l:l + 1, :], w=["bada"])
                wa = W["w_ada"][l].rearrange("(kc p) n -> p kc n", p=128)
                for n in range(12):
                    buf = wst[n % 2]
                    key = "wst%d" % (n % 2)
                    S.dma(buf[:], wa[:, :, n * 512:(n + 1) * 512], w=[key])
                    bk, kb = nb()
                    for kc in range(8):
                        S.pe(MM(bk[:], cbc[:, kc, :], buf[:, kc, :], kc == 0, False), r=[key, "cbc"], w=[kb])
                    S.pe(MM(bk[:], onesf[0:1, :], bada[0:1, n * 512:(n + 1) * 512], False, True), r=["bada", "onesf"], w=[kb])
                    S.act(ACTF(modt[:, n * 512:(n + 1) * 512], bk[:], AF.Copy), r=[kb], w=["modt"])
                S.dve(STT(modt[:, 1024:2048], modt[:, 1024:2048], 1.0, gbc[:, 0:1024], ALU.add, ALU.mult), r=["modt", "gbc"], w=["modt"])
                S.dve(STT(modt[:, 4096:5120], modt[:, 4096:5120], 1.0, gbc[:, 1024:2048], ALU.add, ALU.mult), r=["modt", "gbc"], w=["modt"])
                S.dma(mod_d, modt[:], r=["modt"], w=["mod_d"])
                S.flush()

        def norm_mod_T(X, kx, At, sht, hb, khb, junk, ssq, ms, rstd, tmp, idx, dstT, kdst):
            S.act(ACTF(junk[:], X, AF.Square, accum=ssq[:]), r=[kx], w=["ssq%d" % idx])
            S.dve(TS(ms[:], ssq[:], 1.0 / 1024, 1e-6, ALU.mult, ALU.add), r=["ssq%d" % idx], w=["ms%d" % idx])
            S.pool(TT(rstd[:], ms[:], m05[:, 0:1], ALU.pow), r=["ms%d" % idx, "m05"], w=["rstd%d" % idx])
            S.dve(STT(tmp[:], X, rstd[:, 0:1], At, ALU.mult, ALU.mult), r=[kx, "rstd%d" % idx, "modp"], w=["tmpn"])
            S.pool(TT(hb[:], tmp[:], sht, ALU.add), r=["tmpn", "modp"], w=[khb])
            bk, kb = nb()
            bkb = bfv(bk)
            for kc in range(8):
                S.pe(TR(bkb[:, kc * 128:(kc + 1) * 128], hb[:, kc * 128:(kc + 1) * 128], identb[:]), r=[khb, "identb"], w=[kb])
            S.act(ACTF(dstT, bkb.rearrange("p (k t) -> p k t", k=8), AF.Copy), r=[kb], w=[kdst])

        def phase_A1(l, xsrc):
            with contextlib.ExitStack() as st:
                def sb(name, shape, dt):
                    return st.enter_context(SBT(name, shape, dt))
                WF = sb("WF", [128, 8, 1600], BF16)
                WV = sb("WV", [128, 8, 520], BF16)
                A1t = sb("A1t", [128, 1024], F32)
                sh1t = sb("sh1t", [128, 1024], F32)
                xb = [sb("xb%d" % i, [128, 1024], F32) for i in range(2)]
                junk = sb("junk", [128, 1024], F32)
                tmp = sb("tmpn", [128, 1024], F32)
                hb = [sb("hb%d" % i, [128, 1024], BF16) for i in range(2)]
                hT = [sb("hT%d" % i, [128, 8, 512], BF16) for i in range(2)]
                fst = [sb("fst%d" % i, [128, 512], BF16) for i in range(2)]
                vst = [sb("vst%d" % i, [128, 8, 65], BF16) for i in range(2)]
                wist = [sb("wist%d" % i, [128, 8], F32) for i in range(2)]
                ssq = [sb("ssq%d" % i, [128, 1], F32) for i in range(2)]
                ms = [sb("ms%d" % i, [128, 1], F32) for i in range(2)]
                rstd = [sb("rstd%d" % i, [128, 1], F32) for i in range(2)]
                wv = W["w_in"][l].rearrange("(kc p) n -> p kc n", p=128)
                S.dma(WF[:, :, 0:1024], wv[:, :, 1536:2560], w=["WF"], eng="pool")
                S.dma(WF[:, :, 1024:1600], wv[:, :, 3072:3648], w=["WF"], eng="pool")
                S.dma(WV[:, :, 0:512], wv[:, :, 2560:3072], w=["WV"], eng="pool")
                S.dma(WV[:, :, 512:520], wv[:, :, 3648:3656], w=["WV"], eng="pool")
                S.dma(A1t[:], mod_d[:, 1024:2048], r=["mod_d"], w=["modp"])
                S.dma(sh1t[:], mod_d[:, 0:1024], r=["mod_d"], w=["modp"])
                for i in range(2):
                    S.pool(MEMSET(vst[i][:, :, 64:65], 1.0), w=["vst%d" % i])

                def load(t):
                    S.dma(xb[t % 2][:], xsrc[t * 128:(t + 1) * 128, :], r=["xs_d"], w=["xb%d" % (t % 2)])
                load(0)
                for b in range(NB):
                    hTb = hT[b % 2]
                    kh = "hT%d" % (b % 2)
                    for j in range(4):
                        t = b * 4 + j
                        if t + 1 < NT:
                            load(t + 1)
                        i2 = t % 2
                        norm_mod_T(xb[i2][:], "xb%d" % i2, A1t[:], sh1t[:], hb[i2], "hb%d" % i2, junk, ssq[i2], ms[i2],
                                   rstd[i2], tmp, i2, hTb[:, :, j * 128:(j + 1) * 128], kh)
                        bk, kb = nb()
                        bk2, kb2 = nb()
                        for kc in range(8):
                            S.pe(MM(bk[:], hTb[:, kc, j * 128:(j + 1) * 128], WV[:, kc, 0:512], kc == 0, kc == 7), r=[kh, "WV"], w=[kb])
                        for kc in range(8):
                            S.pe(MM(bk2[:, 0:8], hTb[:, kc, j * 128:(j + 1) * 128], WV[:, kc, 512:520], kc == 0, kc == 7), r=[kh, "WV"], w=[kb2])
                        S.dve(CP(vst[i2][:, :, 0:64], bk[:].rearrange("p (h d) -> p h d", h=8)), r=[kb], w=["vst%d" % i2])
                        S.act(ACTF(wist[i2][:], bk2[:, 0:8], AF.Copy), r=[kb2], w=["wist%d" % i2])
                        S.dma(vaug_d[t * 128:(t + 1) * 128, :], vst[i2][:].rearrange("p h d -> p (h d)"), r=["vst%d" % i2], w=["vaug_d"])
                        S.dma(wi_d[t * 128:(t + 1) * 128, :], wist[i2][:], r=["wist%d" % i2], w=["wi_d"])
                    for c in range(13):
                        rows = 128 if c < 12 else 64
                        bk, kb = nb()
                        for kc in range(8):
                            S.pe(MM(bk[0:rows, :], WF[:, kc, c * 128:c * 128 + rows], hTb[:, kc, :], kc == 0, kc == 7), r=[kh, "WF"], w=[kb])
                        f = fst[c % 2]
                        kf = "fst%d" % (c % 2)
                        if c % 2 == 0:
                            S.act(ACTF(f[0:rows, :], bk[0:rows, :], AF.Copy), r=[kb], w=[kf])
                        else:
                            S.dve(CP(f[0:rows, :], bk[0:rows, :]), r=[kb], w=[kf])
                        S.dma(featT_d[c * 128:c * 128 + rows, b * 512:(b + 1) * 512], f[0:rows, :], r=[kf], w=["featT_d"])
                    S.dma(hT_d[:, b * 512:(b + 1) * 512].rearrange("(kc p) t -> p kc t", p=128), hTb[:], r=[kh], w=["hT_d"])
                S.flush()

        def phase_A2(l):
            with contextlib.ExitStack() as st:
                def sb(name, shape, dt):
                    return st.enter_context(SBT(name, shape, dt))
                WT1 = sb("WT1", [128, 8, 1536], BF16)
                WT2 = sb("WT2", [128, 8, 1536], BF16)
                LA1 = sb("LA1", [128, 8, 288], BF16)
                LA2 = sb("LA2", [128, 8, 288], BF16)
                Bwa = sb("Bwa", [128, 512], BF16)
                Bg = sb("Bg", [128, 512], BF16)
                Bv = sb("Bv", [32, 512], BF16)
                brow = sb("brow", [1, 3, 512], F32)
                pbc = sb("pbc", [128, 5, 512], F32)
                muT = sb("muT", [128, 32], F32)
                omT = sb("omT", [128, 32], F32)
                with contextlib.ExitStack() as st2:
                    Wr = st2.enter_context(SBT("Wr", [128, 8, 1536], BF16))
                    mubc = st2.enter_context(SBT("mubc", [128, 1536], F32))
                    ombc = st2.enter_context(SBT("ombc", [128, 1536], F32))
                    LA = st2.enter_context(SBT("LA", [128, 8, 288], F32))
                    mu32 = st2.enter_context(SBT("mu32", [32, 128], F32))
                    wv = W["w_in"][l].rearrange("(kc p) n -> p kc n", p=128)
                    S.dma(Wr[:, :, 0:768], wv[:, :, 0:768], w=["Wr"], eng="pool")
                    S.dma(Wr[:, :, 768:1536], wv[:, :, 768:1536], w=["Wr"], eng="pool")
                    S.dma(mubc[:], W["mu_rkv"][l:l + 1].rearrange("o a b -> o (a b)").partition_broadcast(128), w=["mubc"])
                    S.dve(TS(ombc[:], mubc[:], -1.0, 1.0, ALU.mult, ALU.add), r=["mubc"], w=["ombc"])
                    S.dve(TT(WT2[:], Wr[:], mubc[:].unsqueeze(1).to_broadcast([128, 8, 1536]), ALU.mult), r=["Wr", "mubc"], w=["WT2"])
                    S.pool(TT(WT1[:], Wr[:], ombc[:].unsqueeze(1).to_broadcast([128, 8, 1536]), ALU.mult), r=["Wr", "ombc"], w=["WT1"])
                    S.dma(LA[:, :, 0:64], W["decay_a"][l].rearrange("(kc p) n -> p kc n", p=128), w=["LA"])
                    S.dma(LA[:, :, 64:128], W["iclr_a"][l].rearrange("(kc p) n -> p kc n", p=128), w=["LA"])
                    S.dma(LA[:, :, 128:256], W["gate_a"][l].rearrange("(kc p) n -> p kc n", p=128), w=["LA"])
                    if l > 0:
                        S.dma(LA[:, :, 256:288], W["vres_a"][l - 1].rearrange("(kc p) n -> p kc n", p=128), w=["LA"])
                    else:
                        S.dve(MEMSET(LA[:, :, 256:288], 0.0), w=["LA"])
                    S.dve(MEMSET(mu32[:], 0.0), w=["mu32"])
                    S.dma(mu32[0:24, :], W["mu_lora"][l].rearrange("a (kc p) -> (a kc) p", p=128), r=["mu32"], w=["mu32"])
                    if l > 0:
                        S.dma(mu32[24:32, :], W["vres_mu"][l - 1:l, :].rearrange("o (kc p) -> (o kc) p", p=128), r=["mu32"], w=["mu32"])
                    bk, kb = nb()
                    S.pe(MM(bk[:, 0:32], mu32[:], identf[0:32, 0:32], True, True), r=["mu32", "cst"], w=[kb])
                    S.act(ACTF(muT[:], bk[:, 0:32], AF.Copy), r=[kb], w=["muT"])
                    S.dve(TS(omT[:], muT[:], -1.0, 1.0, ALU.mult, ALU.add), r=["muT"], w=["omT"])
                    for gi, (c0, c1) in enumerate(((0, 64), (64, 128), (128, 256), (256, 288))):
                        wdt = c1 - c0
                        S.dve(TT(LA2[:, :, c0:c1], LA[:, :, c0:c1], muT[:, gi * 8:(gi + 1) * 8].unsqueeze(2).to_broadcast([128, 8, wdt]), ALU.mult), r=["LA", "muT"], w=["LA2"])
                        S.dve(TT(LA1[:, :, c0:c1], LA[:, :, c0:c1], omT[:, gi * 8:(gi + 1) * 8].unsqueeze(2).to_broadcast([128, 8, wdt]), ALU.mult), r=["LA", "omT"], w=["LA1"])
                    S.dma(Bwa[0:64, :], W["decay_b"][l], w=["Bwa"], eng="pool")
                    S.dma(Bwa[64:128, :], W["iclr_b"][l], w=["Bwa"], eng="pool")
                    S.dma(Bg[:], W["gate_b"][l], w=["Bg"], eng="pool")
                    if l > 0:
                        S.dma(Bv[:], W["vres_b"][l - 1], w=["Bv"], eng="pool")
                    S.dma(brow[:, 0, :], W["decay_w0"][l:l + 1, :], w=["brow"])
                    S.dma(brow[:, 1, :], W["iclr_a0"][l:l + 1, :], w=["brow"])
                    if l > 0:
                        S.dma(brow[:, 2, :], W["vres_v0"][l - 1:l, :], w=["brow"])
                    S.dma(pbc[:, 0, :], W["k_k"][l:l + 1, :].partition_broadcast(128), w=["pbc"])
                    S.dma(pbc[:, 1, :], W["k_a"][l:l + 1, :].partition_broadcast(128), w=["pbc"])
                    S.dma(pbc[:, 2, :], W["r_k"][l:l + 1].rearrange("o a b -> o (a b)").partition_broadcast(128), w=["pbc"])
                    S.dma(pbc[:, 3, :], W["lnx_g"][l:l + 1, :].partition_broadcast(128), w=["pbc"])
                    S.dma(pbc[:, 4, :], W["lnx_b"][l:l + 1, :].partition_broadcast(128), w=["pbc"])
                    S.flush()

                chk(1)
                hTb2 = [sb("hTb%d" % i, [128, 8, 512], BF16) for i in range(1)]
                hTp2 = [sb("hTp%d" % i, [128, 8, 512], BF16) for i in range(1)]
                L1wa = sb("L1wa", [128, 512], BF16)
                sg = sb("sg", [128, 512], BF16)
                sgt = sb("sgt", [128, 512], F32)
                L1v = sb("L1v", [32, 512], BF16)

                def f32t(name):
                    return sb(name, [128, 512], F32)

                def b16t(name):
                    return sb(name, [128, 512], BF16)
                r32, k32, v32 = f32t("r32"), f32t("k32"), f32t("v32")
                kkr, kk, a32, b32, km = f32t("kkr"), f32t("kk"), f32t("a32"), f32t("b32"), f32t("km")
                lw, Ginc, Ginv, Gexc, g32 = f32t("lw"), f32t("Ginc"), f32t("Ginv"), f32t("Gexc"), f32t("g32")
                t1, t2, U0, cen, vf = f32t("t1"), f32t("t2"), f32t("U0"), f32t("cen"), f32t("vf")
                y32 = f32t("y32")
                Kd, Rd, Bi, Ki, Vb, Zb, Ub, ob = (b16t(n) for n in ("Kd", "Rd", "Bi", "Ki", "Vb", "Zb", "Ub", "ob"))
                s8 = [sb("s8_%d" % i, [128, 8], F32) for i in range(6)]
                FT = sb("FT", [64, 8, 4, 128], BF16)
                GE = sb("GE", [128, 8, 512], BF16)
                MMb = [sb("MMb%d" % i, [128, 8, 128], F32) for i in range(2)]
                NNb = [sb("NNb%d" % i, [128, 8, 128], F32) for i in range(2)]
                TTb = [sb("TTb%d" % i, [128, 8, 128], F32) for i in range(2)]
                TTh = sb("TTh", [128, 8, 128], BF16)
                WTs = sb("WTs", [64, 8, 128], BF16)
                P32 = sb("P32", [64, 8, 64], F32)
                Pb = sb("Pb", [64, 8, 64], BF16)
                GC = sb("GC", [64, 8], F32)
                rwT = sb("rwT", [128, 4, 128], BF16)
                S.dve(MEMSET(P32[:], 0.0), w=["P32"])
                S.dve(MEMSET(Pb[:], 0.0), w=["Pb"])
                P32f = P32[:].rearrange("p h v -> p (h v)")
                ev = [0]

                def evac(out, in_, r, w):
                    ev[0] += 1
                    if ev[0] % 2:
                        S.act(ACTF(out, in_, AF.Copy), r=r, w=w)
                    else:
                        S.dve(CP(out, in_), r=r, w=w)

                for b in range(NB):
                    hTb = hTb2[0]
                    kh = "hTb0"
                    src = hT_d.rearrange("(kc p) t -> p kc t", p=128)
                    if b == 1:
                        chk(8)
                    hTp = hTp2[0]
                    S.dma(hTb[:], src[:, :, b * 512:(b + 1) * 512], r=["hT_d"], w=[kh])
                    if b == 0:
                        S.dve(MEMSET(hTp[:, :, 0:1], 0.0), w=[kh])
                        S.dma(hTp[:, :, 1:512], src[:, :, 0:511], r=["hT_d"], w=[kh])
                    else:
                        S.dma(hTp[:], src[:, :, b * 512 - 1:b * 512 + 511], r=["hT_d"], w=[kh])
                    if b == 1:
                        chk(9)
                    for gi, (c0, c1) in enumerate(((0, 128), (128, 256), (256, 288))):
                        if gi == 2 and l == 0:
                            continue
                        rows = c1 - c0
                        bk, kb = nb()
                        for kc in range(8):
                            S.pe(MM(bk[0:rows, :], LA1[:, kc, c0:c1], hTb[:, kc, :], kc == 0, False), r=[kh, "LA1"], w=[kb])
                        for kc in range(8):
                            S.pe(MM(bk[0:rows, :], LA2[:, kc, c0:c1], hTp[:, kc, :], False, kc == 7), r=[kh, "LA2"], w=[kb])
                        if gi == 0:
                            S.act(ACTF(L1wa[0:64, :], bk[0:64, :], AF.Tanh), r=[kb], w=["L1wa"])
                            S.act(ACTF(L1wa[64:128, :], bk[64:128, :], AF.Copy), r=[kb], w=["L1wa"])
                        elif gi == 1:
                            S.act(ACTF(sgt[:], bk[:], AF.Tanh, scale=0.5), r=[kb], w=["sgt"])
                            S.dve(TS(sg[:], sgt[:], 0.5, 0.5, ALU.mult, ALU.add), r=["sgt"], w=["sg"])
                        else:
                            S.act(ACTF(L1v[:], bk[0:32, :], AF.Copy), r=[kb], w=["L1v"])
                    chk(2)
                    for j in range(4):
                        t = b * 4 + j
                        lo = j * 128
                        tsl = slice(j * 128, (j + 1) * 128)
                        if l > 0:
                            S.dma(vf[:], vfirst_d[t * 128:(t + 1) * 128, :], r=["vfirst_d"], w=["vf"])
                        for g, (dst, kd) in enumerate(((r32, "r32"), (k32, "k32"), (v32, "v32"))):
                            bk, kb = nb()
                            for kc in range(8):
                                S.pe(MM(bk[:], hTb[:, kc, lo:lo + 128], WT1[:, kc, g * 512:(g + 1) * 512], kc == 0, False), r=[kh, "WT1"], w=[kb])
                            for kc in range(8):
                                S.pe(MM(bk[:], hTp[:, kc, lo:lo + 128], WT2[:, kc, g * 512:(g + 1) * 512], False, kc == 7), r=[kh, "WT2"], w=[kb])
                            evac(dst[:], bk[:], [kb], [kd])
                        bkw, kbw = nb()
                        S.pe(MM(bkw[:], L1wa[0:64, tsl], Bwa[0:64, :], True, False), r=["L1wa", "Bwa"], w=[kbw])
                        S.pe(MM(bkw[:], onesf[0:1, :], brow[0:1, 0, :], False, True), r=["onesf", "brow"], w=[kbw])
                        bka, kba = nb()
                        S.pe(MM(bka[:], L1wa[64:128, tsl], Bwa[64:128, :], True, False), r=["L1wa", "Bwa"], w=[kba])
                        S.pe(MM(bka[:], onesf[0:1, :], brow[0:1, 1, :], False, True), r=["onesf", "brow"], w=[kba])
                        bkg, kbg = nb()
                        S.pe(MM(bkg[:], sg[:, tsl], Bg[:], True, True), r=["sg", "Bg"], w=[kbg])
                        S.act(ACTF(lw[:], bkw[:], AF.Tanh, scale=0.5), r=[kbw], w=["lw"])
                        S.dve(TS(lw[:], lw[:], -0.5 * math.exp(-0.5), -0.5 * math.exp(-0.5), ALU.mult, ALU.add), r=["lw"], w=["lw"])
                        S.act(ACTF(a32[:], bka[:], AF.Tanh, scale=0.5), r=[kba], w=["a32"])
                        S.pool(TS(a32[:], a32[:], 0.5, 0.5, ALU.mult, ALU.add), r=["a32"], w=["a32"])
                        S.act(ACTF(g32[:], bkg[:], AF.Copy), r=[kbg], w=["g32"])
                        if l > 0:
                            bkv, kbv = nb()
                            S.pe(MM(bkv[:], L1v[0:32, tsl], Bv[0:32, :], True, False), r=["L1v", "Bv"], w=[kbv])
                            S.pe(MM(bkv[:], onesf[0:1, :], brow[0:1, 2, :], False, True), r=["onesf", "brow"], w=[kbv])
                            S.act(ACTF(t1[:], bkv[:], AF.Tanh, scale=0.5), r=[kbv], w=["t1"])
                            S.pool(TS(t1[:], t1[:], 0.5, 0.5, ALU.mult, ALU.add), r=["t1"], w=["t1"])
                            S.pool(TT(t2[:], vf[:], v32[:], ALU.subtract), r=["vf", "v32"], w=["t2"])
                            S.pool(TT(t2[:], t2[:], t1[:], ALU.mult), r=["t2", "t1"], w=["t2"])
                            S.pool(TT(v32[:], v32[:], t2[:], ALU.add), r=["v32", "t2"], w=["v32"])
                        else:
                            S.dma(vfirst_d[t * 128:(t + 1) * 128, :], v32[:], r=["v32"], w=["vfirst_d"])
                        S.act(ACTF(Vb[:], v32[:], AF.Copy), r=["v32"], w=["Vb"])
                        if dbg:
                            rows = slice(t * 128, (t + 1) * 128)
                            S.dma(dbg_d["dbg_v"][rows, :], v32[:], r=["v32"], w=["dbgv"])
                            S.dma(dbg_d["dbg_r"][rows, :], r32[:], r=["r32"], w=["dbgr"])
                            S.dma(dbg_d["dbg_k"][rows, :], k32[:], r=["k32"], w=["dbgk"])
                            S.dma(dbg_d["dbg_a"][rows, :], a32[:], r=["a32"], w=["dbga"])
                            S.dma(dbg_d["dbg_lw"][rows, :], lw[:], r=["lw"], w=["dbglw"])
                        bkc, kbc = nb()
                        S.pe(MM(bkc[:], tri_incl, lw[:], True, True), r=["cst", "lw"], w=[kbc])
                        bke, kbe = nb()
                        S.pe(MM(bke[:], tri_strict, lw[:], True, True), r=["cst", "lw"], w=[kbe])
                        bkG, kbG = nb()
                        for h in range(8):
                            S.pe(MM(bkG[0:64, h:h + 1], lw[:, h * 64:(h + 1) * 64], onesf[:, 0:1], True, True), r=["lw", "onesf"], w=[kbG])
                        S.act(ACTF(Ginc[:], bkc[:], AF.Exp), r=[kbc], w=["Ginc"])
                        S.act(ACTF(Ginv[:], bkc[:], AF.Exp, scale=-1.0), r=[kbc], w=["Ginv"])
                        S.act(ACTF(Gexc[:], bke[:], AF.Exp), r=[kbe], w=["Gexc"])
                        S.act(ACTF(GC[:], bkG[0:64, 0:8], AF.Exp), r=[kbG], w=["GC"])
                        S.dve(TT(kkr[:], k32[:], pbc[:, 0, :], ALU.mult), r=["k32", "pbc"], w=["kkr"])
                        S.act(ACTF(t2[:], kkr[:], AF.Square), r=["kkr"], w=["t2"])
                        S.dve(RED(s8[0][:], t2[:].rearrange("p (h d) -> p h d", h=8), ALU.add), r=["t2"], w=["s8_0"])
                        S.dve(TS(s8[0][:], s8[0][:], 1e-24, None, ALU.max), r=["s8_0"], w=["s8_0"])
                        S.pool(TT(s8[1][:], s8[0][:], m05[:, 0:8], ALU.pow), r=["s8_0", "m05"], w=["s8_1"])
                        S.dve(TT(kk[:].rearrange("p (h d) -> p h d", h=8), kkr[:].rearrange("p (h d) -> p h d", h=8),
                                 s8[1][:].unsqueeze(2).to_broadcast([128, 8, 64]), ALU.mult), r=["kkr", "s8_1"], w=["kk"])
                        S.pool(TT(b32[:], kk[:], a32[:], ALU.mult), r=["kk", "a32"], w=["b32"])
                        S.dve(STT(t1[:], a32[:], -1.0, pbc[:, 1, :], ALU.add, ALU.mult), r=["a32", "pbc"], w=["t1"])
                        S.dve(STT(km[:], t1[:], 1.0, k32[:], ALU.add, ALU.mult), r=["t1", "k32"], w=["km"])
                        S.dve(TT(Kd[:], kk[:], Gexc[:], ALU.mult), r=["kk", "Gexc"], w=["Kd"])
                        S.pool(TT(Bi[:], b32[:], Ginv[:], ALU.mult), r=["b32", "Ginv"], w=["Bi"])
                        S.dve(TT(Ki[:], km[:], Ginv[:], ALU.mult), r=["km", "Ginv"], w=["Ki"])
                        S.pool(TT(Rd[:], r32[:], Ginc[:], ALU.mult), r=["r32", "Ginc"], w=["Rd"])
                        S.pool(TT(t2[:], r32[:], km[:], ALU.mult), r=["r32", "km"], w=["t2"])
                        S.pool(TT(t2[:], t2[:], pbc[:, 2, :], ALU.mult), r=["t2", "pbc"], w=["t2"])
                        S.dve(RED(s8[2][:], t2[:].rearrange("p (h d) -> p h d", h=8), ALU.add), r=["t2"], w=["s8_2"])
                        chk(3)
                        for q, (srcT, ks) in enumerate(((Kd, "Kd"), (Rd, "Rd"), (Bi, "Bi"), (Ki, "Ki"))):
                            bk, kb = nb()
                            bkb = bfv(bk)
                            for h in range(8):
                                S.pe(TR(bkb[0:64, h * 128:(h + 1) * 128], srcT[:, h * 64:(h + 1) * 64], identb[:]), r=[ks, "identb"], w=[kb])
                            evac(FT[:, :, q, :], bkb[0:64, :].rearrange("p (h t) -> p h t", h=8), [kb], ["FT"])
                        for h in range(8):
                            bk, kb = nb()
                            rhs = FT[:, h, 0:2, :].rearrange("p a t -> p (a t)")
                            S.pe(MM(bk[:, 0:256], FT[:, h, 2, :], rhs, True, True), r=["FT"], w=[kb])
                            S.pe(MM(bk[:, 256:512], FT[:, h, 3, :], rhs, True, True), r=["FT"], w=[kb])
                            S.dve(TT(GE[:, h, :], bk[:], mask4[:], ALU.mult), r=[kb, "mask4"], w=["GE"])
                            S.dve(TT(NNb[0][:, h, :], bk[:, 0:128], tri_strict, ALU.mult), r=[kb, "cst"], w=["NNb0"])
                        for g in range(2):
                            bk, kb = nb()
                            for hh in range(4):
                                h = g * 4 + hh
                                S.pe(MM(bk[:, hh * 128:(hh + 1) * 128], FT[:, h, 0, :], FT[:, h, 2, :], True, True), r=["FT"], w=[kb])
                            S.dve(TT(MMb[0][:, g * 4:(g + 1) * 4, :], bk[:].rearrange("p (h t) -> p h t", h=4),
                                     lowm4[:].rearrange("p (h t) -> p h t", h=4), ALU.mult), r=[kb, "lowm4"], w=["MMb0"])
                        chk(4)
                        S.pool(TT(TTb[0][:], identf.unsqueeze(1).to_broadcast([128, 8, 128]), NNb[0][:], ALU.subtract),
                               r=["cst", "NNb0"], w=["TTb0"])
                        cur = 0
                        for step in range(1, 7):
                            nxt = 1 - cur
                            for g in range(2):
                                hs = range(g * 4, g * 4 + 4)
                                bkM, kbM = nb()
                                for hh, h in enumerate(hs):
                                    Nprev = NNb[cur][:, h, :]
                                    S.pe(MM(bkM[:, hh * 128:(hh + 1) * 128], Nprev, MMb[cur][:, h, :], True, True),
                                         r=["NNb%d" % cur, "MMb%d" % cur], w=[kbM])
                                evac(MMb[nxt][:, g * 4:(g + 1) * 4, :], bkM[:].rearrange("p (h t) -> p h t", h=4), [kbM], ["MMb%d" % nxt])
                                if step < 6:
                                    bkN, kbN = nb()
                                    for hh, h in enumerate(hs):
                                        Nprev = NNb[cur][:, h, :]
                                        S.pe(MM(bkN[:, hh * 128:(hh + 1) * 128], MMb[cur][:, h, :], Nprev, True, True),
                                             r=["NNb%d" % cur, "MMb%d" % cur], w=[kbN])
                                    evac(NNb[nxt][:, g * 4:(g + 1) * 4, :], bkN[:].rearrange("p (h t) -> p h t", h=4), [kbN], ["NNb%d" % nxt])
                                bkT, kbT = nb()
                                for hh, h in enumerate(hs):
                                    S.pe(MM(bkT[:, hh * 128:(hh + 1) * 128], MMb[nxt][:, h, :], TTb[cur][:, h, :], True, False),
                                         r=["MMb%d" % nxt, "TTb%d" % cur], w=[kbT])
                                    S.pe(MM(bkT[:, hh * 128:(hh + 1) * 128], identf, TTb[cur][:, h, :], False, True),
                                         r=["cst", "TTb%d" % cur], w=[kbT])
                                evac(TTb[nxt][:, g * 4:(g + 1) * 4, :], bkT[:].rearrange("p (h t) -> p h t", h=4), [kbT], ["TTb%d" % nxt])
                            cur = nxt
                        chk(5)
                        S.act(ACTF(TTh[:], TTb[cur][:], AF.Copy), r=["TTb%d" % cur], w=["TTh"])
                        TTf = TTh
                        kT = "TTh"
                        for g in range(2):
                            bk, kb = nb()
                            for hh in range(4):
                                h = g * 4 + hh
                                S.pe(MM(bk[0:64, hh * 128:(hh + 1) * 128], Kd[:, h * 64:(h + 1) * 64], TTf[:, h, :], True, True), r=["Kd", kT], w=[kb])
                            evac(WTs[:, g * 4:(g + 1) * 4, :], bk[0:64, :].rearrange("p (h t) -> p h t", h=4), [kb], ["WTs"])
                        bk, kb = nb()
                        for h in range(8):
                            S.pe(MM(bk[:, h * 64:(h + 1) * 64], GE[:, h, 256:384], Vb[:, h * 64:(h + 1) * 64], True, True), r=["GE", "Vb"], w=[kb])
                        evac(Zb[:], bk[:], [kb], ["Zb"])
                        bk, kb = nb()
                        for h in range(8):
                            S.pe(MM(bk[:, h * 64:(h + 1) * 64], TTf[:, h, :], Zb[:, h * 64:(h + 1) * 64], True, True), r=[kT, "Zb"], w=[kb])
                        evac(U0[:], bk[:], [kb], ["U0"])
                        chk(6)
                        bk, kb = nb()
                        for h in range(8):
                            S.pe(MM(bk[:, h * 64:(h + 1) * 64], WTs[:, h, :], Pb[:, h, :], True, True), r=["WTs", "Pb"], w=[kb])
                        S.dve(STT(Ub[:], bk[:], -1.0, U0[:], ALU.mult, ALU.subtract), r=[kb, "U0"], w=["Ub"])
                        bkY, kbY = nb()
                        for h in range(8):
                            hs_ = slice(h * 64, (h + 1) * 64)
                            S.pe(MM(bkY[:, hs_], FT[:, h, 1, :], Pb[:, h, :], True, False), r=["FT", "Pb"], w=[kbY])
                            S.pe(MM(bkY[:, hs_], GE[:, h, 128:256], Ub[:, hs_], False, False), r=["GE", "Ub"], w=[kbY])
                            S.pe(MM(bkY[:, hs_], GE[:, h, 384:512], Vb[:, hs_], False, True), r=["GE", "Vb"], w=[kbY])
                        bkX, kbX = nb()
                        S.pe(MM(bkX[0:64, :], identf[0:64, 0:64], P32f, True, False), r=["cst", "P32"], w=[kbX])
                        for h in range(8):
                            hs_ = slice(h * 64, (h + 1) * 64)
                            S.pe(MM(bkX[0:64, hs_], Bi[:, hs_], Ub[:, hs_], False, False), r=["Bi", "Ub"], w=[kbX])
                            S.pe(MM(bkX[0:64, hs_], Ki[:, hs_], Vb[:, hs_], False, h == 7), r=["Ki", "Vb"], w=[kbX])
                        S.dve(TT(P32[:], bkX[0:64, :].rearrange("p (h v) -> p h v", h=8), GC[:].unsqueeze(2).to_broadcast([64, 8, 64]), ALU.mult),
                              r=[kbX, "GC"], w=["P32"])
                        S.act(ACTF(Pb[:], P32[:], AF.Copy), r=["P32"], w=["Pb"])
                        chk(7)
                        S.act(ACTF(y32[:], bkY[:], AF.Copy), r=[kbY], w=["y32"])
                        if dbg:
                            S.dma(dbg_d["dbg_y"][t * 128:(t + 1) * 128, :], y32[:], r=["y32"], w=["dbgy"])
                        chk(10)
                        Y3 = y32[:].rearrange("p (h d) -> p h d", h=8)
                        S.dve(RED(s8[3][:], Y3, ALU.add), r=["y32"], w=["s8_3"])
                        S.dve(TS(s8[3][:], s8[3][:], 1.0 / 64, None, ALU.mult), r=["s8_3"], w=["s8_3"])
                        S.dve(TT(cen[:].rearrange("p (h d) -> p h d", h=8), Y3, s8[3][:].unsqueeze(2).to_broadcast([128, 8, 64]), ALU.subtract),
                              r=["y32", "s8_3"], w=["cen"])
                        S.act(ACTF(t2[:], cen[:], AF.Square), r=["cen"], w=["t2"])
                        S.dve(RED(s8[4][:], t2[:].rearrange("p (h d) -> p h d", h=8), ALU.add), r=["t2"], w=["s8_4"])
                        S.dve(TS(s8[4][:], s8[4][:], 1.0 / 64, 64e-5, ALU.mult, ALU.add), r=["s8_4"], w=["s8_4"])
                        S.pool(TT(s8[5][:], s8[4][:], m05[:, 0:8], ALU.pow), r=["s8_4", "m05"], w=["s8_5"])
                        S.dve(TT(cen[:].rearrange("p (h d) -> p h d", h=8), cen[:].rearrange("p (h d) -> p h d", h=8),
                                 s8[5][:].unsqueeze(2).to_broadcast([128, 8, 64]), ALU.mult), r=["cen", "s8_5"], w=["cen"])
                        S.pool(TT(cen[:], cen[:], pbc[:, 3, :], ALU.mult), r=["cen", "pbc"], w=["cen"])
                        S.pool(TT(cen[:], cen[:], pbc[:, 4, :], ALU.add), r=["cen", "pbc"], w=["cen"])
                        S.dve(TT(t2[:].rearrange("p (h d) -> p h d", h=8), v32[:].rearrange("p (h d) -> p h d", h=8),
                                 s8[2][:].unsqueeze(2).to_broadcast([128, 8, 64]), ALU.mult), r=["v32", "s8_2"], w=["t2"])
                        S.pool(TT(cen[:], cen[:], t2[:], ALU.add), r=["cen", "t2"], w=["cen"])
                        S.dve(TT(ob[:], cen[:], g32[:], ALU.mult), r=["cen", "g32"], w=["ob"])
                        chk(11)
                        bk, kb = nb()
                        bkb = bfv(bk)
                        for c in range(4):
                            S.pe(TR(bkb[:, c * 128:(c + 1) * 128], ob[:, c * 128:(c + 1) * 128], identb[:]), r=["ob", "identb"], w=[kb])
                        evac(rwT[:], bkb[:, 0:512].rearrange("p (c t) -> p c t", c=4), [kb], ["rwT"])
                        S.dma(rwkvT_d[:, t * 128:(t + 1) * 128].rearrange("(c p) t -> p c t", p=128), rwT[:], r=["rwT"], w=["rwkvT_d"])
                        chk(12)
                S.flush()

        def phase_B(l):
            with contextlib.ExitStack() as st:
                def sb(name, shape, dt):
                    return st.enter_context(SBT(name, shape, dt))
                c31b = sb("c31b", [128, 8], F32)
                nc31 = sb("nc31", [128, 8], F32)
                Rn = sb("Rn", [128, 8, 256], BF16)
                aog = sb("aog", [64, 8], F32)
                statL = sb("statL", [65, 64], F32)
                S.dma(c31b[:], c31_d.partition_broadcast(128), w=["c31b"])
                aog8 = sb("aog8", [8, 64], F32)
                S.dma(aog8[:], W["attn_out_g"][l].rearrange("(h d) -> h d", d=64), w=["aog8"])
                bk, kb = nb()
                S.pe(MM(bk[0:64, 0:8], aog8[:], identf[0:8, 0:8], True, True), r=["aog8", "cst"], w=[kb])
                S.dve(CP(aog[:], bk[0:64, 0:8]), r=[kb], w=["aog"])
                S.dve(TS(nc31[:], c31b[:], -1.0, None, ALU.mult), r=["c31b"], w=["nc31"])
                with contextlib.ExitStack() as st2:
                    bnr = st2.enter_context(SBT("bnr", [128, 2048], F32))
                    S.dma(bnr[:], bnear_d, w=["bnr"])
                    for h in range(8):
                        S.act(ACTF(Rn[:, h, :], bnr[:, h * 256:(h + 1) * 256], AF.Exp, bias=nc31[:, h:h + 1]), r=["bnr", "nc31"], w=["Rn"])
                    S.flush()
                kaT = sb("kaT", [128, 4, T], BF16)
                kiT2 = sb("kiT2", [128, T], BF16)
                Va = sb("Va", [128, NT, 520], BF16)
                qaTb = [sb("qaTb%d" % i, [128, 4, 512], BF16) for i in range(2)]
                qiTb = [sb("qiTb%d" % i, [128, 4, 512], BF16) for i in range(2)]
                wib = [sb("wib%d" % i, [128, 4, 8], F32) for i in range(2)]
                wabs = sb("wabs", [128, 4, 8], F32)
                wsgn = sb("wsgn", [128, 4, 8], F32)
                scb = [sb("sc%d" % i, [128, T], F32) for i in range(2)]
                rl = [sb("rl%d" % i, [128, 512], F32) for i in range(3)]
                msk = sb("msk", [128, T], BF16)
                maskT = sb("maskT", [128, NT, 512], BF16)
                Eb = [sb("Eb%d" % i, [128, 512], BF16) for i in range(3)]
                Pt = [sb("Pt%d" % i, [128, 512], BF16) for i in range(3)]
                attTb = sb("attTb", [128, 4, 512], BF16)
                Osb = sb("Osb", [65, 512], F32)
                SQ = sb("SQ", [65, 512], F32)
                rs = sb("rs", [64, 512], F32)
                bis = [sb("bis%d" % i, [128, 1], F32) for i in range(6)]
                wtab = sb("wtab", [128, NBIS + 2], F32)
                ctab = sb("ctab", [128, NBIS + 2], F32)
                S.dma(kaT[:], featT_d[512:1024, :].rearrange("(c p) t -> p c t", p=128), r=["featT_d"], w=["kaT"])
                S.dma(kiT2[0:64, :], featT_d[1536:1600, :], r=["featT_d"], w=["kiT2"])
                S.dma(kiT2[64:128, :], featT_d[1536:1600, :], r=["featT_d"], w=["kiT2"])
                vsrc = vaug_d.rearrange("(n p) c -> p n c", p=128)
                for n0 in range(0, NT, 8):
                    S.dma(Va[:, n0:n0 + 8, :], vsrc[:, n0:n0 + 8, :], r=["vaug_d"], w=["Va"])
                S.dve(MEMSET(statL[0:64, :], 1.0 / 64), w=["statL"])
                S.dve(MEMSET(statL[64:65, :], 1e-6), w=["statL"])
                for k in range(NBIS + 2):
                    S.pool(MEMSET(ctab[:, k:k + 1], 2.0 ** (-k)), w=["ctab"])
                cw = 0.125 * (8 ** -0.5)

                def loadq(b):
                    i = b % 2
                    S.dma(qaTb[i][:], featT_d[0:512, b * 512:(b + 1) * 512].rearrange("(c p) t -> p c t", p=128), r=["featT_d"], w=["qaTb%d" % i])
                    S.dma(qiTb[i][:], featT_d[1024:1536, b * 512:(b + 1) * 512].rearrange("(c p) t -> p c t", p=128), r=["featT_d"], w=["qiTb%d" % i])
                    S.dma(wib[i][:], wi_d[b * 512:(b + 1) * 512, :].rearrange("(j p) h -> p j h", p=128), r=["wi_d"], w=["wib%d" % i])
                loadq(0)
                eidx = [0]
                for b in range(NB):
                    if b + 1 < NB:
                        loadq(b + 1)
                    i2 = b % 2
                    qa, qi, wi_ = qaTb[i2], qiTb[i2], wib[i2]
                    kqa, kqi, kwi = "qaTb%d" % i2, "qiTb%d" % i2, "wib%d" % i2
                    S.dve(STT(wabs[:], wi_[:], -1.0, wi_[:], ALU.mult, ALU.max), r=[kwi], w=["wabs"])
                    S.dve(TS(wabs[:], wabs[:], cw, None, ALU.mult), r=["wabs"], w=["wabs"])
                    S.act(ACTF(wsgn[:], wi_[:], AF.Sign), r=[kwi], w=["wsgn"])
                    nk = 4 * b + 4
                    def indexer(jq):
                        j = 4 * b + jq
                        Lk = (j + 1) * 128
                        qsl = slice(jq * 128, (jq + 1) * 128)
                        sc = scb[jq % 2]
                        ksc = "sc%d" % (jq % 2)
                        nch = (Lk + 511) // 512
                        for kc in range(nch):
                            ncol = min(512, Lk - kc * 512)
                            csl = slice(kc * 512, kc * 512 + ncol)
                            for ih in range(8):
                                pr, hf = ih // 2, ih % 2
                                ps_ = slice(hf * 64, (hf + 1) * 64)
                                bk, kb = nb()
                                S.pe(MM(bk[:, 0:ncol], qi[ps_, pr, qsl], kiT2[ps_, csl], True, True), r=[kqi, "kiT2"], w=[kb])
                                rb = rl[eidx[0] % 3]
                                krb = "rl%d" % (eidx[0] % 3)
                                eidx[0] += 1
                                S.act(ACTF(rb[:, 0:ncol], bk[:, 0:ncol], AF.Relu, scale=wabs[:, jq, ih:ih + 1]), r=[kb, "wabs"], w=[krb])
                                if ih == 0:
                                    S.dve(TS(sc[:, csl], rb[:, 0:ncol], wsgn[:, jq, 0:1], None, ALU.mult), r=[krb, "wsgn"], w=[ksc])
                                else:
                                    S.dve(STT(sc[:, csl], rb[:, 0:ncol], wsgn[:, jq, ih:ih + 1], sc[:, csl], ALU.mult, ALU.add), r=[krb, "wsgn", ksc], w=[ksc])
                                yield

                    gens = [indexer(jq) for jq in range(4)]
                    for _ in gens[0]:
                        pass
                    for jq in range(4):
                        j = 4 * b + jq
                        Lk = (j + 1) * 128
                        qsl = slice(jq * 128, (jq + 1) * 128)
                        sc = scb[jq % 2]
                        ksc = "sc%d" % (jq % 2)
                        nxt = gens[jq + 1] if jq < 3 else None
                        per_round = (((Lk + 128 + 511) // 512) * 8 + NBIS - 1) // NBIS if nxt is not None else 0
                        A_, mid, cnt, inc = bis[0], bis[1], bis[2], bis[3]
                        S.dve(RED(A_[:], sc[:, 0:Lk], ALU.max, absval=True), r=[ksc], w=["bis0"])
                        S.dve(TS(A_[:], A_[:], 1.001, 1e-6, ALU.mult, ALU.add), r=["bis0"], w=["bis0"])
                        S.dve(TT(sc[:, j * 128:(j + 1) * 128], sc[:, j * 128:(j + 1) * 128], caus, ALU.add), r=[ksc, "cst"], w=[ksc])
                        if dbg:
                            S.dma(dbg_d["dbg_score"][j * 128:(j + 1) * 128, 0:Lk], sc[:, 0:Lk], r=[ksc], w=["dbgs"])
                        S.dve(TS(wtab[:], ctab[:], A_[:, 0:1], None, ALU.mult), r=["ctab", "bis0"], w=["wtab"])
                        S.dve(TS(mid[:], A_[:], 0.0, None, ALU.mult), r=["bis0"], w=["bis1"])
                        for k in range(1, NBIS + 1):
                            if k % 2 == 1:
                                S.dve(TS(msk[:, 0:Lk], sc[:, 0:Lk], mid[:, 0:1], 0.0, ALU.is_ge, ALU.add, accum=cnt[:]), r=[ksc, "bis1"], w=["bis2", "msk"])
                                S.dve(STT(inc[:], cnt[:], NSEL - 0.5, wtab[:, k - 1:k], ALU.is_ge, ALU.mult), r=["bis2", "wtab"], w=["bis3"])
                            else:
                                S.dve(TS(bis[4][:], mid[:], -1.0, None, ALU.mult), r=["bis1"], w=["bis4"])
                                S.act(ACTF(msk[:, 0:Lk], sc[:, 0:Lk], AF.Sign, bias=bis[4][:, 0:1], accum=cnt[:]), r=[ksc, "bis4"], w=["bis2", "msk"])
                                S.dve(STT(inc[:], cnt[:], 2.0 * NSEL - 1.0 - Lk, wtab[:, k - 1:k], ALU.is_ge, ALU.mult), r=["bis2", "wtab"], w=["bis3"])
                            S.dve(STT(mid[:], inc[:], wtab[:, k:k + 1], mid[:], ALU.subtract, ALU.add), r=["bis3", "wtab", "bis1"], w=["bis1"])
                            if nxt is not None:
                                for _ in range(per_round):
                                    if next(nxt, "done") == "done":
                                        break
                        S.dve(TT(bis[5][:], mid[:], wtab[:, NBIS:NBIS + 1], ALU.subtract), r=["bis1", "wtab"], w=["bis5"])
                        if dbg:
                            S.dma(dbg_d["dbg_thr"][j * 128:(j + 1) * 128, :], bis[5][:], r=["bis5"], w=["dbgt"])
                        S.dve(TS(msk[:, 0:Lk], sc[:, 0:Lk], bis[5][:, 0:1], None, ALU.is_ge), r=[ksc, "bis5"], w=["msk"])
                        for i0 in range(0, j + 1, 8):
                            n8 = min(8, j + 1 - i0)
                            bk, kb = nb()
                            bkb = bfv(bk)
                            for ii in range(n8):
                                S.pe(TR(bkb[:, ii * 128:(ii + 1) * 128], msk[:, (i0 + ii) * 128:(i0 + ii + 1) * 128], identb[:]), r=["msk", "identb"], w=[kb])
                            S.act(ACTF(maskT[:, i0:i0 + n8, qsl], bkb[:, 0:n8 * 128].rearrange("p (i t) -> p i t", i=n8), AF.Copy), r=[kb], w=["maskT"])
                        if nxt is not None:
                            for _ in nxt:
                                pass
                    for h in range(8):
                        pr, hf = h // 2, h % 2
                        ps_ = slice(hf * 64, (hf + 1) * 64)
                        bkO, kbO = banks[6 + h % 2], "bank%d" % (6 + h % 2)
                        for i in range(nk):
                            m = i - 4 * b
                            c0 = max(0, m) * 128
                            ncol = 512 - c0
                            bk, kb = nb()
                            S.pe(MM(bk[:, 0:ncol], kaT[ps_, pr, i * 128:(i + 1) * 128], qa[ps_, pr, c0:512], True, True), r=["kaT", kqa], w=[kb])
                            ei = eidx[0] % 3
                            eidx[0] += 1
                            E, kE = Eb[ei], "Eb%d" % ei
                            P_, kP = Pt[ei], "Pt%d" % ei
                            S.act(ACTF(E[:, 0:ncol], bk[:, 0:ncol], AF.Exp, bias=c31b[:, h:h + 1], scale=0.125), r=[kb, "c31b"], w=[kE])
                            eng = S.dve if (eidx[0] % 2) else S.pool
                            eng(TT(P_[:, 0:ncol], E[:, 0:ncol], maskT[:, i, c0:512], ALU.mult), r=[kE, "maskT"], w=[kP])
                            if m >= 0:
                                nn = min(256, ncol)
                                eng(TT(P_[:, 0:nn], P_[:, 0:nn], Rn[:, h, 0:nn], ALU.mult), r=[kP, "Rn"], w=[kP])
                            elif m == -1:
                                eng(TT(P_[:, 0:128], P_[:, 0:128], Rn[:, h, 128:256], ALU.mult), r=[kP, "Rn"], w=[kP])
                            S.pe(MM(bkO[0:65, c0:512], Va[:, i, h * 65:(h + 1) * 65], P_[:, 0:ncol], i == 0, i == nk - 1), r=["Va", kP], w=[kbO])
                        S.act(ACTF(Osb[:], bkO[0:65, :], AF.Copy), r=[kbO], w=["Osb"])
                        S.act(ACTF(SQ[:], bkO[0:65, :], AF.Square), r=[kbO], w=["SQ"])
                        bk, kb = nb()
                        S.pe(MM(bk[0:64, :], statL[:], SQ[:], True, True), r=["statL", "SQ"], w=[kb])
                        S.act(ACTF(rs[:], bk[0:64, :], AF.Ln), r=[kb], w=["rs"])
                        S.act(ACTF(rs[:], rs[:], AF.Exp, scale=-0.5), r=["rs"], w=["rs"])
                        S.dve(STT(attTb[ps_, pr, :], Osb[0:64, :], aog[:, h:h + 1], rs[:], ALU.mult, ALU.mult), r=["Osb", "aog", "rs"], w=["attTb"])
                    S.dma(attT_d[:, b * 512:(b + 1) * 512].rearrange("(c p) t -> p c t", p=128), attTb[:], r=["attTb"], w=["attT_d"])
                S.flush()

        def phase_B2(l, xsrc):
            with contextlib.ExitStack() as st:
                def sb(name, shape, dt):
                    return st.enter_context(SBT(name, shape, dt))
                wo = sb("wo", [128, 8, 1024], BF16)
                gt1 = sb("gt1", [128, 1024], F32)
                A2t = sb("A2t", [128, 1024], F32)
                sh2t = sb("sh2t", [128, 1024], F32)
                xb = [sb("xb%d" % i, [128, 1024], F32) for i in range(2)]
                mixT = [sb("mixT%d" % i, [128, 8, 128], BF16) for i in range(2)]
                x1 = [sb("x1_%d" % i, [128, 1024], F32) for i in range(2)]
                junk = sb("junk", [128, 1024], F32)
                tmp = sb("tmpn", [128, 1024], F32)
                tmp2 = sb("tmpm", [128, 1024], F32)
                hb = [sb("hb%d" % i, [128, 1024], BF16) for i in range(2)]
                h2s = [sb("h2s%d" % i, [128, 8, 128], BF16) for i in range(2)]
                ssq = [sb("ssq%d" % i, [128, 1], F32) for i in range(2)]
                ms = [sb("ms%d" % i, [128, 1], F32) for i in range(2)]
                rstd = [sb("rstd%d" % i, [128, 1], F32) for i in range(2)]
                wov = W["w_out"][l].rearrange("(kc p) n -> p kc n", p=128)
                S.dma(wo[:], wov, w=["wo"], eng="pool")
                S.dma(gt1[:], mod_d[:, 2048:3072], r=["mod_d"], w=["modp"])
                S.dma(A2t[:], mod_d[:, 4096:5120], r=["mod_d"], w=["modp"])
                S.dma(sh2t[:], mod_d[:, 3072:4096], r=["mod_d"], w=["modp"])

                def load(t):
                    i = t % 2
                    S.dma(xb[i][:], xsrc[t * 128:(t + 1) * 128, :], r=["xs_d"], w=["xb%d" % i])
                    S.dma(mixT[i][:, 0:4, :], rwkvT_d[:, t * 128:(t + 1) * 128].rearrange("(c p) t -> p c t", p=128), r=["rwkvT_d"], w=["mixT%d" % i])
                    S.dma(mixT[i][:, 4:8, :], attT_d[:, t * 128:(t + 1) * 128].rearrange("(c p) t -> p c t", p=128), r=["attT_d"], w=["mixT%d" % i])
                load(0)
                for t in range(NT):
                    if t + 1 < NT:
                        load(t + 1)
                    i = t % 2
                    for hf in range(2):
                        bk, kb = nb()
                        for kc in range(8):
                            S.pe(MM(bk[:], mixT[i][:, kc, :], wo[:, kc, hf * 512:(hf + 1) * 512], kc == 0, kc == 7), r=["mixT%d" % i, "wo"], w=[kb])
                        csl = slice(hf * 512, (hf + 1) * 512)
                        S.dve(TT(tmp2[:, csl], bk[:], gt1[:, csl], ALU.mult), r=[kb, "modp"], w=["tmpm"])
                    S.pool(TT(x1[i][:], tmp2[:], xb[i][:], ALU.add), r=["tmpm", "xb%d" % i], w=["x1_%d" % i])
                    S.dma(xs_d[t * 128:(t + 1) * 128, :], x1[i][:], r=["x1_%d" % i], w=["xs_d2"])
                    norm_mod_T(x1[i][:], "x1_%d" % i, A2t[:], sh2t[:], hb[i], "hb%d" % i, junk, ssq[i], ms[i], rstd[i], tmp, i,
                               h2s[i][:], "h2s%d" % i)
                    S.dma(h2T_d[:, t * 128:(t + 1) * 128].rearrange("(kc p) t -> p kc t", p=128), h2s[i][:], r=["h2s%d" % i], w=["h2T_d"])
                S.flush()

        def phase_C(l, last):
            TB = 256
            NBC = T // TB
            with contextlib.ExitStack() as st:
                def sb(name, shape, dt):
                    return st.enter_context(SBT(name, shape, dt))
                w1 = sb("w1", [128, 8, 4096], BF16)
                w2 = sb("w2", [128, 32, 1024], BF16)
                gt2 = sb("gt2", [128, 1024], F32)
                fg = sb("fg", [128, 1024], F32)
                h2b = [sb("h2b%d" % i, [128, 8, TB], BF16) for i in range(2)]
                uT = sb("uT", [128, 32, TB], BF16)
                sq = [sb("sq%d" % i, [128, TB], F32) for i in range(2)]
                xb = [sb("xb%d" % i, [128, 1024], F32) for i in range(2)]
                x2 = [sb("x2_%d" % i, [128, 1024], F32) for i in range(2)]
                tmp2 = sb("tmpm", [128, 1024], F32)
                junk = sb("junk", [128, 1024], F32)
                ssq = [sb("ssq%d" % i, [128, 1], F32) for i in range(2)]
                ms = [sb("ms%d" % i, [128, 1], F32) for i in range(2)]
                rstd = [sb("rstd%d" % i, [128, 1], F32) for i in range(2)]
                w1v = W["w_mlp1"][l].rearrange("(kc p) n -> p kc n", p=128)
                w2v = W["w_mlp2"][l].rearrange("(fc p) n -> p fc n", p=128)
                for q in range(4):
                    S.dma(w1[:, :, q * 1024:(q + 1) * 1024], w1v[:, :, q * 1024:(q + 1) * 1024], w=["w1"], eng="pool")
                for q in range(4):
                    S.dma(w2[:, q * 8:(q + 1) * 8, :], w2v[:, q * 8:(q + 1) * 8, :], w=["w2"], eng="pool")
                S.dma(gt2[:], mod_d[:, 5120:6144], r=["mod_d"], w=["modp"])
                if last:
                    S.dma(fg[:], W["final_g"].partition_broadcast(128), w=["fg"])

                def load(bb):
                    i = bb % 2
                    S.dma(h2b[i][:], h2T_d[:, bb * TB:(bb + 1) * TB].rearrange("(kc p) t -> p kc t", p=128), r=["h2T_d"], w=["h2b%d" % i])
                load(0)
                xi = [0]
                for bb in range(NBC):
                    if bb + 1 < NBC:
                        load(bb + 1)
                    i = bb % 2
                    for fc in range(32):
                        bk, kb = nb()
                        for kc in range(8):
                            S.pe(MM(bk[:, 0:TB], w1[:, kc, fc * 128:(fc + 1) * 128], h2b[i][:, kc, :], kc == 0, kc == 7), r=["w1", "h2b%d" % i], w=[kb])
                        s_ = sq[fc % 2]
                        ks_ = "sq%d" % (fc % 2)
                        S.act(ACTF(s_[:], bk[:, 0:TB], AF.Square), r=[kb], w=[ks_])
                        S.dve(STT(uT[:, fc, :], bk[:, 0:TB], 0.0, s_[:], ALU.is_gt, ALU.mult), r=[kb, ks_], w=["uT"])
                    for jj in range(TB // 128):
                        t = bb * (TB // 128) + jj
                        xi_ = xi[0] % 2
                        xi[0] += 1
                        S.dma(xb[xi_][:], xs_d[t * 128:(t + 1) * 128, :], r=["xs_d"], w=["xb%d" % xi_])
                        for hf in range(2):
                            bk, kb = nb()
                            for fc in range(32):
                                S.pe(MM(bk[:], uT[:, fc, jj * 128:(jj + 1) * 128], w2[:, fc, hf * 512:(hf + 1) * 512], fc == 0, fc == 31), r=["uT", "w2"], w=[kb])
                            csl = slice(hf * 512, (hf + 1) * 512)
                            S.dve(TT(tmp2[:, csl], bk[:], gt2[:, csl], ALU.mult), r=[kb, "modp"], w=["tmpm"])
                        S.pool(TT(x2[xi_][:], tmp2[:], xb[xi_][:], ALU.add), r=["tmpm", "xb%d" % xi_], w=["x2_%d" % xi_])
                        if not last:
                            S.dma(xs_d[t * 128:(t + 1) * 128, :], x2[xi_][:], r=["x2_%d" % xi_], w=["xs_d2"])
                        else:
                            S.act(ACTF(junk[:], x2[xi_][:], AF.Square, accum=ssq[xi_][:]), r=["x2_%d" % xi_], w=["ssq%d" % xi_])
                            S.dve(TS(ms[xi_][:], ssq[xi_][:], 1.0 / 1024, 1e-6, ALU.mult, ALU.add), r=["ssq%d" % xi_], w=["ms%d" % xi_])
                            S.pool(TT(rstd[xi_][:], ms[xi_][:], m05[:, 0:1], ALU.pow), r=["ms%d" % xi_, "m05"], w=["rstd%d" % xi_])
                            S.dve(STT(x2[xi_][:], x2[xi_][:], rstd[xi_][:, 0:1], fg[:], ALU.mult, ALU.mult), r=["x2_%d" % xi_, "rstd%d" % xi_, "fg"], w=["x2_%d" % xi_])
                            S.dma(out_d[t * 128:(t + 1) * 128, :], x2[xi_][:], r=["x2_%d" % xi_], w=["out_d"])
                S.flush()

        order = []
        for l in range(L):
            order += [("M", l), ("A1", l), ("A2", l), ("B", l), ("B2", l), ("C", l)]
        for ph, l in order:
            xsrc = x_d if l == 0 else xs_d
            if ph == "M":
                phase_M(l)
            elif ph == "A1":
                phase_A1(l, xsrc)
            elif ph == "A2":
                try:
                    phase_A2(l)
                except _Stop:
                    S.flush()
                    break
            elif ph == "B":
                phase_B(l)
            elif ph == "B2":
                phase_B2(l, xsrc)
            elif ph == "C":
                phase_C(l, l == L - 1)
            if stop_after == (ph, l):
                break
        S.flush(final=True)
        nops = S.nops
    return nc, nops


def _t5_bucket(n):
    n = np.maximum(n, 0)
    nf = np.maximum(n, 1).astype(np.float32)
    large = 16 + (np.log(nf / np.float32(16)) / np.float32(math.log(128 / 16)) * np.float32(16)).astype(np.int32)
    large = np.minimum(large, 31)
    return np.where(n < 16, n, large)


def _consts():
    s = np.arange(128)[:, None]
    t = np.arange(128)[None, :]
    c = np.zeros((128, 640), np.float32)
    c[:, 0:128] = (s == t)
    c[:, 128:256] = (s <= t)
    c[:, 256:384] = (s < t)
    c[:, 384:512] = (s > t)
    c[:, 512:640] = np.where(t <= s, 0.0, -1e30)
    return c


def host_inputs(inputs, T, L):
    B = inputs["x"].shape[0]
    f = lambda a: np.ascontiguousarray(np.asarray(a, dtype=np.float32))
    rel_bias = f(inputs["rel_bias"])
    tk = np.arange(128)[:, None, None, None]
    d = np.arange(2)[None, None, :, None]
    tq = np.arange(128)[None, None, None, :]
    hh = np.arange(8)[None, :, None, None]
    bidx = _t5_bucket(128 * d + tq - tk) + 0 * hh
    bnear = rel_bias[bidx, hh + 0 * bidx].reshape(128, 2048)
    shared = {"consts": _consts(), "bnear": f(bnear), "c31": f(rel_bias[31:32, :])}
    for k in WSHAPES:
        shared[k] = f(inputs[k])[:L]
    for k in VSHAPES:
        shared[k] = f(inputs[k])[:max(L - 1, 1)]
    shared["final_g"] = f(inputs["final_g"]).reshape(1, 1024)
    maps = []
    for bi in range(B):
        m = dict(shared)
        m["x"] = f(inputs["x"][bi, :T])
        m["c8"] = f(np.asarray(inputs["c"][bi]).reshape(8, 128).T)
        maps.append(m)
    return maps


_CACHE = {}


def kernel(**inputs):
    T = inputs["x"].shape[1]
    L = inputs["w_ada"].shape[0]
    B = inputs["x"].shape[0]
    key = (T, L)
    if key not in _CACHE:
        _CACHE[key] = build(T, L)[0]
    nc = _CACHE[key]
    maps = host_inputs(inputs, T, L)
    res = run_bass_kernel_spmd(nc, maps, core_ids=list(range(B)))
    return np.stack([np.asarray(r["out"], dtype=np.float32) for r in res.results], axis=0)
```

```python
import contextlib
import os
import math
import numpy as np
import ml_dtypes
import concourse.bass as bass
import concourse.mybir as mybir
from concourse.bass_utils import run_bass_kernel_spmd

F32 = mybir.dt.float32
BF16 = mybir.dt.bfloat16
AF = mybir.ActivationFunctionType
ALU = mybir.AluOpType
AX = mybir.AxisListType

ENG = ("pe", "dve", "act", "pool", "sp")
NDMA = 12
NBIS = 16


class _Stop(Exception):
    pass


_DEAD = [False]


def chk(n):
    if int(os.environ.get("A2STOP", "0")) == n:
        _DEAD[0] = True


class Op:
    __slots__ = ("eng", "fn", "deps", "signal", "sig_val", "dma", "dma_slot", "dma_val", "sem")

    def __init__(self, eng, fn, dma):
        self.eng = eng
        self.fn = fn
        self.deps = []
        self.signal = False
        self.sig_val = None
        self.dma = dma
        self.dma_slot = None
        self.dma_val = None
        self.sem = None


class Sched:
    def __init__(self, nc, stack):
        self.nc = nc
        self.stack = stack
        self.nsw = 0
        self.sw_dmas = []
        self.esem = {e: stack.enter_context(nc.semaphore("s_" + e)) for e in ENG if e != "sp"}
        self.dsem = [stack.enter_context(nc.semaphore("d_%d" % i)) for i in range(NDMA)]
        self.ops = {e: [] for e in ENG}
        self.last_w = {}
        self.readers = {}
        self.dma_count = 0
        self.dma_last = [None] * NDMA
        self.sigc = {e: 0 for e in ENG}
        self.waited = {e: {} for e in ENG}
        self.bar = {e: [] for e in ENG}
        self.nops = 0

    def _dep(self, op, prod):
        if prod is None or prod is op:
            return
        if (not prod.dma) and (not op.dma) and prod.eng == op.eng == "pe":
            return
        if not prod.dma:
            prod.signal = True
        op.deps.append(prod)

    def add(self, eng, fn, r=(), w=(), dma=False):
        op = Op(eng, fn, dma)
        if _DEAD[0]:
            return op
        if self.bar[eng]:
            for p in self.bar[eng]:
                op.deps.append(p)
            self.bar[eng] = []
        for k in r:
            self._dep(op, self.last_w.get(k))
        for k in w:
            self._dep(op, self.last_w.get(k))
            for rd in self.readers.get(k, ()):
                self._dep(op, rd)
        for k in r:
            self.readers.setdefault(k, []).append(op)
        for k in w:
            self.last_w[k] = op
            self.readers[k] = []
        if dma and eng == "pool":
            op.sem = self.stack.enter_context(self.nc.semaphore("w_%d" % self.nsw))
            op.dma_slot = "w%d" % self.nsw
            self.nsw += 1
            op.dma_val = 16
            self.sw_dmas.append(op)
        elif dma:
            slot = self.dma_count % NDMA
            self.dma_count += 1
            prev = self.dma_last[slot]
            op.dma_slot = slot
            op.sem = self.dsem[slot]
            op.dma_val = (prev.dma_val if prev else 0) + 16
            if prev is not None:
                op.deps.append(prev)
            self.dma_last[slot] = op
        self.ops[eng].append(op)
        self.nops += 1
        return op

    def pe(self, fn, r=(), w=()):
        return self.add("pe", fn, r, w)

    def dve(self, fn, r=(), w=()):
        return self.add("dve", fn, r, w)

    def act(self, fn, r=(), w=()):
        return self.add("act", fn, r, w)

    def pool(self, fn, r=(), w=()):
        return self.add("pool", fn, r, w)

    def dma(self, out, in_, r=(), w=(), eng="sp"):
        return self.add(eng, lambda e: e.dma_start(out=out, in_=in_), r, w, dma=True)

    def flush(self, final=False):
        nc = self.nc
        lasts = []
        for e in ENG:
            nd = [op for op in self.ops[e] if not op.dma]
            if nd:
                nd[-1].signal = True
                lasts.append(nd[-1])
        for e in ENG:
            for op in self.ops[e]:
                if op.signal and not op.dma:
                    self.sigc[e] += 1
                    op.sig_val = self.sigc[e]
        dlast = [p for p in self.dma_last if p is not None] + self.sw_dmas
        self.sw_dmas = []
        with nc.Block() as block:
            engobj = {"pe": block.tensor, "dve": block.vector, "act": block.scalar,
                      "pool": block.gpsimd, "sp": block.sync}
            for ename in ENG:
                ops = self.ops[ename]
                if not ops and not (final and ename == "sp"):
                    continue

                def body(e, ops=ops, ename=ename):
                    waited = self.waited[ename]
                    semof = {}
                    for op in ops:
                        need = {}
                        for p in op.deps:
                            if p.dma:
                                key = ("d", p.dma_slot)
                                semof[key] = p.sem
                                val = p.dma_val
                            else:
                                key = ("e", p.eng)
                                val = p.sig_val
                            if need.get(key, 0) < val:
                                need[key] = val
                        for key, val in need.items():
                            if waited.get(key, 0) >= val:
                                continue
                            waited[key] = val
                            sem = semof[key] if key[0] == "d" else self.esem[key[1]]
                            e.wait_ge(sem, val)
                        ins = op.fn(e)
                        if op.dma:
                            ins.then_inc(op.sem, 16)
                        elif op.signal:
                            ins.then_inc(self.esem[ename], 1)
                    if final and ename == "sp":
                        for p in dlast:
                            if waited.get(("d", p.dma_slot), 0) < p.dma_val:
                                e.wait_ge(p.sem, p.dma_val)
                        for p in lasts:
                            e.wait_ge(self.esem[p.eng], p.sig_val)
                engobj[ename](body)
        barrier = lasts + dlast
        self.ops = {e: [] for e in ENG}
        self.last_w = {}
        self.readers = {}
        self.bar = {e: list(barrier) for e in ENG}


def MM(out, lhsT, rhs, start=True, stop=True):
    return lambda e: e.matmul(out, lhsT=lhsT, rhs=rhs, start=start, stop=stop)


def TR(out, in_, ident):
    return lambda e: e.transpose(out, in_, ident)


def ACTF(out, in_, func, bias=0.0, scale=1.0, accum=None):
    if accum is None:
        return lambda e: e.activation(out=out, in_=in_, func=func, bias=bias, scale=scale)
    return lambda e: e.activation(out=out, in_=in_, func=func, bias=bias, scale=scale, accum_out=accum)


def TT(out, a, b, op):
    return lambda e: e.tensor_tensor(out=out, in0=a, in1=b, op=op)


def TS(out, a, s1, s2=None, op0=ALU.mult, op1=None, accum=None):
    if accum is not None:
        return lambda e: e.tensor_scalar(out=out, in0=a, scalar1=s1, scalar2=s2, op0=op0, op1=op1, accum_out=accum)
    if op1 is None:
        return lambda e: e.tensor_scalar(out=out, in0=a, scalar1=s1, scalar2=None, op0=op0)
    return lambda e: e.tensor_scalar(out=out, in0=a, scalar1=s1, scalar2=s2, op0=op0, op1=op1)


def STT(out, a, s, b, op0, op1):
    return lambda e: e.scalar_tensor_tensor(out=out, in0=a, scalar=s, in1=b, op0=op0, op1=op1)


def CP(out, in_):
    return lambda e: e.tensor_copy(out, in_)


def RED(out, in_, op, axis=AX.X, absval=False):
    if absval:
        return lambda e: e.tensor_reduce(out=out, in_=in_, axis=axis, op=op, apply_absolute_value=True)
    return lambda e: e.tensor_reduce(out=out, in_=in_, axis=axis, op=op)


def MEMSET(ap, v):
    return lambda e: e.memset(ap, v)


WSHAPES = {
    "w_ada": (1024, 6144), "b_ada": (6144,), "norm1_g": (1024,), "norm2_g": (1024,),
    "w_in": (1024, 3656), "mu_rkv": (3, 512), "mu_lora": (3, 1024), "decay_w0": (512,),
    "decay_a": (1024, 64), "decay_b": (64, 512), "iclr_a0": (512,), "iclr_a": (1024, 64),
    "iclr_b": (64, 512), "gate_a": (1024, 128), "gate_b": (128, 512), "k_k": (512,), "k_a": (512,),
    "r_k": (8, 64), "lnx_g": (512,), "lnx_b": (512,), "attn_out_g": (512,),
    "w_out": (1024, 1024), "w_mlp1": (1024, 4096), "w_mlp2": (4096, 1024),
}
VSHAPES = {"vres_mu": (1024,), "vres_v0": (512,), "vres_a": (1024, 32), "vres_b": (32, 512)}


def build(T, L, dbg=False, stop_after=None):
    _DEAD[0] = False
    NT = T // 128
    NB = T // 512
    NSEL = min(256, T // 4)
    nc = bass.Bass("TRN2", target_bir_lowering=False)
    W = {}

    def din(name, shape):
        W[name] = nc.dram_tensor(name, list(shape), F32, kind="ExternalInput").ap()
        return W[name]

    x_d = din("x", [T, 1024])
    c8_d = din("c8", [128, 8])
    consts_d = din("consts", [128, 640])
    bnear_d = din("bnear", [128, 2048])
    c31_d = din("c31", [1, 8])
    for k, s in WSHAPES.items():
        din(k, (L,) + s)
    for k, s in VSHAPES.items():
        din(k, (max(L - 1, 1),) + s)
    din("final_g", [1, 1024])
    out_d = nc.dram_tensor("out", [T, 1024], F32, kind="ExternalOutput").ap()

    def dscr(name, shape, dt):
        return nc.dram_tensor(name, list(shape), dt, kind=("ExternalOutput" if dbg else "Internal")).ap()

    xs_d = dscr("xs", [T, 1024], F32)
    mod_d = dscr("modd", [128, 6144], F32)
    featT_d = dscr("featT", [1600, T], BF16)
    vaug_d = dscr("vaug", [T, 520], BF16)
    wi_d = dscr("wid", [T, 8], F32)
    hT_d = dscr("hTd", [1024, T], BF16)
    vfirst_d = dscr("vfirst", [T, 512], F32)
    rwkvT_d = dscr("rwkvT", [512, T], BF16)
    attT_d = dscr("attT", [512, T], BF16)
    h2T_d = dscr("h2T", [1024, T], BF16)
    dbg_d = {}
    if dbg:
        for nm, shp in (("dbg_y", [T, 512]), ("dbg_score", [T, T]), ("dbg_thr", [T, 1]), ("dbg_v", [T, 512]),
                        ("dbg_r", [T, 512]), ("dbg_k", [T, 512]), ("dbg_a", [T, 512]), ("dbg_lw", [T, 512])):
            dbg_d[nm] = nc.dram_tensor(nm, shp, F32, kind="ExternalOutput").ap()

    uniq = [0]

    def SBT(name, shape, dt):
        uniq[0] += 1
        return nc.sbuf_tensor("%s_%d" % (name, uniq[0]), shape, dt)

    with contextlib.ExitStack() as gst:
        S = Sched(nc, gst)

        def gsb(name, shape, dt):
            return gst.enter_context(SBT(name, shape, dt))

        banks = [gst.enter_context(nc.psum_tensor("bank%d" % i, [128, 512], F32)) for i in range(8)]
        bank_i = [0]

        NROT = 6

        def nb():
            i = bank_i[0] % NROT
            bank_i[0] += 1
            return banks[i], "bank%d" % i

        def bfv(bank):
            return bank[:].bitcast(BF16)

        cst = gsb("cst", [128, 640], F32)
        identb = gsb("identb", [128, 128], BF16)
        mask4 = gsb("mask4", [128, 512], F32)
        lowm4 = gsb("lowm4", [128, 512], F32)
        onesf = gsb("onesf", [128, 128], F32)
        m05 = gsb("m05", [128, 512], F32)
        cbc = gsb("cbc", [128, 8, 128], F32)
        identf = cst[:, 0:128]
        tri_incl = cst[:, 128:256]
        tri_strict = cst[:, 256:384]
        low_strict = cst[:, 384:512]
        caus = cst[:, 512:640]

        with contextlib.ExitStack() as st:
            c8 = st.enter_context(SBT("c8s", [128, 8], F32))
            c8t = st.enter_context(SBT("c8t", [128, 8], F32))
            S.dma(cst[:], consts_d, w=["cst"])
            S.dma(c8[:], c8_d, w=["c8"])
            S.dve(CP(identb[:], identf), r=["cst"], w=["identb"])
            for i in range(4):
                S.dve(CP(mask4[:, i * 128:(i + 1) * 128], tri_strict if i % 2 == 0 else tri_incl), r=["cst"], w=["mask4"])
                S.pool(CP(lowm4[:, i * 128:(i + 1) * 128], low_strict), r=["cst"], w=["lowm4"])
            S.pool(MEMSET(onesf[:], 1.0), w=["onesf"])
            S.pool(MEMSET(m05[:], -0.5), w=["m05"])
            S.act(ACTF(c8t[:], c8[:], AF.Tanh, scale=0.5), r=["c8"], w=["c8t"])
            S.dve(TS(c8t[:], c8t[:], 0.5, 0.5, ALU.mult, ALU.add), r=["c8t"], w=["c8t"])
            S.dve(TT(c8t[:], c8t[:], c8[:], ALU.mult), r=["c8t", "c8"], w=["c8t"])
            S.dve(CP(cbc[:], c8t[:].unsqueeze(2).to_broadcast([128, 8, 128])), r=["c8t"], w=["cbc"])
            S.flush()

        def phase_M(l):
            with contextlib.ExitStack() as st:
                def sb(name, shape, dt):
                    return st.enter_context(SBT(name, shape, dt))
                wst = [sb("wst%d" % i, [128, 8, 512], F32) for i in range(2)]
                modt = sb("modt", [128, 6144], F32)
                gbc = sb("gbc", [128, 2048], F32)
                bada = sb("bada", [1, 6144], F32)
                S.dma(gbc[:, 0:1024], W["norm1_g"][l:l + 1, :].partition_broadcast(128), w=["gbc"])
                S.dma(gbc[:, 1024:2048], W["norm2_g"][l:l + 1, :].partition_broadcast(128), w=["gbc"])
                S.dma(bada[:], W["b_ada"][l:l + 1, :], w=["bada"])
                wa = W["w_ada"][l].rearrange("(kc p) n -> p kc n", p=128)
                for n in range(12):
                    buf = wst[n % 2]
                    key = "wst%d" % (n % 2)
                    S.dma(buf[:], wa[:, :, n * 512:(n + 1) * 512], w=[key])
                    bk, kb = nb()
                    for kc in range(8):
                        S.pe(MM(bk[:], cbc[:, kc, :], buf[:, kc, :], kc == 0, False), r=[key, "cbc"], w=[kb])
                    S.pe(MM(bk[:], onesf[0:1, :], bada[0:1, n * 512:(n + 1) * 512], False, True), r=["bada", "onesf"], w=[kb])
                    S.act(ACTF(modt[:, n * 512:(n + 1) * 512], bk[:], AF.Copy), r=[kb], w=["modt"])
                S.dve(STT(modt[:, 1024:2048], modt[:, 1024:2048], 1.0, gbc[:, 0:1024], ALU.add, ALU.mult), r=["modt", "gbc"], w=["modt"])
                S.dve(STT(modt[:, 4096:5120], modt[:, 4096:5120], 1.0, gbc[:, 1024:2048], ALU.add, ALU.mult), r=["modt", "gbc"], w=["modt"])
                S.dma(mod_d, modt[:], r=["modt"], w=["mod_d"])
                S.flush()

        def norm_mod_T(X, kx, At, sht, hb, khb, junk, ssq, ms, rstd, tmp, idx, dstT, kdst):
            S.act(ACTF(junk[:], X, AF.Square, accum=ssq[:]), r=[kx], w=["ssq%d" % idx])
            S.dve(TS(ms[:], ssq[:], 1.0 / 1024, 1e-6, ALU.mult, ALU.add), r=["ssq%d" % idx], w=["ms%d" % idx])
            S.pool(TT(rstd[:], ms[:], m05[:, 0:1], ALU.pow), r=["ms%d" % idx, "m05"], w=["rstd%d" % idx])
            S.dve(STT(tmp[:], X, rstd[:, 0:1], At, ALU.mult, ALU.mult), r=[kx, "rstd%d" % idx, "modp"], w=["tmpn"])
            S.pool(TT(hb[:], tmp[:], sht, ALU.add), r=["tmpn", "modp"], w=[khb])
            bk, kb = nb()
            bkb = bfv(bk)
            for kc in range(8):
                S.pe(TR(bkb[:, kc * 128:(kc + 1) * 128], hb[:, kc * 128:(kc + 1) * 128], identb[:]), r=[khb, "identb"], w=[kb])
            S.act(ACTF(dstT, bkb.rearrange("p (k t) -> p k t", k=8), AF.Copy), r=[kb], w=[kdst])

        def phase_A1(l, xsrc):
            with contextlib.ExitStack() as st:
                def sb(name, shape, dt):
                    return st.enter_context(SBT(name, shape, dt))
                WF = sb("WF", [128, 8, 1600], BF16)
                WV = sb("WV", [128, 8, 520], BF16)
                A1t = sb("A1t", [128, 1024], F32)
                sh1t = sb("sh1t", [128, 1024], F32)
                xb = [sb("xb%d" % i, [128, 1024], F32) for i in range(2)]
                junk = sb("junk", [128, 1024], F32)
                tmp = sb("tmpn", [128, 1024], F32)
                hb = [sb("hb%d" % i, [128, 1024], BF16) for i in range(2)]
                hT = [sb("hT%d" % i, [128, 8, 512], BF16) for i in range(2)]
                fst = [sb("fst%d" % i, [128, 512], BF16) for i in range(2)]
                vst = [sb("vst%d" % i, [128, 8, 65], BF16) for i in range(2)]
                wist = [sb("wist%d" % i, [128, 8], F32) for i in range(2)]
                ssq = [sb("ssq%d" % i, [128, 1], F32) for i in range(2)]
                ms = [sb("ms%d" % i, [128, 1], F32) for i in range(2)]
                rstd = [sb("rstd%d" % i, [128, 1], F32) for i in range(2)]
                wv = W["w_in"][l].rearrange("(kc p) n -> p kc n", p=128)
                S.dma(WF[:, :, 0:1024], wv[:, :, 1536:2560], w=["WF"], eng="pool")
                S.dma(WF[:, :, 1024:1600], wv[:, :, 3072:3648], w=["WF"], eng="pool")
                S.dma(WV[:, :, 0:512], wv[:, :, 2560:3072], w=["WV"], eng="pool")
                S.dma(WV[:, :, 512:520], wv[:, :, 3648:3656], w=["WV"], eng="pool")
                S.dma(A1t[:], mod_d[:, 1024:2048], r=["mod_d"], w=["modp"])
                S.dma(sh1t[:], mod_d[:, 0:1024], r=["mod_d"], w=["modp"])
                for i in range(2):
                    S.pool(MEMSET(vst[i][:, :, 64:65], 1.0), w=["vst%d" % i])

                def load(t):
                    S.dma(xb[t % 2][:], xsrc[t * 128:(t + 1) * 128, :], r=["xs_d"], w=["xb%d" % (t % 2)])
                load(0)
                for b in range(NB):
                    hTb = hT[b % 2]
                    kh = "hT%d" % (b % 2)
                    for j in range(4):
                        t = b * 4 + j
                        if t + 1 < NT:
                            load(t + 1)
                        i2 = t % 2
                        norm_mod_T(xb[i2][:], "xb%d" % i2, A1t[:], sh1t[:], hb[i2], "hb%d" % i2, junk, ssq[i2], ms[i2],
                                   rstd[i2], tmp, i2, hTb[:, :, j * 128:(j + 1) * 128], kh)
                        bk, kb = nb()
                        bk2, kb2 = nb()
                        for kc in range(8):
                            S.pe(MM(bk[:], hTb[:, kc, j * 128:(j + 1) * 128], WV[:, kc, 0:512], kc == 0, kc == 7), r=[kh, "WV"], w=[kb])
                        for kc in range(8):
                            S.pe(MM(bk2[:, 0:8], hTb[:, kc, j * 128:(j + 1) * 128], WV[:, kc, 512:520], kc == 0, kc == 7), r=[kh, "WV"], w=[kb2])
                        S.dve(CP(vst[i2][:, :, 0:64], bk[:].rearrange("p (h d) -> p h d", h=8)), r=[kb], w=["vst%d" % i2])
                        S.act(ACTF(wist[i2][:], bk2[:, 0:8], AF.Copy), r=[kb2], w=["wist%d" % i2])
                        S.dma(vaug_d[t * 128:(t + 1) * 128, :], vst[i2][:].rearrange("p h d -> p (h d)"), r=["vst%d" % i2], w=["vaug_d"])
                        S.dma(wi_d[t * 128:(t + 1) * 128, :], wist[i2][:], r=["wist%d" % i2], w=["wi_d"])
                    for c in range(13):
                        rows = 128 if c < 12 else 64
                        bk, kb = nb()
                        for kc in range(8):
                            S.pe(MM(bk[0:rows, :], WF[:, kc, c * 128:c * 128 + rows], hTb[:, kc, :], kc == 0, kc == 7), r=[kh, "WF"], w=[kb])
                        f = fst[c % 2]
                        kf = "fst%d" % (c % 2)
                        if c % 2 == 0:
                            S.act(ACTF(f[0:rows, :], bk[0:rows, :], AF.Copy), r=[kb], w=[kf])
                        else:
                            S.dve(CP(f[0:rows, :], bk[0:rows, :]), r=[kb], w=[kf])
                        S.dma(featT_d[c * 128:c * 128 + rows, b * 512:(b + 1) * 512], f[0:rows, :], r=[kf], w=["featT_d"])
                    S.dma(hT_d[:, b * 512:(b + 1) * 512].rearrange("(kc p) t -> p kc t", p=128), hTb[:], r=[kh], w=["hT_d"])
                S.flush()

        def phase_A2(l):
            with contextlib.ExitStack() as st:
                def sb(name, shape, dt):
                    return st.enter_context(SBT(name, shape, dt))
                WT1 = sb("WT1", [128, 8, 1536], BF16)
                WT2 = sb("WT2", [128, 8, 1536], BF16)
                LA1 = sb("LA1", [128, 8, 288], BF16)
                LA2 = sb("LA2", [128, 8, 288], BF16)
                Bwa = sb("Bwa", [128, 512], BF16)
                Bg = sb("Bg", [128, 512], BF16)
                Bv = sb("Bv", [32, 512], BF16)
                brow = sb("brow", [1, 3, 512], F32)
                pbc = sb("pbc", [128, 5, 512], F32)
                muT = sb("muT", [128, 32], F32)
                omT = sb("omT", [128, 32], F32)
                with contextlib.ExitStack() as st2:
                    Wr = st2.enter_context(SBT("Wr", [128, 8, 1536], BF16))
                    mubc = st2.enter_context(SBT("mubc", [128, 1536], F32))
                    ombc = st2.enter_context(SBT("ombc", [128, 1536], F32))
                    LA = st2.enter_context(SBT("LA", [128, 8, 288], F32))
                    mu32 = st2.enter_context(SBT("mu32", [32, 128], F32))
                    wv = W["w_in"][l].rearrange("(kc p) n -> p kc n", p=128)
                    S.dma(Wr[:, :, 0:768], wv[:, :, 0:768], w=["Wr"], eng="pool")
                    S.dma(Wr[:, :, 768:1536], wv[:, :, 768:1536], w=["Wr"], eng="pool")
                    S.dma(mubc[:], W["mu_rkv"][l:l + 1].rearrange("o a b -> o (a b)").partition_broadcast(128), w=["mubc"])
                    S.dve(TS(ombc[:], mubc[:], -1.0, 1.0, ALU.mult, ALU.add), r=["mubc"], w=["ombc"])
                    S.dve(TT(WT2[:], Wr[:], mubc[:].unsqueeze(1).to_broadcast([128, 8, 1536]), ALU.mult), r=["Wr", "mubc"], w=["WT2"])
                    S.pool(TT(WT1[:], Wr[:], ombc[:].unsqueeze(1).to_broadcast([128, 8, 1536]), ALU.mult), r=["Wr", "ombc"], w=["WT1"])
                    S.dma(LA[:, :, 0:64], W["decay_a"][l].rearrange("(kc p) n -> p kc n", p=128), w=["LA"])
                    S.dma(LA[:, :, 64:128], W["iclr_a"][l].rearrange("(kc p) n -> p kc n", p=128), w=["LA"])
                    S.dma(LA[:, :, 128:256], W["gate_a"][l].rearrange("(kc p) n -> p kc n", p=128), w=["LA"])
                    if l > 0:
                        S.dma(LA[:, :, 256:288], W["vres_a"][l - 1].rearrange("(kc p) n -> p kc n", p=128), w=["LA"])
                    else:
                        S.dve(MEMSET(LA[:, :, 256:288], 0.0), w=["LA"])
                    S.dve(MEMSET(mu32[:], 0.0), w=["mu32"])
                    S.dma(mu32[0:24, :], W["mu_lora"][l].rearrange("a (kc p) -> (a kc) p", p=128), r=["mu32"], w=["mu32"])
                    if l > 0:
                        S.dma(mu32[24:32, :], W["vres_mu"][l - 1:l, :].rearrange("o (kc p) -> (o kc) p", p=128), r=["mu32"], w=["mu32"])
                    bk, kb = nb()
                    S.pe(MM(bk[:, 0:32], mu32[:], identf[0:32, 0:32], True, True), r=["mu32", "cst"], w=[kb])
                    S.act(ACTF(muT[:], bk[:, 0:32], AF.Copy), r=[kb], w=["muT"])
                    S.dve(TS(omT[:], muT[:], -1.0, 1.0, ALU.mult, ALU.add), r=["muT"], w=["omT"])
                    for gi, (c0, c1) in enumerate(((0, 64), (64, 128), (128, 256), (256, 288))):
                        wdt = c1 - c0
                        S.dve(TT(LA2[:, :, c0:c1], LA[:, :, c0:c1], muT[:, gi * 8:(gi + 1) * 8].unsqueeze(2).to_broadcast([128, 8, wdt]), ALU.mult), r=["LA", "muT"], w=["LA2"])
                        S.dve(TT(LA1[:, :, c0:c1], LA[:, :, c0:c1], omT[:, gi * 8:(gi + 1) * 8].unsqueeze(2).to_broadcast([128, 8, wdt]), ALU.mult), r=["LA", "omT"], w=["LA1"])
                    S.dma(Bwa[0:64, :], W["decay_b"][l], w=["Bwa"], eng="pool")
                    S.dma(Bwa[64:128, :], W["iclr_b"][l], w=["Bwa"], eng="pool")
                    S.dma(Bg[:], W["gate_b"][l], w=["Bg"], eng="pool")
                    if l > 0:
                        S.dma(Bv[:], W["vres_b"][l - 1], w=["Bv"], eng="pool")
                    S.dma(brow[:, 0, :], W["decay_w0"][l:l + 1, :], w=["brow"])
                    S.dma(brow[:, 1, :], W["iclr_a0"][l:l + 1, :], w=["brow"])
                    if l > 0:
                        S.dma(brow[:, 2, :], W["vres_v0"][l - 1:l, :], w=["brow"])
                    S.dma(pbc[:, 0, :], W["k_k"][l:l + 1, :].partition_broadcast(128), w=["pbc"])
                    S.dma(pbc[:, 1, :], W["k_a"][l:l + 1, :].partition_broadcast(128), w=["pbc"])
                    S.dma(pbc[:, 2, :], W["r_k"][l:l + 1].rearrange("o a b -> o (a b)").partition_broadcast(128), w=["pbc"])
                    S.dma(pbc[:, 3, :], W["lnx_g"][l:l + 1, :].partition_broadcast(128), w=["pbc"])
                    S.dma(pbc[:, 4, :], W["lnx_b"][l:l + 1, :].partition_broadcast(128), w=["pbc"])
                    S.flush()

                chk(1)
                hTb2 = [sb("hTb%d" % i, [128, 8, 512], BF16) for i in range(1)]
                hTp2 = [sb("hTp%d" % i, [128, 8, 512], BF16) for i in range(1)]
                L1wa = sb("L1wa", [128, 512], BF16)
                sg = sb("sg", [128, 512], BF16)
                sgt = sb("sgt", [128, 512], F32)
                L1v = sb("L1v", [32, 512], BF16)

                def f32t(name):
                    return sb(name, [128, 512], F32)

                def b16t(name):
                    return sb(name, [128, 512], BF16)
                r32, k32, v32 = f32t("r32"), f32t("k32"), f32t("v32")
                kkr, kk, a32, b32, km = f32t("kkr"), f32t("kk"), f32t("a32"), f32t("b32"), f32t("km")
                lw, Ginc, Ginv, Gexc, g32 = f32t("lw"), f32t("Ginc"), f32t("Ginv"), f32t("Gexc"), f32t("g32")
                t1, t2, U0, cen, vf = f32t("t1"), f32t("t2"), f32t("U0"), f32t("cen"), f32t("vf")
                y32 = f32t("y32")
                Kd, Rd, Bi, Ki, Vb, Zb, Ub, ob = (b16t(n) for n in ("Kd", "Rd", "Bi", "Ki", "Vb", "Zb", "Ub", "ob"))
                s8 = [sb("s8_%d" % i, [128, 8], F32) for i in range(6)]
                FT = sb("FT", [64, 8, 4, 128], BF16)
                GE = sb("GE", [128, 8, 512], BF16)
                MMb = [sb("MMb%d" % i, [128, 8, 128], F32) for i in range(2)]
                NNb = [sb("NNb%d" % i, [128, 8, 128], F32) for i in range(2)]
                TTb = [sb("TTb%d" % i, [128, 8, 128], F32) for i in range(2)]
                TTh = sb("TTh", [128, 8, 128], BF16)
                WTs = sb("WTs", [64, 8, 128], BF16)
                P32 = sb("P32", [64, 8, 64], F32)
                Pb = sb("Pb", [64, 8, 64], BF16)
                GC = sb("GC", [64, 8], F32)
                rwT = sb("rwT", [128, 4, 128], BF16)
                S.dve(MEMSET(P32[:], 0.0), w=["P32"])
                S.dve(MEMSET(Pb[:], 0.0), w=["Pb"])
                P32f = P32[:].rearrange("p h v -> p (h v)")
                ev = [0]

                def evac(out, in_, r, w):
                    ev[0] += 1
                    if ev[0] % 2:
                        S.act(ACTF(out, in_, AF.Copy), r=r, w=w)
                    else:
                        S.dve(CP(out, in_), r=r, w=w)

                for b in range(NB):
                    hTb = hTb2[0]
                    kh = "hTb0"
                    src = hT_d.rearrange("(kc p) t -> p kc t", p=128)
                    if b == 1:
                        chk(8)
                    hTp = hTp2[0]
                    S.dma(hTb[:], src[:, :, b * 512:(b + 1) * 512], r=["hT_d"], w=[kh])
                    if b == 0:
                        S.dve(MEMSET(hTp[:, :, 0:1], 0.0), w=[kh])
                        S.dma(hTp[:, :, 1:512], src[:, :, 0:511], r=["hT_d"], w=[kh])
                    else:
                        S.dma(hTp[:], src[:, :, b * 512 - 1:b * 512 + 511], r=["hT_d"], w=[kh])
                    if b == 1:
                        chk(9)
                    for gi, (c0, c1) in enumerate(((0, 128), (128, 256), (256, 288))):
                        if gi == 2 and l == 0:
                            continue
                        rows = c1 - c0
                        bk, kb = nb()
                        for kc in range(8):
                            S.pe(MM(bk[0:rows, :], LA1[:, kc, c0:c1], hTb[:, kc, :], kc == 0, False), r=[kh, "LA1"], w=[kb])
                        for kc in range(8):
                            S.pe(MM(bk[0:rows, :], LA2[:, kc, c0:c1], hTp[:, kc, :], False, kc == 7), r=[kh, "LA2"], w=[kb])
                        if gi == 0:
                            S.act(ACTF(L1wa[0:64, :], bk[0:64, :], AF.Tanh), r=[kb], w=["L1wa"])
                            S.act(ACTF(L1wa[64:128, :], bk[64:128, :], AF.Copy), r=[kb], w=["L1wa"])
                        elif gi == 1:
                            S.act(ACTF(sgt[:], bk[:], AF.Tanh, scale=0.5), r=[kb], w=["sgt"])
                            S.dve(TS(sg[:], sgt[:], 0.5, 0.5, ALU.mult, ALU.add), r=["sgt"], w=["sg"])
                        else:
                            S.act(ACTF(L1v[:], bk[0:32, :], AF.Copy), r=[kb], w=["L1v"])
                    chk(2)
                    for j in range(4):
                        t = b * 4 + j
                        lo = j * 128
                        tsl = slice(j * 128, (j + 1) * 128)
                        if l > 0:
                            S.dma(vf[:], vfirst_d[t * 128:(t + 1) * 128, :], r=["vfirst_d"], w=["vf"])
                        for g, (dst, kd) in enumerate(((r32, "r32"), (k32, "k32"), (v32, "v32"))):
                            bk, kb = nb()
                            for kc in range(8):
                                S.pe(MM(bk[:], hTb[:, kc, lo:lo + 128], WT1[:, kc, g * 512:(g + 1) * 512], kc == 0, False), r=[kh, "WT1"], w=[kb])
                            for kc in range(8):
                                S.pe(MM(bk[:], hTp[:, kc, lo:lo + 128], WT2[:, kc, g * 512:(g + 1) * 512], False, kc == 7), r=[kh, "WT2"], w=[kb])
                            evac(dst[:], bk[:], [kb], [kd])
                        bkw, kbw = nb()
                        S.pe(MM(bkw[:], L1wa[0:64, tsl], Bwa[0:64, :], True, False), r=["L1wa", "Bwa"], w=[kbw])
                        S.pe(MM(bkw[:], onesf[0:1, :], brow[0:1, 0, :], False, True), r=["onesf", "brow"], w=[kbw])
                        bka, kba = nb()
                        S.pe(MM(bka[:], L1wa[64:128, tsl], Bwa[64:128, :], True, False), r=["L1wa", "Bwa"], w=[kba])
                        S.pe(MM(bka[:], onesf[0:1, :], brow[0:1, 1, :], False, True), r=["onesf", "brow"], w=[kba])
                        bkg, kbg = nb()
                        S.pe(MM(bkg[:], sg[:, tsl], Bg[:], True, True), r=["sg", "Bg"], w=[kbg])
                        S.act(ACTF(lw[:], bkw[:], AF.Tanh, scale=0.5), r=[kbw], w=["lw"])
                        S.dve(TS(lw[:], lw[:], -0.5 * math.exp(-0.5), -0.5 * math.exp(-0.5), ALU.mult, ALU.add), r=["lw"], w=["lw"])
                        S.act(ACTF(a32[:], bka[:], AF.Tanh, scale=0.5), r=[kba], w=["a32"])
                        S.pool(TS(a32[:], a32[:], 0.5, 0.5, ALU.mult, ALU.add), r=["a32"], w=["a32"])
                        S.act(ACTF(g32[:], bkg[:], AF.Copy), r=[kbg], w=["g32"])
                        if l > 0:
                            bkv, kbv = nb()
                            S.pe(MM(bkv[:], L1v[0:32, tsl], Bv[0:32, :], True, False), r=["L1v", "Bv"], w=[kbv])
                            S.pe(MM(bkv[:], onesf[0:1, :], brow[0:1, 2, :], False, True), r=["onesf", "brow"], w=[kbv])
                            S.act(ACTF(t1[:], bkv[:], AF.Tanh, scale=0.5), r=[kbv], w=["t1"])
                            S.pool(TS(t1[:], t1[:], 0.5, 0.5, ALU.mult, ALU.add), r=["t1"], w=["t1"])
                            S.pool(TT(t2[:], vf[:], v32[:], ALU.subtract), r=["vf", "v32"], w=["t2"])
                            S.pool(TT(t2[:], t2[:], t1[:], ALU.mult), r=["t2", "t1"], w=["t2"])
                            S.pool(TT(v32[:], v32[:], t2[:], ALU.add), r=["v32", "t2"], w=["v32"])
                        else:
                            S.dma(vfirst_d[t * 128:(t + 1) * 128, :], v32[:], r=["v32"], w=["vfirst_d"])
                        S.act(ACTF(Vb[:], v32[:], AF.Copy), r=["v32"], w=["Vb"])
                        if dbg:
                            rows = slice(t * 128, (t + 1) * 128)
                            S.dma(dbg_d["dbg_v"][rows, :], v32[:], r=["v32"], w=["dbgv"])
                            S.dma(dbg_d["dbg_r"][rows, :], r32[:], r=["r32"], w=["dbgr"])
                            S.dma(dbg_d["dbg_k"][rows, :], k32[:], r=["k32"], w=["dbgk"])
                            S.dma(dbg_d["dbg_a"][rows, :], a32[:], r=["a32"], w=["dbga"])
                            S.dma(dbg_d["dbg_lw"][rows, :], lw[:], r=["lw"], w=["dbglw"])
                        bkc, kbc = nb()
                        S.pe(MM(bkc[:], tri_incl, lw[:], True, True), r=["cst", "lw"], w=[kbc])
                        bke, kbe = nb()
                        S.pe(MM(bke[:], tri_strict, lw[:], True, True), r=["cst", "lw"], w=[kbe])
                        bkG, kbG = nb()
                        for h in range(8):
                            S.pe(MM(bkG[0:64, h:h + 1], lw[:, h * 64:(h + 1) * 64], onesf[:, 0:1], True, True), r=["lw", "onesf"], w=[kbG])
                        S.act(ACTF(Ginc[:], bkc[:], AF.Exp), r=[kbc], w=["Ginc"])
                        S.act(ACTF(Ginv[:], bkc[:], AF.Exp, scale=-1.0), r=[kbc], w=["Ginv"])
                        S.act(ACTF(Gexc[:], bke[:], AF.Exp), r=[kbe], w=["Gexc"])
                        S.act(ACTF(GC[:], bkG[0:64, 0:8], AF.Exp), r=[kbG], w=["GC"])
                        S.dve(TT(kkr[:], k32[:], pbc[:, 0, :], ALU.mult), r=["k32", "pbc"], w=["kkr"])
                        S.act(ACTF(t2[:], kkr[:], AF.Square), r=["kkr"], w=["t2"])
                        S.dve(RED(s8[0][:], t2[:].rearrange("p (h d) -> p h d", h=8), ALU.add), r=["t2"], w=["s8_0"])
                        S.dve(TS(s8[0][:], s8[0][:], 1e-24, None, ALU.max), r=["s8_0"], w=["s8_0"])
                        S.pool(TT(s8[1][:], s8[0][:], m05[:, 0:8], ALU.pow), r=["s8_0", "m05"], w=["s8_1"])
                        S.dve(TT(kk[:].rearrange("p (h d) -> p h d", h=8), kkr[:].rearrange("p (h d) -> p h d", h=8),
                                 s8[1][:].unsqueeze(2).to_broadcast([128, 8, 64]), ALU.mult), r=["kkr", "s8_1"], w=["kk"])
                        S.pool(TT(b32[:], kk[:], a32[:], ALU.mult), r=["kk", "a32"], w=["b32"])
                        S.dve(STT(t1[:], a32[:], -1.0, pbc[:, 1, :], ALU.add, ALU.mult), r=["a32", "pbc"], w=["t1"])
                        S.dve(STT(km[:], t1[:], 1.0, k32[:], ALU.add, ALU.mult), r=["t1", "k32"], w=["km"])
                        S.dve(TT(Kd[:], kk[:], Gexc[:], ALU.mult), r=["kk", "Gexc"], w=["Kd"])
                        S.pool(TT(Bi[:], b32[:], Ginv[:], ALU.mult), r=["b32", "Ginv"], w=["Bi"])
                        S.dve(TT(Ki[:], km[:], Ginv[:], ALU.mult), r=["km", "Ginv"], w=["Ki"])
                        S.pool(TT(Rd[:], r32[:], Ginc[:], ALU.mult), r=["r32", "Ginc"], w=["Rd"])
                        S.pool(TT(t2[:], r32[:], km[:], ALU.mult), r=["r32", "km"], w=["t2"])
                        S.pool(TT(t2[:], t2[:], pbc[:, 2, :], ALU.mult), r=["t2", "pbc"], w=["t2"])
                        S.dve(RED(s8[2][:], t2[:].rearrange("p (h d) -> p h d", h=8), ALU.add), r=["t2"], w=["s8_2"])
                        chk(3)
                        for q, (srcT, ks) in enumerate(((Kd, "Kd"), (Rd, "Rd"), (Bi, "Bi"), (Ki, "Ki"))):
                            bk, kb = nb()
                            bkb = bfv(bk)
                            for h in range(8):
                                S.pe(TR(bkb[0:64, h * 128:(h + 1) * 128], srcT[:, h * 64:(h + 1) * 64], identb[:]), r=[ks, "identb"], w=[kb])
                            evac(FT[:, :, q, :], bkb[0:64, :].rearrange("p (h t) -> p h t", h=8), [kb], ["FT"])
                        for h in range(8):
                            bk, kb = nb()
                            rhs = FT[:, h, 0:2, :].rearrange("p a t -> p (a t)")
                            S.pe(MM(bk[:, 0:256], FT[:, h, 2, :], rhs, True, True), r=["FT"], w=[kb])
                            S.pe(MM(bk[:, 256:512], FT[:, h, 3, :], rhs, True, True), r=["FT"], w=[kb])
                            S.dve(TT(GE[:, h, :], bk[:], mask4[:], ALU.mult), r=[kb, "mask4"], w=["GE"])
                            S.dve(TT(NNb[0][:, h, :], bk[:, 0:128], tri_strict, ALU.mult), r=[kb, "cst"], w=["NNb0"])
                        for g in range(2):
                            bk, kb = nb()
                            for hh in range(4):
                                h = g * 4 + hh
                                S.pe(MM(bk[:, hh * 128:(hh + 1) * 128], FT[:, h, 0, :], FT[:, h, 2, :], True, True), r=["FT"], w=[kb])
                            S.dve(TT(MMb[0][:, g * 4:(g + 1) * 4, :], bk[:].rearrange("p (h t) -> p h t", h=4),
                                     lowm4[:].rearrange("p (h t) -> p h t", h=4), ALU.mult), r=[kb, "lowm4"], w=["MMb0"])
                        chk(4)
                        S.pool(TT(TTb[0][:], identf.unsqueeze(1).to_broadcast([128, 8, 128]), NNb[0][:], ALU.subtract),
                               r=["cst", "NNb0"], w=["TTb0"])
                        cur = 0
                        for step in range(1, 7):
                            nxt = 1 - cur
                            sqb = []
                            for g in range(2):
                                hs = range(g * 4, g * 4 + 4)
                                bkM, kbM = nb()
                                for hh, h in enumerate(hs):
                                    S.pe(MM(bkM[:, hh * 128:(hh + 1) * 128], NNb[cur][:, h, :], MMb[cur][:, h, :], True, True),
                                         r=["NNb%d" % cur, "MMb%d" % cur], w=[kbM])
                                bkN = kbN = None
                                if step < 6:
                                    bkN, kbN = nb()
                                    for hh, h in enumerate(hs):
                                        S.pe(MM(bkN[:, hh * 128:(hh + 1) * 128], MMb[cur][:, h, :], NNb[cur][:, h, :], True, True),
                                             r=["NNb%d" % cur, "MMb%d" % cur], w=[kbN])
                                sqb.append((bkM, kbM, bkN, kbN))
                            for g in range(2):
                                bkM, kbM, bkN, kbN = sqb[g]
                                evac(MMb[nxt][:, g * 4:(g + 1) * 4, :], bkM[:].rearrange("p (h t) -> p h t", h=4), [kbM], ["MMb%d" % nxt])
                                if bkN is not None:
                                    evac(NNb[nxt][:, g * 4:(g + 1) * 4, :], bkN[:].rearrange("p (h t) -> p h t", h=4), [kbN], ["NNb%d" % nxt])
                            for g in range(2):
                                hs = range(g * 4, g * 4 + 4)
                                bkT, kbT = nb()
                                for hh, h in enumerate(hs):
                                    S.pe(MM(bkT[:, hh * 128:(hh + 1) * 128], MMb[nxt][:, h, :], TTb[cur][:, h, :], True, False),
                                         r=["MMb%d" % nxt, "TTb%d" % cur], w=[kbT])
                                    S.pe(MM(bkT[:, hh * 128:(hh + 1) * 128], identf, TTb[cur][:, h, :], False, True),
                                         r=["cst", "TTb%d" % cur], w=[kbT])
                                evac(TTb[nxt][:, g * 4:(g + 1) * 4, :], bkT[:].rearrange("p (h t) -> p h t", h=4), [kbT], ["TTb%d" % nxt])
                            cur = nxt
                        S.act(ACTF(TTh[:], TTb[cur][:], AF.Copy), r=["TTb%d" % cur], w=["TTh"])
                        TTf = TTh
                        kT = "TTh"
                        for g in range(2):
                            bk, kb = nb()
                            for hh in range(4):
                                h = g * 4 + hh
                                S.pe(MM(bk[0:64, hh * 128:(hh + 1) * 128], Kd[:, h * 64:(h + 1) * 64], TTf[:, h, :], True, True), r=["Kd", kT], w=[kb])
                            evac(WTs[:, g * 4:(g + 1) * 4, :], bk[0:64, :].rearrange("p (h t) -> p h t", h=4), [kb], ["WTs"])
                        bk, kb = nb()
                        for h in range(8):
                            S.pe(MM(bk[:, h * 64:(h + 1) * 64], GE[:, h, 256:384], Vb[:, h * 64:(h + 1) * 64], True, True), r=["GE", "Vb"], w=[kb])
                        evac(Zb[:], bk[:], [kb], ["Zb"])
                        bk, kb = nb()
                        for h in range(8):
                            S.pe(MM(bk[:, h * 64:(h + 1) * 64], TTf[:, h, :], Zb[:, h * 64:(h + 1) * 64], True, True), r=[kT, "Zb"], w=[kb])
                        evac(U0[:], bk[:], [kb], ["U0"])
                        chk(6)
                        bk, kb = nb()
                        for h in range(8):
                            S.pe(MM(bk[:, h * 64:(h + 1) * 64], WTs[:, h, :], Pb[:, h, :], True, True), r=["WTs", "Pb"], w=[kb])
                        S.dve(STT(Ub[:], bk[:], -1.0, U0[:], ALU.mult, ALU.subtract), r=[kb, "U0"], w=["Ub"])
                        bkY, kbY = nb()
                        for h in range(8):
                            hs_ = slice(h * 64, (h + 1) * 64)
                            S.pe(MM(bkY[:, hs_], FT[:, h, 1, :], Pb[:, h, :], True, False), r=["FT", "Pb"], w=[kbY])
                            S.pe(MM(bkY[:, hs_], GE[:, h, 128:256], Ub[:, hs_], False, False), r=["GE", "Ub"], w=[kbY])
                            S.pe(MM(bkY[:, hs_], GE[:, h, 384:512], Vb[:, hs_], False, True), r=["GE", "Vb"], w=[kbY])
                        bkX, kbX = nb()
                        S.pe(MM(bkX[0:64, :], identf[0:64, 0:64], P32f, True, False), r=["cst", "P32"], w=[kbX])
                        for h in range(8):
                            hs_ = slice(h * 64, (h + 1) * 64)
                            S.pe(MM(bkX[0:64, hs_], Bi[:, hs_], Ub[:, hs_], False, False), r=["Bi", "Ub"], w=[kbX])
                            S.pe(MM(bkX[0:64, hs_], Ki[:, hs_], Vb[:, hs_], False, h == 7), r=["Ki", "Vb"], w=[kbX])
                        S.dve(TT(P32[:], bkX[0:64, :].rearrange("p (h v) -> p h v", h=8), GC[:].unsqueeze(2).to_broadcast([64, 8, 64]), ALU.mult),
                              r=[kbX, "GC"], w=["P32"])
                        S.act(ACTF(Pb[:], P32[:], AF.Copy), r=["P32"], w=["Pb"])
                        chk(7)
                        S.act(ACTF(y32[:], bkY[:], AF.Copy), r=[kbY], w=["y32"])
                        if dbg:
                            S.dma(dbg_d["dbg_y"][t * 128:(t + 1) * 128, :], y32[:], r=["y32"], w=["dbgy"])
                        chk(10)
                        Y3 = y32[:].rearrange("p (h d) -> p h d", h=8)
                        S.dve(RED(s8[3][:], Y3, ALU.add), r=["y32"], w=["s8_3"])
                        S.dve(TS(s8[3][:], s8[3][:], 1.0 / 64, None, ALU.mult), r=["s8_3"], w=["s8_3"])
                        S.dve(TT(cen[:].rearrange("p (h d) -> p h d", h=8), Y3, s8[3][:].unsqueeze(2).to_broadcast([128, 8, 64]), ALU.subtract),
                              r=["y32", "s8_3"], w=["cen"])
                        S.act(ACTF(t2[:], cen[:], AF.Square), r=["cen"], w=["t2"])
                        S.dve(RED(s8[4][:], t2[:].rearrange("p (h d) -> p h d", h=8), ALU.add), r=["t2"], w=["s8_4"])
                        S.dve(TS(s8[4][:], s8[4][:], 1.0 / 64, 64e-5, ALU.mult, ALU.add), r=["s8_4"], w=["s8_4"])
                        S.pool(TT(s8[5][:], s8[4][:], m05[:, 0:8], ALU.pow), r=["s8_4", "m05"], w=["s8_5"])
                        S.dve(TT(cen[:].rearrange("p (h d) -> p h d", h=8), cen[:].rearrange("p (h d) -> p h d", h=8),
                                 s8[5][:].unsqueeze(2).to_broadcast([128, 8, 64]), ALU.mult), r=["cen", "s8_5"], w=["cen"])
                        S.pool(TT(cen[:], cen[:], pbc[:, 3, :], ALU.mult), r=["cen", "pbc"], w=["cen"])
                        S.pool(TT(cen[:], cen[:], pbc[:, 4, :], ALU.add), r=["cen", "pbc"], w=["cen"])
                        S.dve(TT(t2[:].rearrange("p (h d) -> p h d", h=8), v32[:].rearrange("p (h d) -> p h d", h=8),
                                 s8[2][:].unsqueeze(2).to_broadcast([128, 8, 64]), ALU.mult), r=["v32", "s8_2"], w=["t2"])
                        S.pool(TT(cen[:], cen[:], t2[:], ALU.add), r=["cen", "t2"], w=["cen"])
                        S.dve(TT(ob[:], cen[:], g32[:], ALU.mult), r=["cen", "g32"], w=["ob"])
                        chk(11)
                        bk, kb = nb()
                        bkb = bfv(bk)
                        for c in range(4):
                            S.pe(TR(bkb[:, c * 128:(c + 1) * 128], ob[:, c * 128:(c + 1) * 128], identb[:]), r=["ob", "identb"], w=[kb])
                        evac(rwT[:], bkb[:, 0:512].rearrange("p (c t) -> p c t", c=4), [kb], ["rwT"])
                        S.dma(rwkvT_d[:, t * 128:(t + 1) * 128].rearrange("(c p) t -> p c t", p=128), rwT[:], r=["rwT"], w=["rwkvT_d"])
                        chk(12)
                S.flush()

        def phase_B(l):
            with contextlib.ExitStack() as st:
                def sb(name, shape, dt):
                    return st.enter_context(SBT(name, shape, dt))
                c31b = sb("c31b", [128, 8], F32)
                nc31 = sb("nc31", [128, 8], F32)
                Rn = sb("Rn", [128, 8, 256], BF16)
                aog = sb("aog", [64, 8], F32)
                statL = sb("statL", [65, 64], F32)
                S.dma(c31b[:], c31_d.partition_broadcast(128), w=["c31b"])
                aog8 = sb("aog8", [8, 64], F32)
                S.dma(aog8[:], W["attn_out_g"][l].rearrange("(h d) -> h d", d=64), w=["aog8"])
                bk, kb = nb()
                S.pe(MM(bk[0:64, 0:8], aog8[:], identf[0:8, 0:8], True, True), r=["aog8", "cst"], w=[kb])
                S.dve(CP(aog[:], bk[0:64, 0:8]), r=[kb], w=["aog"])
                S.dve(TS(nc31[:], c31b[:], -1.0, None, ALU.mult), r=["c31b"], w=["nc31"])
                with contextlib.ExitStack() as st2:
                    bnr = st2.enter_context(SBT("bnr", [128, 2048], F32))
                    S.dma(bnr[:], bnear_d, w=["bnr"])
                    for h in range(8):
                        S.act(ACTF(Rn[:, h, :], bnr[:, h * 256:(h + 1) * 256], AF.Exp, bias=nc31[:, h:h + 1]), r=["bnr", "nc31"], w=["Rn"])
                    S.flush()
                kaT = sb("kaT", [128, 4, T], BF16)
                kiT2 = sb("kiT2", [128, T], BF16)
                Va = sb("Va", [128, NT, 520], BF16)
                qaTb = [sb("qaTb%d" % i, [128, 4, 512], BF16) for i in range(2)]
                qiTb = [sb("qiTb%d" % i, [128, 4, 512], BF16) for i in range(2)]
                wib = [sb("wib%d" % i, [128, 4, 8], F32) for i in range(2)]
                wabs = sb("wabs", [128, 4, 8], F32)
                wsgn = sb("wsgn", [128, 4, 8], F32)
                scb = [sb("sc%d" % i, [128, T], F32) for i in range(2)]
                rl = [sb("rl%d" % i, [128, 512], F32) for i in range(3)]
                msk = sb("msk", [128, T], BF16)
                maskT = sb("maskT", [128, NT, 512], BF16)
                NEP = 4
                Eb = [sb("Eb%d" % i, [128, 512], BF16) for i in range(NEP)]
                Pt = [sb("Pt%d" % i, [128, 512], BF16) for i in range(NEP)]
                attTb = sb("attTb", [128, 4, 512], BF16)
                Osb = sb("Osb", [65, 512], F32)
                SQ = sb("SQ", [65, 512], F32)
                rs = sb("rs", [64, 512], F32)
                bis = [sb("bis%d" % i, [128, 1], F32) for i in range(6)]
                wtab = sb("wtab", [128, NBIS + 2], F32)
                ctab = sb("ctab", [128, NBIS + 2], F32)
                S.dma(kaT[:], featT_d[512:1024, :].rearrange("(c p) t -> p c t", p=128), r=["featT_d"], w=["kaT"])
                S.dma(kiT2[0:64, :], featT_d[1536:1600, :], r=["featT_d"], w=["kiT2"])
                S.dma(kiT2[64:128, :], featT_d[1536:1600, :], r=["featT_d"], w=["kiT2"])
                vsrc = vaug_d.rearrange("(n p) c -> p n c", p=128)
                for n0 in range(0, NT, 8):
                    S.dma(Va[:, n0:n0 + 8, :], vsrc[:, n0:n0 + 8, :], r=["vaug_d"], w=["Va"])
                S.dve(MEMSET(statL[0:64, :], 1.0 / 64), w=["statL"])
                S.dve(MEMSET(statL[64:65, :], 1e-6), w=["statL"])
                for k in range(NBIS + 2):
                    S.pool(MEMSET(ctab[:, k:k + 1], 2.0 ** (-k)), w=["ctab"])
                cw = 0.125 * (8 ** -0.5)

                def loadq(b):
                    i = b % 2
                    S.dma(qaTb[i][:], featT_d[0:512, b * 512:(b + 1) * 512].rearrange("(c p) t -> p c t", p=128), r=["featT_d"], w=["qaTb%d" % i])
                    S.dma(qiTb[i][:], featT_d[1024:1536, b * 512:(b + 1) * 512].rearrange("(c p) t -> p c t", p=128), r=["featT_d"], w=["qiTb%d" % i])
                    S.dma(wib[i][:], wi_d[b * 512:(b + 1) * 512, :].rearrange("(j p) h -> p j h", p=128), r=["wi_d"], w=["wib%d" % i])
                loadq(0)
                eidx = [0]
                for b in range(NB):
                    if b + 1 < NB:
                        loadq(b + 1)
                    i2 = b % 2
                    qa, qi, wi_ = qaTb[i2], qiTb[i2], wib[i2]
                    kqa, kqi, kwi = "qaTb%d" % i2, "qiTb%d" % i2, "wib%d" % i2
                    S.dve(STT(wabs[:], wi_[:], -1.0, wi_[:], ALU.mult, ALU.max), r=[kwi], w=["wabs"])
                    S.dve(TS(wabs[:], wabs[:], cw, None, ALU.mult), r=["wabs"], w=["wabs"])
                    S.act(ACTF(wsgn[:], wi_[:], AF.Sign), r=[kwi], w=["wsgn"])
                    nk = 4 * b + 4
                    def indexer(jq):
                        j = 4 * b + jq
                        Lk = (j + 1) * 128
                        qsl = slice(jq * 128, (jq + 1) * 128)
                        sc = scb[jq % 2]
                        ksc = "sc%d" % (jq % 2)
                        nch = (Lk + 511) // 512
                        for kc in range(nch):
                            ncol = min(512, Lk - kc * 512)
                            csl = slice(kc * 512, kc * 512 + ncol)
                            for ih in range(8):
                                pr, hf = ih // 2, ih % 2
                                ps_ = slice(hf * 64, (hf + 1) * 64)
                                bk, kb = nb()
                                S.pe(MM(bk[:, 0:ncol], qi[ps_, pr, qsl], kiT2[ps_, csl], True, True), r=[kqi, "kiT2"], w=[kb])
                                rb = rl[eidx[0] % 3]
                                krb = "rl%d" % (eidx[0] % 3)
                                eidx[0] += 1
                                S.act(ACTF(rb[:, 0:ncol], bk[:, 0:ncol], AF.Relu, scale=wabs[:, jq, ih:ih + 1]), r=[kb, "wabs"], w=[krb])
                                if ih == 0:
                                    S.dve(TS(sc[:, csl], rb[:, 0:ncol], wsgn[:, jq, 0:1], None, ALU.mult), r=[krb, "wsgn"], w=[ksc])
                                else:
                                    S.dve(STT(sc[:, csl], rb[:, 0:ncol], wsgn[:, jq, ih:ih + 1], sc[:, csl], ALU.mult, ALU.add), r=[krb, "wsgn", ksc], w=[ksc])
                                yield

                    gens = [indexer(jq) for jq in range(4)]
                    for _ in gens[0]:
                        pass
                    for jq in range(4):
                        j = 4 * b + jq
                        Lk = (j + 1) * 128
                        qsl = slice(jq * 128, (jq + 1) * 128)
                        sc = scb[jq % 2]
                        ksc = "sc%d" % (jq % 2)
                        nxt = gens[jq + 1] if jq < 3 else None
                        per_round = (((Lk + 128 + 511) // 512) * 8 + NBIS - 1) // NBIS if nxt is not None else 0
                        A_, mid, cnt, inc = bis[0], bis[1], bis[2], bis[3]
                        S.dve(RED(A_[:], sc[:, 0:Lk], ALU.max, absval=True), r=[ksc], w=["bis0"])
                        S.dve(TS(A_[:], A_[:], 1.001, 1e-6, ALU.mult, ALU.add), r=["bis0"], w=["bis0"])
                        S.dve(TT(sc[:, j * 128:(j + 1) * 128], sc[:, j * 128:(j + 1) * 128], caus, ALU.add), r=[ksc, "cst"], w=[ksc])
                        if dbg:
                            S.dma(dbg_d["dbg_score"][j * 128:(j + 1) * 128, 0:Lk], sc[:, 0:Lk], r=[ksc], w=["dbgs"])
                        S.dve(TS(wtab[:], ctab[:], A_[:, 0:1], None, ALU.mult), r=["ctab", "bis0"], w=["wtab"])
                        S.dve(TS(mid[:], A_[:], 0.0, None, ALU.mult), r=["bis0"], w=["bis1"])
                        for k in range(1, NBIS + 1):
                            if k % 2 == 1:
                                S.dve(TS(msk[:, 0:Lk], sc[:, 0:Lk], mid[:, 0:1], 0.0, ALU.is_ge, ALU.add, accum=cnt[:]), r=[ksc, "bis1"], w=["bis2", "msk"])
                                S.dve(STT(inc[:], cnt[:], NSEL - 0.5, wtab[:, k - 1:k], ALU.is_ge, ALU.mult), r=["bis2", "wtab"], w=["bis3"])
                            else:
                                S.dve(TS(bis[4][:], mid[:], -1.0, None, ALU.mult), r=["bis1"], w=["bis4"])
                                S.act(ACTF(msk[:, 0:Lk], sc[:, 0:Lk], AF.Sign, bias=bis[4][:, 0:1], accum=cnt[:]), r=[ksc, "bis4"], w=["bis2", "msk"])
                                S.dve(STT(inc[:], cnt[:], 2.0 * NSEL - 1.0 - Lk, wtab[:, k - 1:k], ALU.is_ge, ALU.mult), r=["bis2", "wtab"], w=["bis3"])
                            S.dve(STT(mid[:], inc[:], wtab[:, k:k + 1], mid[:], ALU.subtract, ALU.add), r=["bis3", "wtab", "bis1"], w=["bis1"])
                            if nxt is not None:
                                for _ in range(per_round):
                                    if next(nxt, "done") == "done":
                                        break
                        S.dve(TT(bis[5][:], mid[:], wtab[:, NBIS:NBIS + 1], ALU.subtract), r=["bis1", "wtab"], w=["bis5"])
                        if dbg:
                            S.dma(dbg_d["dbg_thr"][j * 128:(j + 1) * 128, :], bis[5][:], r=["bis5"], w=["dbgt"])
                        S.dve(TS(msk[:, 0:Lk], sc[:, 0:Lk], bis[5][:, 0:1], None, ALU.is_ge), r=[ksc, "bis5"], w=["msk"])
                        for i0 in range(0, j + 1, 8):
                            n8 = min(8, j + 1 - i0)
                            bk, kb = nb()
                            bkb = bfv(bk)
                            for ii in range(n8):
                                S.pe(TR(bkb[:, ii * 128:(ii + 1) * 128], msk[:, (i0 + ii) * 128:(i0 + ii + 1) * 128], identb[:]), r=["msk", "identb"], w=[kb])
                            S.act(ACTF(maskT[:, i0:i0 + n8, qsl], bkb[:, 0:n8 * 128].rearrange("p (i t) -> p i t", i=n8), AF.Copy), r=[kb], w=["maskT"])
                        if nxt is not None:
                            for _ in nxt:
                                pass
                    for hp in range(4):
                        hpair = (2 * hp, 2 * hp + 1)
                        for i in range(nk):
                            m = i - 4 * b
                            c0 = max(0, m) * 128
                            ncol = 512 - c0
                            st_ = []
                            for h in hpair:
                                pr, hf = h // 2, h % 2
                                ps_ = slice(hf * 64, (hf + 1) * 64)
                                bk, kb = nb()
                                S.pe(MM(bk[:, 0:ncol], kaT[ps_, pr, i * 128:(i + 1) * 128], qa[ps_, pr, c0:512], True, True), r=["kaT", kqa], w=[kb])
                                ei = eidx[0] % NEP
                                eidx[0] += 1
                                st_.append((h, bk, kb, ei))
                            for h, bk, kb, ei in st_:
                                S.act(ACTF(Eb[ei][:, 0:ncol], bk[:, 0:ncol], AF.Exp, bias=c31b[:, h:h + 1], scale=0.125), r=[kb, "c31b"], w=["Eb%d" % ei])
                            for h, bk, kb, ei in st_:
                                E, kE, P_, kP = Eb[ei], "Eb%d" % ei, Pt[ei], "Pt%d" % ei
                                eng = S.dve if (h % 2) else S.pool
                                eng(TT(P_[:, 0:ncol], E[:, 0:ncol], maskT[:, i, c0:512], ALU.mult), r=[kE, "maskT"], w=[kP])
                                if m >= 0:
                                    nn = min(256, ncol)
                                    eng(TT(P_[:, 0:nn], P_[:, 0:nn], Rn[:, h, 0:nn], ALU.mult), r=[kP, "Rn"], w=[kP])
                                elif m == -1:
                                    eng(TT(P_[:, 0:128], P_[:, 0:128], Rn[:, h, 128:256], ALU.mult), r=[kP, "Rn"], w=[kP])
                            for h, bk, kb, ei in st_:
                                bkO, kbO = banks[6 + h % 2], "bank%d" % (6 + h % 2)
                                S.pe(MM(bkO[0:65, c0:512], Va[:, i, h * 65:(h + 1) * 65], Pt[ei][:, 0:ncol], i == 0, i == nk - 1), r=["Va", "Pt%d" % ei], w=[kbO])
                        for h in hpair:
                            pr, hf = h // 2, h % 2
                            ps_ = slice(hf * 64, (hf + 1) * 64)
                            bkO, kbO = banks[6 + h % 2], "bank%d" % (6 + h % 2)
                            S.act(ACTF(Osb[:], bkO[0:65, :], AF.Copy), r=[kbO], w=["Osb"])
                            S.act(ACTF(SQ[:], bkO[0:65, :], AF.Square), r=[kbO], w=["SQ"])
                            bk, kb = nb()
                            S.pe(MM(bk[0:64, :], statL[:], SQ[:], True, True), r=["statL", "SQ"], w=[kb])
                            S.act(ACTF(rs[:], bk[0:64, :], AF.Ln), r=[kb], w=["rs"])
                            S.act(ACTF(rs[:], rs[:], AF.Exp, scale=-0.5), r=["rs"], w=["rs"])
                            S.dve(STT(attTb[ps_, pr, :], Osb[0:64, :], aog[:, h:h + 1], rs[:], ALU.mult, ALU.mult), r=["Osb", "aog", "rs"], w=["attTb"])
                    S.dma(attT_d[:, b * 512:(b + 1) * 512].rearrange("(c p) t -> p c t", p=128), attTb[:], r=["attTb"], w=["attT_d"])
                S.flush()

        def phase_B2(l, xsrc):
            with contextlib.ExitStack() as st:
                def sb(name, shape, dt):
                    return st.enter_context(SBT(name, shape, dt))
                wo = sb("wo", [128, 8, 1024], BF16)
                gt1 = sb("gt1", [128, 1024], F32)
                A2t = sb("A2t", [128, 1024], F32)
                sh2t = sb("sh2t", [128, 1024], F32)
                xb = [sb("xb%d" % i, [128, 1024], F32) for i in range(2)]
                mixT = [sb("mixT%d" % i, [128, 8, 128], BF16) for i in range(2)]
                x1 = [sb("x1_%d" % i, [128, 1024], F32) for i in range(2)]
                junk = sb("junk", [128, 1024], F32)
                tmp = sb("tmpn", [128, 1024], F32)
                tmp2 = sb("tmpm", [128, 1024], F32)
                hb = [sb("hb%d" % i, [128, 1024], BF16) for i in range(2)]
                h2s = [sb("h2s%d" % i, [128, 8, 128], BF16) for i in range(2)]
                ssq = [sb("ssq%d" % i, [128, 1], F32) for i in range(2)]
                ms = [sb("ms%d" % i, [128, 1], F32) for i in range(2)]
                rstd = [sb("rstd%d" % i, [128, 1], F32) for i in range(2)]
                wov = W["w_out"][l].rearrange("(kc p) n -> p kc n", p=128)
                S.dma(wo[:], wov, w=["wo"], eng="pool")
                S.dma(gt1[:], mod_d[:, 2048:3072], r=["mod_d"], w=["modp"])
                S.dma(A2t[:], mod_d[:, 4096:5120], r=["mod_d"], w=["modp"])
                S.dma(sh2t[:], mod_d[:, 3072:4096], r=["mod_d"], w=["modp"])

                def load(t):
                    i = t % 2
                    S.dma(xb[i][:], xsrc[t * 128:(t + 1) * 128, :], r=["xs_d"], w=["xb%d" % i])
                    S.dma(mixT[i][:, 0:4, :], rwkvT_d[:, t * 128:(t + 1) * 128].rearrange("(c p) t -> p c t", p=128), r=["rwkvT_d"], w=["mixT%d" % i])
                    S.dma(mixT[i][:, 4:8, :], attT_d[:, t * 128:(t + 1) * 128].rearrange("(c p) t -> p c t", p=128), r=["attT_d"], w=["mixT%d" % i])
                load(0)
                for t in range(NT):
                    if t + 1 < NT:
                        load(t + 1)
                    i = t % 2
                    for hf in range(2):
                        bk, kb = nb()
                        for kc in range(8):
                            S.pe(MM(bk[:], mixT[i][:, kc, :], wo[:, kc, hf * 512:(hf + 1) * 512], kc == 0, kc == 7), r=["mixT%d" % i, "wo"], w=[kb])
                        csl = slice(hf * 512, (hf + 1) * 512)
                        S.dve(TT(tmp2[:, csl], bk[:], gt1[:, csl], ALU.mult), r=[kb, "modp"], w=["tmpm"])
                    S.pool(TT(x1[i][:], tmp2[:], xb[i][:], ALU.add), r=["tmpm", "xb%d" % i], w=["x1_%d" % i])
                    S.dma(xs_d[t * 128:(t + 1) * 128, :], x1[i][:], r=["x1_%d" % i], w=["xs_d2"])
                    norm_mod_T(x1[i][:], "x1_%d" % i, A2t[:], sh2t[:], hb[i], "hb%d" % i, junk, ssq[i], ms[i], rstd[i], tmp, i,
                               h2s[i][:], "h2s%d" % i)
                    S.dma(h2T_d[:, t * 128:(t + 1) * 128].rearrange("(kc p) t -> p kc t", p=128), h2s[i][:], r=["h2s%d" % i], w=["h2T_d"])
                S.flush()

        def phase_C(l, last):
            TB = 256
            NBC = T // TB
            with contextlib.ExitStack() as st:
                def sb(name, shape, dt):
                    return st.enter_context(SBT(name, shape, dt))
                w1 = sb("w1", [128, 8, 4096], BF16)
                w2 = sb("w2", [128, 32, 1024], BF16)
                gt2 = sb("gt2", [128, 1024], F32)
                fg = sb("fg", [128, 1024], F32)
                h2b = [sb("h2b%d" % i, [128, 8, TB], BF16) for i in range(2)]
                uT = sb("uT", [128, 32, TB], BF16)
                sq = [sb("sq%d" % i, [128, TB], F32) for i in range(2)]
                xb = [sb("xb%d" % i, [128, 1024], F32) for i in range(2)]
                x2 = [sb("x2_%d" % i, [128, 1024], F32) for i in range(2)]
                tmp2 = sb("tmpm", [128, 1024], F32)
                junk = sb("junk", [128, 1024], F32)
                ssq = [sb("ssq%d" % i, [128, 1], F32) for i in range(2)]
                ms = [sb("ms%d" % i, [128, 1], F32) for i in range(2)]
                rstd = [sb("rstd%d" % i, [128, 1], F32) for i in range(2)]
                w1v = W["w_mlp1"][l].rearrange("(kc p) n -> p kc n", p=128)
                w2v = W["w_mlp2"][l].rearrange("(fc p) n -> p fc n", p=128)
                for q in range(4):
                    S.dma(w1[:, :, q * 1024:(q + 1) * 1024], w1v[:, :, q * 1024:(q + 1) * 1024], w=["w1"], eng="pool")
                for q in range(4):
                    S.dma(w2[:, q * 8:(q + 1) * 8, :], w2v[:, q * 8:(q + 1) * 8, :], w=["w2"], eng="pool")
                S.dma(gt2[:], mod_d[:, 5120:6144], r=["mod_d"], w=["modp"])
                if last:
                    S.dma(fg[:], W["final_g"].partition_broadcast(128), w=["fg"])

                def load(bb):
                    i = bb % 2
                    S.dma(h2b[i][:], h2T_d[:, bb * TB:(bb + 1) * TB].rearrange("(kc p) t -> p kc t", p=128), r=["h2T_d"], w=["h2b%d" % i])
                load(0)
                xi = [0]
                for bb in range(NBC):
                    if bb + 1 < NBC:
                        load(bb + 1)
                    i = bb % 2
                    for fc in range(32):
                        bk, kb = nb()
                        for kc in range(8):
                            S.pe(MM(bk[:, 0:TB], w1[:, kc, fc * 128:(fc + 1) * 128], h2b[i][:, kc, :], kc == 0, kc == 7), r=["w1", "h2b%d" % i], w=[kb])
                        s_ = sq[fc % 2]
                        ks_ = "sq%d" % (fc % 2)
                        S.act(ACTF(s_[:], bk[:, 0:TB], AF.Square), r=[kb], w=[ks_])
                        S.dve(STT(uT[:, fc, :], bk[:, 0:TB], 0.0, s_[:], ALU.is_gt, ALU.mult), r=[kb, ks_], w=["uT"])
                    for jj in range(TB // 128):
                        t = bb * (TB // 128) + jj
                        xi_ = xi[0] % 2
                        xi[0] += 1
                        S.dma(xb[xi_][:], xs_d[t * 128:(t + 1) * 128, :], r=["xs_d"], w=["xb%d" % xi_])
                        for hf in range(2):
                            bk, kb = nb()
                            for fc in range(32):
                                S.pe(MM(bk[:], uT[:, fc, jj * 128:(jj + 1) * 128], w2[:, fc, hf * 512:(hf + 1) * 512], fc == 0, fc == 31), r=["uT", "w2"], w=[kb])
                            csl = slice(hf * 512, (hf + 1) * 512)
                            S.dve(TT(tmp2[:, csl], bk[:], gt2[:, csl], ALU.mult), r=[kb, "modp"], w=["tmpm"])
                        S.pool(TT(x2[xi_][:], tmp2[:], xb[xi_][:], ALU.add), r=["tmpm", "xb%d" % xi_], w=["x2_%d" % xi_])
                        if not last:
                            S.dma(xs_d[t * 128:(t + 1) * 128, :], x2[xi_][:], r=["x2_%d" % xi_], w=["xs_d2"])
                        else:
                            S.act(ACTF(junk[:], x2[xi_][:], AF.Square, accum=ssq[xi_][:]), r=["x2_%d" % xi_], w=["ssq%d" % xi_])
                            S.dve(TS(ms[xi_][:], ssq[xi_][:], 1.0 / 1024, 1e-6, ALU.mult, ALU.add), r=["ssq%d" % xi_], w=["ms%d" % xi_])
                            S.pool(TT(rstd[xi_][:], ms[xi_][:], m05[:, 0:1], ALU.pow), r=["ms%d" % xi_, "m05"], w=["rstd%d" % xi_])
                            S.dve(STT(x2[xi_][:], x2[xi_][:], rstd[xi_][:, 0:1], fg[:], ALU.mult, ALU.mult), r=["x2_%d" % xi_, "rstd%d" % xi_, "fg"], w=["x2_%d" % xi_])
                            S.dma(out_d[t * 128:(t + 1) * 128, :], x2[xi_][:], r=["x2_%d" % xi_], w=["out_d"])
                S.flush()

        order = []
        for l in range(L):
            order += [("M", l), ("A1", l), ("A2", l), ("B", l), ("B2", l), ("C", l)]
        for ph, l in order:
            xsrc = x_d if l == 0 else xs_d
            if ph == "M":
                phase_M(l)
            elif ph == "A1":
                phase_A1(l, xsrc)
            elif ph == "A2":
                try:
                    phase_A2(l)
                except _Stop:
                    S.flush()
                    break
            elif ph == "B":
                phase_B(l)
            elif ph == "B2":
                phase_B2(l, xsrc)
            elif ph == "C":
                phase_C(l, l == L - 1)
            if stop_after == (ph, l):
                break
        S.flush(final=True)
        nops = S.nops
    return nc, nops


def _t5_bucket(n):
    n = np.maximum(n, 0)
    nf = np.maximum(n, 1).astype(np.float32)
    large = 16 + (np.log(nf / np.float32(16)) / np.float32(math.log(128 / 16)) * np.float32(16)).astype(np.int32)
    large = np.minimum(large, 31)
    return np.where(n < 16, n, large)


def _consts():
    s = np.arange(128)[:, None]
    t = np.arange(128)[None, :]
    c = np.zeros((128, 640), np.float32)
    c[:, 0:128] = (s == t)
    c[:, 128:256] = (s <= t)
    c[:, 256:384] = (s < t)
    c[:, 384:512] = (s > t)
    c[:, 512:640] = np.where(t <= s, 0.0, -1e30)
    return c


def host_inputs(inputs, T, L):
    B = inputs["x"].shape[0]
    f = lambda a: np.ascontiguousarray(np.asarray(a, dtype=np.float32))
    rel_bias = f(inputs["rel_bias"])
    tk = np.arange(128)[:, None, None, None]
    d = np.arange(2)[None, None, :, None]
    tq = np.arange(128)[None, None, None, :]
    hh = np.arange(8)[None, :, None, None]
    bidx = _t5_bucket(128 * d + tq - tk) + 0 * hh
    bnear = rel_bias[bidx, hh + 0 * bidx].reshape(128, 2048)
    shared = {"consts": _consts(), "bnear": f(bnear), "c31": f(rel_bias[31:32, :])}
    for k in WSHAPES:
        shared[k] = f(inputs[k])[:L]
    for k in VSHAPES:
        shared[k] = f(inputs[k])[:max(L - 1, 1)]
    shared["final_g"] = f(inputs["final_g"]).reshape(1, 1024)
    maps = []
    for bi in range(B):
        m = dict(shared)
        m["x"] = f(inputs["x"][bi, :T])
        m["c8"] = f(np.asarray(inputs["c"][bi]).reshape(8, 128).T)
        maps.append(m)
    return maps


_CACHE = {}


def kernel(**inputs):
    T = inputs["x"].shape[1]
    L = inputs["w_ada"].shape[0]
    B = inputs["x"].shape[0]
    key = (T, L)
    if key not in _CACHE:
        _CACHE[key] = build(T, L)[0]
    nc = _CACHE[key]
    maps = host_inputs(inputs, T, L)
    res = run_bass_kernel_spmd(nc, maps, core_ids=list(range(B)))
    return np.stack([np.asarray(r["out"], dtype=np.float32) for r in res.results], axis=0)
```

```python
import contextlib
import os
import math
import numpy as np
import ml_dtypes
import concourse.bass as bass
import concourse.mybir as mybir
from concourse.bass_utils import run_bass_kernel_spmd

F32 = mybir.dt.float32
BF16 = mybir.dt.bfloat16
AF = mybir.ActivationFunctionType
ALU = mybir.AluOpType
AX = mybir.AxisListType

ENG = ("pe", "dve", "act", "pool", "sp")
NDMA = 12
NBIS = 16


class _Stop(Exception):
    pass


_DEAD = [False]


def chk(n):
    if int(os.environ.get("A2STOP", "0")) == n:
        _DEAD[0] = True


class Op:
    __slots__ = ("eng", "fn", "deps", "signal", "sig_val", "dma", "dma_slot", "dma_val", "sem")

    def __init__(self, eng, fn, dma):
        self.eng = eng
        self.fn = fn
        self.deps = []
        self.signal = False
        self.sig_val = None
        self.dma = dma
        self.dma_slot = None
        self.dma_val = None
        self.sem = None


class Sched:
    def __init__(self, nc, stack):
        self.nc = nc
        self.stack = stack
        self.nsw = 0
        self.sw_dmas = []
        self.esem = {e: stack.enter_context(nc.semaphore("s_" + e)) for e in ENG if e != "sp"}
        self.dsem = [stack.enter_context(nc.semaphore("d_%d" % i)) for i in range(NDMA)]
        self.ops = {e: [] for e in ENG}
        self.last_w = {}
        self.readers = {}
        self.dma_count = 0
        self.dma_last = [None] * NDMA
        self.sigc = {e: 0 for e in ENG}
        self.waited = {e: {} for e in ENG}
        self.bar = {e: [] for e in ENG}
        self.nops = 0

    def _dep(self, op, prod):
        if prod is None or prod is op:
            return
        if (not prod.dma) and (not op.dma) and prod.eng == op.eng == "pe":
            return
        if not prod.dma:
            prod.signal = True
        op.deps.append(prod)

    def add(self, eng, fn, r=(), w=(), dma=False):
        op = Op(eng, fn, dma)
        if _DEAD[0]:
            return op
        if self.bar[eng]:
            for p in self.bar[eng]:
                op.deps.append(p)
            self.bar[eng] = []
        for k in r:
            self._dep(op, self.last_w.get(k))
        for k in w:
            self._dep(op, self.last_w.get(k))
            for rd in self.readers.get(k, ()):
                self._dep(op, rd)
        for k in r:
            self.readers.setdefault(k, []).append(op)
        for k in w:
            self.last_w[k] = op
            self.readers[k] = []
        if dma and eng == "pool":
            op.sem = self.stack.enter_context(self.nc.semaphore("w_%d" % self.nsw))
            op.dma_slot = "w%d" % self.nsw
            self.nsw += 1
            op.dma_val = 16
            self.sw_dmas.append(op)
        elif dma:
            slot = self.dma_count % NDMA
            self.dma_count += 1
            prev = self.dma_last[slot]
            op.dma_slot = slot
            op.sem = self.dsem[slot]
            op.dma_val = (prev.dma_val if prev else 0) + 16
            if prev is not None:
                op.deps.append(prev)
            self.dma_last[slot] = op
        self.ops[eng].append(op)
        self.nops += 1
        return op

    def pe(self, fn, r=(), w=()):
        return self.add("pe", fn, r, w)

    def dve(self, fn, r=(), w=()):
        return self.add("dve", fn, r, w)

    def act(self, fn, r=(), w=()):
        return self.add("act", fn, r, w)

    def pool(self, fn, r=(), w=()):
        return self.add("pool", fn, r, w)

    def dma(self, out, in_, r=(), w=(), eng="sp"):
        return self.add(eng, lambda e: e.dma_start(out=out, in_=in_), r, w, dma=True)

    def flush(self, final=False):
        nc = self.nc
        lasts = []
        for e in ENG:
            nd = [op for op in self.ops[e] if not op.dma]
            if nd:
                nd[-1].signal = True
                lasts.append(nd[-1])
        for e in ENG:
            for op in self.ops[e]:
                if op.signal and not op.dma:
                    self.sigc[e] += 1
                    op.sig_val = self.sigc[e]
        dlast = [p for p in self.dma_last if p is not None] + self.sw_dmas
        self.sw_dmas = []
        with nc.Block() as block:
            engobj = {"pe": block.tensor, "dve": block.vector, "act": block.scalar,
                      "pool": block.gpsimd, "sp": block.sync}
            for ename in ENG:
                ops = self.ops[ename]
                if not ops and not (final and ename == "sp"):
                    continue

                def body(e, ops=ops, ename=ename):
                    waited = self.waited[ename]
                    semof = {}
                    for op in ops:
                        need = {}
                        for p in op.deps:
                            if p.dma:
                                key = ("d", p.dma_slot)
                                semof[key] = p.sem
                                val = p.dma_val
                            else:
                                key = ("e", p.eng)
                                val = p.sig_val
                            if need.get(key, 0) < val:
                                need[key] = val
                        for key, val in need.items():
                            if waited.get(key, 0) >= val:
                                continue
                            waited[key] = val
                            sem = semof[key] if key[0] == "d" else self.esem[key[1]]
                            e.wait_ge(sem, val)
                        ins = op.fn(e)
                        if op.dma:
                            ins.then_inc(op.sem, 16)
                        elif op.signal:
                            ins.then_inc(self.esem[ename], 1)
                    if final and ename == "sp":
                        for p in dlast:
                            if waited.get(("d", p.dma_slot), 0) < p.dma_val:
                                e.wait_ge(p.sem, p.dma_val)
                        for p in lasts:
                            e.wait_ge(self.esem[p.eng], p.sig_val)
                engobj[ename](body)
        barrier = lasts + dlast
        self.ops = {e: [] for e in ENG}
        self.last_w = {}
        self.readers = {}
        self.bar = {e: list(barrier) for e in ENG}


def MM(out, lhsT, rhs, start=True, stop=True):
    return lambda e: e.matmul(out, lhsT=lhsT, rhs=rhs, start=start, stop=stop)


def TR(out, in_, ident):
    return lambda e: e.transpose(out, in_, ident)


def ACTF(out, in_, func, bias=0.0, scale=1.0, accum=None):
    if accum is None:
        return lambda e: e.activation(out=out, in_=in_, func=func, bias=bias, scale=scale)
    return lambda e: e.activation(out=out, in_=in_, func=func, bias=bias, scale=scale, accum_out=accum)


def TT(out, a, b, op):
    return lambda e: e.tensor_tensor(out=out, in0=a, in1=b, op=op)


def TS(out, a, s1, s2=None, op0=ALU.mult, op1=None, accum=None):
    if accum is not None:
        return lambda e: e.tensor_scalar(out=out, in0=a, scalar1=s1, scalar2=s2, op0=op0, op1=op1, accum_out=accum)
    if op1 is None:
        return lambda e: e.tensor_scalar(out=out, in0=a, scalar1=s1, scalar2=None, op0=op0)
    return lambda e: e.tensor_scalar(out=out, in0=a, scalar1=s1, scalar2=s2, op0=op0, op1=op1)


def STT(out, a, s, b, op0, op1):
    return lambda e: e.scalar_tensor_tensor(out=out, in0=a, scalar=s, in1=b, op0=op0, op1=op1)


def CP(out, in_):
    return lambda e: e.tensor_copy(out, in_)


def RED(out, in_, op, axis=AX.X, absval=False):
    if absval:
        return lambda e: e.tensor_reduce(out=out, in_=in_, axis=axis, op=op, apply_absolute_value=True)
    return lambda e: e.tensor_reduce(out=out, in_=in_, axis=axis, op=op)


def MEMSET(ap, v):
    return lambda e: e.memset(ap, v)


WSHAPES = {
    "w_ada": (1024, 6144), "b_ada": (6144,), "norm1_g": (1024,), "norm2_g": (1024,),
    "w_in": (1024, 3656), "mu_rkv": (3, 512), "mu_lora": (3, 1024), "decay_w0": (512,),
    "decay_a": (1024, 64), "decay_b": (64, 512), "iclr_a0": (512,), "iclr_a": (1024, 64),
    "iclr_b": (64, 512), "gate_a": (1024, 128), "gate_b": (128, 512), "k_k": (512,), "k_a": (512,),
    "r_k": (8, 64), "lnx_g": (512,), "lnx_b": (512,), "attn_out_g": (512,),
    "w_out": (1024, 1024), "w_mlp1": (1024, 4096), "w_mlp2": (4096, 1024),
}
VSHAPES = {"vres_mu": (1024,), "vres_v0": (512,), "vres_a": (1024, 32), "vres_b": (32, 512)}


def build(T, L, dbg=False, stop_after=None):
    _DEAD[0] = False
    NT = T // 128
    NB = T // 512
    NSEL = min(256, T // 4)
    nc = bass.Bass("TRN2", target_bir_lowering=False)
    W = {}

    def din(name, shape):
        W[name] = nc.dram_tensor(name, list(shape), F32, kind="ExternalInput").ap()
        return W[name]

    x_d = din("x", [T, 1024])
    c8_d = din("c8", [128, 8])
    consts_d = din("consts", [128, 640])
    bnear_d = din("bnear", [128, 2048])
    c31_d = din("c31", [1, 8])
    for k, s in WSHAPES.items():
        din(k, (L,) + s)
    for k, s in VSHAPES.items():
        din(k, (max(L - 1, 1),) + s)
    din("final_g", [1, 1024])
    out_d = nc.dram_tensor("out", [T, 1024], F32, kind="ExternalOutput").ap()

    def dscr(name, shape, dt):
        return nc.dram_tensor(name, list(shape), dt, kind=("ExternalOutput" if dbg else "Internal")).ap()

    xs_d = dscr("xs", [T, 1024], F32)
    mod_d = dscr("modd", [128, 6144], F32)
    featT_d = dscr("featT", [1600, T], BF16)
    vaug_d = dscr("vaug", [T, 520], BF16)
    wi_d = dscr("wid", [T, 8], F32)
    hT_d = dscr("hTd", [1024, T], BF16)
    vfirst_d = dscr("vfirst", [T, 512], F32)
    rwkvT_d = dscr("rwkvT", [512, T], BF16)
    attT_d = dscr("attT", [512, T], BF16)
    h2T_d = dscr("h2T", [1024, T], BF16)
    dbg_d = {}
    if dbg:
        for nm, shp in (("dbg_y", [T, 512]), ("dbg_score", [T, T]), ("dbg_thr", [T, 1]), ("dbg_v", [T, 512]),
                        ("dbg_r", [T, 512]), ("dbg_k", [T, 512]), ("dbg_a", [T, 512]), ("dbg_lw", [T, 512])):
            dbg_d[nm] = nc.dram_tensor(nm, shp, F32, kind="ExternalOutput").ap()

    uniq = [0]

    def SBT(name, shape, dt):
        uniq[0] += 1
        return nc.sbuf_tensor("%s_%d" % (name, uniq[0]), shape, dt)

    with contextlib.ExitStack() as gst:
        S = Sched(nc, gst)

        def gsb(name, shape, dt):
            return gst.enter_context(SBT(name, shape, dt))

        banks = [gst.enter_context(nc.psum_tensor("bank%d" % i, [128, 512], F32)) for i in range(8)]
        bank_i = [0]

        NROT = 6

        nrot = [8]

        def nb():
            i = bank_i[0] % nrot[0]
            bank_i[0] += 1
            return banks[i], "bank%d" % i

        def bfv(bank):
            return bank[:].bitcast(BF16)

        cst = gsb("cst", [128, 640], F32)
        identb = gsb("identb", [128, 128], BF16)
        mask4 = gsb("mask4", [128, 512], F32)
        lowm4 = gsb("lowm4", [128, 512], F32)
        onesf = gsb("onesf", [128, 128], F32)
        m05 = gsb("m05", [128, 512], F32)
        cbc = gsb("cbc", [128, 8, 128], F32)
        identf = cst[:, 0:128]
        tri_incl = cst[:, 128:256]
        tri_strict = cst[:, 256:384]
        low_strict = cst[:, 384:512]
        caus = cst[:, 512:640]

        with contextlib.ExitStack() as st:
            c8 = st.enter_context(SBT("c8s", [128, 8], F32))
            c8t = st.enter_context(SBT("c8t", [128, 8], F32))
            S.dma(cst[:], consts_d, w=["cst"])
            S.dma(c8[:], c8_d, w=["c8"])
            S.dve(CP(identb[:], identf), r=["cst"], w=["identb"])
            for i in range(4):
                S.dve(CP(mask4[:, i * 128:(i + 1) * 128], tri_strict if i % 2 == 0 else tri_incl), r=["cst"], w=["mask4"])
                S.pool(CP(lowm4[:, i * 128:(i + 1) * 128], low_strict), r=["cst"], w=["lowm4"])
            S.pool(MEMSET(onesf[:], 1.0), w=["onesf"])
            S.pool(MEMSET(m05[:], -0.5), w=["m05"])
            S.act(ACTF(c8t[:], c8[:], AF.Tanh, scale=0.5), r=["c8"], w=["c8t"])
            S.dve(TS(c8t[:], c8t[:], 0.5, 0.5, ALU.mult, ALU.add), r=["c8t"], w=["c8t"])
            S.dve(TT(c8t[:], c8t[:], c8[:], ALU.mult), r=["c8t", "c8"], w=["c8t"])
            S.dve(CP(cbc[:], c8t[:].unsqueeze(2).to_broadcast([128, 8, 128])), r=["c8t"], w=["cbc"])
            S.flush()

        def phase_M(l):
            with contextlib.ExitStack() as st:
                def sb(name, shape, dt):
                    return st.enter_context(SBT(name, shape, dt))
                wst = [sb("wst%d" % i, [128, 8, 512], F32) for i in range(2)]
                modt = sb("modt", [128, 6144], F32)
                gbc = sb("gbc", [128, 2048], F32)
                bada = sb("bada", [1, 6144], F32)
                S.dma(gbc[:, 0:1024], W["norm1_g"][l:l + 1, :].partition_broadcast(128), w=["gbc"])
                S.dma(gbc[:, 1024:2048], W["norm2_g"][l:l + 1, :].partition_broadcast(128), w=["gbc"])
                S.dma(bada[:], W["b_ada"][l:l + 1, :], w=["bada"])
                wa = W["w_ada"][l].rearrange("(kc p) n -> p kc n", p=128)
                for n in range(12):
                    buf = wst[n % 2]
                    key = "wst%d" % (n % 2)
                    S.dma(buf[:], wa[:, :, n * 512:(n + 1) * 512], w=[key])
                    bk, kb = nb()
                    for kc in range(8):
                        S.pe(MM(bk[:], cbc[:, kc, :], buf[:, kc, :], kc == 0, False), r=[key, "cbc"], w=[kb])
                    S.pe(MM(bk[:], onesf[0:1, :], bada[0:1, n * 512:(n + 1) * 512], False, True), r=["bada", "onesf"], w=[kb])
                    S.act(ACTF(modt[:, n * 512:(n + 1) * 512], bk[:], AF.Copy), r=[kb], w=["modt"])
                S.dve(STT(modt[:, 1024:2048], modt[:, 1024:2048], 1.0, gbc[:, 0:1024], ALU.add, ALU.mult), r=["modt", "gbc"], w=["modt"])
                S.dve(STT(modt[:, 4096:5120], modt[:, 4096:5120], 1.0, gbc[:, 1024:2048], ALU.add, ALU.mult), r=["modt", "gbc"], w=["modt"])
                S.dma(mod_d, modt[:], r=["modt"], w=["mod_d"])
                S.flush()

        def norm_mod_T(X, kx, At, sht, hb, khb, junk, ssq, ms, rstd, tmp, idx, dstT, kdst):
            S.act(ACTF(junk[:], X, AF.Square, accum=ssq[:]), r=[kx], w=["ssq%d" % idx])
            S.dve(TS(ms[:], ssq[:], 1.0 / 1024, 1e-6, ALU.mult, ALU.add), r=["ssq%d" % idx], w=["ms%d" % idx])
            S.pool(TT(rstd[:], ms[:], m05[:, 0:1], ALU.pow), r=["ms%d" % idx, "m05"], w=["rstd%d" % idx])
            S.dve(STT(tmp[:], X, rstd[:, 0:1], At, ALU.mult, ALU.mult), r=[kx, "rstd%d" % idx, "modp"], w=["tmpn"])
            S.pool(TT(hb[:], tmp[:], sht, ALU.add), r=["tmpn", "modp"], w=[khb])
            bk, kb = nb()
            bkb = bfv(bk)
            for kc in range(8):
                S.pe(TR(bkb[:, kc * 128:(kc + 1) * 128], hb[:, kc * 128:(kc + 1) * 128], identb[:]), r=[khb, "identb"], w=[kb])
            S.act(ACTF(dstT, bkb.rearrange("p (k t) -> p k t", k=8), AF.Copy), r=[kb], w=[kdst])

        def phase_A1(l, xsrc):
            with contextlib.ExitStack() as st:
                def sb(name, shape, dt):
                    return st.enter_context(SBT(name, shape, dt))
                WF = sb("WF", [128, 8, 1600], BF16)
                WV = sb("WV", [128, 8, 520], BF16)
                A1t = sb("A1t", [128, 1024], F32)
                sh1t = sb("sh1t", [128, 1024], F32)
                xb = [sb("xb%d" % i, [128, 1024], F32) for i in range(2)]
                junk = sb("junk", [128, 1024], F32)
                tmp = sb("tmpn", [128, 1024], F32)
                hb = [sb("hb%d" % i, [128, 1024], BF16) for i in range(2)]
                hT = [sb("hT%d" % i, [128, 8, 512], BF16) for i in range(2)]
                fst = [sb("fst%d" % i, [128, 512], BF16) for i in range(2)]
                vst = [sb("vst%d" % i, [128, 8, 65], BF16) for i in range(2)]
                wist = [sb("wist%d" % i, [128, 8], F32) for i in range(2)]
                ssq = [sb("ssq%d" % i, [128, 1], F32) for i in range(2)]
                ms = [sb("ms%d" % i, [128, 1], F32) for i in range(2)]
                rstd = [sb("rstd%d" % i, [128, 1], F32) for i in range(2)]
                wv = W["w_in"][l].rearrange("(kc p) n -> p kc n", p=128)
                S.dma(WF[:, :, 0:1024], wv[:, :, 1536:2560], w=["WF"], eng="pool")
                S.dma(WF[:, :, 1024:1600], wv[:, :, 3072:3648], w=["WF"], eng="pool")
                S.dma(WV[:, :, 0:512], wv[:, :, 2560:3072], w=["WV"], eng="pool")
                S.dma(WV[:, :, 512:520], wv[:, :, 3648:3656], w=["WV"], eng="pool")
                S.dma(A1t[:], mod_d[:, 1024:2048], r=["mod_d"], w=["modp"])
                S.dma(sh1t[:], mod_d[:, 0:1024], r=["mod_d"], w=["modp"])
                for i in range(2):
                    S.pool(MEMSET(vst[i][:, :, 64:65], 1.0), w=["vst%d" % i])

                def load(t):
                    S.dma(xb[t % 2][:], xsrc[t * 128:(t + 1) * 128, :], r=["xs_d"], w=["xb%d" % (t % 2)])
                load(0)
                for b in range(NB):
                    hTb = hT[b % 2]
                    kh = "hT%d" % (b % 2)
                    for j in range(4):
                        t = b * 4 + j
                        if t + 1 < NT:
                            load(t + 1)
                        i2 = t % 2
                        norm_mod_T(xb[i2][:], "xb%d" % i2, A1t[:], sh1t[:], hb[i2], "hb%d" % i2, junk, ssq[i2], ms[i2],
                                   rstd[i2], tmp, i2, hTb[:, :, j * 128:(j + 1) * 128], kh)
                        bk, kb = nb()
                        bk2, kb2 = nb()
                        for kc in range(8):
                            S.pe(MM(bk[:], hTb[:, kc, j * 128:(j + 1) * 128], WV[:, kc, 0:512], kc == 0, kc == 7), r=[kh, "WV"], w=[kb])
                        for kc in range(8):
                            S.pe(MM(bk2[:, 0:8], hTb[:, kc, j * 128:(j + 1) * 128], WV[:, kc, 512:520], kc == 0, kc == 7), r=[kh, "WV"], w=[kb2])
                        S.dve(CP(vst[i2][:, :, 0:64], bk[:].rearrange("p (h d) -> p h d", h=8)), r=[kb], w=["vst%d" % i2])
                        S.act(ACTF(wist[i2][:], bk2[:, 0:8], AF.Copy), r=[kb2], w=["wist%d" % i2])
                        S.dma(vaug_d[t * 128:(t + 1) * 128, :], vst[i2][:].rearrange("p h d -> p (h d)"), r=["vst%d" % i2], w=["vaug_d"])
                        S.dma(wi_d[t * 128:(t + 1) * 128, :], wist[i2][:], r=["wist%d" % i2], w=["wi_d"])
                    for c in range(13):
                        rows = 128 if c < 12 else 64
                        bk, kb = nb()
                        for kc in range(8):
                            S.pe(MM(bk[0:rows, :], WF[:, kc, c * 128:c * 128 + rows], hTb[:, kc, :], kc == 0, kc == 7), r=[kh, "WF"], w=[kb])
                        f = fst[c % 2]
                        kf = "fst%d" % (c % 2)
                        if c % 2 == 0:
                            S.act(ACTF(f[0:rows, :], bk[0:rows, :], AF.Copy), r=[kb], w=[kf])
                        else:
                            S.dve(CP(f[0:rows, :], bk[0:rows, :]), r=[kb], w=[kf])
                        S.dma(featT_d[c * 128:c * 128 + rows, b * 512:(b + 1) * 512], f[0:rows, :], r=[kf], w=["featT_d"])
                    S.dma(hT_d[:, b * 512:(b + 1) * 512].rearrange("(kc p) t -> p kc t", p=128), hTb[:], r=[kh], w=["hT_d"])
                S.flush()

        def phase_A2(l):
            with contextlib.ExitStack() as st:
                def sb(name, shape, dt):
                    return st.enter_context(SBT(name, shape, dt))
                WT1 = sb("WT1", [128, 8, 1536], BF16)
                WT2 = sb("WT2", [128, 8, 1536], BF16)
                LA1 = sb("LA1", [128, 8, 288], BF16)
                LA2 = sb("LA2", [128, 8, 288], BF16)
                Bwa = sb("Bwa", [128, 512], BF16)
                Bg = sb("Bg", [128, 512], BF16)
                Bv = sb("Bv", [32, 512], BF16)
                brow = sb("brow", [1, 3, 512], F32)
                pbc = sb("pbc", [128, 5, 512], F32)
                muT = sb("muT", [128, 32], F32)
                omT = sb("omT", [128, 32], F32)
                with contextlib.ExitStack() as st2:
                    Wr = st2.enter_context(SBT("Wr", [128, 8, 1536], BF16))
                    mubc = st2.enter_context(SBT("mubc", [128, 1536], F32))
                    ombc = st2.enter_context(SBT("ombc", [128, 1536], F32))
                    LA = st2.enter_context(SBT("LA", [128, 8, 288], F32))
                    mu32 = st2.enter_context(SBT("mu32", [32, 128], F32))
                    wv = W["w_in"][l].rearrange("(kc p) n -> p kc n", p=128)
                    S.dma(Wr[:, :, 0:768], wv[:, :, 0:768], w=["Wr"], eng="pool")
                    S.dma(Wr[:, :, 768:1536], wv[:, :, 768:1536], w=["Wr"], eng="pool")
                    S.dma(mubc[:], W["mu_rkv"][l:l + 1].rearrange("o a b -> o (a b)").partition_broadcast(128), w=["mubc"])
                    S.dve(TS(ombc[:], mubc[:], -1.0, 1.0, ALU.mult, ALU.add), r=["mubc"], w=["ombc"])
                    S.dve(TT(WT2[:], Wr[:], mubc[:].unsqueeze(1).to_broadcast([128, 8, 1536]), ALU.mult), r=["Wr", "mubc"], w=["WT2"])
                    S.pool(TT(WT1[:], Wr[:], ombc[:].unsqueeze(1).to_broadcast([128, 8, 1536]), ALU.mult), r=["Wr", "ombc"], w=["WT1"])
                    S.dma(LA[:, :, 0:64], W["decay_a"][l].rearrange("(kc p) n -> p kc n", p=128), w=["LA"])
                    S.dma(LA[:, :, 64:128], W["iclr_a"][l].rearrange("(kc p) n -> p kc n", p=128), w=["LA"])
                    S.dma(LA[:, :, 128:256], W["gate_a"][l].rearrange("(kc p) n -> p kc n", p=128), w=["LA"])
                    if l > 0:
                        S.dma(LA[:, :, 256:288], W["vres_a"][l - 1].rearrange("(kc p) n -> p kc n", p=128), w=["LA"])
                    else:
                        S.dve(MEMSET(LA[:, :, 256:288], 0.0), w=["LA"])
                    S.dve(MEMSET(mu32[:], 0.0), w=["mu32"])
                    S.dma(mu32[0:24, :], W["mu_lora"][l].rearrange("a (kc p) -> (a kc) p", p=128), r=["mu32"], w=["mu32"])
                    if l > 0:
                        S.dma(mu32[24:32, :], W["vres_mu"][l - 1:l, :].rearrange("o (kc p) -> (o kc) p", p=128), r=["mu32"], w=["mu32"])
                    bk, kb = nb()
                    S.pe(MM(bk[:, 0:32], mu32[:], identf[0:32, 0:32], True, True), r=["mu32", "cst"], w=[kb])
                    S.act(ACTF(muT[:], bk[:, 0:32], AF.Copy), r=[kb], w=["muT"])
                    S.dve(TS(omT[:], muT[:], -1.0, 1.0, ALU.mult, ALU.add), r=["muT"], w=["omT"])
                    for gi, (c0, c1) in enumerate(((0, 64), (64, 128), (128, 256), (256, 288))):
                        wdt = c1 - c0
                        S.dve(TT(LA2[:, :, c0:c1], LA[:, :, c0:c1], muT[:, gi * 8:(gi + 1) * 8].unsqueeze(2).to_broadcast([128, 8, wdt]), ALU.mult), r=["LA", "muT"], w=["LA2"])
                        S.dve(TT(LA1[:, :, c0:c1], LA[:, :, c0:c1], omT[:, gi * 8:(gi + 1) * 8].unsqueeze(2).to_broadcast([128, 8, wdt]), ALU.mult), r=["LA", "omT"], w=["LA1"])
                    S.dma(Bwa[0:64, :], W["decay_b"][l], w=["Bwa"], eng="pool")
                    S.dma(Bwa[64:128, :], W["iclr_b"][l], w=["Bwa"], eng="pool")
                    S.dma(Bg[:], W["gate_b"][l], w=["Bg"], eng="pool")
                    if l > 0:
                        S.dma(Bv[:], W["vres_b"][l - 1], w=["Bv"], eng="pool")
                    S.dma(brow[:, 0, :], W["decay_w0"][l:l + 1, :], w=["brow"])
                    S.dma(brow[:, 1, :], W["iclr_a0"][l:l + 1, :], w=["brow"])
                    if l > 0:
                        S.dma(brow[:, 2, :], W["vres_v0"][l - 1:l, :], w=["brow"])
                    S.dma(pbc[:, 0, :], W["k_k"][l:l + 1, :].partition_broadcast(128), w=["pbc"])
                    S.dma(pbc[:, 1, :], W["k_a"][l:l + 1, :].partition_broadcast(128), w=["pbc"])
                    S.dma(pbc[:, 2, :], W["r_k"][l:l + 1].rearrange("o a b -> o (a b)").partition_broadcast(128), w=["pbc"])
                    S.dma(pbc[:, 3, :], W["lnx_g"][l:l + 1, :].partition_broadcast(128), w=["pbc"])
                    S.dma(pbc[:, 4, :], W["lnx_b"][l:l + 1, :].partition_broadcast(128), w=["pbc"])
                    S.flush()

                chk(1)
                hTb2 = [sb("hTb%d" % i, [128, 8, 512], BF16) for i in range(1)]
                hTp2 = [sb("hTp%d" % i, [128, 8, 512], BF16) for i in range(1)]
                L1wa = sb("L1wa", [128, 512], BF16)
                sg = sb("sg", [128, 512], BF16)
                sgt = sb("sgt", [128, 512], F32)
                L1v = sb("L1v", [32, 512], BF16)

                def f32t(name):
                    return sb(name, [128, 512], F32)

                def b16t(name):
                    return sb(name, [128, 512], BF16)
                r32, k32, v32 = f32t("r32"), f32t("k32"), f32t("v32")
                kkr, kk, a32, b32, km = f32t("kkr"), f32t("kk"), f32t("a32"), f32t("b32"), f32t("km")
                lw, Ginc, Ginv, Gexc, g32 = f32t("lw"), f32t("Ginc"), f32t("Ginv"), f32t("Gexc"), f32t("g32")
                t1, t2, U0, cen, vf = f32t("t1"), f32t("t2"), f32t("U0"), f32t("cen"), f32t("vf")
                y32 = f32t("y32")
                Kd, Rd, Bi, Ki, Vb, Zb, Ub, ob = (b16t(n) for n in ("Kd", "Rd", "Bi", "Ki", "Vb", "Zb", "Ub", "ob"))
                s8 = [sb("s8_%d" % i, [128, 8], F32) for i in range(6)]
                FT = sb("FT", [64, 8, 4, 128], BF16)
                GE = sb("GE", [128, 8, 512], BF16)
                MMb = [sb("MMb%d" % i, [128, 8, 128], F32) for i in range(2)]
                NNb = [sb("NNb%d" % i, [128, 8, 128], F32) for i in range(2)]
                TTb = [sb("TTb%d" % i, [128, 8, 128], F32) for i in range(2)]
                TTh = sb("TTh", [128, 8, 128], BF16)
                WTs = sb("WTs", [64, 8, 128], BF16)
                P32 = sb("P32", [64, 8, 64], F32)
                Pb = sb("Pb", [64, 8, 64], BF16)
                GC = sb("GC", [64, 8], F32)
                rwT = sb("rwT", [128, 4, 128], BF16)
                S.dve(MEMSET(P32[:], 0.0), w=["P32"])
                S.dve(MEMSET(Pb[:], 0.0), w=["Pb"])
                P32f = P32[:].rearrange("p h v -> p (h v)")
                ev = [0]

                def evac(out, in_, r, w):
                    ev[0] += 1
                    if ev[0] % 2:
                        S.act(ACTF(out, in_, AF.Copy), r=r, w=w)
                    else:
                        S.dve(CP(out, in_), r=r, w=w)

                for b in range(NB):
                    hTb = hTb2[0]
                    kh = "hTb0"
                    src = hT_d.rearrange("(kc p) t -> p kc t", p=128)
                    if b == 1:
                        chk(8)
                    hTp = hTp2[0]
                    S.dma(hTb[:], src[:, :, b * 512:(b + 1) * 512], r=["hT_d"], w=[kh])
                    if b == 0:
                        S.dve(MEMSET(hTp[:, :, 0:1], 0.0), w=[kh])
                        S.dma(hTp[:, :, 1:512], src[:, :, 0:511], r=["hT_d"], w=[kh])
                    else:
                        S.dma(hTp[:], src[:, :, b * 512 - 1:b * 512 + 511], r=["hT_d"], w=[kh])
                    if b == 1:
                        chk(9)
                    for gi, (c0, c1) in enumerate(((0, 128), (128, 256), (256, 288))):
                        if gi == 2 and l == 0:
                            continue
                        rows = c1 - c0
                        bk, kb = nb()
                        for kc in range(8):
                            S.pe(MM(bk[0:rows, :], LA1[:, kc, c0:c1], hTb[:, kc, :], kc == 0, False), r=[kh, "LA1"], w=[kb])
                        for kc in range(8):
                            S.pe(MM(bk[0:rows, :], LA2[:, kc, c0:c1], hTp[:, kc, :], False, kc == 7), r=[kh, "LA2"], w=[kb])
                        if gi == 0:
                            S.act(ACTF(L1wa[0:64, :], bk[0:64, :], AF.Tanh), r=[kb], w=["L1wa"])
                            S.act(ACTF(L1wa[64:128, :], bk[64:128, :], AF.Copy), r=[kb], w=["L1wa"])
                        elif gi == 1:
                            S.act(ACTF(sgt[:], bk[:], AF.Tanh, scale=0.5), r=[kb], w=["sgt"])
                            S.dve(TS(sg[:], sgt[:], 0.5, 0.5, ALU.mult, ALU.add), r=["sgt"], w=["sg"])
                        else:
                            S.act(ACTF(L1v[:], bk[0:32, :], AF.Copy), r=[kb], w=["L1v"])
                    chk(2)
                    for j in range(4):
                        t = b * 4 + j
                        lo = j * 128
                        tsl = slice(j * 128, (j + 1) * 128)
                        if l > 0:
                            S.dma(vf[:], vfirst_d[t * 128:(t + 1) * 128, :], r=["vfirst_d"], w=["vf"])
                        for g, (dst, kd) in enumerate(((r32, "r32"), (k32, "k32"), (v32, "v32"))):
                            bk, kb = nb()
                            for kc in range(8):
                                S.pe(MM(bk[:], hTb[:, kc, lo:lo + 128], WT1[:, kc, g * 512:(g + 1) * 512], kc == 0, False), r=[kh, "WT1"], w=[kb])
                            for kc in range(8):
                                S.pe(MM(bk[:], hTp[:, kc, lo:lo + 128], WT2[:, kc, g * 512:(g + 1) * 512], False, kc == 7), r=[kh, "WT2"], w=[kb])
                            evac(dst[:], bk[:], [kb], [kd])
                        bkw, kbw = nb()
                        S.pe(MM(bkw[:], L1wa[0:64, tsl], Bwa[0:64, :], True, False), r=["L1wa", "Bwa"], w=[kbw])
                        S.pe(MM(bkw[:], onesf[0:1, :], brow[0:1, 0, :], False, True), r=["onesf", "brow"], w=[kbw])
                        bka, kba = nb()
                        S.pe(MM(bka[:], L1wa[64:128, tsl], Bwa[64:128, :], True, False), r=["L1wa", "Bwa"], w=[kba])
                        S.pe(MM(bka[:], onesf[0:1, :], brow[0:1, 1, :], False, True), r=["onesf", "brow"], w=[kba])
                        bkg, kbg = nb()
                        S.pe(MM(bkg[:], sg[:, tsl], Bg[:], True, True), r=["sg", "Bg"], w=[kbg])
                        S.act(ACTF(lw[:], bkw[:], AF.Tanh, scale=0.5), r=[kbw], w=["lw"])
                        S.dve(TS(lw[:], lw[:], -0.5 * math.exp(-0.5), -0.5 * math.exp(-0.5), ALU.mult, ALU.add), r=["lw"], w=["lw"])
                        S.act(ACTF(a32[:], bka[:], AF.Tanh, scale=0.5), r=[kba], w=["a32"])
                        S.pool(TS(a32[:], a32[:], 0.5, 0.5, ALU.mult, ALU.add), r=["a32"], w=["a32"])
                        S.act(ACTF(g32[:], bkg[:], AF.Copy), r=[kbg], w=["g32"])
                        if l > 0:
                            bkv, kbv = nb()
                            S.pe(MM(bkv[:], L1v[0:32, tsl], Bv[0:32, :], True, False), r=["L1v", "Bv"], w=[kbv])
                            S.pe(MM(bkv[:], onesf[0:1, :], brow[0:1, 2, :], False, True), r=["onesf", "brow"], w=[kbv])
                            S.act(ACTF(t1[:], bkv[:], AF.Tanh, scale=0.5), r=[kbv], w=["t1"])
                            S.pool(TS(t1[:], t1[:], 0.5, 0.5, ALU.mult, ALU.add), r=["t1"], w=["t1"])
                            S.pool(TT(t2[:], vf[:], v32[:], ALU.subtract), r=["vf", "v32"], w=["t2"])
                            S.pool(TT(t2[:], t2[:], t1[:], ALU.mult), r=["t2", "t1"], w=["t2"])
                            S.pool(TT(v32[:], v32[:], t2[:], ALU.add), r=["v32", "t2"], w=["v32"])
                        else:
                            S.dma(vfirst_d[t * 128:(t + 1) * 128, :], v32[:], r=["v32"], w=["vfirst_d"])
                        S.act(ACTF(Vb[:], v32[:], AF.Copy), r=["v32"], w=["Vb"])
                        if dbg:
                            rows = slice(t * 128, (t + 1) * 128)
                            S.dma(dbg_d["dbg_v"][rows, :], v32[:], r=["v32"], w=["dbgv"])
                            S.dma(dbg_d["dbg_r"][rows, :], r32[:], r=["r32"], w=["dbgr"])
                            S.dma(dbg_d["dbg_k"][rows, :], k32[:], r=["k32"], w=["dbgk"])
                            S.dma(dbg_d["dbg_a"][rows, :], a32[:], r=["a32"], w=["dbga"])
                            S.dma(dbg_d["dbg_lw"][rows, :], lw[:], r=["lw"], w=["dbglw"])
                        bkc, kbc = nb()
                        S.pe(MM(bkc[:], tri_incl, lw[:], True, True), r=["cst", "lw"], w=[kbc])
                        bke, kbe = nb()
                        S.pe(MM(bke[:], tri_strict, lw[:], True, True), r=["cst", "lw"], w=[kbe])
                        bkG, kbG = nb()
                        for h in range(8):
                            S.pe(MM(bkG[0:64, h:h + 1], lw[:, h * 64:(h + 1) * 64], onesf[:, 0:1], True, True), r=["lw", "onesf"], w=[kbG])
                        S.act(ACTF(Ginc[:], bkc[:], AF.Exp), r=[kbc], w=["Ginc"])
                        S.act(ACTF(Ginv[:], bkc[:], AF.Exp, scale=-1.0), r=[kbc], w=["Ginv"])
                        S.act(ACTF(Gexc[:], bke[:], AF.Exp), r=[kbe], w=["Gexc"])
                        S.act(ACTF(GC[:], bkG[0:64, 0:8], AF.Exp), r=[kbG], w=["GC"])
                        S.dve(TT(kkr[:], k32[:], pbc[:, 0, :], ALU.mult), r=["k32", "pbc"], w=["kkr"])
                        S.act(ACTF(t2[:], kkr[:], AF.Square), r=["kkr"], w=["t2"])
                        S.dve(RED(s8[0][:], t2[:].rearrange("p (h d) -> p h d", h=8), ALU.add), r=["t2"], w=["s8_0"])
                        S.dve(TS(s8[0][:], s8[0][:], 1e-24, None, ALU.max), r=["s8_0"], w=["s8_0"])
                        S.pool(TT(s8[1][:], s8[0][:], m05[:, 0:8], ALU.pow), r=["s8_0", "m05"], w=["s8_1"])
                        S.dve(TT(kk[:].rearrange("p (h d) -> p h d", h=8), kkr[:].rearrange("p (h d) -> p h d", h=8),
                                 s8[1][:].unsqueeze(2).to_broadcast([128, 8, 64]), ALU.mult), r=["kkr", "s8_1"], w=["kk"])
                        S.pool(TT(b32[:], kk[:], a32[:], ALU.mult), r=["kk", "a32"], w=["b32"])
                        S.dve(STT(t1[:], a32[:], -1.0, pbc[:, 1, :], ALU.add, ALU.mult), r=["a32", "pbc"], w=["t1"])
                        S.dve(STT(km[:], t1[:], 1.0, k32[:], ALU.add, ALU.mult), r=["t1", "k32"], w=["km"])
                        S.dve(TT(Kd[:], kk[:], Gexc[:], ALU.mult), r=["kk", "Gexc"], w=["Kd"])
                        S.pool(TT(Bi[:], b32[:], Ginv[:], ALU.mult), r=["b32", "Ginv"], w=["Bi"])
                        S.dve(TT(Ki[:], km[:], Ginv[:], ALU.mult), r=["km", "Ginv"], w=["Ki"])
                        S.pool(TT(Rd[:], r32[:], Ginc[:], ALU.mult), r=["r32", "Ginc"], w=["Rd"])
                        S.pool(TT(t2[:], r32[:], km[:], ALU.mult), r=["r32", "km"], w=["t2"])
                        S.pool(TT(t2[:], t2[:], pbc[:, 2, :], ALU.mult), r=["t2", "pbc"], w=["t2"])
                        S.dve(RED(s8[2][:], t2[:].rearrange("p (h d) -> p h d", h=8), ALU.add), r=["t2"], w=["s8_2"])
                        chk(3)
                        for q, (srcT, ks) in enumerate(((Kd, "Kd"), (Rd, "Rd"), (Bi, "Bi"), (Ki, "Ki"))):
                            bk, kb = nb()
                            bkb = bfv(bk)
                            for h in range(8):
                                S.pe(TR(bkb[0:64, h * 128:(h + 1) * 128], srcT[:, h * 64:(h + 1) * 64], identb[:]), r=[ks, "identb"], w=[kb])
                            evac(FT[:, :, q, :], bkb[0:64, :].rearrange("p (h t) -> p h t", h=8), [kb], ["FT"])
                        for h in range(8):
                            bk, kb = nb()
                            rhs = FT[:, h, 0:2, :].rearrange("p a t -> p (a t)")
                            S.pe(MM(bk[:, 0:256], FT[:, h, 2, :], rhs, True, True), r=["FT"], w=[kb])
                            S.pe(MM(bk[:, 256:512], FT[:, h, 3, :], rhs, True, True), r=["FT"], w=[kb])
                            S.dve(TT(GE[:, h, :], bk[:], mask4[:], ALU.mult), r=[kb, "mask4"], w=["GE"])
                            S.dve(TT(NNb[0][:, h, :], bk[:, 0:128], tri_strict, ALU.mult), r=[kb, "cst"], w=["NNb0"])
                        for g in range(2):
                            bk, kb = nb()
                            for hh in range(4):
                                h = g * 4 + hh
                                S.pe(MM(bk[:, hh * 128:(hh + 1) * 128], FT[:, h, 0, :], FT[:, h, 2, :], True, True), r=["FT"], w=[kb])
                            S.dve(TT(MMb[0][:, g * 4:(g + 1) * 4, :], bk[:].rearrange("p (h t) -> p h t", h=4),
                                     lowm4[:].rearrange("p (h t) -> p h t", h=4), ALU.mult), r=[kb, "lowm4"], w=["MMb0"])
                        chk(4)
                        S.pool(TT(TTb[0][:], identf.unsqueeze(1).to_broadcast([128, 8, 128]), NNb[0][:], ALU.subtract),
                               r=["cst", "NNb0"], w=["TTb0"])
                        cur = 0
                        for step in range(1, 7):
                            nxt = 1 - cur
                            sqb = []
                            for g in range(2):
                                hs = range(g * 4, g * 4 + 4)
                                bkM, kbM = nb()
                                for hh, h in enumerate(hs):
                                    S.pe(MM(bkM[:, hh * 128:(hh + 1) * 128], NNb[cur][:, h, :], MMb[cur][:, h, :], True, True),
                                         r=["NNb%d" % cur, "MMb%d" % cur], w=[kbM])
                                bkN = kbN = None
                                if step < 6:
                                    bkN, kbN = nb()
                                    for hh, h in enumerate(hs):
                                        S.pe(MM(bkN[:, hh * 128:(hh + 1) * 128], MMb[cur][:, h, :], NNb[cur][:, h, :], True, True),
                                             r=["NNb%d" % cur, "MMb%d" % cur], w=[kbN])
                                sqb.append((bkM, kbM, bkN, kbN))
                            for g in range(2):
                                bkM, kbM, bkN, kbN = sqb[g]
                                evac(MMb[nxt][:, g * 4:(g + 1) * 4, :], bkM[:].rearrange("p (h t) -> p h t", h=4), [kbM], ["MMb%d" % nxt])
                                if bkN is not None:
                                    evac(NNb[nxt][:, g * 4:(g + 1) * 4, :], bkN[:].rearrange("p (h t) -> p h t", h=4), [kbN], ["NNb%d" % nxt])
                            for g in range(2):
                                hs = range(g * 4, g * 4 + 4)
                                bkT, kbT = nb()
                                for hh, h in enumerate(hs):
                                    S.pe(MM(bkT[:, hh * 128:(hh + 1) * 128], MMb[nxt][:, h, :], TTb[cur][:, h, :], True, True),
                                         r=["MMb%d" % nxt, "TTb%d" % cur], w=[kbT])
                                S.dve(TT(TTb[nxt][:, g * 4:(g + 1) * 4, :].rearrange("p h t -> p (h t)"), bkT[:],
                                         TTb[cur][:, g * 4:(g + 1) * 4, :].rearrange("p h t -> p (h t)"), ALU.add),
                                      r=[kbT, "TTb%d" % cur], w=["TTb%d" % nxt])
                            cur = nxt
                        S.act(ACTF(TTh[:], TTb[cur][:], AF.Copy), r=["TTb%d" % cur], w=["TTh"])
                        TTf = TTh
                        kT = "TTh"
                        for g in range(2):
                            bk, kb = nb()
                            for hh in range(4):
                                h = g * 4 + hh
                                S.pe(MM(bk[0:64, hh * 128:(hh + 1) * 128], Kd[:, h * 64:(h + 1) * 64], TTf[:, h, :], True, True), r=["Kd", kT], w=[kb])
                            evac(WTs[:, g * 4:(g + 1) * 4, :], bk[0:64, :].rearrange("p (h t) -> p h t", h=4), [kb], ["WTs"])
                        bk, kb = nb()
                        for h in range(8):
                            S.pe(MM(bk[:, h * 64:(h + 1) * 64], GE[:, h, 256:384], Vb[:, h * 64:(h + 1) * 64], True, True), r=["GE", "Vb"], w=[kb])
                        evac(Zb[:], bk[:], [kb], ["Zb"])
                        bk, kb = nb()
                        for h in range(8):
                            S.pe(MM(bk[:, h * 64:(h + 1) * 64], TTf[:, h, :], Zb[:, h * 64:(h + 1) * 64], True, True), r=[kT, "Zb"], w=[kb])
                        evac(U0[:], bk[:], [kb], ["U0"])
                        chk(6)
                        bk, kb = nb()
                        for h in range(8):
                            S.pe(MM(bk[:, h * 64:(h + 1) * 64], WTs[:, h, :], Pb[:, h, :], True, True), r=["WTs", "Pb"], w=[kb])
                        S.dve(STT(Ub[:], bk[:], -1.0, U0[:], ALU.mult, ALU.subtract), r=[kb, "U0"], w=["Ub"])
                        bkY, kbY = nb()
                        for h in range(8):
                            hs_ = slice(h * 64, (h + 1) * 64)
                            S.pe(MM(bkY[:, hs_], FT[:, h, 1, :], Pb[:, h, :], True, False), r=["FT", "Pb"], w=[kbY])
                            S.pe(MM(bkY[:, hs_], GE[:, h, 128:256], Ub[:, hs_], False, False), r=["GE", "Ub"], w=[kbY])
                            S.pe(MM(bkY[:, hs_], GE[:, h, 384:512], Vb[:, hs_], False, True), r=["GE", "Vb"], w=[kbY])
                        bkX, kbX = nb()
                        S.pe(MM(bkX[0:64, :], identf[0:64, 0:64], P32f, True, False), r=["cst", "P32"], w=[kbX])
                        for h in range(8):
                            hs_ = slice(h * 64, (h + 1) * 64)
                            S.pe(MM(bkX[0:64, hs_], Bi[:, hs_], Ub[:, hs_], False, False), r=["Bi", "Ub"], w=[kbX])
                            S.pe(MM(bkX[0:64, hs_], Ki[:, hs_], Vb[:, hs_], False, h == 7), r=["Ki", "Vb"], w=[kbX])
                        S.dve(TT(P32[:], bkX[0:64, :].rearrange("p (h v) -> p h v", h=8), GC[:].unsqueeze(2).to_broadcast([64, 8, 64]), ALU.mult),
                              r=[kbX, "GC"], w=["P32"])
                        S.act(ACTF(Pb[:], P32[:], AF.Copy), r=["P32"], w=["Pb"])
                        chk(7)
                        S.act(ACTF(y32[:], bkY[:], AF.Copy), r=[kbY], w=["y32"])
                        if dbg:
                            S.dma(dbg_d["dbg_y"][t * 128:(t + 1) * 128, :], y32[:], r=["y32"], w=["dbgy"])
                        chk(10)
                        Y3 = y32[:].rearrange("p (h d) -> p h d", h=8)
                        S.dve(RED(s8[3][:], Y3, ALU.add), r=["y32"], w=["s8_3"])
                        S.dve(TS(s8[3][:], s8[3][:], 1.0 / 64, None, ALU.mult), r=["s8_3"], w=["s8_3"])
                        S.dve(TT(cen[:].rearrange("p (h d) -> p h d", h=8), Y3, s8[3][:].unsqueeze(2).to_broadcast([128, 8, 64]), ALU.subtract),
                              r=["y32", "s8_3"], w=["cen"])
                        S.act(ACTF(t2[:], cen[:], AF.Square), r=["cen"], w=["t2"])
                        S.dve(RED(s8[4][:], t2[:].rearrange("p (h d) -> p h d", h=8), ALU.add), r=["t2"], w=["s8_4"])
                        S.dve(TS(s8[4][:], s8[4][:], 1.0 / 64, 64e-5, ALU.mult, ALU.add), r=["s8_4"], w=["s8_4"])
                        S.pool(TT(s8[5][:], s8[4][:], m05[:, 0:8], ALU.pow), r=["s8_4", "m05"], w=["s8_5"])
                        S.dve(TT(cen[:].rearrange("p (h d) -> p h d", h=8), cen[:].rearrange("p (h d) -> p h d", h=8),
                                 s8[5][:].unsqueeze(2).to_broadcast([128, 8, 64]), ALU.mult), r=["cen", "s8_5"], w=["cen"])
                        S.pool(TT(cen[:], cen[:], pbc[:, 3, :], ALU.mult), r=["cen", "pbc"], w=["cen"])
                        S.pool(TT(cen[:], cen[:], pbc[:, 4, :], ALU.add), r=["cen", "pbc"], w=["cen"])
                        S.dve(TT(t2[:].rearrange("p (h d) -> p h d", h=8), v32[:].rearrange("p (h d) -> p h d", h=8),
                                 s8[2][:].unsqueeze(2).to_broadcast([128, 8, 64]), ALU.mult), r=["v32", "s8_2"], w=["t2"])
                        S.pool(TT(cen[:], cen[:], t2[:], ALU.add), r=["cen", "t2"], w=["cen"])
                        S.dve(TT(ob[:], cen[:], g32[:], ALU.mult), r=["cen", "g32"], w=["ob"])
                        chk(11)
                        bk, kb = nb()
                        bkb = bfv(bk)
                        for c in range(4):
                            S.pe(TR(bkb[:, c * 128:(c + 1) * 128], ob[:, c * 128:(c + 1) * 128], identb[:]), r=["ob", "identb"], w=[kb])
                        evac(rwT[:], bkb[:, 0:512].rearrange("p (c t) -> p c t", c=4), [kb], ["rwT"])
                        S.dma(rwkvT_d[:, t * 128:(t + 1) * 128].rearrange("(c p) t -> p c t", p=128), rwT[:], r=["rwT"], w=["rwkvT_d"])
                        chk(12)
                S.flush()

        def phase_B(l):
            nrot[0] = NROT
            with contextlib.ExitStack() as st:
                def sb(name, shape, dt):
                    return st.enter_context(SBT(name, shape, dt))
                c31b = sb("c31b", [128, 8], F32)
                nc31 = sb("nc31", [128, 8], F32)
                Rn = sb("Rn", [128, 8, 256], BF16)
                aog = sb("aog", [64, 8], F32)
                statL = sb("statL", [65, 64], F32)
                S.dma(c31b[:], c31_d.partition_broadcast(128), w=["c31b"])
                aog8 = sb("aog8", [8, 64], F32)
                S.dma(aog8[:], W["attn_out_g"][l].rearrange("(h d) -> h d", d=64), w=["aog8"])
                bk, kb = nb()
                S.pe(MM(bk[0:64, 0:8], aog8[:], identf[0:8, 0:8], True, True), r=["aog8", "cst"], w=[kb])
                S.dve(CP(aog[:], bk[0:64, 0:8]), r=[kb], w=["aog"])
                S.dve(TS(nc31[:], c31b[:], -1.0, None, ALU.mult), r=["c31b"], w=["nc31"])
                with contextlib.ExitStack() as st2:
                    bnr = st2.enter_context(SBT("bnr", [128, 2048], F32))
                    S.dma(bnr[:], bnear_d, w=["bnr"])
                    for h in range(8):
                        S.act(ACTF(Rn[:, h, :], bnr[:, h * 256:(h + 1) * 256], AF.Exp, bias=nc31[:, h:h + 1]), r=["bnr", "nc31"], w=["Rn"])
                    S.flush()
                kaT = sb("kaT", [128, 4, T], BF16)
                kiT2 = sb("kiT2", [128, T], BF16)
                Va = sb("Va", [128, NT, 520], BF16)
                qaTb = [sb("qaTb%d" % i, [128, 4, 512], BF16) for i in range(2)]
                qiTb = [sb("qiTb%d" % i, [128, 4, 512], BF16) for i in range(2)]
                wib = [sb("wib%d" % i, [128, 4, 8], F32) for i in range(2)]
                wabs = sb("wabs", [128, 4, 8], F32)
                wsgn = sb("wsgn", [128, 4, 8], F32)
                scb = [sb("sc%d" % i, [128, T], F32) for i in range(2)]
                rl = [sb("rl%d" % i, [128, 512], F32) for i in range(3)]
                msk = sb("msk", [128, T], BF16)
                maskT = sb("maskT", [128, NT, 512], BF16)
                NEP = 4
                Eb = [sb("Eb%d" % i, [128, 512], BF16) for i in range(NEP)]
                Pt = [sb("Pt%d" % i, [128, 512], BF16) for i in range(NEP)]
                attTb = sb("attTb", [128, 4, 512], BF16)
                Osb = sb("Osb", [65, 512], F32)
                SQ = sb("SQ", [65, 512], F32)
                rs = sb("rs", [64, 512], F32)
                bis = [sb("bis%d" % i, [128, 1], F32) for i in range(6)]
                wtab = sb("wtab", [128, NBIS + 2], F32)
                ctab = sb("ctab", [128, NBIS + 2], F32)
                S.dma(kaT[:], featT_d[512:1024, :].rearrange("(c p) t -> p c t", p=128), r=["featT_d"], w=["kaT"])
                S.dma(kiT2[0:64, :], featT_d[1536:1600, :], r=["featT_d"], w=["kiT2"])
                S.dma(kiT2[64:128, :], featT_d[1536:1600, :], r=["featT_d"], w=["kiT2"])
                vsrc = vaug_d.rearrange("(n p) c -> p n c", p=128)
                for n0 in range(0, NT, 8):
                    S.dma(Va[:, n0:n0 + 8, :], vsrc[:, n0:n0 + 8, :], r=["vaug_d"], w=["Va"])
                S.dve(MEMSET(statL[0:64, :], 1.0 / 64), w=["statL"])
                S.dve(MEMSET(statL[64:65, :], 1e-6), w=["statL"])
                for k in range(NBIS + 2):
                    S.pool(MEMSET(ctab[:, k:k + 1], 2.0 ** (-k)), w=["ctab"])
                cw = 0.125 * (8 ** -0.5)

                def loadq(b):
                    i = b % 2
                    S.dma(qaTb[i][:], featT_d[0:512, b * 512:(b + 1) * 512].rearrange("(c p) t -> p c t", p=128), r=["featT_d"], w=["qaTb%d" % i])
                    S.dma(qiTb[i][:], featT_d[1024:1536, b * 512:(b + 1) * 512].rearrange("(c p) t -> p c t", p=128), r=["featT_d"], w=["qiTb%d" % i])
                    S.dma(wib[i][:], wi_d[b * 512:(b + 1) * 512, :].rearrange("(j p) h -> p j h", p=128), r=["wi_d"], w=["wib%d" % i])
                loadq(0)
                eidx = [0]
                for b in range(NB):
                    if b + 1 < NB:
                        loadq(b + 1)
                    i2 = b % 2
                    qa, qi, wi_ = qaTb[i2], qiTb[i2], wib[i2]
                    kqa, kqi, kwi = "qaTb%d" % i2, "qiTb%d" % i2, "wib%d" % i2
                    S.dve(STT(wabs[:], wi_[:], -1.0, wi_[:], ALU.mult, ALU.max), r=[kwi], w=["wabs"])
                    S.dve(TS(wabs[:], wabs[:], cw, None, ALU.mult), r=["wabs"], w=["wabs"])
                    S.act(ACTF(wsgn[:], wi_[:], AF.Sign), r=[kwi], w=["wsgn"])
                    nk = 4 * b + 4
                    def indexer(jq):
                        j = 4 * b + jq
                        Lk = (j + 1) * 128
                        qsl = slice(jq * 128, (jq + 1) * 128)
                        sc = scb[jq % 2]
                        ksc = "sc%d" % (jq % 2)
                        nch = (Lk + 511) // 512
                        for kc in range(nch):
                            ncol = min(512, Lk - kc * 512)
                            csl = slice(kc * 512, kc * 512 + ncol)
                            for ih in range(8):
                                pr, hf = ih // 2, ih % 2
                                ps_ = slice(hf * 64, (hf + 1) * 64)
                                bk, kb = nb()
                                S.pe(MM(bk[:, 0:ncol], qi[ps_, pr, qsl], kiT2[ps_, csl], True, True), r=[kqi, "kiT2"], w=[kb])
                                rb = rl[eidx[0] % 3]
                                krb = "rl%d" % (eidx[0] % 3)
                                eidx[0] += 1
                                S.act(ACTF(rb[:, 0:ncol], bk[:, 0:ncol], AF.Relu, scale=wabs[:, jq, ih:ih + 1]), r=[kb, "wabs"], w=[krb])
                                if ih == 0:
                                    S.dve(TS(sc[:, csl], rb[:, 0:ncol], wsgn[:, jq, 0:1], None, ALU.mult), r=[krb, "wsgn"], w=[ksc])
                                else:
                                    S.dve(STT(sc[:, csl], rb[:, 0:ncol], wsgn[:, jq, ih:ih + 1], sc[:, csl], ALU.mult, ALU.add), r=[krb, "wsgn", ksc], w=[ksc])
                                yield

                    gens = [indexer(jq) for jq in range(4)]
                    for _ in gens[0]:
                        pass
                    for jq in range(4):
                        j = 4 * b + jq
                        Lk = (j + 1) * 128
                        qsl = slice(jq * 128, (jq + 1) * 128)
                        sc = scb[jq % 2]
                        ksc = "sc%d" % (jq % 2)
                        nxt = gens[jq + 1] if jq < 3 else None
                        per_round = (((Lk + 128 + 511) // 512) * 8 + NBIS - 1) // NBIS if nxt is not None else 0
                        A_, mid, cnt, inc = bis[0], bis[1], bis[2], bis[3]
                        S.dve(RED(A_[:], sc[:, 0:Lk], ALU.max, absval=True), r=[ksc], w=["bis0"])
                        S.dve(TS(A_[:], A_[:], 1.001, 1e-6, ALU.mult, ALU.add), r=["bis0"], w=["bis0"])
                        S.dve(TT(sc[:, j * 128:(j + 1) * 128], sc[:, j * 128:(j + 1) * 128], caus, ALU.add), r=[ksc, "cst"], w=[ksc])
                        if dbg:
                            S.dma(dbg_d["dbg_score"][j * 128:(j + 1) * 128, 0:Lk], sc[:, 0:Lk], r=[ksc], w=["dbgs"])
                        S.dve(TS(wtab[:], ctab[:], A_[:, 0:1], None, ALU.mult), r=["ctab", "bis0"], w=["wtab"])
                        S.dve(TS(mid[:], A_[:], 0.0, None, ALU.mult), r=["bis0"], w=["bis1"])
                        for k in range(1, NBIS + 1):
                            if k % 2 == 1:
                                S.dve(TS(msk[:, 0:Lk], sc[:, 0:Lk], mid[:, 0:1], 0.0, ALU.is_ge, ALU.add, accum=cnt[:]), r=[ksc, "bis1"], w=["bis2", "msk"])
                                S.dve(STT(inc[:], cnt[:], NSEL - 0.5, wtab[:, k - 1:k], ALU.is_ge, ALU.mult), r=["bis2", "wtab"], w=["bis3"])
                            else:
                                S.dve(TS(bis[4][:], mid[:], -1.0, None, ALU.mult), r=["bis1"], w=["bis4"])
                                S.act(ACTF(msk[:, 0:Lk], sc[:, 0:Lk], AF.Sign, bias=bis[4][:, 0:1], accum=cnt[:]), r=[ksc, "bis4"], w=["bis2", "msk"])
                                S.dve(STT(inc[:], cnt[:], 2.0 * NSEL - 1.0 - Lk, wtab[:, k - 1:k], ALU.is_ge, ALU.mult), r=["bis2", "wtab"], w=["bis3"])
                            S.dve(STT(mid[:], inc[:], wtab[:, k:k + 1], mid[:], ALU.subtract, ALU.add), r=["bis3", "wtab", "bis1"], w=["bis1"])
                            if nxt is not None:
                                for _ in range(per_round):
                                    if next(nxt, "done") == "done":
                                        break
                        S.dve(TT(bis[5][:], mid[:], wtab[:, NBIS:NBIS + 1], ALU.subtract), r=["bis1", "wtab"], w=["bis5"])
                        if dbg:
                            S.dma(dbg_d["dbg_thr"][j * 128:(j + 1) * 128, :], bis[5][:], r=["bis5"], w=["dbgt"])
                        S.dve(TS(msk[:, 0:Lk], sc[:, 0:Lk], bis[5][:, 0:1], None, ALU.is_ge), r=[ksc, "bis5"], w=["msk"])
                        for i0 in range(0, j + 1, 8):
                            n8 = min(8, j + 1 - i0)
                            bk, kb = nb()
                            bkb = bfv(bk)
                            for ii in range(n8):
                                S.pe(TR(bkb[:, ii * 128:(ii + 1) * 128], msk[:, (i0 + ii) * 128:(i0 + ii + 1) * 128], identb[:]), r=["msk", "identb"], w=[kb])
                            S.act(ACTF(maskT[:, i0:i0 + n8, qsl], bkb[:, 0:n8 * 128].rearrange("p (i t) -> p i t", i=n8), AF.Copy), r=[kb], w=["maskT"])
                        if nxt is not None:
                            for _ in nxt:
                                pass
                    for hp in range(4):
                        hpair = (2 * hp, 2 * hp + 1)
                        for i in range(nk):
                            m = i - 4 * b
                            c0 = max(0, m) * 128
                            ncol = 512 - c0
                            st_ = []
                            for h in hpair:
                                pr, hf = h // 2, h % 2
                                ps_ = slice(hf * 64, (hf + 1) * 64)
                                bk, kb = nb()
                                S.pe(MM(bk[:, 0:ncol], kaT[ps_, pr, i * 128:(i + 1) * 128], qa[ps_, pr, c0:512], True, True), r=["kaT", kqa], w=[kb])
                                ei = eidx[0] % NEP
                                eidx[0] += 1
                                st_.append((h, bk, kb, ei))
                            for h, bk, kb, ei in st_:
                                S.act(ACTF(Eb[ei][:, 0:ncol], bk[:, 0:ncol], AF.Exp, bias=c31b[:, h:h + 1], scale=0.125), r=[kb, "c31b"], w=["Eb%d" % ei])
                            for h, bk, kb, ei in st_:
                                E, kE, P_, kP = Eb[ei], "Eb%d" % ei, Pt[ei], "Pt%d" % ei
                                eng = S.dve if (h % 2) else S.pool
                                eng(TT(P_[:, 0:ncol], E[:, 0:ncol], maskT[:, i, c0:512], ALU.mult), r=[kE, "maskT"], w=[kP])
                                if m >= 0:
                                    nn = min(256, ncol)
                                    eng(TT(P_[:, 0:nn], P_[:, 0:nn], Rn[:, h, 0:nn], ALU.mult), r=[kP, "Rn"], w=[kP])
                                elif m == -1:
                                    eng(TT(P_[:, 0:128], P_[:, 0:128], Rn[:, h, 128:256], ALU.mult), r=[kP, "Rn"], w=[kP])
                            for h, bk, kb, ei in st_:
                                bkO, kbO = banks[6 + h % 2], "bank%d" % (6 + h % 2)
                                S.pe(MM(bkO[0:65, c0:512], Va[:, i, h * 65:(h + 1) * 65], Pt[ei][:, 0:ncol], i == 0, i == nk - 1), r=["Va", "Pt%d" % ei], w=[kbO])
                        for h in hpair:
                            pr, hf = h // 2, h % 2
                            ps_ = slice(hf * 64, (hf + 1) * 64)
                            bkO, kbO = banks[6 + h % 2], "bank%d" % (6 + h % 2)
                            S.act(ACTF(Osb[:], bkO[0:65, :], AF.Copy), r=[kbO], w=["Osb"])
                            S.act(ACTF(SQ[:], bkO[0:65, :], AF.Square), r=[kbO], w=["SQ"])
                            bk, kb = nb()
                            S.pe(MM(bk[0:64, :], statL[:], SQ[:], True, True), r=["statL", "SQ"], w=[kb])
                            S.act(ACTF(rs[:], bk[0:64, :], AF.Ln), r=[kb], w=["rs"])
                            S.act(ACTF(rs[:], rs[:], AF.Exp, scale=-0.5), r=["rs"], w=["rs"])
                            S.dve(STT(attTb[ps_, pr, :], Osb[0:64, :], aog[:, h:h + 1], rs[:], ALU.mult, ALU.mult), r=["Osb", "aog", "rs"], w=["attTb"])
                    S.dma(attT_d[:, b * 512:(b + 1) * 512].rearrange("(c p) t -> p c t", p=128), attTb[:], r=["attTb"], w=["attT_d"])
                S.flush()
            nrot[0] = 8

        def phase_B2(l, xsrc):
            with contextlib.ExitStack() as st:
                def sb(name, shape, dt):
                    return st.enter_context(SBT(name, shape, dt))
                wo = sb("wo", [128, 8, 1024], BF16)
                gt1 = sb("gt1", [128, 1024], F32)
                A2t = sb("A2t", [128, 1024], F32)
                sh2t = sb("sh2t", [128, 1024], F32)
                xb = [sb("xb%d" % i, [128, 1024], F32) for i in range(2)]
                mixT = [sb("mixT%d" % i, [128, 8, 128], BF16) for i in range(2)]
                x1 = [sb("x1_%d" % i, [128, 1024], F32) for i in range(2)]
                junk = sb("junk", [128, 1024], F32)
                tmp = sb("tmpn", [128, 1024], F32)
                tmp2 = sb("tmpm", [128, 1024], F32)
                hb = [sb("hb%d" % i, [128, 1024], BF16) for i in range(2)]
                h2s = [sb("h2s%d" % i, [128, 8, 128], BF16) for i in range(2)]
                ssq = [sb("ssq%d" % i, [128, 1], F32) for i in range(2)]
                ms = [sb("ms%d" % i, [128, 1], F32) for i in range(2)]
                rstd = [sb("rstd%d" % i, [128, 1], F32) for i in range(2)]
                wov = W["w_out"][l].rearrange("(kc p) n -> p kc n", p=128)
                S.dma(wo[:], wov, w=["wo"], eng="pool")
                S.dma(gt1[:], mod_d[:, 2048:3072], r=["mod_d"], w=["modp"])
                S.dma(A2t[:], mod_d[:, 4096:5120], r=["mod_d"], w=["modp"])
                S.dma(sh2t[:], mod_d[:, 3072:4096], r=["mod_d"], w=["modp"])

                def load(t):
                    i = t % 2
                    S.dma(xb[i][:], xsrc[t * 128:(t + 1) * 128, :], r=["xs_d"], w=["xb%d" % i])
                    S.dma(mixT[i][:, 0:4, :], rwkvT_d[:, t * 128:(t + 1) * 128].rearrange("(c p) t -> p c t", p=128), r=["rwkvT_d"], w=["mixT%d" % i])
                    S.dma(mixT[i][:, 4:8, :], attT_d[:, t * 128:(t + 1) * 128].rearrange("(c p) t -> p c t", p=128), r=["attT_d"], w=["mixT%d" % i])
                load(0)
                for t in range(NT):
                    if t + 1 < NT:
                        load(t + 1)
                    i = t % 2
                    for hf in range(2):
                        bk, kb = nb()
                        for kc in range(8):
                            S.pe(MM(bk[:], mixT[i][:, kc, :], wo[:, kc, hf * 512:(hf + 1) * 512], kc == 0, kc == 7), r=["mixT%d" % i, "wo"], w=[kb])
                        csl = slice(hf * 512, (hf + 1) * 512)
                        S.dve(TT(tmp2[:, csl], bk[:], gt1[:, csl], ALU.mult), r=[kb, "modp"], w=["tmpm"])
                    S.pool(TT(x1[i][:], tmp2[:], xb[i][:], ALU.add), r=["tmpm", "xb%d" % i], w=["x1_%d" % i])
                    S.dma(xs_d[t * 128:(t + 1) * 128, :], x1[i][:], r=["x1_%d" % i], w=["xs_d2"])
                    norm_mod_T(x1[i][:], "x1_%d" % i, A2t[:], sh2t[:], hb[i], "hb%d" % i, junk, ssq[i], ms[i], rstd[i], tmp, i,
                               h2s[i][:], "h2s%d" % i)
                    S.dma(h2T_d[:, t * 128:(t + 1) * 128].rearrange("(kc p) t -> p kc t", p=128), h2s[i][:], r=["h2s%d" % i], w=["h2T_d"])
                S.flush()

        def phase_C(l, last):
            TB = 256
            NBC = T // TB
            with contextlib.ExitStack() as st:
                def sb(name, shape, dt):
                    return st.enter_context(SBT(name, shape, dt))
                w1 = sb("w1", [128, 8, 4096], BF16)
                w2 = sb("w2", [128, 32, 1024], BF16)
                gt2 = sb("gt2", [128, 1024], F32)
                fg = sb("fg", [128, 1024], F32)
                h2b = [sb("h2b%d" % i, [128, 8, TB], BF16) for i in range(2)]
                uT = sb("uT", [128, 32, TB], BF16)
                sq = [sb("sq%d" % i, [128, TB], F32) for i in range(2)]
                xb = [sb("xb%d" % i, [128, 1024], F32) for i in range(2)]
                x2 = [sb("x2_%d" % i, [128, 1024], F32) for i in range(2)]
                tmp2 = sb("tmpm", [128, 1024], F32)
                junk = sb("junk", [128, 1024], F32)
                ssq = [sb("ssq%d" % i, [128, 1], F32) for i in range(2)]
                ms = [sb("ms%d" % i, [128, 1], F32) for i in range(2)]
                rstd = [sb("rstd%d" % i, [128, 1], F32) for i in range(2)]
                w1v = W["w_mlp1"][l].rearrange("(kc p) n -> p kc n", p=128)
                w2v = W["w_mlp2"][l].rearrange("(fc p) n -> p fc n", p=128)
                for q in range(4):
                    S.dma(w1[:, :, q * 1024:(q + 1) * 1024], w1v[:, :, q * 1024:(q + 1) * 1024], w=["w1"], eng="pool")
                for q in range(4):
                    S.dma(w2[:, q * 8:(q + 1) * 8, :], w2v[:, q * 8:(q + 1) * 8, :], w=["w2"], eng="pool")
                S.dma(gt2[:], mod_d[:, 5120:6144], r=["mod_d"], w=["modp"])
                if last:
                    S.dma(fg[:], W["final_g"].partition_broadcast(128), w=["fg"])

                def load(bb):
                    i = bb % 2
                    S.dma(h2b[i][:], h2T_d[:, bb * TB:(bb + 1) * TB].rearrange("(kc p) t -> p kc t", p=128), r=["h2T_d"], w=["h2b%d" % i])
                load(0)
                xi = [0]
                for bb in range(NBC):
                    if bb + 1 < NBC:
                        load(bb + 1)
                    i = bb % 2
                    for fc in range(32):
                        bk, kb = nb()
                        for kc in range(8):
                            S.pe(MM(bk[:, 0:TB], w1[:, kc, fc * 128:(fc + 1) * 128], h2b[i][:, kc, :], kc == 0, kc == 7), r=["w1", "h2b%d" % i], w=[kb])
                        s_ = sq[fc % 2]
                        ks_ = "sq%d" % (fc % 2)
                        S.act(ACTF(s_[:], bk[:, 0:TB], AF.Square), r=[kb], w=[ks_])
                        S.dve(STT(uT[:, fc, :], bk[:, 0:TB], 0.0, s_[:], ALU.is_gt, ALU.mult), r=[kb, ks_], w=["uT"])
                    for jj in range(TB // 128):
                        t = bb * (TB // 128) + jj
                        xi_ = xi[0] % 2
                        xi[0] += 1
                        S.dma(xb[xi_][:], xs_d[t * 128:(t + 1) * 128, :], r=["xs_d"], w=["xb%d" % xi_])
                        for hf in range(2):
                            bk, kb = nb()
                            for fc in range(32):
                                S.pe(MM(bk[:], uT[:, fc, jj * 128:(jj + 1) * 128], w2[:, fc, hf * 512:(hf + 1) * 512], fc == 0, fc == 31), r=["uT", "w2"], w=[kb])
                            csl = slice(hf * 512, (hf + 1) * 512)
                            S.dve(TT(tmp2[:, csl], bk[:], gt2[:, csl], ALU.mult), r=[kb, "modp"], w=["tmpm"])
                        S.pool(TT(x2[xi_][:], tmp2[:], xb[xi_][:], ALU.add), r=["tmpm", "xb%d" % xi_], w=["x2_%d" % xi_])
                        if not last:
                            S.dma(xs_d[t * 128:(t + 1) * 128, :], x2[xi_][:], r=["x2_%d" % xi_], w=["xs_d2"])
                        else:
                            S.act(ACTF(junk[:], x2[xi_][:], AF.Square, accum=ssq[xi_][:]), r=["x2_%d" % xi_], w=["ssq%d" % xi_])
                            S.dve(TS(ms[xi_][:], ssq[xi_][:], 1.0 / 1024, 1e-6, ALU.mult, ALU.add), r=["ssq%d" % xi_], w=["ms%d" % xi_])
                            S.pool(TT(rstd[xi_][:], ms[xi_][:], m05[:, 0:1], ALU.pow), r=["ms%d" % xi_, "m05"], w=["rstd%d" % xi_])
                            S.dve(STT(x2[xi_][:], x2[xi_][:], rstd[xi_][:, 0:1], fg[:], ALU.mult, ALU.mult), r=["x2_%d" % xi_, "rstd%d" % xi_, "fg"], w=["x2_%d" % xi_])
                            S.dma(out_d[t * 128:(t + 1) * 128, :], x2[xi_][:], r=["x2_%d" % xi_], w=["out_d"])
                S.flush()

        order = []
        for l in range(L):
            order += [("M", l), ("A1", l), ("A2", l), ("B", l), ("B2", l), ("C", l)]
        for ph, l in order:
            xsrc = x_d if l == 0 else xs_d
            if ph == "M":
                phase_M(l)
            elif ph == "A1":
                phase_A1(l, xsrc)
            elif ph == "A2":
                try:
                    phase_A2(l)
                except _Stop:
                    S.flush()
                    break
            elif ph == "B":
                phase_B(l)
            elif ph == "B2":
                phase_B2(l, xsrc)
            elif ph == "C":
                phase_C(l, l == L - 1)
            if stop_after == (ph, l):
                break
        S.flush(final=True)
        nops = S.nops
    return nc, nops


def _t5_bucket(n):
    n = np.maximum(n, 0)
    nf = np.maximum(n, 1).astype(np.float32)
    large = 16 + (np.log(nf / np.float32(16)) / np.float32(math.log(128 / 16)) * np.float32(16)).astype(np.int32)
    large = np.minimum(large, 31)
    return np.where(n < 16, n, large)


def _consts():
    s = np.arange(128)[:, None]
    t = np.arange(128)[None, :]
    c = np.zeros((128, 640), np.float32)
    c[:, 0:128] = (s == t)
    c[:, 128:256] = (s <= t)
    c[:, 256:384] = (s < t)
    c[:, 384:512] = (s > t)
    c[:, 512:640] = np.where(t <= s, 0.0, -1e30)
    return c


def host_inputs(inputs, T, L):
    B = inputs["x"].shape[0]
    f = lambda a: np.ascontiguousarray(np.asarray(a, dtype=np.float32))
    rel_bias = f(inputs["rel_bias"])
    tk = np.arange(128)[:, None, None, None]
    d = np.arange(2)[None, None, :, None]
    tq = np.arange(128)[None, None, None, :]
    hh = np.arange(8)[None, :, None, None]
    bidx = _t5_bucket(128 * d + tq - tk) + 0 * hh
    bnear = rel_bias[bidx, hh + 0 * bidx].reshape(128, 2048)
    shared = {"consts": _consts(), "bnear": f(bnear), "c31": f(rel_bias[31:32, :])}
    for k in WSHAPES:
        shared[k] = f(inputs[k])[:L]
    for k in VSHAPES:
        shared[k] = f(inputs[k])[:max(L - 1, 1)]
    shared["final_g"] = f(inputs["final_g"]).reshape(1, 1024)
    maps = []
    for bi in range(B):
        m = dict(shared)
        m["x"] = f(inputs["x"][bi, :T])
        m["c8"] = f(np.asarray(inputs["c"][bi]).reshape(8, 128).T)
        maps.append(m)
    return maps


_CACHE = {}


def kernel(**inputs):
    T = inputs["x"].shape[1]
    L = inputs["w_ada"].shape[0]
    B = inputs["x"].shape[0]
    key = (T, L)
    if key not in _CACHE:
        _CACHE[key] = build(T, L)[0]
    nc = _CACHE[key]
    maps = host_inputs(inputs, T, L)
    res = run_bass_kernel_spmd(nc, maps, core_ids=list(range(B)))
    return np.stack([np.asarray(r["out"], dtype=np.float32) for r in res.results], axis=0)
```

```python
import contextlib
import os
import math
import numpy as np
import ml_dtypes
import concourse.bass as bass
import concourse.mybir as mybir
from concourse.bass_utils import run_bass_kernel_spmd

F32 = mybir.dt.float32
BF16 = mybir.dt.bfloat16
AF = mybir.ActivationFunctionType
ALU = mybir.AluOpType
AX = mybir.AxisListType

ENG = ("pe", "dve", "act", "pool", "sp")
NDMA = 12
NBIS = 16


class _Stop(Exception):
    pass


_DEAD = [False]


def chk(n):
    if int(os.environ.get("A2STOP", "0")) == n:
        _DEAD[0] = True


class Op:
    __slots__ = ("eng", "fn", "deps", "signal", "sig_val", "dma", "dma_slot", "dma_val", "sem")

    def __init__(self, eng, fn, dma):
        self.eng = eng
        self.fn = fn
        self.deps = []
        self.signal = False
        self.sig_val = None
        self.dma = dma
        self.dma_slot = None
        self.dma_val = None
        self.sem = None


class Sched:
    def __init__(self, nc, stack):
        self.nc = nc
        self.stack = stack
        self.nsw = 0
        self.sw_dmas = []
        self.esem = {e: stack.enter_context(nc.semaphore("s_" + e)) for e in ENG if e != "sp"}
        self.dsem = [stack.enter_context(nc.semaphore("d_%d" % i)) for i in range(NDMA)]
        self.ops = {e: [] for e in ENG}
        self.last_w = {}
        self.readers = {}
        self.dma_count = 0
        self.dma_last = [None] * NDMA
        self.sigc = {e: 0 for e in ENG}
        self.waited = {e: {} for e in ENG}
        self.bar = {e: [] for e in ENG}
        self.nops = 0

    def _dep(self, op, prod):
        if prod is None or prod is op:
            return
        if (not prod.dma) and (not op.dma) and prod.eng == op.eng == "pe":
            return
        if not prod.dma:
            prod.signal = True
        op.deps.append(prod)

    def add(self, eng, fn, r=(), w=(), dma=False):
        op = Op(eng, fn, dma)
        if _DEAD[0]:
            return op
        if self.bar[eng]:
            for p in self.bar[eng]:
                op.deps.append(p)
            self.bar[eng] = []
        for k in r:
            self._dep(op, self.last_w.get(k))
        for k in w:
            self._dep(op, self.last_w.get(k))
            for rd in self.readers.get(k, ()):
                self._dep(op, rd)
        for k in r:
            self.readers.setdefault(k, []).append(op)
        for k in w:
            self.last_w[k] = op
            self.readers[k] = []
        if dma and eng == "pool":
            op.sem = self.stack.enter_context(self.nc.semaphore("w_%d" % self.nsw))
            op.dma_slot = "w%d" % self.nsw
            self.nsw += 1
            op.dma_val = 16
            self.sw_dmas.append(op)
        elif dma:
            slot = self.dma_count % NDMA
            self.dma_count += 1
            prev = self.dma_last[slot]
            op.dma_slot = slot
            op.sem = self.dsem[slot]
            op.dma_val = (prev.dma_val if prev else 0) + 16
            if prev is not None:
                op.deps.append(prev)
            self.dma_last[slot] = op
        self.ops[eng].append(op)
        self.nops += 1
        return op

    def pe(self, fn, r=(), w=()):
        return self.add("pe", fn, r, w)

    def dve(self, fn, r=(), w=()):
        return self.add("dve", fn, r, w)

    def act(self, fn, r=(), w=()):
        return self.add("act", fn, r, w)

    def pool(self, fn, r=(), w=()):
        return self.add("pool", fn, r, w)

    def dma(self, out, in_, r=(), w=(), eng="sp"):
        return self.add(eng, lambda e: e.dma_start(out=out, in_=in_), r, w, dma=True)

    def flush(self, final=False):
        nc = self.nc
        lasts = []
        for e in ENG:
            nd = [op for op in self.ops[e] if not op.dma]
            if nd:
                nd[-1].signal = True
                lasts.append(nd[-1])
        for e in ENG:
            for op in self.ops[e]:
                if op.signal and not op.dma:
                    self.sigc[e] += 1
                    op.sig_val = self.sigc[e]
        dlast = [p for p in self.dma_last if p is not None] + self.sw_dmas
        self.sw_dmas = []
        with nc.Block() as block:
            engobj = {"pe": block.tensor, "dve": block.vector, "act": block.scalar,
                      "pool": block.gpsimd, "sp": block.sync}
            for ename in ENG:
                ops = self.ops[ename]
                if not ops and not (final and ename == "sp"):
                    continue

                def body(e, ops=ops, ename=ename):
                    waited = self.waited[ename]
                    semof = {}
                    for op in ops:
                        need = {}
                        for p in op.deps:
                            if p.dma:
                                key = ("d", p.dma_slot)
                                semof[key] = p.sem
                                val = p.dma_val
                            else:
                                key = ("e", p.eng)
                                val = p.sig_val
                            if need.get(key, 0) < val:
                                need[key] = val
                        for key, val in need.items():
                            if waited.get(key, 0) >= val:
                                continue
                            waited[key] = val
                            sem = semof[key] if key[0] == "d" else self.esem[key[1]]
                            e.wait_ge(sem, val)
                        ins = op.fn(e)
                        if op.dma:
                            ins.then_inc(op.sem, 16)
                        elif op.signal:
                            ins.then_inc(self.esem[ename], 1)
                    if final and ename == "sp":
                        for p in dlast:
                            if waited.get(("d", p.dma_slot), 0) < p.dma_val:
                                e.wait_ge(p.sem, p.dma_val)
                        for p in lasts:
                            e.wait_ge(self.esem[p.eng], p.sig_val)
                engobj[ename](body)
        barrier = lasts + dlast
        self.ops = {e: [] for e in ENG}
        self.last_w = {}
        self.readers = {}
        self.bar = {e: list(barrier) for e in ENG}


def MM(out, lhsT, rhs, start=True, stop=True):
    return lambda e: e.matmul(out, lhsT=lhsT, rhs=rhs, start=start, stop=stop)


def TR(out, in_, ident):
    return lambda e: e.transpose(out, in_, ident)


def ACTF(out, in_, func, bias=0.0, scale=1.0, accum=None):
    if accum is None:
        return lambda e: e.activation(out=out, in_=in_, func=func, bias=bias, scale=scale)
    return lambda e: e.activation(out=out, in_=in_, func=func, bias=bias, scale=scale, accum_out=accum)


def TT(out, a, b, op):
    return lambda e: e.tensor_tensor(out=out, in0=a, in1=b, op=op)


def TS(out, a, s1, s2=None, op0=ALU.mult, op1=None, accum=None):
    if accum is not None:
        return lambda e: e.tensor_scalar(out=out, in0=a, scalar1=s1, scalar2=s2, op0=op0, op1=op1, accum_out=accum)
    if op1 is None:
        return lambda e: e.tensor_scalar(out=out, in0=a, scalar1=s1, scalar2=None, op0=op0)
    return lambda e: e.tensor_scalar(out=out, in0=a, scalar1=s1, scalar2=s2, op0=op0, op1=op1)


def STT(out, a, s, b, op0, op1):
    return lambda e: e.scalar_tensor_tensor(out=out, in0=a, scalar=s, in1=b, op0=op0, op1=op1)


def CP(out, in_):
    return lambda e: e.tensor_copy(out, in_)


def RED(out, in_, op, axis=AX.X, absval=False):
    if absval:
        return lambda e: e.tensor_reduce(out=out, in_=in_, axis=axis, op=op, apply_absolute_value=True)
    return lambda e: e.tensor_reduce(out=out, in_=in_, axis=axis, op=op)


def MEMSET(ap, v):
    return lambda e: e.memset(ap, v)


WSHAPES = {
    "w_ada": (1024, 6144), "b_ada": (6144,), "norm1_g": (1024,), "norm2_g": (1024,),
    "w_in": (1024, 3656), "mu_rkv": (3, 512), "mu_lora": (3, 1024), "decay_w0": (512,),
    "decay_a": (1024, 64), "decay_b": (64, 512), "iclr_a0": (512,), "iclr_a": (1024, 64),
    "iclr_b": (64, 512), "gate_a": (1024, 128), "gate_b": (128, 512), "k_k": (512,), "k_a": (512,),
    "r_k": (8, 64), "lnx_g": (512,), "lnx_b": (512,), "attn_out_g": (512,),
    "w_out": (1024, 1024), "w_mlp1": (1024, 4096), "w_mlp2": (4096, 1024),
}
VSHAPES = {"vres_mu": (1024,), "vres_v0": (512,), "vres_a": (1024, 32), "vres_b": (32, 512)}


def build(T, L, dbg=False, stop_after=None):
    _DEAD[0] = False
    NT = T // 128
    NB = T // 512
    NSEL = min(256, T // 4)
    nc = bass.Bass("TRN2", target_bir_lowering=False)
    W = {}

    def din(name, shape):
        W[name] = nc.dram_tensor(name, list(shape), F32, kind="ExternalInput").ap()
        return W[name]

    x_d = din("x", [T, 1024])
    c8_d = din("c8", [128, 8])
    consts_d = din("consts", [128, 640])
    bnear_d = din("bnear", [128, 2048])
    c31_d = din("c31", [1, 8])
    for k, s in WSHAPES.items():
        din(k, (L,) + s)
    for k, s in VSHAPES.items():
        din(k, (max(L - 1, 1),) + s)
    din("final_g", [1, 1024])
    out_d = nc.dram_tensor("out", [T, 1024], F32, kind="ExternalOutput").ap()

    def dscr(name, shape, dt):
        return nc.dram_tensor(name, list(shape), dt, kind=("ExternalOutput" if dbg else "Internal")).ap()

    xs_d = dscr("xs", [T, 1024], F32)
    mod_d = dscr("modd", [128, 6144], F32)
    featT_d = dscr("featT", [1600, T], BF16)
    vaug_d = dscr("vaug", [T, 520], BF16)
    wi_d = dscr("wid", [T, 8], F32)
    hT_d = dscr("hTd", [1024, T], BF16)
    vfirst_d = dscr("vfirst", [T, 512], F32)
    rwkvT_d = dscr("rwkvT", [512, T], BF16)
    attT_d = dscr("attT", [512, T], BF16)
    h2T_d = dscr("h2T", [1024, T], BF16)
    dbg_d = {}
    if dbg:
        for nm, shp in (("dbg_y", [T, 512]), ("dbg_score", [T, T]), ("dbg_thr", [T, 1]), ("dbg_v", [T, 512]),
                        ("dbg_r", [T, 512]), ("dbg_k", [T, 512]), ("dbg_a", [T, 512]), ("dbg_lw", [T, 512])):
            dbg_d[nm] = nc.dram_tensor(nm, shp, F32, kind="ExternalOutput").ap()

    uniq = [0]

    def SBT(name, shape, dt):
        uniq[0] += 1
        return nc.sbuf_tensor("%s_%d" % (name, uniq[0]), shape, dt)

    with contextlib.ExitStack() as gst:
        S = Sched(nc, gst)

        def gsb(name, shape, dt):
            return gst.enter_context(SBT(name, shape, dt))

        banks = [gst.enter_context(nc.psum_tensor("bank%d" % i, [128, 512], F32)) for i in range(8)]
        bank_i = [0]

        NROT = 6

        nrot = [8]

        def nb():
            i = bank_i[0] % nrot[0]
            bank_i[0] += 1
            return banks[i], "bank%d" % i

        def bfv(bank):
            return bank[:].bitcast(BF16)

        cst = gsb("cst", [128, 640], F32)
        identb = gsb("identb", [128, 128], BF16)
        mask4 = gsb("mask4", [128, 512], F32)
        lowm4 = gsb("lowm4", [128, 512], F32)
        onesf = gsb("onesf", [128, 128], F32)
        m05 = gsb("m05", [128, 512], F32)
        cbc = gsb("cbc", [128, 8, 128], F32)
        identf = cst[:, 0:128]
        tri_incl = cst[:, 128:256]
        tri_strict = cst[:, 256:384]
        low_strict = cst[:, 384:512]
        caus = cst[:, 512:640]

        with contextlib.ExitStack() as st:
            c8 = st.enter_context(SBT("c8s", [128, 8], F32))
            c8t = st.enter_context(SBT("c8t", [128, 8], F32))
            S.dma(cst[:], consts_d, w=["cst"])
            S.dma(c8[:], c8_d, w=["c8"])
            S.dve(CP(identb[:], identf), r=["cst"], w=["identb"])
            for i in range(4):
                S.dve(CP(mask4[:, i * 128:(i + 1) * 128], tri_strict if i % 2 == 0 else tri_incl), r=["cst"], w=["mask4"])
                S.pool(CP(lowm4[:, i * 128:(i + 1) * 128], low_strict), r=["cst"], w=["lowm4"])
            S.pool(MEMSET(onesf[:], 1.0), w=["onesf"])
            S.pool(MEMSET(m05[:], -0.5), w=["m05"])
            S.act(ACTF(c8t[:], c8[:], AF.Tanh, scale=0.5), r=["c8"], w=["c8t"])
            S.dve(TS(c8t[:], c8t[:], 0.5, 0.5, ALU.mult, ALU.add), r=["c8t"], w=["c8t"])
            S.dve(TT(c8t[:], c8t[:], c8[:], ALU.mult), r=["c8t", "c8"], w=["c8t"])
            S.dve(CP(cbc[:], c8t[:].unsqueeze(2).to_broadcast([128, 8, 128])), r=["c8t"], w=["cbc"])
            S.flush()

        def phase_M(l):
            with contextlib.ExitStack() as st:
                def sb(name, shape, dt):
                    return st.enter_context(SBT(name, shape, dt))
                wst = [sb("wst%d" % i, [128, 8, 512], F32) for i in range(2)]
                modt = sb("modt", [128, 6144], F32)
                gbc = sb("gbc", [128, 2048], F32)
                bada = sb("bada", [1, 6144], F32)
                S.dma(gbc[:, 0:1024], W["norm1_g"][l:l + 1, :].partition_broadcast(128), w=["gbc"])
                S.dma(gbc[:, 1024:2048], W["norm2_g"][l:l + 1, :].partition_broadcast(128), w=["gbc"])
                S.dma(bada[:], W["b_ada"][l:l + 1, :], w=["bada"])
                wa = W["w_ada"][l].rearrange("(kc p) n -> p kc n", p=128)
                for n in range(12):
                    buf = wst[n % 2]
                    key = "wst%d" % (n % 2)
                    S.dma(buf[:], wa[:, :, n * 512:(n + 1) * 512], w=[key])
                    bk, kb = nb()
                    for kc in range(8):
                        S.pe(MM(bk[:], cbc[:, kc, :], buf[:, kc, :], kc == 0, False), r=[key, "cbc"], w=[kb])
                    S.pe(MM(bk[:], onesf[0:1, :], bada[0:1, n * 512:(n + 1) * 512], False, True), r=["bada", "onesf"], w=[kb])
                    S.act(ACTF(modt[:, n * 512:(n + 1) * 512], bk[:], AF.Copy), r=[kb], w=["modt"])
                S.dve(STT(modt[:, 1024:2048], modt[:, 1024:2048], 1.0, gbc[:, 0:1024], ALU.add, ALU.mult), r=["modt", "gbc"], w=["modt"])
                S.dve(STT(modt[:, 4096:5120], modt[:, 4096:5120], 1.0, gbc[:, 1024:2048], ALU.add, ALU.mult), r=["modt", "gbc"], w=["modt"])
                S.dma(mod_d, modt[:], r=["modt"], w=["mod_d"])
                S.flush()

        def norm_mod_T(X, kx, At, sht, hb, khb, junk, ssq, ms, rstd, tmp, idx, dstT, kdst):
            S.act(ACTF(junk[:], X, AF.Square, accum=ssq[:]), r=[kx], w=["ssq%d" % idx])
            S.dve(TS(ms[:], ssq[:], 1.0 / 1024, 1e-6, ALU.mult, ALU.add), r=["ssq%d" % idx], w=["ms%d" % idx])
            S.pool(TT(rstd[:], ms[:], m05[:, 0:1], ALU.pow), r=["ms%d" % idx, "m05"], w=["rstd%d" % idx])
            S.dve(STT(tmp[:], X, rstd[:, 0:1], At, ALU.mult, ALU.mult), r=[kx, "rstd%d" % idx, "modp"], w=["tmpn"])
            S.pool(TT(hb[:], tmp[:], sht, ALU.add), r=["tmpn", "modp"], w=[khb])
            bk, kb = nb()
            bkb = bfv(bk)
            for kc in range(8):
                S.pe(TR(bkb[:, kc * 128:(kc + 1) * 128], hb[:, kc * 128:(kc + 1) * 128], identb[:]), r=[khb, "identb"], w=[kb])
            S.act(ACTF(dstT, bkb.rearrange("p (k t) -> p k t", k=8), AF.Copy), r=[kb], w=[kdst])

        def phase_A1(l, xsrc):
            with contextlib.ExitStack() as st:
                def sb(name, shape, dt):
                    return st.enter_context(SBT(name, shape, dt))
                WF = sb("WF", [128, 8, 1600], BF16)
                WV = sb("WV", [128, 8, 520], BF16)
                A1t = sb("A1t", [128, 1024], F32)
                sh1t = sb("sh1t", [128, 1024], F32)
                xb = [sb("xb%d" % i, [128, 1024], F32) for i in range(2)]
                junk = sb("junk", [128, 1024], F32)
                tmp = sb("tmpn", [128, 1024], F32)
                hb = [sb("hb%d" % i, [128, 1024], BF16) for i in range(2)]
                hT = [sb("hT%d" % i, [128, 8, 512], BF16) for i in range(2)]
                fst = [sb("fst%d" % i, [128, 512], BF16) for i in range(2)]
                vst = [sb("vst%d" % i, [128, 8, 65], BF16) for i in range(2)]
                wist = [sb("wist%d" % i, [128, 8], F32) for i in range(2)]
                ssq = [sb("ssq%d" % i, [128, 1], F32) for i in range(2)]
                ms = [sb("ms%d" % i, [128, 1], F32) for i in range(2)]
                rstd = [sb("rstd%d" % i, [128, 1], F32) for i in range(2)]
                wv = W["w_in"][l].rearrange("(kc p) n -> p kc n", p=128)
                S.dma(WF[:, :, 0:1024], wv[:, :, 1536:2560], w=["WF"], eng="pool")
                S.dma(WF[:, :, 1024:1600], wv[:, :, 3072:3648], w=["WF"], eng="pool")
                S.dma(WV[:, :, 0:512], wv[:, :, 2560:3072], w=["WV"], eng="pool")
                S.dma(WV[:, :, 512:520], wv[:, :, 3648:3656], w=["WV"], eng="pool")
                S.dma(A1t[:], mod_d[:, 1024:2048], r=["mod_d"], w=["modp"])
                S.dma(sh1t[:], mod_d[:, 0:1024], r=["mod_d"], w=["modp"])
                for i in range(2):
                    S.pool(MEMSET(vst[i][:, :, 64:65], 1.0), w=["vst%d" % i])

                def load(t):
                    S.dma(xb[t % 2][:], xsrc[t * 128:(t + 1) * 128, :], r=["xs_d"], w=["xb%d" % (t % 2)])
                load(0)
                for b in range(NB):
                    hTb = hT[b % 2]
                    kh = "hT%d" % (b % 2)
                    for j in range(4):
                        t = b * 4 + j
                        if t + 1 < NT:
                            load(t + 1)
                        i2 = t % 2
                        norm_mod_T(xb[i2][:], "xb%d" % i2, A1t[:], sh1t[:], hb[i2], "hb%d" % i2, junk, ssq[i2], ms[i2],
                                   rstd[i2], tmp, i2, hTb[:, :, j * 128:(j + 1) * 128], kh)
                        bk, kb = nb()
                        bk2, kb2 = nb()
                        for kc in range(8):
                            S.pe(MM(bk[:], hTb[:, kc, j * 128:(j + 1) * 128], WV[:, kc, 0:512], kc == 0, kc == 7), r=[kh, "WV"], w=[kb])
                        for kc in range(8):
                            S.pe(MM(bk2[:, 0:8], hTb[:, kc, j * 128:(j + 1) * 128], WV[:, kc, 512:520], kc == 0, kc == 7), r=[kh, "WV"], w=[kb2])
                        S.dve(CP(vst[i2][:, :, 0:64], bk[:].rearrange("p (h d) -> p h d", h=8)), r=[kb], w=["vst%d" % i2])
                        S.act(ACTF(wist[i2][:], bk2[:, 0:8], AF.Copy), r=[kb2], w=["wist%d" % i2])
                        S.dma(vaug_d[t * 128:(t + 1) * 128, :], vst[i2][:].rearrange("p h d -> p (h d)"), r=["vst%d" % i2], w=["vaug_d"])
                        S.dma(wi_d[t * 128:(t + 1) * 128, :], wist[i2][:], r=["wist%d" % i2], w=["wi_d"])
                    for c in range(13):
                        rows = 128 if c < 12 else 64
                        bk, kb = nb()
                        for kc in range(8):
                            S.pe(MM(bk[0:rows, :], WF[:, kc, c * 128:c * 128 + rows], hTb[:, kc, :], kc == 0, kc == 7), r=[kh, "WF"], w=[kb])
                        f = fst[c % 2]
                        kf = "fst%d" % (c % 2)
                        if c % 2 == 0:
                            S.act(ACTF(f[0:rows, :], bk[0:rows, :], AF.Copy), r=[kb], w=[kf])
                        else:
                            S.dve(CP(f[0:rows, :], bk[0:rows, :]), r=[kb], w=[kf])
                        S.dma(featT_d[c * 128:c * 128 + rows, b * 512:(b + 1) * 512], f[0:rows, :], r=[kf], w=["featT_d"])
                    S.dma(hT_d[:, b * 512:(b + 1) * 512].rearrange("(kc p) t -> p kc t", p=128), hTb[:], r=[kh], w=["hT_d"])
                S.flush()

        def phase_A2(l):
            with contextlib.ExitStack() as st:
                def sb(name, shape, dt):
                    return st.enter_context(SBT(name, shape, dt))
                WT1 = sb("WT1", [128, 8, 1536], BF16)
                WT2 = sb("WT2", [128, 8, 1536], BF16)
                LA1 = sb("LA1", [128, 8, 288], BF16)
                LA2 = sb("LA2", [128, 8, 288], BF16)
                Bwa = sb("Bwa", [128, 512], BF16)
                Bg = sb("Bg", [128, 512], BF16)
                Bv = sb("Bv", [32, 512], BF16)
                brow = sb("brow", [1, 3, 512], F32)
                pbc = sb("pbc", [128, 5, 512], F32)
                muT = sb("muT", [128, 32], F32)
                omT = sb("omT", [128, 32], F32)
                with contextlib.ExitStack() as st2:
                    Wr = st2.enter_context(SBT("Wr", [128, 8, 1536], BF16))
                    mubc = st2.enter_context(SBT("mubc", [128, 1536], F32))
                    ombc = st2.enter_context(SBT("ombc", [128, 1536], F32))
                    LA = st2.enter_context(SBT("LA", [128, 8, 288], F32))
                    mu32 = st2.enter_context(SBT("mu32", [32, 128], F32))
                    wv = W["w_in"][l].rearrange("(kc p) n -> p kc n", p=128)
                    S.dma(Wr[:, :, 0:768], wv[:, :, 0:768], w=["Wr"], eng="pool")
                    S.dma(Wr[:, :, 768:1536], wv[:, :, 768:1536], w=["Wr"], eng="pool")
                    S.dma(mubc[:], W["mu_rkv"][l:l + 1].rearrange("o a b -> o (a b)").partition_broadcast(128), w=["mubc"])
                    S.dve(TS(ombc[:], mubc[:], -1.0, 1.0, ALU.mult, ALU.add), r=["mubc"], w=["ombc"])
                    S.dve(TT(WT2[:], Wr[:], mubc[:].unsqueeze(1).to_broadcast([128, 8, 1536]), ALU.mult), r=["Wr", "mubc"], w=["WT2"])
                    S.pool(TT(WT1[:], Wr[:], ombc[:].unsqueeze(1).to_broadcast([128, 8, 1536]), ALU.mult), r=["Wr", "ombc"], w=["WT1"])
                    S.dma(LA[:, :, 0:64], W["decay_a"][l].rearrange("(kc p) n -> p kc n", p=128), w=["LA"])
                    S.dma(LA[:, :, 64:128], W["iclr_a"][l].rearrange("(kc p) n -> p kc n", p=128), w=["LA"])
                    S.dma(LA[:, :, 128:256], W["gate_a"][l].rearrange("(kc p) n -> p kc n", p=128), w=["LA"])
                    if l > 0:
                        S.dma(LA[:, :, 256:288], W["vres_a"][l - 1].rearrange("(kc p) n -> p kc n", p=128), w=["LA"])
                    else:
                        S.dve(MEMSET(LA[:, :, 256:288], 0.0), w=["LA"])
                    S.dve(MEMSET(mu32[:], 0.0), w=["mu32"])
                    S.dma(mu32[0:24, :], W["mu_lora"][l].rearrange("a (kc p) -> (a kc) p", p=128), r=["mu32"], w=["mu32"])
                    if l > 0:
                        S.dma(mu32[24:32, :], W["vres_mu"][l - 1:l, :].rearrange("o (kc p) -> (o kc) p", p=128), r=["mu32"], w=["mu32"])
                    bk, kb = nb()
                    S.pe(MM(bk[:, 0:32], mu32[:], identf[0:32, 0:32], True, True), r=["mu32", "cst"], w=[kb])
                    S.act(ACTF(muT[:], bk[:, 0:32], AF.Copy), r=[kb], w=["muT"])
                    S.dve(TS(omT[:], muT[:], -1.0, 1.0, ALU.mult, ALU.add), r=["muT"], w=["omT"])
                    for gi, (c0, c1) in enumerate(((0, 64), (64, 128), (128, 256), (256, 288))):
                        wdt = c1 - c0
                        S.dve(TT(LA2[:, :, c0:c1], LA[:, :, c0:c1], muT[:, gi * 8:(gi + 1) * 8].unsqueeze(2).to_broadcast([128, 8, wdt]), ALU.mult), r=["LA", "muT"], w=["LA2"])
                        S.dve(TT(LA1[:, :, c0:c1], LA[:, :, c0:c1], omT[:, gi * 8:(gi + 1) * 8].unsqueeze(2).to_broadcast([128, 8, wdt]), ALU.mult), r=["LA", "omT"], w=["LA1"])
                    S.dma(Bwa[0:64, :], W["decay_b"][l], w=["Bwa"], eng="pool")
                    S.dma(Bwa[64:128, :], W["iclr_b"][l], w=["Bwa"], eng="pool")
                    S.dma(Bg[:], W["gate_b"][l], w=["Bg"], eng="pool")
                    if l > 0:
                        S.dma(Bv[:], W["vres_b"][l - 1], w=["Bv"], eng="pool")
                    S.dma(brow[:, 0, :], W["decay_w0"][l:l + 1, :], w=["brow"])
                    S.dma(brow[:, 1, :], W["iclr_a0"][l:l + 1, :], w=["brow"])
                    if l > 0:
                        S.dma(brow[:, 2, :], W["vres_v0"][l - 1:l, :], w=["brow"])
                    S.dma(pbc[:, 0, :], W["k_k"][l:l + 1, :].partition_broadcast(128), w=["pbc"])
                    S.dma(pbc[:, 1, :], W["k_a"][l:l + 1, :].partition_broadcast(128), w=["pbc"])
                    S.dma(pbc[:, 2, :], W["r_k"][l:l + 1].rearrange("o a b -> o (a b)").partition_broadcast(128), w=["pbc"])
                    S.dma(pbc[:, 3, :], W["lnx_g"][l:l + 1, :].partition_broadcast(128), w=["pbc"])
                    S.dma(pbc[:, 4, :], W["lnx_b"][l:l + 1, :].partition_broadcast(128), w=["pbc"])
                    S.flush()

                chk(1)
                hTb2 = [sb("hTb%d" % i, [128, 8, 512], BF16) for i in range(1)]
                hTp2 = [sb("hTp%d" % i, [128, 8, 512], BF16) for i in range(1)]
                L1wa = sb("L1wa", [128, 512], BF16)
                sg = sb("sg", [128, 512], BF16)
                sgt = sb("sgt", [128, 512], F32)
                L1v = sb("L1v", [32, 512], BF16)

                def f32t(name):
                    return sb(name, [128, 512], F32)

                def b16t(name):
                    return sb(name, [128, 512], BF16)
                r32, k32, v32 = f32t("r32"), f32t("k32"), f32t("v32")
                kkr, kk, a32, b32, km = f32t("kkr"), f32t("kk"), f32t("a32"), f32t("b32"), f32t("km")
                lw, Ginc, Ginv, Gexc, g32 = f32t("lw"), f32t("Ginc"), f32t("Ginv"), f32t("Gexc"), f32t("g32")
                t1, t2, U0, cen, vf = f32t("t1"), f32t("t2"), f32t("U0"), f32t("cen"), f32t("vf")
                y32 = f32t("y32")
                Kd, Rd, Bi, Ki, Vb, Zb, Ub, ob = (b16t(n) for n in ("Kd", "Rd", "Bi", "Ki", "Vb", "Zb", "Ub", "ob"))
                s8 = [sb("s8_%d" % i, [128, 8], F32) for i in range(6)]
                FT = sb("FT", [64, 8, 4, 128], BF16)
                GE = sb("GE", [128, 8, 512], BF16)
                MMb = [sb("MMb%d" % i, [128, 8, 128], F32) for i in range(2)]
                NNb = [sb("NNb%d" % i, [128, 8, 128], F32) for i in range(2)]
                TTb = [sb("TTb%d" % i, [128, 8, 128], F32) for i in range(2)]
                TTh = sb("TTh", [128, 8, 128], BF16)
                WTs = sb("WTs", [64, 8, 128], BF16)
                P32 = sb("P32", [64, 8, 64], F32)
                Pb = sb("Pb", [64, 8, 64], BF16)
                GC = sb("GC", [64, 8], F32)
                rwT = sb("rwT", [128, 4, 128], BF16)
                S.dve(MEMSET(P32[:], 0.0), w=["P32"])
                S.dve(MEMSET(Pb[:], 0.0), w=["Pb"])
                P32f = P32[:].rearrange("p h v -> p (h v)")
                ev = [0]

                def evac(out, in_, r, w):
                    ev[0] += 1
                    if ev[0] % 2:
                        S.act(ACTF(out, in_, AF.Copy), r=r, w=w)
                    else:
                        S.dve(CP(out, in_), r=r, w=w)

                for b in range(NB):
                    hTb = hTb2[0]
                    kh = "hTb0"
                    src = hT_d.rearrange("(kc p) t -> p kc t", p=128)
                    if b == 1:
                        chk(8)
                    hTp = hTp2[0]
                    S.dma(hTb[:], src[:, :, b * 512:(b + 1) * 512], r=["hT_d"], w=[kh])
                    if b == 0:
                        S.dve(MEMSET(hTp[:, :, 0:1], 0.0), w=[kh])
                        S.dma(hTp[:, :, 1:512], src[:, :, 0:511], r=["hT_d"], w=[kh])
                    else:
                        S.dma(hTp[:], src[:, :, b * 512 - 1:b * 512 + 511], r=["hT_d"], w=[kh])
                    if b == 1:
                        chk(9)
                    for gi, (c0, c1) in enumerate(((0, 128), (128, 256), (256, 288))):
                        if gi == 2 and l == 0:
                            continue
                        rows = c1 - c0
                        bk, kb = nb()
                        for kc in range(8):
                            S.pe(MM(bk[0:rows, :], LA1[:, kc, c0:c1], hTb[:, kc, :], kc == 0, False), r=[kh, "LA1"], w=[kb])
                        for kc in range(8):
                            S.pe(MM(bk[0:rows, :], LA2[:, kc, c0:c1], hTp[:, kc, :], False, kc == 7), r=[kh, "LA2"], w=[kb])
                        if gi == 0:
                            S.act(ACTF(L1wa[0:64, :], bk[0:64, :], AF.Tanh), r=[kb], w=["L1wa"])
                            S.act(ACTF(L1wa[64:128, :], bk[64:128, :], AF.Copy), r=[kb], w=["L1wa"])
                        elif gi == 1:
                            S.act(ACTF(sgt[:], bk[:], AF.Tanh, scale=0.5), r=[kb], w=["sgt"])
                            S.dve(TS(sg[:], sgt[:], 0.5, 0.5, ALU.mult, ALU.add), r=["sgt"], w=["sg"])
                        else:
                            S.act(ACTF(L1v[:], bk[0:32, :], AF.Copy), r=[kb], w=["L1v"])
                    chk(2)
                    for j in range(4):
                        t = b * 4 + j
                        lo = j * 128
                        tsl = slice(j * 128, (j + 1) * 128)
                        if l > 0:
                            S.dma(vf[:], vfirst_d[t * 128:(t + 1) * 128, :], r=["vfirst_d"], w=["vf"])
                        for g, (dst, kd) in enumerate(((r32, "r32"), (k32, "k32"), (v32, "v32"))):
                            bk, kb = nb()
                            for kc in range(8):
                                S.pe(MM(bk[:], hTb[:, kc, lo:lo + 128], WT1[:, kc, g * 512:(g + 1) * 512], kc == 0, False), r=[kh, "WT1"], w=[kb])
                            for kc in range(8):
                                S.pe(MM(bk[:], hTp[:, kc, lo:lo + 128], WT2[:, kc, g * 512:(g + 1) * 512], False, kc == 7), r=[kh, "WT2"], w=[kb])
                            evac(dst[:], bk[:], [kb], [kd])
                        bkw, kbw = nb()
                        S.pe(MM(bkw[:], L1wa[0:64, tsl], Bwa[0:64, :], True, False), r=["L1wa", "Bwa"], w=[kbw])
                        S.pe(MM(bkw[:], onesf[0:1, :], brow[0:1, 0, :], False, True), r=["onesf", "brow"], w=[kbw])
                        bka, kba = nb()
                        S.pe(MM(bka[:], L1wa[64:128, tsl], Bwa[64:128, :], True, False), r=["L1wa", "Bwa"], w=[kba])
                        S.pe(MM(bka[:], onesf[0:1, :], brow[0:1, 1, :], False, True), r=["onesf", "brow"], w=[kba])
                        bkg, kbg = nb()
                        S.pe(MM(bkg[:], sg[:, tsl], Bg[:], True, True), r=["sg", "Bg"], w=[kbg])
                        S.act(ACTF(lw[:], bkw[:], AF.Tanh, scale=0.5), r=[kbw], w=["lw"])
                        S.dve(TS(lw[:], lw[:], -0.5 * math.exp(-0.5), -0.5 * math.exp(-0.5), ALU.mult, ALU.add), r=["lw"], w=["lw"])
                        S.act(ACTF(a32[:], bka[:], AF.Tanh, scale=0.5), r=[kba], w=["a32"])
                        S.pool(TS(a32[:], a32[:], 0.5, 0.5, ALU.mult, ALU.add), r=["a32"], w=["a32"])
                        S.act(ACTF(g32[:], bkg[:], AF.Copy), r=[kbg], w=["g32"])
                        if l > 0:
                            bkv, kbv = nb()
                            S.pe(MM(bkv[:], L1v[0:32, tsl], Bv[0:32, :], True, False), r=["L1v", "Bv"], w=[kbv])
                            S.pe(MM(bkv[:], onesf[0:1, :], brow[0:1, 2, :], False, True), r=["onesf", "brow"], w=[kbv])
                            S.act(ACTF(t1[:], bkv[:], AF.Tanh, scale=0.5), r=[kbv], w=["t1"])
                            S.pool(TS(t1[:], t1[:], 0.5, 0.5, ALU.mult, ALU.add), r=["t1"], w=["t1"])
                            S.pool(TT(t2[:], vf[:], v32[:], ALU.subtract), r=["vf", "v32"], w=["t2"])
                            S.pool(TT(t2[:], t2[:], t1[:], ALU.mult), r=["t2", "t1"], w=["t2"])
                            S.pool(TT(v32[:], v32[:], t2[:], ALU.add), r=["v32", "t2"], w=["v32"])
                        else:
                            S.dma(vfirst_d[t * 128:(t + 1) * 128, :], v32[:], r=["v32"], w=["vfirst_d"])
                        S.act(ACTF(Vb[:], v32[:], AF.Copy), r=["v32"], w=["Vb"])
                        if dbg:
                            rows = slice(t * 128, (t + 1) * 128)
                            S.dma(dbg_d["dbg_v"][rows, :], v32[:], r=["v32"], w=["dbgv"])
                            S.dma(dbg_d["dbg_r"][rows, :], r32[:], r=["r32"], w=["dbgr"])
                            S.dma(dbg_d["dbg_k"][rows, :], k32[:], r=["k32"], w=["dbgk"])
                            S.dma(dbg_d["dbg_a"][rows, :], a32[:], r=["a32"], w=["dbga"])
                            S.dma(dbg_d["dbg_lw"][rows, :], lw[:], r=["lw"], w=["dbglw"])
                        bkc, kbc = nb()
                        S.pe(MM(bkc[:], tri_incl, lw[:], True, True), r=["cst", "lw"], w=[kbc])
                        bke, kbe = nb()
                        S.pe(MM(bke[:], tri_strict, lw[:], True, True), r=["cst", "lw"], w=[kbe])
                        bkG, kbG = nb()
                        for h in range(8):
                            S.pe(MM(bkG[0:64, h:h + 1], lw[:, h * 64:(h + 1) * 64], onesf[:, 0:1], True, True), r=["lw", "onesf"], w=[kbG])
                        S.act(ACTF(Ginc[:], bkc[:], AF.Exp), r=[kbc], w=["Ginc"])
                        S.act(ACTF(Ginv[:], bkc[:], AF.Exp, scale=-1.0), r=[kbc], w=["Ginv"])
                        S.act(ACTF(Gexc[:], bke[:], AF.Exp), r=[kbe], w=["Gexc"])
                        S.act(ACTF(GC[:], bkG[0:64, 0:8], AF.Exp), r=[kbG], w=["GC"])
                        S.dve(TT(kkr[:], k32[:], pbc[:, 0, :], ALU.mult), r=["k32", "pbc"], w=["kkr"])
                        S.act(ACTF(t2[:], kkr[:], AF.Square), r=["kkr"], w=["t2"])
                        S.dve(RED(s8[0][:], t2[:].rearrange("p (h d) -> p h d", h=8), ALU.add), r=["t2"], w=["s8_0"])
                        S.dve(TS(s8[0][:], s8[0][:], 1e-24, None, ALU.max), r=["s8_0"], w=["s8_0"])
                        S.pool(TT(s8[1][:], s8[0][:], m05[:, 0:8], ALU.pow), r=["s8_0", "m05"], w=["s8_1"])
                        S.dve(TT(kk[:].rearrange("p (h d) -> p h d", h=8), kkr[:].rearrange("p (h d) -> p h d", h=8),
                                 s8[1][:].unsqueeze(2).to_broadcast([128, 8, 64]), ALU.mult), r=["kkr", "s8_1"], w=["kk"])
                        S.pool(TT(b32[:], kk[:], a32[:], ALU.mult), r=["kk", "a32"], w=["b32"])
                        S.dve(STT(t1[:], a32[:], -1.0, pbc[:, 1, :], ALU.add, ALU.mult), r=["a32", "pbc"], w=["t1"])
                        S.dve(STT(km[:], t1[:], 1.0, k32[:], ALU.add, ALU.mult), r=["t1", "k32"], w=["km"])
                        S.dve(TT(Kd[:], kk[:], Gexc[:], ALU.mult), r=["kk", "Gexc"], w=["Kd"])
                        S.pool(TT(Bi[:], b32[:], Ginv[:], ALU.mult), r=["b32", "Ginv"], w=["Bi"])
                        S.dve(TT(Ki[:], km[:], Ginv[:], ALU.mult), r=["km", "Ginv"], w=["Ki"])
                        S.pool(TT(Rd[:], r32[:], Ginc[:], ALU.mult), r=["r32", "Ginc"], w=["Rd"])
                        S.pool(TT(t2[:], r32[:], km[:], ALU.mult), r=["r32", "km"], w=["t2"])
                        S.pool(TT(t2[:], t2[:], pbc[:, 2, :], ALU.mult), r=["t2", "pbc"], w=["t2"])
                        S.dve(RED(s8[2][:], t2[:].rearrange("p (h d) -> p h d", h=8), ALU.add), r=["t2"], w=["s8_2"])
                        chk(3)
                        for q, (srcT, ks) in enumerate(((Kd, "Kd"), (Rd, "Rd"), (Bi, "Bi"), (Ki, "Ki"))):
                            bk, kb = nb()
                            bkb = bfv(bk)
                            for h in range(8):
                                S.pe(TR(bkb[0:64, h * 128:(h + 1) * 128], srcT[:, h * 64:(h + 1) * 64], identb[:]), r=[ks, "identb"], w=[kb])
                            evac(FT[:, :, q, :], bkb[0:64, :].rearrange("p (h t) -> p h t", h=8), [kb], ["FT"])
                        for h in range(8):
                            bk, kb = nb()
                            rhs = FT[:, h, 0:2, :].rearrange("p a t -> p (a t)")
                            S.pe(MM(bk[:, 0:256], FT[:, h, 2, :], rhs, True, True), r=["FT"], w=[kb])
                            S.pe(MM(bk[:, 256:512], FT[:, h, 3, :], rhs, True, True), r=["FT"], w=[kb])
                            S.dve(TT(GE[:, h, :], bk[:], mask4[:], ALU.mult), r=[kb, "mask4"], w=["GE"])
                            S.dve(TT(NNb[0][:, h, :], bk[:, 0:128], tri_strict, ALU.mult), r=[kb, "cst"], w=["NNb0"])
                        for g in range(2):
                            bk, kb = nb()
                            for hh in range(4):
                                h = g * 4 + hh
                                S.pe(MM(bk[:, hh * 128:(hh + 1) * 128], FT[:, h, 0, :], FT[:, h, 2, :], True, True), r=["FT"], w=[kb])
                            S.dve(TT(MMb[0][:, g * 4:(g + 1) * 4, :], bk[:].rearrange("p (h t) -> p h t", h=4),
                                     lowm4[:].rearrange("p (h t) -> p h t", h=4), ALU.mult), r=[kb, "lowm4"], w=["MMb0"])
                        chk(4)
                        S.pool(TT(TTb[0][:], identf.unsqueeze(1).to_broadcast([128, 8, 128]), NNb[0][:], ALU.subtract),
                               r=["cst", "NNb0"], w=["TTb0"])
                        cur = 0
                        for step in range(1, 7):
                            nxt = 1 - cur
                            sqb = []
                            for g in range(2):
                                hs = range(g * 4, g * 4 + 4)
                                bkM, kbM = nb()
                                for hh, h in enumerate(hs):
                                    S.pe(MM(bkM[:, hh * 128:(hh + 1) * 128], NNb[cur][:, h, :], MMb[cur][:, h, :], True, True),
                                         r=["NNb%d" % cur, "MMb%d" % cur], w=[kbM])
                                bkN = kbN = None
                                if step < 6:
                                    bkN, kbN = nb()
                                    for hh, h in enumerate(hs):
                                        S.pe(MM(bkN[:, hh * 128:(hh + 1) * 128], MMb[cur][:, h, :], NNb[cur][:, h, :], True, True),
                                             r=["NNb%d" % cur, "MMb%d" % cur], w=[kbN])
                                sqb.append((bkM, kbM, bkN, kbN))
                            for g in range(2):
                                bkM, kbM, bkN, kbN = sqb[g]
                                evac(MMb[nxt][:, g * 4:(g + 1) * 4, :], bkM[:].rearrange("p (h t) -> p h t", h=4), [kbM], ["MMb%d" % nxt])
                                if bkN is not None:
                                    evac(NNb[nxt][:, g * 4:(g + 1) * 4, :], bkN[:].rearrange("p (h t) -> p h t", h=4), [kbN], ["NNb%d" % nxt])
                            for g in range(2):
                                hs = range(g * 4, g * 4 + 4)
                                bkT, kbT = nb()
                                for hh, h in enumerate(hs):
                                    S.pe(MM(bkT[:, hh * 128:(hh + 1) * 128], MMb[nxt][:, h, :], TTb[cur][:, h, :], True, True),
                                         r=["MMb%d" % nxt, "TTb%d" % cur], w=[kbT])
                                S.dve(TT(TTb[nxt][:, g * 4:(g + 1) * 4, :].rearrange("p h t -> p (h t)"), bkT[:],
                                         TTb[cur][:, g * 4:(g + 1) * 4, :].rearrange("p h t -> p (h t)"), ALU.add),
                                      r=[kbT, "TTb%d" % cur], w=["TTb%d" % nxt])
                            cur = nxt
                        S.act(ACTF(TTh[:], TTb[cur][:], AF.Copy), r=["TTb%d" % cur], w=["TTh"])
                        TTf = TTh
                        kT = "TTh"
                        for g in range(2):
                            bk, kb = nb()
                            for hh in range(4):
                                h = g * 4 + hh
                                S.pe(MM(bk[0:64, hh * 128:(hh + 1) * 128], Kd[:, h * 64:(h + 1) * 64], TTf[:, h, :], True, True), r=["Kd", kT], w=[kb])
                            evac(WTs[:, g * 4:(g + 1) * 4, :], bk[0:64, :].rearrange("p (h t) -> p h t", h=4), [kb], ["WTs"])
                        bk, kb = nb()
                        for h in range(8):
                            S.pe(MM(bk[:, h * 64:(h + 1) * 64], GE[:, h, 256:384], Vb[:, h * 64:(h + 1) * 64], True, True), r=["GE", "Vb"], w=[kb])
                        evac(Zb[:], bk[:], [kb], ["Zb"])
                        bk, kb = nb()
                        for h in range(8):
                            S.pe(MM(bk[:, h * 64:(h + 1) * 64], TTf[:, h, :], Zb[:, h * 64:(h + 1) * 64], True, True), r=[kT, "Zb"], w=[kb])
                        evac(U0[:], bk[:], [kb], ["U0"])
                        chk(6)
                        bk, kb = nb()
                        for h in range(8):
                            S.pe(MM(bk[:, h * 64:(h + 1) * 64], WTs[:, h, :], Pb[:, h, :], True, True), r=["WTs", "Pb"], w=[kb])
                        S.dve(STT(Ub[:], bk[:], -1.0, U0[:], ALU.mult, ALU.subtract), r=[kb, "U0"], w=["Ub"])
                        bkY, kbY = nb()
                        for h in range(8):
                            hs_ = slice(h * 64, (h + 1) * 64)
                            S.pe(MM(bkY[:, hs_], FT[:, h, 1, :], Pb[:, h, :], True, False), r=["FT", "Pb"], w=[kbY])
                            S.pe(MM(bkY[:, hs_], GE[:, h, 128:256], Ub[:, hs_], False, False), r=["GE", "Ub"], w=[kbY])
                            S.pe(MM(bkY[:, hs_], GE[:, h, 384:512], Vb[:, hs_], False, True), r=["GE", "Vb"], w=[kbY])
                        bkX, kbX = nb()
                        S.pe(MM(bkX[0:64, :], identf[0:64, 0:64], P32f, True, False), r=["cst", "P32"], w=[kbX])
                        for h in range(8):
                            hs_ = slice(h * 64, (h + 1) * 64)
                            S.pe(MM(bkX[0:64, hs_], Bi[:, hs_], Ub[:, hs_], False, False), r=["Bi", "Ub"], w=[kbX])
                            S.pe(MM(bkX[0:64, hs_], Ki[:, hs_], Vb[:, hs_], False, h == 7), r=["Ki", "Vb"], w=[kbX])
                        S.dve(TT(P32[:], bkX[0:64, :].rearrange("p (h v) -> p h v", h=8), GC[:].unsqueeze(2).to_broadcast([64, 8, 64]), ALU.mult),
                              r=[kbX, "GC"], w=["P32"])
                        S.act(ACTF(Pb[:], P32[:], AF.Copy), r=["P32"], w=["Pb"])
                        chk(7)
                        S.act(ACTF(y32[:], bkY[:], AF.Copy), r=[kbY], w=["y32"])
                        if dbg:
                            S.dma(dbg_d["dbg_y"][t * 128:(t + 1) * 128, :], y32[:], r=["y32"], w=["dbgy"])
                        chk(10)
                        Y3 = y32[:].rearrange("p (h d) -> p h d", h=8)
                        S.dve(RED(s8[3][:], Y3, ALU.add), r=["y32"], w=["s8_3"])
                        S.dve(TS(s8[3][:], s8[3][:], 1.0 / 64, None, ALU.mult), r=["s8_3"], w=["s8_3"])
                        S.dve(TT(cen[:].rearrange("p (h d) -> p h d", h=8), Y3, s8[3][:].unsqueeze(2).to_broadcast([128, 8, 64]), ALU.subtract),
                              r=["y32", "s8_3"], w=["cen"])
                        S.act(ACTF(t2[:], cen[:], AF.Square), r=["cen"], w=["t2"])
                        S.dve(RED(s8[4][:], t2[:].rearrange("p (h d) -> p h d", h=8), ALU.add), r=["t2"], w=["s8_4"])
                        S.dve(TS(s8[4][:], s8[4][:], 1.0 / 64, 64e-5, ALU.mult, ALU.add), r=["s8_4"], w=["s8_4"])
                        S.pool(TT(s8[5][:], s8[4][:], m05[:, 0:8], ALU.pow), r=["s8_4", "m05"], w=["s8_5"])
                        S.dve(TT(cen[:].rearrange("p (h d) -> p h d", h=8), cen[:].rearrange("p (h d) -> p h d", h=8),
                                 s8[5][:].unsqueeze(2).to_broadcast([128, 8, 64]), ALU.mult), r=["cen", "s8_5"], w=["cen"])
                        S.pool(TT(cen[:], cen[:], pbc[:, 3, :], ALU.mult), r=["cen", "pbc"], w=["cen"])
                        S.pool(TT(cen[:], cen[:], pbc[:, 4, :], ALU.add), r=["cen", "pbc"], w=["cen"])
                        S.dve(TT(t2[:].rearrange("p (h d) -> p h d", h=8), v32[:].rearrange("p (h d) -> p h d", h=8),
                                 s8[2][:].unsqueeze(2).to_broadcast([128, 8, 64]), ALU.mult), r=["v32", "s8_2"], w=["t2"])
                        S.pool(TT(cen[:], cen[:], t2[:], ALU.add), r=["cen", "t2"], w=["cen"])
                        S.dve(TT(ob[:], cen[:], g32[:], ALU.mult), r=["cen", "g32"], w=["ob"])
                        chk(11)
                        bk, kb = nb()
                        bkb = bfv(bk)
                        for c in range(4):
                            S.pe(TR(bkb[:, c * 128:(c + 1) * 128], ob[:, c * 128:(c + 1) * 128], identb[:]), r=["ob", "identb"], w=[kb])
                        evac(rwT[:], bkb[:, 0:512].rearrange("p (c t) -> p c t", c=4), [kb], ["rwT"])
                        S.dma(rwkvT_d[:, t * 128:(t + 1) * 128].rearrange("(c p) t -> p c t", p=128), rwT[:], r=["rwT"], w=["rwkvT_d"])
                        chk(12)
                S.flush()

        def phase_B(l):
            nrot[0] = NROT
            with contextlib.ExitStack() as st:
                def sb(name, shape, dt):
                    return st.enter_context(SBT(name, shape, dt))
                c31b = sb("c31b", [128, 8], F32)
                nc31 = sb("nc31", [128, 8], F32)
                Rn = sb("Rn", [128, 8, 256], BF16)
                aog = sb("aog", [64, 8], F32)
                statL = sb("statL", [65, 64], F32)
                S.dma(c31b[:], c31_d.partition_broadcast(128), w=["c31b"])
                aog8 = sb("aog8", [8, 64], F32)
                S.dma(aog8[:], W["attn_out_g"][l].rearrange("(h d) -> h d", d=64), w=["aog8"])
                bk, kb = nb()
                S.pe(MM(bk[0:64, 0:8], aog8[:], identf[0:8, 0:8], True, True), r=["aog8", "cst"], w=[kb])
                S.dve(CP(aog[:], bk[0:64, 0:8]), r=[kb], w=["aog"])
                S.dve(TS(nc31[:], c31b[:], -1.0, None, ALU.mult), r=["c31b"], w=["nc31"])
                with contextlib.ExitStack() as st2:
                    bnr = st2.enter_context(SBT("bnr", [128, 2048], F32))
                    S.dma(bnr[:], bnear_d, w=["bnr"])
                    for h in range(8):
                        S.act(ACTF(Rn[:, h, :], bnr[:, h * 256:(h + 1) * 256], AF.Exp, bias=nc31[:, h:h + 1]), r=["bnr", "nc31"], w=["Rn"])
                    S.flush()
                kaT = sb("kaT", [128, 4, T], BF16)
                kiT2 = sb("kiT2", [128, T], BF16)
                Va = sb("Va", [128, NT, 520], BF16)
                qaTb = [sb("qaTb%d" % i, [128, 4, 512], BF16) for i in range(2)]
                qiTb = [sb("qiTb%d" % i, [128, 4, 512], BF16) for i in range(2)]
                wib = [sb("wib%d" % i, [128, 4, 8], F32) for i in range(2)]
                wabs = sb("wabs", [128, 4, 8], F32)
                wsgn = sb("wsgn", [128, 4, 8], F32)
                scb = [sb("sc%d" % i, [128, T], F32) for i in range(2)]
                NRL = 5
                rl = [sb("rl%d" % i, [128, 512], F32) for i in range(NRL)]
                msk = sb("msk", [128, T], BF16)
                maskT = sb("maskT", [128, NT, 512], BF16)
                NEP = 4
                Eb = [sb("Eb%d" % i, [128, 512], BF16) for i in range(NEP)]
                Pt = [sb("Pt%d" % i, [128, 512], BF16) for i in range(NEP)]
                attTb = sb("attTb", [128, 4, 512], BF16)
                Osb = sb("Osb", [65, 512], F32)
                SQ = sb("SQ", [65, 512], F32)
                rs = sb("rs", [64, 512], F32)
                bis = [sb("bis%d" % i, [128, 1], F32) for i in range(6)]
                wtab = sb("wtab", [128, NBIS + 2], F32)
                ctab = sb("ctab", [128, NBIS + 2], F32)
                S.dma(kaT[:], featT_d[512:1024, :].rearrange("(c p) t -> p c t", p=128), r=["featT_d"], w=["kaT"])
                S.dma(kiT2[0:64, :], featT_d[1536:1600, :], r=["featT_d"], w=["kiT2"])
                S.dma(kiT2[64:128, :], featT_d[1536:1600, :], r=["featT_d"], w=["kiT2"])
                vsrc = vaug_d.rearrange("(n p) c -> p n c", p=128)
                for n0 in range(0, NT, 8):
                    S.dma(Va[:, n0:n0 + 8, :], vsrc[:, n0:n0 + 8, :], r=["vaug_d"], w=["Va"])
                S.dve(MEMSET(statL[0:64, :], 1.0 / 64), w=["statL"])
                S.dve(MEMSET(statL[64:65, :], 1e-6), w=["statL"])
                for k in range(NBIS + 2):
                    S.pool(MEMSET(ctab[:, k:k + 1], 2.0 ** (-k)), w=["ctab"])
                cw = 0.125 * (8 ** -0.5)

                def loadq(b):
                    i = b % 2
                    S.dma(qaTb[i][:], featT_d[0:512, b * 512:(b + 1) * 512].rearrange("(c p) t -> p c t", p=128), r=["featT_d"], w=["qaTb%d" % i])
                    S.dma(qiTb[i][:], featT_d[1024:1536, b * 512:(b + 1) * 512].rearrange("(c p) t -> p c t", p=128), r=["featT_d"], w=["qiTb%d" % i])
                    S.dma(wib[i][:], wi_d[b * 512:(b + 1) * 512, :].rearrange("(j p) h -> p j h", p=128), r=["wi_d"], w=["wib%d" % i])
                loadq(0)
                eidx = [0]
                for b in range(NB):
                    if b + 1 < NB:
                        loadq(b + 1)
                    i2 = b % 2
                    qa, qi, wi_ = qaTb[i2], qiTb[i2], wib[i2]
                    kqa, kqi, kwi = "qaTb%d" % i2, "qiTb%d" % i2, "wib%d" % i2
                    S.dve(STT(wabs[:], wi_[:], -1.0, wi_[:], ALU.mult, ALU.max), r=[kwi], w=["wabs"])
                    S.dve(TS(wabs[:], wabs[:], cw, None, ALU.mult), r=["wabs"], w=["wabs"])
                    S.act(ACTF(wsgn[:], wi_[:], AF.Sign), r=[kwi], w=["wsgn"])
                    nk = 4 * b + 4
                    def indexer(jq):
                        j = 4 * b + jq
                        Lk = (j + 1) * 128
                        qsl = slice(jq * 128, (jq + 1) * 128)
                        sc = scb[jq % 2]
                        ksc = "sc%d" % (jq % 2)
                        nch = (Lk + 511) // 512
                        for kc in range(nch):
                            ncol = min(512, Lk - kc * 512)
                            csl = slice(kc * 512, kc * 512 + ncol)
                            for ih in range(8):
                                pr, hf = ih // 2, ih % 2
                                ps_ = slice(hf * 64, (hf + 1) * 64)
                                bk, kb = nb()
                                S.pe(MM(bk[:, 0:ncol], qi[ps_, pr, qsl], kiT2[ps_, csl], True, True), r=[kqi, "kiT2"], w=[kb])
                                rb = rl[eidx[0] % NRL]
                                krb = "rl%d" % (eidx[0] % NRL)
                                eidx[0] += 1
                                S.act(ACTF(rb[:, 0:ncol], bk[:, 0:ncol], AF.Relu, scale=wabs[:, jq, ih:ih + 1]), r=[kb, "wabs"], w=[krb])
                                if ih == 0:
                                    yield (TS(sc[:, csl], rb[:, 0:ncol], wsgn[:, jq, 0:1], None, ALU.mult), [krb, "wsgn"], [ksc])
                                else:
                                    yield (STT(sc[:, csl], rb[:, 0:ncol], wsgn[:, jq, ih:ih + 1], sc[:, csl], ALU.mult, ALU.add), [krb, "wsgn", ksc], [ksc])

                    def do_acc(a):
                        S.dve(a[0], r=a[1], w=a[2])

                    gens = [indexer(jq) for jq in range(4)]
                    for a in gens[0]:
                        do_acc(a)
                    for jq in range(4):
                        j = 4 * b + jq
                        Lk = (j + 1) * 128
                        qsl = slice(jq * 128, (jq + 1) * 128)
                        sc = scb[jq % 2]
                        ksc = "sc%d" % (jq % 2)
                        nxt = gens[jq + 1] if jq < 3 else None
                        pending = []
                        A_, mid, cnt, inc = bis[0], bis[1], bis[2], bis[3]
                        S.dve(RED(A_[:], sc[:, 0:Lk], ALU.max, absval=True), r=[ksc], w=["bis0"])
                        S.dve(TS(A_[:], A_[:], 1.001, 1e-6, ALU.mult, ALU.add), r=["bis0"], w=["bis0"])
                        S.dve(TT(sc[:, j * 128:(j + 1) * 128], sc[:, j * 128:(j + 1) * 128], caus, ALU.add), r=[ksc, "cst"], w=[ksc])
                        if dbg:
                            S.dma(dbg_d["dbg_score"][j * 128:(j + 1) * 128, 0:Lk], sc[:, 0:Lk], r=[ksc], w=["dbgs"])
                        S.dve(TS(wtab[:], ctab[:], A_[:, 0:1], None, ALU.mult), r=["ctab", "bis0"], w=["wtab"])
                        S.dve(TS(mid[:], A_[:], 0.0, None, ALU.mult), r=["bis0"], w=["bis1"])
                        for k in range(1, NBIS + 1):
                            if k % 2 == 1:
                                if nxt is not None:
                                    for _ in range(NRL):
                                        a = next(nxt, None)
                                        if a is None:
                                            break
                                        pending.append(a)
                                S.dve(TS(msk[:, 0:Lk], sc[:, 0:Lk], mid[:, 0:1], 0.0, ALU.is_ge, ALU.add, accum=cnt[:]), r=[ksc, "bis1"], w=["bis2", "msk"])
                                S.dve(STT(inc[:], cnt[:], NSEL - 0.5, wtab[:, k - 1:k], ALU.is_ge, ALU.mult), r=["bis2", "wtab"], w=["bis3"])
                            else:
                                S.dve(TS(bis[4][:], mid[:], -1.0, None, ALU.mult), r=["bis1"], w=["bis4"])
                                S.act(ACTF(msk[:, 0:Lk], sc[:, 0:Lk], AF.Sign, bias=bis[4][:, 0:1], accum=cnt[:]), r=[ksc, "bis4"], w=["bis2", "msk"])
                                for a in pending:
                                    do_acc(a)
                                del pending[:]
                                S.dve(STT(inc[:], cnt[:], 2.0 * NSEL - 1.0 - Lk, wtab[:, k - 1:k], ALU.is_ge, ALU.mult), r=["bis2", "wtab"], w=["bis3"])
                            S.dve(STT(mid[:], inc[:], wtab[:, k:k + 1], mid[:], ALU.subtract, ALU.add), r=["bis3", "wtab", "bis1"], w=["bis1"])
                        S.dve(TT(bis[5][:], mid[:], wtab[:, NBIS:NBIS + 1], ALU.subtract), r=["bis1", "wtab"], w=["bis5"])
                        if dbg:
                            S.dma(dbg_d["dbg_thr"][j * 128:(j + 1) * 128, :], bis[5][:], r=["bis5"], w=["dbgt"])
                        S.dve(TS(msk[:, 0:Lk], sc[:, 0:Lk], bis[5][:, 0:1], None, ALU.is_ge), r=[ksc, "bis5"], w=["msk"])
                        for i0 in range(0, j + 1, 8):
                            n8 = min(8, j + 1 - i0)
                            bk, kb = nb()
                            bkb = bfv(bk)
                            for ii in range(n8):
                                S.pe(TR(bkb[:, ii * 128:(ii + 1) * 128], msk[:, (i0 + ii) * 128:(i0 + ii + 1) * 128], identb[:]), r=["msk", "identb"], w=[kb])
                            S.act(ACTF(maskT[:, i0:i0 + n8, qsl], bkb[:, 0:n8 * 128].rearrange("p (i t) -> p i t", i=n8), AF.Copy), r=[kb], w=["maskT"])
                        for a in pending:
                            do_acc(a)
                        del pending[:]
                        if nxt is not None:
                            for a in nxt:
                                do_acc(a)
                    for hp in range(4):
                        hpair = (2 * hp, 2 * hp + 1)
                        for i in range(nk):
                            m = i - 4 * b
                            c0 = max(0, m) * 128
                            ncol = 512 - c0
                            st_ = []
                            for h in hpair:
                                pr, hf = h // 2, h % 2
                                ps_ = slice(hf * 64, (hf + 1) * 64)
                                bk, kb = nb()
                                S.pe(MM(bk[:, 0:ncol], kaT[ps_, pr, i * 128:(i + 1) * 128], qa[ps_, pr, c0:512], True, True), r=["kaT", kqa], w=[kb])
                                ei = eidx[0] % NEP
                                eidx[0] += 1
                                st_.append((h, bk, kb, ei))
                            for h, bk, kb, ei in st_:
                                S.act(ACTF(Eb[ei][:, 0:ncol], bk[:, 0:ncol], AF.Exp, bias=c31b[:, h:h + 1], scale=0.125), r=[kb, "c31b"], w=["Eb%d" % ei])
                            for h, bk, kb, ei in st_:
                                E, kE, P_, kP = Eb[ei], "Eb%d" % ei, Pt[ei], "Pt%d" % ei
                                eng = S.dve if (h % 2) else S.pool
                                eng(TT(P_[:, 0:ncol], E[:, 0:ncol], maskT[:, i, c0:512], ALU.mult), r=[kE, "maskT"], w=[kP])
                                if m >= 0:
                                    nn = min(256, ncol)
                                    eng(TT(P_[:, 0:nn], P_[:, 0:nn], Rn[:, h, 0:nn], ALU.mult), r=[kP, "Rn"], w=[kP])
                                elif m == -1:
                                    eng(TT(P_[:, 0:128], P_[:, 0:128], Rn[:, h, 128:256], ALU.mult), r=[kP, "Rn"], w=[kP])
                            for h, bk, kb, ei in st_:
                                bkO, kbO = banks[6 + h % 2], "bank%d" % (6 + h % 2)
                                S.pe(MM(bkO[0:65, c0:512], Va[:, i, h * 65:(h + 1) * 65], Pt[ei][:, 0:ncol], i == 0, i == nk - 1), r=["Va", "Pt%d" % ei], w=[kbO])
                        for h in hpair:
                            pr, hf = h // 2, h % 2
                            ps_ = slice(hf * 64, (hf + 1) * 64)
                            bkO, kbO = banks[6 + h % 2], "bank%d" % (6 + h % 2)
                            S.act(ACTF(Osb[:], bkO[0:65, :], AF.Copy), r=[kbO], w=["Osb"])
                            S.act(ACTF(SQ[:], bkO[0:65, :], AF.Square), r=[kbO], w=["SQ"])
                            bk, kb = nb()
                            S.pe(MM(bk[0:64, :], statL[:], SQ[:], True, True), r=["statL", "SQ"], w=[kb])
                            S.act(ACTF(rs[:], bk[0:64, :], AF.Ln), r=[kb], w=["rs"])
                            S.act(ACTF(rs[:], rs[:], AF.Exp, scale=-0.5), r=["rs"], w=["rs"])
                            S.dve(STT(attTb[ps_, pr, :], Osb[0:64, :], aog[:, h:h + 1], rs[:], ALU.mult, ALU.mult), r=["Osb", "aog", "rs"], w=["attTb"])
                    S.dma(attT_d[:, b * 512:(b + 1) * 512].rearrange("(c p) t -> p c t", p=128), attTb[:], r=["attTb"], w=["attT_d"])
                S.flush()
            nrot[0] = 8

        def phase_B2(l, xsrc):
            with contextlib.ExitStack() as st:
                def sb(name, shape, dt):
                    return st.enter_context(SBT(name, shape, dt))
                wo = sb("wo", [128, 8, 1024], BF16)
                gt1 = sb("gt1", [128, 1024], F32)
                A2t = sb("A2t", [128, 1024], F32)
                sh2t = sb("sh2t", [128, 1024], F32)
                xb = [sb("xb%d" % i, [128, 1024], F32) for i in range(2)]
                mixT = [sb("mixT%d" % i, [128, 8, 128], BF16) for i in range(2)]
                x1 = [sb("x1_%d" % i, [128, 1024], F32) for i in range(2)]
                junk = sb("junk", [128, 1024], F32)
                tmp = sb("tmpn", [128, 1024], F32)
                tmp2 = sb("tmpm", [128, 1024], F32)
                hb = [sb("hb%d" % i, [128, 1024], BF16) for i in range(2)]
                h2s = [sb("h2s%d" % i, [128, 8, 128], BF16) for i in range(2)]
                ssq = [sb("ssq%d" % i, [128, 1], F32) for i in range(2)]
                ms = [sb("ms%d" % i, [128, 1], F32) for i in range(2)]
                rstd = [sb("rstd%d" % i, [128, 1], F32) for i in range(2)]
                wov = W["w_out"][l].rearrange("(kc p) n -> p kc n", p=128)
                S.dma(wo[:], wov, w=["wo"], eng="pool")
                S.dma(gt1[:], mod_d[:, 2048:3072], r=["mod_d"], w=["modp"])
                S.dma(A2t[:], mod_d[:, 4096:5120], r=["mod_d"], w=["modp"])
                S.dma(sh2t[:], mod_d[:, 3072:4096], r=["mod_d"], w=["modp"])

                def load(t):
                    i = t % 2
                    S.dma(xb[i][:], xsrc[t * 128:(t + 1) * 128, :], r=["xs_d"], w=["xb%d" % i])
                    S.dma(mixT[i][:, 0:4, :], rwkvT_d[:, t * 128:(t + 1) * 128].rearrange("(c p) t -> p c t", p=128), r=["rwkvT_d"], w=["mixT%d" % i])
                    S.dma(mixT[i][:, 4:8, :], attT_d[:, t * 128:(t + 1) * 128].rearrange("(c p) t -> p c t", p=128), r=["attT_d"], w=["mixT%d" % i])
                load(0)
                for t in range(NT):
                    if t + 1 < NT:
                        load(t + 1)
                    i = t % 2
                    for hf in range(2):
                        bk, kb = nb()
                        for kc in range(8):
                            S.pe(MM(bk[:], mixT[i][:, kc, :], wo[:, kc, hf * 512:(hf + 1) * 512], kc == 0, kc == 7), r=["mixT%d" % i, "wo"], w=[kb])
                        csl = slice(hf * 512, (hf + 1) * 512)
                        S.dve(TT(tmp2[:, csl], bk[:], gt1[:, csl], ALU.mult), r=[kb, "modp"], w=["tmpm"])
                    S.pool(TT(x1[i][:], tmp2[:], xb[i][:], ALU.add), r=["tmpm", "xb%d" % i], w=["x1_%d" % i])
                    S.dma(xs_d[t * 128:(t + 1) * 128, :], x1[i][:], r=["x1_%d" % i], w=["xs_d2"])
                    norm_mod_T(x1[i][:], "x1_%d" % i, A2t[:], sh2t[:], hb[i], "hb%d" % i, junk, ssq[i], ms[i], rstd[i], tmp, i,
                               h2s[i][:], "h2s%d" % i)
                    S.dma(h2T_d[:, t * 128:(t + 1) * 128].rearrange("(kc p) t -> p kc t", p=128), h2s[i][:], r=["h2s%d" % i], w=["h2T_d"])
                S.flush()

        def phase_C(l, last):
            TB = 256
            NBC = T // TB
            with contextlib.ExitStack() as st:
                def sb(name, shape, dt):
                    return st.enter_context(SBT(name, shape, dt))
                w1 = sb("w1", [128, 8, 4096], BF16)
                w2 = sb("w2", [128, 32, 1024], BF16)
                gt2 = sb("gt2", [128, 1024], F32)
                fg = sb("fg", [128, 1024], F32)
                h2b = [sb("h2b%d" % i, [128, 8, TB], BF16) for i in range(2)]
                uT = sb("uT", [128, 32, TB], BF16)
                sq = [sb("sq%d" % i, [128, TB], F32) for i in range(2)]
                xb = [sb("xb%d" % i, [128, 1024], F32) for i in range(2)]
                x2 = [sb("x2_%d" % i, [128, 1024], F32) for i in range(2)]
                tmp2 = sb("tmpm", [128, 1024], F32)
                junk = sb("junk", [128, 1024], F32)
                ssq = [sb("ssq%d" % i, [128, 1], F32) for i in range(2)]
                ms = [sb("ms%d" % i, [128, 1], F32) for i in range(2)]
                rstd = [sb("rstd%d" % i, [128, 1], F32) for i in range(2)]
                w1v = W["w_mlp1"][l].rearrange("(kc p) n -> p kc n", p=128)
                w2v = W["w_mlp2"][l].rearrange("(fc p) n -> p fc n", p=128)
                for q in range(4):
                    S.dma(w1[:, :, q * 1024:(q + 1) * 1024], w1v[:, :, q * 1024:(q + 1) * 1024], w=["w1"], eng="pool")
                for q in range(4):
                    S.dma(w2[:, q * 8:(q + 1) * 8, :], w2v[:, q * 8:(q + 1) * 8, :], w=["w2"], eng="pool")
                S.dma(gt2[:], mod_d[:, 5120:6144], r=["mod_d"], w=["modp"])
                if last:
                    S.dma(fg[:], W["final_g"].partition_broadcast(128), w=["fg"])

                def load(bb):
                    i = bb % 2
                    S.dma(h2b[i][:], h2T_d[:, bb * TB:(bb + 1) * TB].rearrange("(kc p) t -> p kc t", p=128), r=["h2T_d"], w=["h2b%d" % i])
                load(0)
                xi = [0]
                for bb in range(NBC):
                    if bb + 1 < NBC:
                        load(bb + 1)
                    i = bb % 2
                    for fc in range(32):
                        bk, kb = nb()
                        for kc in range(8):
                            S.pe(MM(bk[:, 0:TB], w1[:, kc, fc * 128:(fc + 1) * 128], h2b[i][:, kc, :], kc == 0, kc == 7), r=["w1", "h2b%d" % i], w=[kb])
                        s_ = sq[fc % 2]
                        ks_ = "sq%d" % (fc % 2)
                        S.act(ACTF(s_[:], bk[:, 0:TB], AF.Square), r=[kb], w=[ks_])
                        S.dve(STT(uT[:, fc, :], bk[:, 0:TB], 0.0, s_[:], ALU.is_gt, ALU.mult), r=[kb, ks_], w=["uT"])
                    for jj in range(TB // 128):
                        t = bb * (TB // 128) + jj
                        xi_ = xi[0] % 2
                        xi[0] += 1
                        S.dma(xb[xi_][:], xs_d[t * 128:(t + 1) * 128, :], r=["xs_d"], w=["xb%d" % xi_])
                        for hf in range(2):
                            bk, kb = nb()
                            for fc in range(32):
                                S.pe(MM(bk[:], uT[:, fc, jj * 128:(jj + 1) * 128], w2[:, fc, hf * 512:(hf + 1) * 512], fc == 0, fc == 31), r=["uT", "w2"], w=[kb])
                            csl = slice(hf * 512, (hf + 1) * 512)
                            S.dve(TT(tmp2[:, csl], bk[:], gt2[:, csl], ALU.mult), r=[kb, "modp"], w=["tmpm"])
                        S.pool(TT(x2[xi_][:], tmp2[:], xb[xi_][:], ALU.add), r=["tmpm", "xb%d" % xi_], w=["x2_%d" % xi_])
                        if not last:
                            S.dma(xs_d[t * 128:(t + 1) * 128, :], x2[xi_][:], r=["x2_%d" % xi_], w=["xs_d2"])
                        else:
                            S.act(ACTF(junk[:], x2[xi_][:], AF.Square, accum=ssq[xi_][:]), r=["x2_%d" % xi_], w=["ssq%d" % xi_])
                            S.dve(TS(ms[xi_][:], ssq[xi_][:], 1.0 / 1024, 1e-6, ALU.mult, ALU.add), r=["ssq%d" % xi_], w=["ms%d" % xi_])
                            S.pool(TT(rstd[xi_][:], ms[xi_][:], m05[:, 0:1], ALU.pow), r=["ms%d" % xi_, "m05"], w=["rstd%d" % xi_])
                            S.dve(STT(x2[xi_][:], x2[xi_][:], rstd[xi_][:, 0:1], fg[:], ALU.mult, ALU.mult), r=["x2_%d" % xi_, "rstd%d" % xi_, "fg"], w=["x2_%d" % xi_])
                            S.dma(out_d[t * 128:(t + 1) * 128, :], x2[xi_][:], r=["x2_%d" % xi_], w=["out_d"])
                S.flush()

        order = []
        for l in range(L):
            order += [("M", l), ("A1", l), ("A2", l), ("B", l), ("B2", l), ("C", l)]
        for ph, l in order:
            xsrc = x_d if l == 0 else xs_d
            if ph == "M":
                phase_M(l)
            elif ph == "A1":
                phase_A1(l, xsrc)
            elif ph == "A2":
                try:
                    phase_A2(l)
                except _Stop:
                    S.flush()
                    break
            elif ph == "B":
                phase_B(l)
            elif ph == "B2":
                phase_B2(l, xsrc)
            elif ph == "C":
                phase_C(l, l == L - 1)
            if stop_after == (ph, l):
                break
        S.flush(final=True)
        nops = S.nops
    return nc, nops


def _t5_bucket(n):
    n = np.maximum(n, 0)
    nf = np.maximum(n, 1).astype(np.float32)
    large = 16 + (np.log(nf / np.float32(16)) / np.float32(math.log(128 / 16)) * np.float32(16)).astype(np.int32)
    large = np.minimum(large, 31)
    return np.where(n < 16, n, large)


def _consts():
    s = np.arange(128)[:, None]
    t = np.arange(128)[None, :]
    c = np.zeros((128, 640), np.float32)
    c[:, 0:128] = (s == t)
    c[:, 128:256] = (s <= t)
    c[:, 256:384] = (s < t)
    c[:, 384:512] = (s > t)
    c[:, 512:640] = np.where(t <= s, 0.0, -1e30)
    return c


def host_inputs(inputs, T, L):
    B = inputs["x"].shape[0]
    f = lambda a: np.ascontiguousarray(np.asarray(a, dtype=np.float32))
    rel_bias = f(inputs["rel_bias"])
    tk = np.arange(128)[:, None, None, None]
    d = np.arange(2)[None, None, :, None]
    tq = np.arange(128)[None, None, None, :]
    hh = np.arange(8)[None, :, None, None]
    bidx = _t5_bucket(128 * d + tq - tk) + 0 * hh
    bnear = rel_bias[bidx, hh + 0 * bidx].reshape(128, 2048)
    shared = {"consts": _consts(), "bnear": f(bnear), "c31": f(rel_bias[31:32, :])}
    for k in WSHAPES:
        shared[k] = f(inputs[k])[:L]
    for k in VSHAPES:
        shared[k] = f(inputs[k])[:max(L - 1, 1)]
    shared["final_g"] = f(inputs["final_g"]).reshape(1, 1024)
    maps = []
    for bi in range(B):
        m = dict(shared)
        m["x"] = f(inputs["x"][bi, :T])
        m["c8"] = f(np.asarray(inputs["c"][bi]).reshape(8, 128).T)
        maps.append(m)
    return maps


_CACHE = {}


def kernel(**inputs):
    T = inputs["x"].shape[1]
    L = inputs["w_ada"].shape[0]
    B = inputs["x"].shape[0]
    key = (T, L)
    if key not in _CACHE:
        _CACHE[key] = build(T, L)[0]
    nc = _CACHE[key]
    maps = host_inputs(inputs, T, L)
    res = run_bass_kernel_spmd(nc, maps, core_ids=list(range(B)))
    return np.stack([np.asarray(r["out"], dtype=np.float32) for r in res.results], axis=0)
```

```python
import contextlib
import os
import math
import numpy as np
import ml_dtypes
import concourse.bass as bass
import concourse.mybir as mybir
from concourse.bass_utils import run_bass_kernel_spmd

F32 = mybir.dt.float32
BF16 = mybir.dt.bfloat16
AF = mybir.ActivationFunctionType
ALU = mybir.AluOpType
AX = mybir.AxisListType

ENG = ("pe", "dve", "act", "pool", "sp")
NDMA = 12
NBIS = 16


class _Stop(Exception):
    pass


_DEAD = [False]


def chk(n):
    if int(os.environ.get("A2STOP", "0")) == n:
        _DEAD[0] = True


class Op:
    __slots__ = ("eng", "fn", "deps", "signal", "sig_val", "dma", "dma_slot", "dma_val", "sem")

    def __init__(self, eng, fn, dma):
        self.eng = eng
        self.fn = fn
        self.deps = []
        self.signal = False
        self.sig_val = None
        self.dma = dma
        self.dma_slot = None
        self.dma_val = None
        self.sem = None


class Sched:
    def __init__(self, nc, stack):
        self.nc = nc
        self.stack = stack
        self.nsw = 0
        self.sw_dmas = []
        self.esem = {e: stack.enter_context(nc.semaphore("s_" + e)) for e in ENG if e != "sp"}
        self.dsem = [stack.enter_context(nc.semaphore("d_%d" % i)) for i in range(NDMA)]
        self.ops = {e: [] for e in ENG}
        self.last_w = {}
        self.readers = {}
        self.dma_count = 0
        self.dma_last = [None] * NDMA
        self.sigc = {e: 0 for e in ENG}
        self.waited = {e: {} for e in ENG}
        self.bar = {e: [] for e in ENG}
        self.nops = 0

    def _dep(self, op, prod):
        if prod is None or prod is op:
            return
        if (not prod.dma) and (not op.dma) and prod.eng == op.eng == "pe":
            return
        if not prod.dma:
            prod.signal = True
        op.deps.append(prod)

    def add(self, eng, fn, r=(), w=(), dma=False):
        op = Op(eng, fn, dma)
        if _DEAD[0]:
            return op
        if self.bar[eng]:
            for p in self.bar[eng]:
                op.deps.append(p)
            self.bar[eng] = []
        for k in r:
            self._dep(op, self.last_w.get(k))
        for k in w:
            self._dep(op, self.last_w.get(k))
            for rd in self.readers.get(k, ()):
                self._dep(op, rd)
        for k in r:
            self.readers.setdefault(k, []).append(op)
        for k in w:
            self.last_w[k] = op
            self.readers[k] = []
        if dma and eng == "pool":
            op.sem = self.stack.enter_context(self.nc.semaphore("w_%d" % self.nsw))
            op.dma_slot = "w%d" % self.nsw
            self.nsw += 1
            op.dma_val = 16
            self.sw_dmas.append(op)
        elif dma:
            slot = self.dma_count % NDMA
            self.dma_count += 1
            prev = self.dma_last[slot]
            op.dma_slot = slot
            op.sem = self.dsem[slot]
            op.dma_val = (prev.dma_val if prev else 0) + 16
            if prev is not None:
                op.deps.append(prev)
            self.dma_last[slot] = op
        self.ops[eng].append(op)
        self.nops += 1
        return op

    def pe(self, fn, r=(), w=()):
        return self.add("pe", fn, r, w)

    def dve(self, fn, r=(), w=()):
        return self.add("dve", fn, r, w)

    def act(self, fn, r=(), w=()):
        return self.add("act", fn, r, w)

    def pool(self, fn, r=(), w=()):
        return self.add("pool", fn, r, w)

    def dma(self, out, in_, r=(), w=(), eng="sp"):
        return self.add(eng, lambda e: e.dma_start(out=out, in_=in_), r, w, dma=True)

    def flush(self, final=False):
        nc = self.nc
        lasts = []
        for e in ENG:
            nd = [op for op in self.ops[e] if not op.dma]
            if nd:
                nd[-1].signal = True
                lasts.append(nd[-1])
        for e in ENG:
            for op in self.ops[e]:
                if op.signal and not op.dma:
                    self.sigc[e] += 1
                    op.sig_val = self.sigc[e]
        dlast = [p for p in self.dma_last if p is not None] + self.sw_dmas
        self.sw_dmas = []
        with nc.Block() as block:
            engobj = {"pe": block.tensor, "dve": block.vector, "act": block.scalar,
                      "pool": block.gpsimd, "sp": block.sync}
            for ename in ENG:
                ops = self.ops[ename]
                if not ops and not (final and ename == "sp"):
                    continue

                def body(e, ops=ops, ename=ename):
                    waited = self.waited[ename]
                    semof = {}
                    for op in ops:
                        need = {}
                        for p in op.deps:
                            if p.dma:
                                key = ("d", p.dma_slot)
                                semof[key] = p.sem
                                val = p.dma_val
                            else:
                                key = ("e", p.eng)
                                val = p.sig_val
                            if need.get(key, 0) < val:
                                need[key] = val
                        for key, val in need.items():
                            if waited.get(key, 0) >= val:
                                continue
                            waited[key] = val
                            sem = semof[key] if key[0] == "d" else self.esem[key[1]]
                            e.wait_ge(sem, val)
                        ins = op.fn(e)
                        if op.dma:
                            ins.then_inc(op.sem, 16)
                        elif op.signal:
                            ins.then_inc(self.esem[ename], 1)
                    if final and ename == "sp":
                        for p in dlast:
                            if waited.get(("d", p.dma_slot), 0) < p.dma_val:
                                e.wait_ge(p.sem, p.dma_val)
                        for p in lasts:
                            e.wait_ge(self.esem[p.eng], p.sig_val)
                engobj[ename](body)
        barrier = lasts + dlast
        self.ops = {e: [] for e in ENG}
        self.last_w = {}
        self.readers = {}
        self.bar = {e: list(barrier) for e in ENG}


def MM(out, lhsT, rhs, start=True, stop=True):
    return lambda e: e.matmul(out, lhsT=lhsT, rhs=rhs, start=start, stop=stop)


def TR(out, in_, ident):
    return lambda e: e.transpose(out, in_, ident)


def ACTF(out, in_, func, bias=0.0, scale=1.0, accum=None):
    if accum is None:
        return lambda e: e.activation(out=out, in_=in_, func=func, bias=bias, scale=scale)
    return lambda e: e.activation(out=out, in_=in_, func=func, bias=bias, scale=scale, accum_out=accum)


def TT(out, a, b, op):
    return lambda e: e.tensor_tensor(out=out, in0=a, in1=b, op=op)


def TS(out, a, s1, s2=None, op0=ALU.mult, op1=None, accum=None):
    if accum is not None:
        return lambda e: e.tensor_scalar(out=out, in0=a, scalar1=s1, scalar2=s2, op0=op0, op1=op1, accum_out=accum)
    if op1 is None:
        return lambda e: e.tensor_scalar(out=out, in0=a, scalar1=s1, scalar2=None, op0=op0)
    return lambda e: e.tensor_scalar(out=out, in0=a, scalar1=s1, scalar2=s2, op0=op0, op1=op1)


def STT(out, a, s, b, op0, op1):
    return lambda e: e.scalar_tensor_tensor(out=out, in0=a, scalar=s, in1=b, op0=op0, op1=op1)


def CP(out, in_):
    return lambda e: e.tensor_copy(out, in_)


def RED(out, in_, op, axis=AX.X, absval=False):
    if absval:
        return lambda e: e.tensor_reduce(out=out, in_=in_, axis=axis, op=op, apply_absolute_value=True)
    return lambda e: e.tensor_reduce(out=out, in_=in_, axis=axis, op=op)


def MEMSET(ap, v):
    return lambda e: e.memset(ap, v)


WSHAPES = {
    "w_ada": (1024, 6144), "b_ada": (6144,), "norm1_g": (1024,), "norm2_g": (1024,),
    "w_in": (1024, 3656), "mu_rkv": (3, 512), "mu_lora": (3, 1024), "decay_w0": (512,),
    "decay_a": (1024, 64), "decay_b": (64, 512), "iclr_a0": (512,), "iclr_a": (1024, 64),
    "iclr_b": (64, 512), "gate_a": (1024, 128), "gate_b": (128, 512), "k_k": (512,), "k_a": (512,),
    "r_k": (8, 64), "lnx_g": (512,), "lnx_b": (512,), "attn_out_g": (512,),
    "w_out": (1024, 1024), "w_mlp1": (1024, 4096), "w_mlp2": (4096, 1024),
}
VSHAPES = {"vres_mu": (1024,), "vres_v0": (512,), "vres_a": (1024, 32), "vres_b": (32, 512)}


def build(T, L, dbg=False, stop_after=None):
    _DEAD[0] = False
    NT = T // 128
    NB = T // 512
    NSEL = min(256, T // 4)
    nc = bass.Bass("TRN2", target_bir_lowering=False)
    W = {}

    def din(name, shape):
        W[name] = nc.dram_tensor(name, list(shape), F32, kind="ExternalInput").ap()
        return W[name]

    x_d = din("x", [T, 1024])
    c8_d = din("c8", [128, 8])
    consts_d = din("consts", [128, 640])
    bnear_d = din("bnear", [128, 2048])
    c31_d = din("c31", [1, 8])
    for k, s in WSHAPES.items():
        din(k, (L,) + s)
    for k, s in VSHAPES.items():
        din(k, (max(L - 1, 1),) + s)
    din("final_g", [1, 1024])
    out_d = nc.dram_tensor("out", [T, 1024], F32, kind="ExternalOutput").ap()

    def dscr(name, shape, dt):
        return nc.dram_tensor(name, list(shape), dt, kind=("ExternalOutput" if dbg else "Internal")).ap()

    xs_d = dscr("xs", [T, 1024], F32)
    mod_d = dscr("modd", [128, 6144], F32)
    featT_d = dscr("featT", [1600, T], BF16)
    vaug_d = dscr("vaug", [T, 520], BF16)
    wi_d = dscr("wid", [T, 8], F32)
    hT_d = dscr("hTd", [1024, T], BF16)
    vfirst_d = dscr("vfirst", [T, 512], F32)
    rwkvT_d = dscr("rwkvT", [512, T], BF16)
    attT_d = dscr("attT", [512, T], BF16)
    h2T_d = dscr("h2T", [1024, T], BF16)
    dbg_d = {}
    if dbg:
        for nm, shp in (("dbg_y", [T, 512]), ("dbg_score", [T, T]), ("dbg_thr", [T, 1]), ("dbg_v", [T, 512]),
                        ("dbg_r", [T, 512]), ("dbg_k", [T, 512]), ("dbg_a", [T, 512]), ("dbg_lw", [T, 512])):
            dbg_d[nm] = nc.dram_tensor(nm, shp, F32, kind="ExternalOutput").ap()

    uniq = [0]

    def SBT(name, shape, dt):
        uniq[0] += 1
        return nc.sbuf_tensor("%s_%d" % (name, uniq[0]), shape, dt)

    with contextlib.ExitStack() as gst:
        S = Sched(nc, gst)

        def gsb(name, shape, dt):
            return gst.enter_context(SBT(name, shape, dt))

        banks = [gst.enter_context(nc.psum_tensor("bank%d" % i, [128, 512], F32)) for i in range(8)]
        bank_i = [0]

        NROT = 6

        nrot = [8]

        def nb():
            i = bank_i[0] % nrot[0]
            bank_i[0] += 1
            return banks[i], "bank%d" % i

        def bfv(bank):
            return bank[:].bitcast(BF16)

        cst = gsb("cst", [128, 640], F32)
        identb = gsb("identb", [128, 128], BF16)
        mask4 = gsb("mask4", [128, 512], F32)
        lowm4 = gsb("lowm4", [128, 512], F32)
        onesf = gsb("onesf", [128, 128], F32)
        m05 = gsb("m05", [128, 512], F32)
        cbc = gsb("cbc", [128, 8, 128], F32)
        identf = cst[:, 0:128]
        tri_incl = cst[:, 128:256]
        tri_strict = cst[:, 256:384]
        low_strict = cst[:, 384:512]
        caus = cst[:, 512:640]

        with contextlib.ExitStack() as st:
            c8 = st.enter_context(SBT("c8s", [128, 8], F32))
            c8t = st.enter_context(SBT("c8t", [128, 8], F32))
            S.dma(cst[:], consts_d, w=["cst"])
            S.dma(c8[:], c8_d, w=["c8"])
            S.dve(CP(identb[:], identf), r=["cst"], w=["identb"])
            for i in range(4):
                S.dve(CP(mask4[:, i * 128:(i + 1) * 128], tri_strict if i % 2 == 0 else tri_incl), r=["cst"], w=["mask4"])
                S.pool(CP(lowm4[:, i * 128:(i + 1) * 128], low_strict), r=["cst"], w=["lowm4"])
            S.pool(MEMSET(onesf[:], 1.0), w=["onesf"])
            S.pool(MEMSET(m05[:], -0.5), w=["m05"])
            S.act(ACTF(c8t[:], c8[:], AF.Tanh, scale=0.5), r=["c8"], w=["c8t"])
            S.dve(TS(c8t[:], c8t[:], 0.5, 0.5, ALU.mult, ALU.add), r=["c8t"], w=["c8t"])
            S.dve(TT(c8t[:], c8t[:], c8[:], ALU.mult), r=["c8t", "c8"], w=["c8t"])
            S.dve(CP(cbc[:], c8t[:].unsqueeze(2).to_broadcast([128, 8, 128])), r=["c8t"], w=["cbc"])
            S.flush()

        def phase_M(l):
            with contextlib.ExitStack() as st:
                def sb(name, shape, dt):
                    return st.enter_context(SBT(name, shape, dt))
                wst = [sb("wst%d" % i, [128, 8, 512], F32) for i in range(2)]
                modt = sb("modt", [128, 6144], F32)
                gbc = sb("gbc", [128, 2048], F32)
                bada = sb("bada", [1, 6144], F32)
                S.dma(gbc[:, 0:1024], W["norm1_g"][l:l + 1, :].partition_broadcast(128), w=["gbc"])
                S.dma(gbc[:, 1024:2048], W["norm2_g"][l:l + 1, :].partition_broadcast(128), w=["gbc"])
                S.dma(bada[:], W["b_ada"][l:l + 1, :], w=["bada"])
                wa = W["w_ada"][l].rearrange("(kc p) n -> p kc n", p=128)
                for n in range(12):
                    buf = wst[n % 2]
                    key = "wst%d" % (n % 2)
                    S.dma(buf[:], wa[:, :, n * 512:(n + 1) * 512], w=[key])
                    bk, kb = nb()
                    for kc in range(8):
                        S.pe(MM(bk[:], cbc[:, kc, :], buf[:, kc, :], kc == 0, False), r=[key, "cbc"], w=[kb])
                    S.pe(MM(bk[:], onesf[0:1, :], bada[0:1, n * 512:(n + 1) * 512], False, True), r=["bada", "onesf"], w=[kb])
                    S.act(ACTF(modt[:, n * 512:(n + 1) * 512], bk[:], AF.Copy), r=[kb], w=["modt"])
                S.dve(STT(modt[:, 1024:2048], modt[:, 1024:2048], 1.0, gbc[:, 0:1024], ALU.add, ALU.mult), r=["modt", "gbc"], w=["modt"])
                S.dve(STT(modt[:, 4096:5120], modt[:, 4096:5120], 1.0, gbc[:, 1024:2048], ALU.add, ALU.mult), r=["modt", "gbc"], w=["modt"])
                S.dma(mod_d, modt[:], r=["modt"], w=["mod_d"])
                S.flush()

        def norm_mod_T(X, kx, At, sht, hb, khb, junk, ssq, ms, rstd, tmp, idx, dstT, kdst):
            S.act(ACTF(junk[:], X, AF.Square, accum=ssq[:]), r=[kx], w=["ssq%d" % idx])
            S.dve(TS(ms[:], ssq[:], 1.0 / 1024, 1e-6, ALU.mult, ALU.add), r=["ssq%d" % idx], w=["ms%d" % idx])
            S.pool(TT(rstd[:], ms[:], m05[:, 0:1], ALU.pow), r=["ms%d" % idx, "m05"], w=["rstd%d" % idx])
            S.dve(STT(tmp[:], X, rstd[:, 0:1], At, ALU.mult, ALU.mult), r=[kx, "rstd%d" % idx, "modp"], w=["tmpn%d" % idx])
            S.pool(TT(hb[:], tmp[:], sht, ALU.add), r=["tmpn%d" % idx, "modp"], w=[khb])
            bk, kb = nb()
            bkb = bfv(bk)
            for kc in range(8):
                S.pe(TR(bkb[:, kc * 128:(kc + 1) * 128], hb[:, kc * 128:(kc + 1) * 128], identb[:]), r=[khb, "identb"], w=[kb])
            S.act(ACTF(dstT, bkb.rearrange("p (k t) -> p k t", k=8), AF.Copy), r=[kb], w=[kdst])

        def phase_A1(l, xsrc):
            with contextlib.ExitStack() as st:
                def sb(name, shape, dt):
                    return st.enter_context(SBT(name, shape, dt))
                WF = sb("WF", [128, 8, 1600], BF16)
                WV = sb("WV", [128, 8, 520], BF16)
                A1t = sb("A1t", [128, 1024], F32)
                sh1t = sb("sh1t", [128, 1024], F32)
                xb = [sb("xb%d" % i, [128, 1024], F32) for i in range(2)]
                junk = sb("junk", [128, 1024], F32)
                tmp = [sb("tmpn%d" % i, [128, 1024], F32) for i in range(2)]
                hb = [sb("hb%d" % i, [128, 1024], BF16) for i in range(2)]
                hT = [sb("hT%d" % i, [128, 8, 512], BF16) for i in range(2)]
                fst = [sb("fst%d" % i, [128, 512], BF16) for i in range(2)]
                vst = [sb("vst%d" % i, [128, 8, 65], BF16) for i in range(2)]
                wist = [sb("wist%d" % i, [128, 8], F32) for i in range(2)]
                ssq = [sb("ssq%d" % i, [128, 1], F32) for i in range(2)]
                ms = [sb("ms%d" % i, [128, 1], F32) for i in range(2)]
                rstd = [sb("rstd%d" % i, [128, 1], F32) for i in range(2)]
                wv = W["w_in"][l].rearrange("(kc p) n -> p kc n", p=128)
                S.dma(WF[:, :, 0:1024], wv[:, :, 1536:2560], w=["WF"], eng="pool")
                S.dma(WF[:, :, 1024:1600], wv[:, :, 3072:3648], w=["WF"], eng="pool")
                S.dma(WV[:, :, 0:512], wv[:, :, 2560:3072], w=["WV"], eng="pool")
                S.dma(WV[:, :, 512:520], wv[:, :, 3648:3656], w=["WV"], eng="pool")
                S.dma(A1t[:], mod_d[:, 1024:2048], r=["mod_d"], w=["modp"])
                S.dma(sh1t[:], mod_d[:, 0:1024], r=["mod_d"], w=["modp"])
                for i in range(2):
                    S.pool(MEMSET(vst[i][:, :, 64:65], 1.0), w=["vst%d" % i])

                def load(t):
                    S.dma(xb[t % 2][:], xsrc[t * 128:(t + 1) * 128, :], r=["xs_d"], w=["xb%d" % (t % 2)])
                load(0)
                for b in range(NB):
                    hTb = hT[b % 2]
                    kh = "hT%d" % (b % 2)
                    for j in range(4):
                        t = b * 4 + j
                        if t + 1 < NT:
                            load(t + 1)
                        i2 = t % 2
                        norm_mod_T(xb[i2][:], "xb%d" % i2, A1t[:], sh1t[:], hb[i2], "hb%d" % i2, junk, ssq[i2], ms[i2],
                                   rstd[i2], tmp[i2], i2, hTb[:, :, j * 128:(j + 1) * 128], kh)
                        bk, kb = nb()
                        bk2, kb2 = nb()
                        for kc in range(8):
                            S.pe(MM(bk[:], hTb[:, kc, j * 128:(j + 1) * 128], WV[:, kc, 0:512], kc == 0, kc == 7), r=[kh, "WV"], w=[kb])
                        for kc in range(8):
                            S.pe(MM(bk2[:, 0:8], hTb[:, kc, j * 128:(j + 1) * 128], WV[:, kc, 512:520], kc == 0, kc == 7), r=[kh, "WV"], w=[kb2])
                        S.dve(CP(vst[i2][:, :, 0:64], bk[:].rearrange("p (h d) -> p h d", h=8)), r=[kb], w=["vst%d" % i2])
                        S.act(ACTF(wist[i2][:], bk2[:, 0:8], AF.Copy), r=[kb2], w=["wist%d" % i2])
                        S.dma(vaug_d[t * 128:(t + 1) * 128, :], vst[i2][:].rearrange("p h d -> p (h d)"), r=["vst%d" % i2], w=["vaug_d"])
                        S.dma(wi_d[t * 128:(t + 1) * 128, :], wist[i2][:], r=["wist%d" % i2], w=["wi_d"])
                    for c in range(13):
                        rows = 128 if c < 12 else 64
                        bk, kb = nb()
                        for kc in range(8):
                            S.pe(MM(bk[0:rows, :], WF[:, kc, c * 128:c * 128 + rows], hTb[:, kc, :], kc == 0, kc == 7), r=[kh, "WF"], w=[kb])
                        f = fst[c % 2]
                        kf = "fst%d" % (c % 2)
                        if c % 2 == 0:
                            S.act(ACTF(f[0:rows, :], bk[0:rows, :], AF.Copy), r=[kb], w=[kf])
                        else:
                            S.dve(CP(f[0:rows, :], bk[0:rows, :]), r=[kb], w=[kf])
                        S.dma(featT_d[c * 128:c * 128 + rows, b * 512:(b + 1) * 512], f[0:rows, :], r=[kf], w=["featT_d"])
                    S.dma(hT_d[:, b * 512:(b + 1) * 512].rearrange("(kc p) t -> p kc t", p=128), hTb[:], r=[kh], w=["hT_d"])
                S.flush()

        def phase_A2(l):
            with contextlib.ExitStack() as st:
                def sb(name, shape, dt):
                    return st.enter_context(SBT(name, shape, dt))
                WT1 = sb("WT1", [128, 8, 1536], BF16)
                WT2 = sb("WT2", [128, 8, 1536], BF16)
                LA1 = sb("LA1", [128, 8, 288], BF16)
                LA2 = sb("LA2", [128, 8, 288], BF16)
                Bwa = sb("Bwa", [128, 512], BF16)
                Bg = sb("Bg", [128, 512], BF16)
                Bv = sb("Bv", [32, 512], BF16)
                brow = sb("brow", [1, 3, 512], F32)
                pbc = sb("pbc", [128, 5, 512], F32)
                muT = sb("muT", [128, 32], F32)
                omT = sb("omT", [128, 32], F32)
                with contextlib.ExitStack() as st2:
                    Wr = st2.enter_context(SBT("Wr", [128, 8, 1536], BF16))
                    mubc = st2.enter_context(SBT("mubc", [128, 1536], F32))
                    ombc = st2.enter_context(SBT("ombc", [128, 1536], F32))
                    LA = st2.enter_context(SBT("LA", [128, 8, 288], F32))
                    mu32 = st2.enter_context(SBT("mu32", [32, 128], F32))
                    wv = W["w_in"][l].rearrange("(kc p) n -> p kc n", p=128)
                    S.dma(Wr[:, :, 0:768], wv[:, :, 0:768], w=["Wr"], eng="pool")
                    S.dma(Wr[:, :, 768:1536], wv[:, :, 768:1536], w=["Wr"], eng="pool")
                    S.dma(mubc[:], W["mu_rkv"][l:l + 1].rearrange("o a b -> o (a b)").partition_broadcast(128), w=["mubc"])
                    S.dve(TS(ombc[:], mubc[:], -1.0, 1.0, ALU.mult, ALU.add), r=["mubc"], w=["ombc"])
                    S.dve(TT(WT2[:], Wr[:], mubc[:].unsqueeze(1).to_broadcast([128, 8, 1536]), ALU.mult), r=["Wr", "mubc"], w=["WT2"])
                    S.pool(TT(WT1[:], Wr[:], ombc[:].unsqueeze(1).to_broadcast([128, 8, 1536]), ALU.mult), r=["Wr", "ombc"], w=["WT1"])
                    S.dma(LA[:, :, 0:64], W["decay_a"][l].rearrange("(kc p) n -> p kc n", p=128), w=["LA"])
                    S.dma(LA[:, :, 64:128], W["iclr_a"][l].rearrange("(kc p) n -> p kc n", p=128), w=["LA"])
                    S.dma(LA[:, :, 128:256], W["gate_a"][l].rearrange("(kc p) n -> p kc n", p=128), w=["LA"])
                    if l > 0:
                        S.dma(LA[:, :, 256:288], W["vres_a"][l - 1].rearrange("(kc p) n -> p kc n", p=128), w=["LA"])
                    else:
                        S.dve(MEMSET(LA[:, :, 256:288], 0.0), w=["LA"])
                    S.dve(MEMSET(mu32[:], 0.0), w=["mu32"])
                    S.dma(mu32[0:24, :], W["mu_lora"][l].rearrange("a (kc p) -> (a kc) p", p=128), r=["mu32"], w=["mu32"])
                    if l > 0:
                        S.dma(mu32[24:32, :], W["vres_mu"][l - 1:l, :].rearrange("o (kc p) -> (o kc) p", p=128), r=["mu32"], w=["mu32"])
                    bk, kb = nb()
                    S.pe(MM(bk[:, 0:32], mu32[:], identf[0:32, 0:32], True, True), r=["mu32", "cst"], w=[kb])
                    S.act(ACTF(muT[:], bk[:, 0:32], AF.Copy), r=[kb], w=["muT"])
                    S.dve(TS(omT[:], muT[:], -1.0, 1.0, ALU.mult, ALU.add), r=["muT"], w=["omT"])
                    for gi, (c0, c1) in enumerate(((0, 64), (64, 128), (128, 256), (256, 288))):
                        wdt = c1 - c0
                        S.dve(TT(LA2[:, :, c0:c1], LA[:, :, c0:c1], muT[:, gi * 8:(gi + 1) * 8].unsqueeze(2).to_broadcast([128, 8, wdt]), ALU.mult), r=["LA", "muT"], w=["LA2"])
                        S.dve(TT(LA1[:, :, c0:c1], LA[:, :, c0:c1], omT[:, gi * 8:(gi + 1) * 8].unsqueeze(2).to_broadcast([128, 8, wdt]), ALU.mult), r=["LA", "omT"], w=["LA1"])
                    S.dma(Bwa[0:64, :], W["decay_b"][l], w=["Bwa"], eng="pool")
                    S.dma(Bwa[64:128, :], W["iclr_b"][l], w=["Bwa"], eng="pool")
                    S.dma(Bg[:], W["gate_b"][l], w=["Bg"], eng="pool")
                    if l > 0:
                        S.dma(Bv[:], W["vres_b"][l - 1], w=["Bv"], eng="pool")
                    S.dma(brow[:, 0, :], W["decay_w0"][l:l + 1, :], w=["brow"])
                    S.dma(brow[:, 1, :], W["iclr_a0"][l:l + 1, :], w=["brow"])
                    if l > 0:
                        S.dma(brow[:, 2, :], W["vres_v0"][l - 1:l, :], w=["brow"])
                    S.dma(pbc[:, 0, :], W["k_k"][l:l + 1, :].partition_broadcast(128), w=["pbc"])
                    S.dma(pbc[:, 1, :], W["k_a"][l:l + 1, :].partition_broadcast(128), w=["pbc"])
                    S.dma(pbc[:, 2, :], W["r_k"][l:l + 1].rearrange("o a b -> o (a b)").partition_broadcast(128), w=["pbc"])
                    S.dma(pbc[:, 3, :], W["lnx_g"][l:l + 1, :].partition_broadcast(128), w=["pbc"])
                    S.dma(pbc[:, 4, :], W["lnx_b"][l:l + 1, :].partition_broadcast(128), w=["pbc"])
                    S.flush()

                chk(1)
                hTb2 = [sb("hTb%d" % i, [128, 8, 512], BF16) for i in range(1)]
                hTp2 = [sb("hTp%d" % i, [128, 8, 512], BF16) for i in range(1)]
                L1wa = sb("L1wa", [128, 512], BF16)
                sg = sb("sg", [128, 512], BF16)
                sgt = sb("sgt", [128, 512], F32)
                L1v = sb("L1v", [32, 512], BF16)

                def f32t(name):
                    return sb(name, [128, 512], F32)

                def b16t(name):
                    return sb(name, [128, 512], BF16)
                r32, k32, v32 = f32t("r32"), f32t("k32"), f32t("v32")
                kkr, kk, a32, b32, km = f32t("kkr"), f32t("kk"), f32t("a32"), f32t("b32"), f32t("km")
                lw, Ginc, Ginv, Gexc, g32 = f32t("lw"), f32t("Ginc"), f32t("Ginv"), f32t("Gexc"), f32t("g32")
                t1, t2, U0, cen, vf = f32t("t1"), f32t("t2"), f32t("U0"), f32t("cen"), f32t("vf")
                y32 = f32t("y32")
                Kd, Rd, Bi, Ki, Vb, Zb, Ub, ob = (b16t(n) for n in ("Kd", "Rd", "Bi", "Ki", "Vb", "Zb", "Ub", "ob"))
                s8 = [sb("s8_%d" % i, [128, 8], F32) for i in range(6)]
                FT = sb("FT", [64, 8, 4, 128], BF16)
                GE = sb("GE", [128, 8, 512], BF16)
                MMb = [sb("MMb%d" % i, [128, 8, 128], F32) for i in range(2)]
                NNb = [sb("NNb%d" % i, [128, 8, 128], F32) for i in range(2)]
                TTb = [sb("TTb%d" % i, [128, 8, 128], F32) for i in range(2)]
                TTh = sb("TTh", [128, 8, 128], BF16)
                WTs = sb("WTs", [64, 8, 128], BF16)
                P32 = sb("P32", [64, 8, 64], F32)
                Pb = sb("Pb", [64, 8, 64], BF16)
                GC = sb("GC", [64, 8], F32)
                rwT = sb("rwT", [128, 4, 128], BF16)
                S.dve(MEMSET(P32[:], 0.0), w=["P32"])
                S.dve(MEMSET(Pb[:], 0.0), w=["Pb"])
                P32f = P32[:].rearrange("p h v -> p (h v)")
                ev = [0]

                def evac(out, in_, r, w):
                    ev[0] += 1
                    if ev[0] % 2:
                        S.act(ACTF(out, in_, AF.Copy), r=r, w=w)
                    else:
                        S.dve(CP(out, in_), r=r, w=w)

                for b in range(NB):
                    hTb = hTb2[0]
                    kh = "hTb0"
                    src = hT_d.rearrange("(kc p) t -> p kc t", p=128)
                    if b == 1:
                        chk(8)
                    hTp = hTp2[0]
                    S.dma(hTb[:], src[:, :, b * 512:(b + 1) * 512], r=["hT_d"], w=[kh])
                    if b == 0:
                        S.dve(MEMSET(hTp[:, :, 0:1], 0.0), w=[kh])
                        S.dma(hTp[:, :, 1:512], src[:, :, 0:511], r=["hT_d"], w=[kh])
                    else:
                        S.dma(hTp[:], src[:, :, b * 512 - 1:b * 512 + 511], r=["hT_d"], w=[kh])
                    if b == 1:
                        chk(9)
                    for gi, (c0, c1) in enumerate(((0, 128), (128, 256), (256, 288))):
                        if gi == 2 and l == 0:
                            continue
                        rows = c1 - c0
                        bk, kb = nb()
                        for kc in range(8):
                            S.pe(MM(bk[0:rows, :], LA1[:, kc, c0:c1], hTb[:, kc, :], kc == 0, False), r=[kh, "LA1"], w=[kb])
                        for kc in range(8):
                            S.pe(MM(bk[0:rows, :], LA2[:, kc, c0:c1], hTp[:, kc, :], False, kc == 7), r=[kh, "LA2"], w=[kb])
                        if gi == 0:
                            S.act(ACTF(L1wa[0:64, :], bk[0:64, :], AF.Tanh), r=[kb], w=["L1wa"])
                            S.act(ACTF(L1wa[64:128, :], bk[64:128, :], AF.Copy), r=[kb], w=["L1wa"])
                        elif gi == 1:
                            S.act(ACTF(sgt[:], bk[:], AF.Tanh, scale=0.5), r=[kb], w=["sgt"])
                            S.dve(TS(sg[:], sgt[:], 0.5, 0.5, ALU.mult, ALU.add), r=["sgt"], w=["sg"])
                        else:
                            S.act(ACTF(L1v[:], bk[0:32, :], AF.Copy), r=[kb], w=["L1v"])
                    chk(2)
                    for j in range(4):
                        t = b * 4 + j
                        lo = j * 128
                        tsl = slice(j * 128, (j + 1) * 128)
                        if l > 0:
                            S.dma(vf[:], vfirst_d[t * 128:(t + 1) * 128, :], r=["vfirst_d"], w=["vf"])
                        for g, (dst, kd) in enumerate(((r32, "r32"), (k32, "k32"), (v32, "v32"))):
                            bk, kb = nb()
                            for kc in range(8):
                                S.pe(MM(bk[:], hTb[:, kc, lo:lo + 128], WT1[:, kc, g * 512:(g + 1) * 512], kc == 0, False), r=[kh, "WT1"], w=[kb])
                            for kc in range(8):
                                S.pe(MM(bk[:], hTp[:, kc, lo:lo + 128], WT2[:, kc, g * 512:(g + 1) * 512], False, kc == 7), r=[kh, "WT2"], w=[kb])
                            evac(dst[:], bk[:], [kb], [kd])
                        bkw, kbw = nb()
                        S.pe(MM(bkw[:], L1wa[0:64, tsl], Bwa[0:64, :], True, False), r=["L1wa", "Bwa"], w=[kbw])
                        S.pe(MM(bkw[:], onesf[0:1, :], brow[0:1, 0, :], False, True), r=["onesf", "brow"], w=[kbw])
                        bka, kba = nb()
                        S.pe(MM(bka[:], L1wa[64:128, tsl], Bwa[64:128, :], True, False), r=["L1wa", "Bwa"], w=[kba])
                        S.pe(MM(bka[:], onesf[0:1, :], brow[0:1, 1, :], False, True), r=["onesf", "brow"], w=[kba])
                        bkg, kbg = nb()
                        S.pe(MM(bkg[:], sg[:, tsl], Bg[:], True, True), r=["sg", "Bg"], w=[kbg])
                        S.act(ACTF(lw[:], bkw[:], AF.Tanh, scale=0.5), r=[kbw], w=["lw"])
                        S.dve(TS(lw[:], lw[:], -0.5 * math.exp(-0.5), -0.5 * math.exp(-0.5), ALU.mult, ALU.add), r=["lw"], w=["lw"])
                        S.act(ACTF(a32[:], bka[:], AF.Tanh, scale=0.5), r=[kba], w=["a32"])
                        S.pool(TS(a32[:], a32[:], 0.5, 0.5, ALU.mult, ALU.add), r=["a32"], w=["a32"])
                        S.act(ACTF(g32[:], bkg[:], AF.Copy), r=[kbg], w=["g32"])
                        if l > 0:
                            bkv, kbv = nb()
                            S.pe(MM(bkv[:], L1v[0:32, tsl], Bv[0:32, :], True, False), r=["L1v", "Bv"], w=[kbv])
                            S.pe(MM(bkv[:], onesf[0:1, :], brow[0:1, 2, :], False, True), r=["onesf", "brow"], w=[kbv])
                            S.act(ACTF(t1[:], bkv[:], AF.Tanh, scale=0.5), r=[kbv], w=["t1"])
                            S.pool(TS(t1[:], t1[:], 0.5, 0.5, ALU.mult, ALU.add), r=["t1"], w=["t1"])
                            S.pool(TT(t2[:], vf[:], v32[:], ALU.subtract), r=["vf", "v32"], w=["t2"])
                            S.pool(TT(t2[:], t2[:], t1[:], ALU.mult), r=["t2", "t1"], w=["t2"])
                            S.pool(TT(v32[:], v32[:], t2[:], ALU.add), r=["v32", "t2"], w=["v32"])
                        else:
                            S.dma(vfirst_d[t * 128:(t + 1) * 128, :], v32[:], r=["v32"], w=["vfirst_d"])
                        S.act(ACTF(Vb[:], v32[:], AF.Copy), r=["v32"], w=["Vb"])
                        if dbg:
                            rows = slice(t * 128, (t + 1) * 128)
                            S.dma(dbg_d["dbg_v"][rows, :], v32[:], r=["v32"], w=["dbgv"])
                            S.dma(dbg_d["dbg_r"][rows, :], r32[:], r=["r32"], w=["dbgr"])
                            S.dma(dbg_d["dbg_k"][rows, :], k32[:], r=["k32"], w=["dbgk"])
                            S.dma(dbg_d["dbg_a"][rows, :], a32[:], r=["a32"], w=["dbga"])
                            S.dma(dbg_d["dbg_lw"][rows, :], lw[:], r=["lw"], w=["dbglw"])
                        bkc, kbc = nb()
                        S.pe(MM(bkc[:], tri_incl, lw[:], True, True), r=["cst", "lw"], w=[kbc])
                        bke, kbe = nb()
                        S.pe(MM(bke[:], tri_strict, lw[:], True, True), r=["cst", "lw"], w=[kbe])
                        bkG, kbG = nb()
                        for h in range(8):
                            S.pe(MM(bkG[0:64, h:h + 1], lw[:, h * 64:(h + 1) * 64], onesf[:, 0:1], True, True), r=["lw", "onesf"], w=[kbG])
                        S.act(ACTF(Ginc[:], bkc[:], AF.Exp), r=[kbc], w=["Ginc"])
                        S.act(ACTF(Ginv[:], bkc[:], AF.Exp, scale=-1.0), r=[kbc], w=["Ginv"])
                        S.act(ACTF(Gexc[:], bke[:], AF.Exp), r=[kbe], w=["Gexc"])
                        S.act(ACTF(GC[:], bkG[0:64, 0:8], AF.Exp), r=[kbG], w=["GC"])
                        S.dve(TT(kkr[:], k32[:], pbc[:, 0, :], ALU.mult), r=["k32", "pbc"], w=["kkr"])
                        S.act(ACTF(t2[:], kkr[:], AF.Square), r=["kkr"], w=["t2"])
                        S.dve(RED(s8[0][:], t2[:].rearrange("p (h d) -> p h d", h=8), ALU.add), r=["t2"], w=["s8_0"])
                        S.dve(TS(s8[0][:], s8[0][:], 1e-24, None, ALU.max), r=["s8_0"], w=["s8_0"])
                        S.pool(TT(s8[1][:], s8[0][:], m05[:, 0:8], ALU.pow), r=["s8_0", "m05"], w=["s8_1"])
                        S.dve(TT(kk[:].rearrange("p (h d) -> p h d", h=8), kkr[:].rearrange("p (h d) -> p h d", h=8),
                                 s8[1][:].unsqueeze(2).to_broadcast([128, 8, 64]), ALU.mult), r=["kkr", "s8_1"], w=["kk"])
                        S.pool(TT(b32[:], kk[:], a32[:], ALU.mult), r=["kk", "a32"], w=["b32"])
                        S.dve(STT(t1[:], a32[:], -1.0, pbc[:, 1, :], ALU.add, ALU.mult), r=["a32", "pbc"], w=["t1"])
                        S.dve(STT(km[:], t1[:], 1.0, k32[:], ALU.add, ALU.mult), r=["t1", "k32"], w=["km"])
                        S.dve(TT(Kd[:], kk[:], Gexc[:], ALU.mult), r=["kk", "Gexc"], w=["Kd"])
                        S.pool(TT(Bi[:], b32[:], Ginv[:], ALU.mult), r=["b32", "Ginv"], w=["Bi"])
                        S.dve(TT(Ki[:], km[:], Ginv[:], ALU.mult), r=["km", "Ginv"], w=["Ki"])
                        S.pool(TT(Rd[:], r32[:], Ginc[:], ALU.mult), r=["r32", "Ginc"], w=["Rd"])
                        S.pool(TT(t2[:], r32[:], km[:], ALU.mult), r=["r32", "km"], w=["t2"])
                        S.pool(TT(t2[:], t2[:], pbc[:, 2, :], ALU.mult), r=["t2", "pbc"], w=["t2"])
                        S.dve(RED(s8[2][:], t2[:].rearrange("p (h d) -> p h d", h=8), ALU.add), r=["t2"], w=["s8_2"])
                        chk(3)
                        for q, (srcT, ks) in enumerate(((Kd, "Kd"), (Rd, "Rd"), (Bi, "Bi"), (Ki, "Ki"))):
                            bk, kb = nb()
                            bkb = bfv(bk)
                            for h in range(8):
                                S.pe(TR(bkb[0:64, h * 128:(h + 1) * 128], srcT[:, h * 64:(h + 1) * 64], identb[:]), r=[ks, "identb"], w=[kb])
                            evac(FT[:, :, q, :], bkb[0:64, :].rearrange("p (h t) -> p h t", h=8), [kb], ["FT"])
                        for h in range(8):
                            bk, kb = nb()
                            rhs = FT[:, h, 0:2, :].rearrange("p a t -> p (a t)")
                            S.pe(MM(bk[:, 0:256], FT[:, h, 2, :], rhs, True, True), r=["FT"], w=[kb])
                            S.pe(MM(bk[:, 256:512], FT[:, h, 3, :], rhs, True, True), r=["FT"], w=[kb])
                            S.dve(TT(GE[:, h, :], bk[:], mask4[:], ALU.mult), r=[kb, "mask4"], w=["GE"])
                            S.dve(TT(NNb[0][:, h, :], bk[:, 0:128], tri_strict, ALU.mult), r=[kb, "cst"], w=["NNb0"])
                        for g in range(2):
                            bk, kb = nb()
                            for hh in range(4):
                                h = g * 4 + hh
                                S.pe(MM(bk[:, hh * 128:(hh + 1) * 128], FT[:, h, 0, :], FT[:, h, 2, :], True, True), r=["FT"], w=[kb])
                            S.dve(TT(MMb[0][:, g * 4:(g + 1) * 4, :], bk[:].rearrange("p (h t) -> p h t", h=4),
                                     lowm4[:].rearrange("p (h t) -> p h t", h=4), ALU.mult), r=[kb, "lowm4"], w=["MMb0"])
                        chk(4)
                        S.pool(TT(TTb[0][:], identf.unsqueeze(1).to_broadcast([128, 8, 128]), NNb[0][:], ALU.subtract),
                               r=["cst", "NNb0"], w=["TTb0"])
                        cur = 0
                        for step in range(1, 7):
                            nxt = 1 - cur
                            sqb = []
                            for g in range(2):
                                hs = range(g * 4, g * 4 + 4)
                                bkM, kbM = nb()
                                for hh, h in enumerate(hs):
                                    S.pe(MM(bkM[:, hh * 128:(hh + 1) * 128], NNb[cur][:, h, :], MMb[cur][:, h, :], True, True),
                                         r=["NNb%d" % cur, "MMb%d" % cur], w=[kbM])
                                bkN = kbN = None
                                if step < 6:
                                    bkN, kbN = nb()
                                    for hh, h in enumerate(hs):
                                        S.pe(MM(bkN[:, hh * 128:(hh + 1) * 128], MMb[cur][:, h, :], NNb[cur][:, h, :], True, True),
                                             r=["NNb%d" % cur, "MMb%d" % cur], w=[kbN])
                                sqb.append((bkM, kbM, bkN, kbN))
                            for g in range(2):
                                bkM, kbM, bkN, kbN = sqb[g]
                                evac(MMb[nxt][:, g * 4:(g + 1) * 4, :], bkM[:].rearrange("p (h t) -> p h t", h=4), [kbM], ["MMb%d" % nxt])
                                if bkN is not None:
                                    evac(NNb[nxt][:, g * 4:(g + 1) * 4, :], bkN[:].rearrange("p (h t) -> p h t", h=4), [kbN], ["NNb%d" % nxt])
                            for g in range(2):
                                hs = range(g * 4, g * 4 + 4)
                                bkT, kbT = nb()
                                for hh, h in enumerate(hs):
                                    S.pe(MM(bkT[:, hh * 128:(hh + 1) * 128], MMb[nxt][:, h, :], TTb[cur][:, h, :], True, True),
                                         r=["MMb%d" % nxt, "TTb%d" % cur], w=[kbT])
                                S.dve(TT(TTb[nxt][:, g * 4:(g + 1) * 4, :].rearrange("p h t -> p (h t)"), bkT[:],
                                         TTb[cur][:, g * 4:(g + 1) * 4, :].rearrange("p h t -> p (h t)"), ALU.add),
                                      r=[kbT, "TTb%d" % cur], w=["TTb%d" % nxt])
                            cur = nxt
                        S.act(ACTF(TTh[:], TTb[cur][:], AF.Copy), r=["TTb%d" % cur], w=["TTh"])
                        TTf = TTh
                        kT = "TTh"
                        for g in range(2):
                            bk, kb = nb()
                            for hh in range(4):
                                h = g * 4 + hh
                                S.pe(MM(bk[0:64, hh * 128:(hh + 1) * 128], Kd[:, h * 64:(h + 1) * 64], TTf[:, h, :], True, True), r=["Kd", kT], w=[kb])
                            evac(WTs[:, g * 4:(g + 1) * 4, :], bk[0:64, :].rearrange("p (h t) -> p h t", h=4), [kb], ["WTs"])
                        bk, kb = nb()
                        for h in range(8):
                            S.pe(MM(bk[:, h * 64:(h + 1) * 64], GE[:, h, 256:384], Vb[:, h * 64:(h + 1) * 64], True, True), r=["GE", "Vb"], w=[kb])
                        evac(Zb[:], bk[:], [kb], ["Zb"])
                        bk, kb = nb()
                        for h in range(8):
                            S.pe(MM(bk[:, h * 64:(h + 1) * 64], TTf[:, h, :], Zb[:, h * 64:(h + 1) * 64], True, True), r=[kT, "Zb"], w=[kb])
                        evac(U0[:], bk[:], [kb], ["U0"])
                        chk(6)
                        bk, kb = nb()
                        for h in range(8):
                            S.pe(MM(bk[:, h * 64:(h + 1) * 64], WTs[:, h, :], Pb[:, h, :], True, True), r=["WTs", "Pb"], w=[kb])
                        S.dve(STT(Ub[:], bk[:], -1.0, U0[:], ALU.mult, ALU.subtract), r=[kb, "U0"], w=["Ub"])
                        bkY, kbY = nb()
                        for h in range(8):
                            hs_ = slice(h * 64, (h + 1) * 64)
                            S.pe(MM(bkY[:, hs_], FT[:, h, 1, :], Pb[:, h, :], True, False), r=["FT", "Pb"], w=[kbY])
                            S.pe(MM(bkY[:, hs_], GE[:, h, 128:256], Ub[:, hs_], False, False), r=["GE", "Ub"], w=[kbY])
                            S.pe(MM(bkY[:, hs_], GE[:, h, 384:512], Vb[:, hs_], False, True), r=["GE", "Vb"], w=[kbY])
                        bkX, kbX = nb()
                        S.pe(MM(bkX[0:64, :], identf[0:64, 0:64], P32f, True, False), r=["cst", "P32"], w=[kbX])
                        for h in range(8):
                            hs_ = slice(h * 64, (h + 1) * 64)
                            S.pe(MM(bkX[0:64, hs_], Bi[:, hs_], Ub[:, hs_], False, False), r=["Bi", "Ub"], w=[kbX])
                            S.pe(MM(bkX[0:64, hs_], Ki[:, hs_], Vb[:, hs_], False, h == 7), r=["Ki", "Vb"], w=[kbX])
                        S.dve(TT(P32[:], bkX[0:64, :].rearrange("p (h v) -> p h v", h=8), GC[:].unsqueeze(2).to_broadcast([64, 8, 64]), ALU.mult),
                              r=[kbX, "GC"], w=["P32"])
                        S.act(ACTF(Pb[:], P32[:], AF.Copy), r=["P32"], w=["Pb"])
                        chk(7)
                        S.act(ACTF(y32[:], bkY[:], AF.Copy), r=[kbY], w=["y32"])
                        if dbg:
                            S.dma(dbg_d["dbg_y"][t * 128:(t + 1) * 128, :], y32[:], r=["y32"], w=["dbgy"])
                        chk(10)
                        Y3 = y32[:].rearrange("p (h d) -> p h d", h=8)
                        S.dve(RED(s8[3][:], Y3, ALU.add), r=["y32"], w=["s8_3"])
                        S.dve(TS(s8[3][:], s8[3][:], 1.0 / 64, None, ALU.mult), r=["s8_3"], w=["s8_3"])
                        S.dve(TT(cen[:].rearrange("p (h d) -> p h d", h=8), Y3, s8[3][:].unsqueeze(2).to_broadcast([128, 8, 64]), ALU.subtract),
                              r=["y32", "s8_3"], w=["cen"])
                        S.act(ACTF(t2[:], cen[:], AF.Square), r=["cen"], w=["t2"])
                        S.dve(RED(s8[4][:], t2[:].rearrange("p (h d) -> p h d", h=8), ALU.add), r=["t2"], w=["s8_4"])
                        S.dve(TS(s8[4][:], s8[4][:], 1.0 / 64, 64e-5, ALU.mult, ALU.add), r=["s8_4"], w=["s8_4"])
                        S.pool(TT(s8[5][:], s8[4][:], m05[:, 0:8], ALU.pow), r=["s8_4", "m05"], w=["s8_5"])
                        S.dve(TT(cen[:].rearrange("p (h d) -> p h d", h=8), cen[:].rearrange("p (h d) -> p h d", h=8),
                                 s8[5][:].unsqueeze(2).to_broadcast([128, 8, 64]), ALU.mult), r=["cen", "s8_5"], w=["cen"])
                        S.pool(TT(cen[:], cen[:], pbc[:, 3, :], ALU.mult), r=["cen", "pbc"], w=["cen"])
                        S.pool(TT(cen[:], cen[:], pbc[:, 4, :], ALU.add), r=["cen", "pbc"], w=["cen"])
                        S.dve(TT(t2[:].rearrange("p (h d) -> p h d", h=8), v32[:].rearrange("p (h d) -> p h d", h=8),
                                 s8[2][:].unsqueeze(2).to_broadcast([128, 8, 64]), ALU.mult), r=["v32", "s8_2"], w=["t2"])
                        S.pool(TT(cen[:], cen[:], t2[:], ALU.add), r=["cen", "t2"], w=["cen"])
                        S.dve(TT(ob[:], cen[:], g32[:], ALU.mult), r=["cen", "g32"], w=["ob"])
                        chk(11)
                        bk, kb = nb()
                        bkb = bfv(bk)
                        for c in range(4):
                            S.pe(TR(bkb[:, c * 128:(c + 1) * 128], ob[:, c * 128:(c + 1) * 128], identb[:]), r=["ob", "identb"], w=[kb])
                        evac(rwT[:], bkb[:, 0:512].rearrange("p (c t) -> p c t", c=4), [kb], ["rwT"])
                        S.dma(rwkvT_d[:, t * 128:(t + 1) * 128].rearrange("(c p) t -> p c t", p=128), rwT[:], r=["rwT"], w=["rwkvT_d"])
                        chk(12)
                S.flush()

        def phase_B(l):
            nrot[0] = NROT
            with contextlib.ExitStack() as st:
                def sb(name, shape, dt):
                    return st.enter_context(SBT(name, shape, dt))
                c31b = sb("c31b", [128, 8], F32)
                nc31 = sb("nc31", [128, 8], F32)
                Rn = sb("Rn", [128, 8, 256], BF16)
                aog = sb("aog", [64, 8], F32)
                statL = sb("statL", [65, 64], F32)
                S.dma(c31b[:], c31_d.partition_broadcast(128), w=["c31b"])
                aog8 = sb("aog8", [8, 64], F32)
                S.dma(aog8[:], W["attn_out_g"][l].rearrange("(h d) -> h d", d=64), w=["aog8"])
                bk, kb = nb()
                S.pe(MM(bk[0:64, 0:8], aog8[:], identf[0:8, 0:8], True, True), r=["aog8", "cst"], w=[kb])
                S.dve(CP(aog[:], bk[0:64, 0:8]), r=[kb], w=["aog"])
                S.dve(TS(nc31[:], c31b[:], -1.0, None, ALU.mult), r=["c31b"], w=["nc31"])
                with contextlib.ExitStack() as st2:
                    bnr = st2.enter_context(SBT("bnr", [128, 2048], F32))
                    S.dma(bnr[:], bnear_d, w=["bnr"])
                    for h in range(8):
                        S.act(ACTF(Rn[:, h, :], bnr[:, h * 256:(h + 1) * 256], AF.Exp, bias=nc31[:, h:h + 1]), r=["bnr", "nc31"], w=["Rn"])
                    S.flush()
                kaT = sb("kaT", [128, 4, T], BF16)
                kiT2 = sb("kiT2", [128, T], BF16)
                Va = sb("Va", [128, NT, 520], BF16)
                qaTb = [sb("qaTb%d" % i, [128, 4, 512], BF16) for i in range(2)]
                qiTb = [sb("qiTb%d" % i, [128, 4, 512], BF16) for i in range(2)]
                wib = [sb("wib%d" % i, [128, 4, 8], F32) for i in range(2)]
                wabs = sb("wabs", [128, 4, 8], F32)
                wsgn = sb("wsgn", [128, 4, 8], F32)
                scb = [sb("sc%d" % i, [128, T], F32) for i in range(2)]
                NRL = 5
                rl = [sb("rl%d" % i, [128, 512], F32) for i in range(NRL)]
                msk = sb("msk", [128, T], BF16)
                maskT = sb("maskT", [128, NT, 512], BF16)
                NEP = 4
                Eb = [sb("Eb%d" % i, [128, 512], BF16) for i in range(NEP)]
                Pt = [sb("Pt%d" % i, [128, 512], BF16) for i in range(NEP)]
                attTb = sb("attTb", [128, 4, 512], BF16)
                Osb = sb("Osb", [65, 512], F32)
                SQ = sb("SQ", [65, 512], F32)
                rs = sb("rs", [64, 512], F32)
                bis = [sb("bis%d" % i, [128, 1], F32) for i in range(6)]
                wtab = sb("wtab", [128, NBIS + 2], F32)
                ctab = sb("ctab", [128, NBIS + 2], F32)
                S.dma(kaT[:], featT_d[512:1024, :].rearrange("(c p) t -> p c t", p=128), r=["featT_d"], w=["kaT"])
                S.dma(kiT2[0:64, :], featT_d[1536:1600, :], r=["featT_d"], w=["kiT2"])
                S.dma(kiT2[64:128, :], featT_d[1536:1600, :], r=["featT_d"], w=["kiT2"])
                vsrc = vaug_d.rearrange("(n p) c -> p n c", p=128)
                for n0 in range(0, NT, 8):
                    S.dma(Va[:, n0:n0 + 8, :], vsrc[:, n0:n0 + 8, :], r=["vaug_d"], w=["Va"])
                S.dve(MEMSET(statL[0:64, :], 1.0 / 64), w=["statL"])
                S.dve(MEMSET(statL[64:65, :], 1e-6), w=["statL"])
                for k in range(NBIS + 2):
                    S.pool(MEMSET(ctab[:, k:k + 1], 2.0 ** (-k)), w=["ctab"])
                cw = 0.125 * (8 ** -0.5)

                def loadq(b):
                    i = b % 2
                    S.dma(qaTb[i][:], featT_d[0:512, b * 512:(b + 1) * 512].rearrange("(c p) t -> p c t", p=128), r=["featT_d"], w=["qaTb%d" % i])
                    S.dma(qiTb[i][:], featT_d[1024:1536, b * 512:(b + 1) * 512].rearrange("(c p) t -> p c t", p=128), r=["featT_d"], w=["qiTb%d" % i])
                    S.dma(wib[i][:], wi_d[b * 512:(b + 1) * 512, :].rearrange("(j p) h -> p j h", p=128), r=["wi_d"], w=["wib%d" % i])
                loadq(0)
                eidx = [0]
                for b in range(NB):
                    if b + 1 < NB:
                        loadq(b + 1)
                    i2 = b % 2
                    qa, qi, wi_ = qaTb[i2], qiTb[i2], wib[i2]
                    kqa, kqi, kwi = "qaTb%d" % i2, "qiTb%d" % i2, "wib%d" % i2
                    S.dve(STT(wabs[:], wi_[:], -1.0, wi_[:], ALU.mult, ALU.max), r=[kwi], w=["wabs"])
                    S.dve(TS(wabs[:], wabs[:], cw, None, ALU.mult), r=["wabs"], w=["wabs"])
                    S.act(ACTF(wsgn[:], wi_[:], AF.Sign), r=[kwi], w=["wsgn"])
                    nk = 4 * b + 4
                    def indexer(jq):
                        j = 4 * b + jq
                        Lk = (j + 1) * 128
                        qsl = slice(jq * 128, (jq + 1) * 128)
                        sc = scb[jq % 2]
                        ksc = "sc%d" % (jq % 2)
                        nch = (Lk + 511) // 512
                        for kc in range(nch):
                            ncol = min(512, Lk - kc * 512)
                            csl = slice(kc * 512, kc * 512 + ncol)
                            for ih in range(8):
                                pr, hf = ih // 2, ih % 2
                                ps_ = slice(hf * 64, (hf + 1) * 64)
                                bk, kb = nb()
                                S.pe(MM(bk[:, 0:ncol], qi[ps_, pr, qsl], kiT2[ps_, csl], True, True), r=[kqi, "kiT2"], w=[kb])
                                rb = rl[eidx[0] % NRL]
                                krb = "rl%d" % (eidx[0] % NRL)
                                eidx[0] += 1
                                S.act(ACTF(rb[:, 0:ncol], bk[:, 0:ncol], AF.Relu, scale=wabs[:, jq, ih:ih + 1]), r=[kb, "wabs"], w=[krb])
                                if ih == 0:
                                    yield (TS(sc[:, csl], rb[:, 0:ncol], wsgn[:, jq, 0:1], None, ALU.mult), [krb, "wsgn"], [ksc])
                                else:
                                    yield (STT(sc[:, csl], rb[:, 0:ncol], wsgn[:, jq, ih:ih + 1], sc[:, csl], ALU.mult, ALU.add), [krb, "wsgn", ksc], [ksc])

                    def do_acc(a):
                        S.dve(a[0], r=a[1], w=a[2])

                    gens = [indexer(jq) for jq in range(4)]
                    for a in gens[0]:
                        do_acc(a)
                    for jq in range(4):
                        j = 4 * b + jq
                        Lk = (j + 1) * 128
                        qsl = slice(jq * 128, (jq + 1) * 128)
                        sc = scb[jq % 2]
                        ksc = "sc%d" % (jq % 2)
                        nxt = gens[jq + 1] if jq < 3 else None
                        pending = []
                        A_, mid, cnt, inc = bis[0], bis[1], bis[2], bis[3]
                        S.dve(RED(A_[:], sc[:, 0:Lk], ALU.max, absval=True), r=[ksc], w=["bis0"])
                        S.dve(TS(A_[:], A_[:], 1.001, 1e-6, ALU.mult, ALU.add), r=["bis0"], w=["bis0"])
                        S.dve(TT(sc[:, j * 128:(j + 1) * 128], sc[:, j * 128:(j + 1) * 128], caus, ALU.add), r=[ksc, "cst"], w=[ksc])
                        if dbg:
                            S.dma(dbg_d["dbg_score"][j * 128:(j + 1) * 128, 0:Lk], sc[:, 0:Lk], r=[ksc], w=["dbgs"])
                        S.dve(TS(wtab[:], ctab[:], A_[:, 0:1], None, ALU.mult), r=["ctab", "bis0"], w=["wtab"])
                        S.dve(TS(mid[:], A_[:], 0.0, None, ALU.mult), r=["bis0"], w=["bis1"])
                        for k in range(1, NBIS + 1):
                            if k % 2 == 1:
                                if nxt is not None:
                                    for _ in range(NRL):
                                        a = next(nxt, None)
                                        if a is None:
                                            break
                                        pending.append(a)
                                S.dve(TS(msk[:, 0:Lk], sc[:, 0:Lk], mid[:, 0:1], 0.0, ALU.is_ge, ALU.add, accum=cnt[:]), r=[ksc, "bis1"], w=["bis2", "msk"])
                                S.dve(STT(inc[:], cnt[:], NSEL - 0.5, wtab[:, k - 1:k], ALU.is_ge, ALU.mult), r=["bis2", "wtab"], w=["bis3"])
                            else:
                                S.dve(TS(bis[4][:], mid[:], -1.0, None, ALU.mult), r=["bis1"], w=["bis4"])
                                S.act(ACTF(msk[:, 0:Lk], sc[:, 0:Lk], AF.Sign, bias=bis[4][:, 0:1], accum=cnt[:]), r=[ksc, "bis4"], w=["bis2", "msk"])
                                for a in pending:
                                    do_acc(a)
                                del pending[:]
                                S.dve(STT(inc[:], cnt[:], 2.0 * NSEL - 1.0 - Lk, wtab[:, k - 1:k], ALU.is_ge, ALU.mult), r=["bis2", "wtab"], w=["bis3"])
                            S.dve(STT(mid[:], inc[:], wtab[:, k:k + 1], mid[:], ALU.subtract, ALU.add), r=["bis3", "wtab", "bis1"], w=["bis1"])
                        S.dve(TT(bis[5][:], mid[:], wtab[:, NBIS:NBIS + 1], ALU.subtract), r=["bis1", "wtab"], w=["bis5"])
                        if dbg:
                            S.dma(dbg_d["dbg_thr"][j * 128:(j + 1) * 128, :], bis[5][:], r=["bis5"], w=["dbgt"])
                        S.dve(TS(msk[:, 0:Lk], sc[:, 0:Lk], bis[5][:, 0:1], None, ALU.is_ge), r=[ksc, "bis5"], w=["msk"])
                        for i0 in range(0, j + 1, 8):
                            n8 = min(8, j + 1 - i0)
                            bk, kb = nb()
                            bkb = bfv(bk)
                            for ii in range(n8):
                                S.pe(TR(bkb[:, ii * 128:(ii + 1) * 128], msk[:, (i0 + ii) * 128:(i0 + ii + 1) * 128], identb[:]), r=["msk", "identb"], w=[kb])
                            S.act(ACTF(maskT[:, i0:i0 + n8, qsl], bkb[:, 0:n8 * 128].rearrange("p (i t) -> p i t", i=n8), AF.Copy), r=[kb], w=["maskT"])
                        for a in pending:
                            do_acc(a)
                        del pending[:]
                        if nxt is not None:
                            for a in nxt:
                                do_acc(a)
                    for hp in range(4):
                        hpair = (2 * hp, 2 * hp + 1)
                        for i in range(nk):
                            m = i - 4 * b
                            c0 = max(0, m) * 128
                            ncol = 512 - c0
                            st_ = []
                            for h in hpair:
                                pr, hf = h // 2, h % 2
                                ps_ = slice(hf * 64, (hf + 1) * 64)
                                bk, kb = nb()
                                S.pe(MM(bk[:, 0:ncol], kaT[ps_, pr, i * 128:(i + 1) * 128], qa[ps_, pr, c0:512], True, True), r=["kaT", kqa], w=[kb])
                                ei = eidx[0] % NEP
                                eidx[0] += 1
                                st_.append((h, bk, kb, ei))
                            for h, bk, kb, ei in st_:
                                S.act(ACTF(Eb[ei][:, 0:ncol], bk[:, 0:ncol], AF.Exp, bias=c31b[:, h:h + 1], scale=0.125), r=[kb, "c31b"], w=["Eb%d" % ei])
                            for h, bk, kb, ei in st_:
                                E, kE, P_, kP = Eb[ei], "Eb%d" % ei, Pt[ei], "Pt%d" % ei
                                eng = S.dve if (h % 2) else S.pool
                                eng(TT(P_[:, 0:ncol], E[:, 0:ncol], maskT[:, i, c0:512], ALU.mult), r=[kE, "maskT"], w=[kP])
                                if m >= 0:
                                    nn = min(256, ncol)
                                    eng(TT(P_[:, 0:nn], P_[:, 0:nn], Rn[:, h, 0:nn], ALU.mult), r=[kP, "Rn"], w=[kP])
                                elif m == -1:
                                    eng(TT(P_[:, 0:128], P_[:, 0:128], Rn[:, h, 128:256], ALU.mult), r=[kP, "Rn"], w=[kP])
                            for h, bk, kb, ei in st_:
                                bkO, kbO = banks[6 + h % 2], "bank%d" % (6 + h % 2)
                                S.pe(MM(bkO[0:65, c0:512], Va[:, i, h * 65:(h + 1) * 65], Pt[ei][:, 0:ncol], i == 0, i == nk - 1), r=["Va", "Pt%d" % ei], w=[kbO])
                        for h in hpair:
                            pr, hf = h // 2, h % 2
                            ps_ = slice(hf * 64, (hf + 1) * 64)
                            bkO, kbO = banks[6 + h % 2], "bank%d" % (6 + h % 2)
                            S.act(ACTF(Osb[:], bkO[0:65, :], AF.Copy), r=[kbO], w=["Osb"])
                            S.act(ACTF(SQ[:], bkO[0:65, :], AF.Square), r=[kbO], w=["SQ"])
                            bk, kb = nb()
                            S.pe(MM(bk[0:64, :], statL[:], SQ[:], True, True), r=["statL", "SQ"], w=[kb])
                            S.act(ACTF(rs[:], bk[0:64, :], AF.Ln), r=[kb], w=["rs"])
                            S.act(ACTF(rs[:], rs[:], AF.Exp, scale=-0.5), r=["rs"], w=["rs"])
                            S.dve(STT(attTb[ps_, pr, :], Osb[0:64, :], aog[:, h:h + 1], rs[:], ALU.mult, ALU.mult), r=["Osb", "aog", "rs"], w=["attTb"])
                    S.dma(attT_d[:, b * 512:(b + 1) * 512].rearrange("(c p) t -> p c t", p=128), attTb[:], r=["attTb"], w=["attT_d"])
                S.flush()
            nrot[0] = 8

        def phase_B2(l, xsrc):
            with contextlib.ExitStack() as st:
                def sb(name, shape, dt):
                    return st.enter_context(SBT(name, shape, dt))
                wo = sb("wo", [128, 8, 1024], BF16)
                gt1 = sb("gt1", [128, 1024], F32)
                A2t = sb("A2t", [128, 1024], F32)
                sh2t = sb("sh2t", [128, 1024], F32)
                xb = [sb("xb%d" % i, [128, 1024], F32) for i in range(2)]
                mixT = [sb("mixT%d" % i, [128, 8, 128], BF16) for i in range(2)]
                x1 = [sb("x1_%d" % i, [128, 1024], F32) for i in range(2)]
                junk = sb("junk", [128, 1024], F32)
                tmp = [sb("tmpn%d" % i, [128, 1024], F32) for i in range(2)]
                tmp2 = [sb("tmpm%d" % i_, [128, 1024], F32) for i_ in range(2)]
                hb = [sb("hb%d" % i, [128, 1024], BF16) for i in range(2)]
                h2s = [sb("h2s%d" % i, [128, 8, 128], BF16) for i in range(2)]
                ssq = [sb("ssq%d" % i, [128, 1], F32) for i in range(2)]
                ms = [sb("ms%d" % i, [128, 1], F32) for i in range(2)]
                rstd = [sb("rstd%d" % i, [128, 1], F32) for i in range(2)]
                wov = W["w_out"][l].rearrange("(kc p) n -> p kc n", p=128)
                S.dma(wo[:], wov, w=["wo"], eng="pool")
                S.dma(gt1[:], mod_d[:, 2048:3072], r=["mod_d"], w=["modp"])
                S.dma(A2t[:], mod_d[:, 4096:5120], r=["mod_d"], w=["modp"])
                S.dma(sh2t[:], mod_d[:, 3072:4096], r=["mod_d"], w=["modp"])

                def load(t):
                    i = t % 2
                    S.dma(xb[i][:], xsrc[t * 128:(t + 1) * 128, :], r=["xs_d"], w=["xb%d" % i])
                    S.dma(mixT[i][:, 0:4, :], rwkvT_d[:, t * 128:(t + 1) * 128].rearrange("(c p) t -> p c t", p=128), r=["rwkvT_d"], w=["mixT%d" % i])
                    S.dma(mixT[i][:, 4:8, :], attT_d[:, t * 128:(t + 1) * 128].rearrange("(c p) t -> p c t", p=128), r=["attT_d"], w=["mixT%d" % i])
                load(0)
                for t in range(NT):
                    if t + 1 < NT:
                        load(t + 1)
                    i = t % 2
                    for hf in range(2):
                        bk, kb = nb()
                        for kc in range(8):
                            S.pe(MM(bk[:], mixT[i][:, kc, :], wo[:, kc, hf * 512:(hf + 1) * 512], kc == 0, kc == 7), r=["mixT%d" % i, "wo"], w=[kb])
                        csl = slice(hf * 512, (hf + 1) * 512)
                        S.dve(TT(tmp2[i][:, csl], bk[:], gt1[:, csl], ALU.mult), r=[kb, "modp"], w=["tmpm%d" % i])
                    S.pool(TT(x1[i][:], tmp2[i][:], xb[i][:], ALU.add), r=["tmpm%d" % i, "xb%d" % i], w=["x1_%d" % i])
                    S.dma(xs_d[t * 128:(t + 1) * 128, :], x1[i][:], r=["x1_%d" % i], w=["xs_d2"])
                    norm_mod_T(x1[i][:], "x1_%d" % i, A2t[:], sh2t[:], hb[i], "hb%d" % i, junk, ssq[i], ms[i], rstd[i], tmp[i], i,
                               h2s[i][:], "h2s%d" % i)
                    S.dma(h2T_d[:, t * 128:(t + 1) * 128].rearrange("(kc p) t -> p kc t", p=128), h2s[i][:], r=["h2s%d" % i], w=["h2T_d"])
                S.flush()

        def phase_C(l, last):
            TB = 256
            NBC = T // TB
            with contextlib.ExitStack() as st:
                def sb(name, shape, dt):
                    return st.enter_context(SBT(name, shape, dt))
                w1 = sb("w1", [128, 8, 4096], BF16)
                w2 = sb("w2", [128, 32, 1024], BF16)
                gt2 = sb("gt2", [128, 1024], F32)
                fg = sb("fg", [128, 1024], F32)
                h2b = [sb("h2b%d" % i, [128, 8, TB], BF16) for i in range(2)]
                uT = sb("uT", [128, 32, TB], BF16)
                sq = [sb("sq%d" % i, [128, TB], F32) for i in range(2)]
                xb = [sb("xb%d" % i, [128, 1024], F32) for i in range(2)]
                x2 = [sb("x2_%d" % i, [128, 1024], F32) for i in range(2)]
                tmp2 = sb("tmpm", [128, 1024], F32)
                junk = sb("junk", [128, 1024], F32)
                ssq = [sb("ssq%d" % i, [128, 1], F32) for i in range(2)]
                ms = [sb("ms%d" % i, [128, 1], F32) for i in range(2)]
                rstd = [sb("rstd%d" % i, [128, 1], F32) for i in range(2)]
                w1v = W["w_mlp1"][l].rearrange("(kc p) n -> p kc n", p=128)
                w2v = W["w_mlp2"][l].rearrange("(fc p) n -> p fc n", p=128)
                for q in range(4):
                    S.dma(w1[:, :, q * 1024:(q + 1) * 1024], w1v[:, :, q * 1024:(q + 1) * 1024], w=["w1"], eng="pool")
                for q in range(4):
                    S.dma(w2[:, q * 8:(q + 1) * 8, :], w2v[:, q * 8:(q + 1) * 8, :], w=["w2"], eng="pool")
                S.dma(gt2[:], mod_d[:, 5120:6144], r=["mod_d"], w=["modp"])
                if last:
                    S.dma(fg[:], W["final_g"].partition_broadcast(128), w=["fg"])

                def load(bb):
                    i = bb % 2
                    S.dma(h2b[i][:], h2T_d[:, bb * TB:(bb + 1) * TB].rearrange("(kc p) t -> p kc t", p=128), r=["h2T_d"], w=["h2b%d" % i])
                load(0)
                xi = [0]
                for bb in range(NBC):
                    if bb + 1 < NBC:
                        load(bb + 1)
                    i = bb % 2
                    for fc in range(32):
                        bk, kb = nb()
                        for kc in range(8):
                            S.pe(MM(bk[:, 0:TB], w1[:, kc, fc * 128:(fc + 1) * 128], h2b[i][:, kc, :], kc == 0, kc == 7), r=["w1", "h2b%d" % i], w=[kb])
                        s_ = sq[fc % 2]
                        ks_ = "sq%d" % (fc % 2)
                        S.act(ACTF(s_[:], bk[:, 0:TB], AF.Square), r=[kb], w=[ks_])
                        S.dve(STT(uT[:, fc, :], bk[:, 0:TB], 0.0, s_[:], ALU.is_gt, ALU.mult), r=[kb, ks_], w=["uT"])
                    for jj in range(TB // 128):
                        t = bb * (TB // 128) + jj
                        xi_ = xi[0] % 2
                        xi[0] += 1
                        S.dma(xb[xi_][:], xs_d[t * 128:(t + 1) * 128, :], r=["xs_d"], w=["xb%d" % xi_])
                        for hf in range(2):
                            bk, kb = nb()
                            for fc in range(32):
                                S.pe(MM(bk[:], uT[:, fc, jj * 128:(jj + 1) * 128], w2[:, fc, hf * 512:(hf + 1) * 512], fc == 0, fc == 31), r=["uT", "w2"], w=[kb])
                            csl = slice(hf * 512, (hf + 1) * 512)
                            S.dve(TT(tmp2[:, csl], bk[:], gt2[:, csl], ALU.mult), r=[kb, "modp"], w=["tmpm"])
                        S.pool(TT(x2[xi_][:], tmp2[:], xb[xi_][:], ALU.add), r=["tmpm", "xb%d" % xi_], w=["x2_%d" % xi_])
                        if not last:
                            S.dma(xs_d[t * 128:(t + 1) * 128, :], x2[xi_][:], r=["x2_%d" % xi_], w=["xs_d2"])
                        else:
                            S.act(ACTF(junk[:], x2[xi_][:], AF.Square, accum=ssq[xi_][:]), r=["x2_%d" % xi_], w=["ssq%d" % xi_])
                            S.dve(TS(ms[xi_][:], ssq[xi_][:], 1.0 / 1024, 1e-6, ALU.mult, ALU.add), r=["ssq%d" % xi_], w=["ms%d" % xi_])
                            S.pool(TT(rstd[xi_][:], ms[xi_][:], m05[:, 0:1], ALU.pow), r=["ms%d" % xi_, "m05"], w=["rstd%d" % xi_])
                            S.dve(STT(x2[xi_][:], x2[xi_][:], rstd[xi_][:, 0:1], fg[:], ALU.mult, ALU.mult), r=["x2_%d" % xi_, "rstd%d" % xi_, "fg"], w=["x2_%d" % xi_])
                            S.dma(out_d[t * 128:(t + 1) * 128, :], x2[xi_][:], r=["x2_%d" % xi_], w=["out_d"])
                S.flush()

        order = []
        for l in range(L):
            order += [("M", l), ("A1", l), ("A2", l), ("B", l), ("B2", l), ("C", l)]
        for ph, l in order:
            xsrc = x_d if l == 0 else xs_d
            if ph == "M":
                phase_M(l)
            elif ph == "A1":
                phase_A1(l, xsrc)
            elif ph == "A2":
                try:
                    phase_A2(l)
                except _Stop:
                    S.flush()
                    break
            elif ph == "B":
                phase_B(l)
            elif ph == "B2":
                phase_B2(l, xsrc)
            elif ph == "C":
                phase_C(l, l == L - 1)
            if stop_after == (ph, l):
                break
        S.flush(final=True)
        nops = S.nops
    return nc, nops


def _t5_bucket(n):
    n = np.maximum(n, 0)
    nf = np.maximum(n, 1).astype(np.float32)
    large = 16 + (np.log(nf / np.float32(16)) / np.float32(math.log(128 / 16)) * np.float32(16)).astype(np.int32)
    large = np.minimum(large, 31)
    return np.where(n < 16, n, large)


def _consts():
    s = np.arange(128)[:, None]
    t = np.arange(128)[None, :]
    c = np.zeros((128, 640), np.float32)
    c[:, 0:128] = (s == t)
    c[:, 128:256] = (s <= t)
    c[:, 256:384] = (s < t)
    c[:, 384:512] = (s > t)
    c[:, 512:640] = np.where(t <= s, 0.0, -1e30)
    return c


def host_inputs(inputs, T, L):
    B = inputs["x"].shape[0]
    f = lambda a: np.ascontiguousarray(np.asarray(a, dtype=np.float32))
    rel_bias = f(inputs["rel_bias"])
    tk = np.arange(128)[:, None, None, None]
    d = np.arange(2)[None, None, :, None]
    tq = np.arange(128)[None, None, None, :]
    hh = np.arange(8)[None, :, None, None]
    bidx = _t5_bucket(128 * d + tq - tk) + 0 * hh
    bnear = rel_bias[bidx, hh + 0 * bidx].reshape(128, 2048)
    shared = {"consts": _consts(), "bnear": f(bnear), "c31": f(rel_bias[31:32, :])}
    for k in WSHAPES:
        shared[k] = f(inputs[k])[:L]
    for k in VSHAPES:
        shared[k] = f(inputs[k])[:max(L - 1, 1)]
    shared["final_g"] = f(inputs["final_g"]).reshape(1, 1024)
    maps = []
    for bi in range(B):
        m = dict(shared)
        m["x"] = f(inputs["x"][bi, :T])
        m["c8"] = f(np.asarray(inputs["c"][bi]).reshape(8, 128).T)
        maps.append(m)
    return maps


_CACHE = {}


def kernel(**inputs):
    T = inputs["x"].shape[1]
    L = inputs["w_ada"].shape[0]
    B = inputs["x"].shape[0]
    key = (T, L)
    if key not in _CACHE:
        _CACHE[key] = build(T, L)[0]
    nc = _CACHE[key]
    maps = host_inputs(inputs, T, L)
    res = run_bass_kernel_spmd(nc, maps, core_ids=list(range(B)))
    return np.stack([np.asarray(r["out"], dtype=np.float32) for r in res.results], axis=0)
```
